# Optimizing a Trainium2 kernel written in Bass

```python
import math
import jax, jax.numpy as jnp
from jax import lax
import numpy as np

D_MODEL = 2048
BATCH = 4
SEQ = 2048
DEPTH = 4
DEC_BATCH = 128
DEC_SEQ = 1
PAST_LEN = 16384
PAGE_SIZE = 128

D_MIX = D_MODEL
M_HEADS = 4
M_DV = D_MIX // (2 * M_HEADS)
M_DK = M_DV // 2
G_HEADS = 8
G_DK = D_MIX // (2 * G_HEADS)
G_DV = G_DK
CONV_W = 4
G_CONV_CH = G_HEADS * (2 * G_DK + G_DV)
X_HEADS = 4
X_DH = 128
N_MEM = 256
D_FF = -(-8 * D_MODEL // (3 * 256)) * 256
CHUNK = 64
GATE_CAP = 15.0
EPS = 1e-6
SPLIT_SIZES = (M_HEADS * M_DK, M_HEADS * M_DK, M_HEADS * M_DV, M_HEADS * M_DV, M_HEADS, M_HEADS,
               G_HEADS * G_DK, G_HEADS * G_DK, G_HEADS * G_DV, G_HEADS * G_DV, G_HEADS, G_HEADS)
N_IN = sum(SPLIT_SIZES)
SPLIT_IDX = tuple(int(s) for s in np.cumsum(SPLIT_SIZES)[:-1])

kernel_name = 'hybrid_mlstm_gdn_memory_decoder_step'


def rmsnorm(x, g):
    xf = x.astype(jnp.float32)
    y = xf * lax.rsqrt(jnp.mean(xf * xf, axis=-1, keepdims=True) + EPS)
    return (y * g.astype(jnp.float32)).astype(x.dtype)


def l2norm(x):
    return x * lax.rsqrt(jnp.sum(x * x, axis=-1, keepdims=True) + EPS)


def softcap(x, cap):
    return cap * jnp.tanh(x / cap)


def to_chunks(a, L):
    B, T = a.shape[0], a.shape[1]
    return jnp.moveaxis(a.reshape((B, T // L, L) + a.shape[2:]), 1, 0)


def from_chunks(a):
    a = jnp.moveaxis(a, 0, 1)
    return a.reshape((a.shape[0], a.shape[1] * a.shape[2]) + a.shape[3:])


def causal_conv(x, buf, w):
    T = x.shape[1]
    xp = jnp.concatenate([buf.astype(x.dtype), x], axis=1)
    y = xp[:, 0:T] * w[0]
    for j in range(1, CONV_W):
        y = y + xp[:, j:j + T] * w[j]
    return jax.nn.silu(y), xp[:, -(CONV_W - 1):]


def mlstm_chunkwise(q, k, v, i_pre, logf, C0, n0, m0):
    L = math.gcd(q.shape[1], CHUNK)
    causal = jnp.tril(jnp.ones((L, L), dtype=bool))

    def step(carry, xs):
        C, n, m = carry
        qc, kc, vc, ic, fc = xs
        b = jnp.cumsum(fc, axis=1)
        D = b[:, :, None, :] - b[:, None, :, :] + ic[:, None, :, :]
        D = jnp.where(causal[None, :, :, None], D, -jnp.inf)
        inter = b + m[:, None, :]
        m_t = jnp.maximum(inter, jnp.max(D, axis=2))
        Wts = jnp.exp(D - m_t[:, :, None, :]) * jnp.einsum('bthd,bshd->btsh', qc, kc)
        a = jnp.exp(inter - m_t)
        num = a[..., None] * jnp.einsum('bthd,bhde->bthe', qc, C) + jnp.einsum('btsh,bshe->bthe', Wts, vc)
        nq = a * jnp.einsum('bthd,bhd->bth', qc, n) + jnp.sum(Wts, axis=2)
        h = num / jnp.maximum(jnp.abs(nq), jnp.exp(-m_t))[..., None]
        m_new = m_t[:, -1]
        decay = jnp.exp(b[:, -1] + m - m_new)
        w_s = jnp.exp(b[:, -1:, :] - b + ic - m_new[:, None, :])
        C_new = decay[..., None, None] * C + jnp.einsum('bsh,bshd,bshe->bhde', w_s, kc, vc)
        n_new = decay[..., None] * n + jnp.einsum('bsh,bshd->bhd', w_s, kc)
        return (C_new, n_new, m_new), h

    xs = (to_chunks(q, L), to_chunks(k, L), to_chunks(v, L), to_chunks(i_pre, L), to_chunks(logf, L))
    (C, n, m), h = lax.scan(step, (C0, n0, m0), xs)
    return from_chunks(h), C, n, m


def gdn_chunked(q, k, v, log_a, beta, S0):
    L = math.gcd(q.shape[1], CHUNK)
    incl = jnp.tril(jnp.ones((L, L), dtype=bool))
    strict = jnp.tril(jnp.ones((L, L), dtype=bool), -1)

    def step(S, xs):
        qc, kc, vc, gc, bc = xs
        g = jnp.cumsum(gc, axis=1)
        G = g[:, :, None, :] - g[:, None, :, :]
        decay_ts = jnp.exp(jnp.where(incl[None, :, :, None], G, -jnp.inf))
        kk = jnp.einsum('bthd,bshd->btsh', kc, kc)
        A = jnp.where(strict[None, :, :, None], decay_ts * kk * bc[:, None, :, :], 0.0)
        eg = jnp.exp(g)
        rhs = vc - eg[..., None] * jnp.einsum('bthd,bhde->bthe', kc, S)
        U = lax.linalg.triangular_solve(jnp.transpose(A, (0, 3, 1, 2)), jnp.transpose(rhs, (0, 2, 1, 3)),
                                        left_side=True, lower=True, unit_diagonal=True)
        wu = bc[..., None] * jnp.transpose(U, (0, 2, 1, 3))
        qk = jnp.einsum('bthd,bshd->btsh', qc, kc) * decay_ts
        o = eg[..., None] * jnp.einsum('bthd,bhde->bthe', qc, S) + jnp.einsum('btsh,bshe->bthe', qk, wu)
        wend = jnp.exp(g[:, -1:, :] - g)
        S_new = jnp.exp(g[:, -1])[..., None, None] * S + jnp.einsum('bsh,bshd,bshe->bhde', wend, kc, wu)
        return S_new, o

    xs = (to_chunks(q, L), to_chunks(k, L), to_chunks(v, L), to_chunks(log_a, L), to_chunks(beta, L))
    S, o = lax.scan(step, S0, xs)
    return from_chunks(o), S


def parallel_mixer(h, w_in, b_i, b_f, m_norm, conv_w, A_log, dt_bias, g_norm, w_out, C0, n0, m0, S0, conv0):
    f32 = jnp.float32
    B, T, _ = h.shape
    proj = h @ w_in
    mq, mk, mv, mo, mi, mf, gq, gk, gv, gz, ga, gb = jnp.split(proj, SPLIT_IDX, axis=-1)
    q = mq.reshape(B, T, M_HEADS, M_DK).astype(f32)
    k = mk.reshape(B, T, M_HEADS, M_DK).astype(f32) * (M_DK ** -0.5)
    v = mv.reshape(B, T, M_HEADS, M_DV).astype(f32)
    i_pre = softcap(mi.astype(f32) + b_i.astype(f32), GATE_CAP)
    logf = jax.nn.log_sigmoid(softcap(mf.astype(f32) + b_f.astype(f32), GATE_CAP))
    hm, C, n, m = mlstm_chunkwise(q, k, v, i_pre, logf, C0.astype(f32), n0.astype(f32), m0.astype(f32))
    hm = rmsnorm(hm, m_norm) * jax.nn.sigmoid(mo.reshape(B, T, M_HEADS, M_DV).astype(f32))
    qkv, conv_new = causal_conv(jnp.concatenate([gq, gk, gv], axis=-1), conv0, conv_w)
    cq, ck, cv = jnp.split(qkv, [G_HEADS * G_DK, 2 * G_HEADS * G_DK], axis=-1)
    q2 = l2norm(cq.reshape(B, T, G_HEADS, G_DK).astype(f32)) * (G_DK ** -0.5)
    k2 = l2norm(ck.reshape(B, T, G_HEADS, G_DK).astype(f32))
    v2 = cv.reshape(B, T, G_HEADS, G_DV).astype(f32)
    log_a = -jnp.exp(A_log.astype(f32)) * jax.nn.softplus(ga.astype(f32) + dt_bias.astype(f32))
    beta = jax.nn.sigmoid(gb.astype(f32))
    o, S = gdn_chunked(q2, k2, v2, log_a, beta, S0.astype(f32))
    o = rmsnorm(o, g_norm) * jax.nn.silu(gz.reshape(B, T, G_HEADS, G_DV).astype(f32))
    mixed = jnp.concatenate([hm.reshape(B, T, -1), o.reshape(B, T, -1)], axis=-1).astype(h.dtype)
    return mixed @ w_out, (C, n, m, S, conv_new)


def mem_kv(mem, g_mem, wk, wv):
    B = mem.shape[0]
    mh = rmsnorm(mem, g_mem)
    return ((mh @ wk).reshape(B, N_MEM, X_HEADS, X_DH), (mh @ wv).reshape(B, N_MEM, X_HEADS, X_DH))


def cross_attn(h, mk, mv, wq, wo):
    B, T, _ = h.shape
    q = (h @ wq).reshape(B, T, X_HEADS, X_DH)
    s = jnp.einsum('bthd,bmhd->bhtm', q, mk.astype(q.dtype)).astype(jnp.float32) * (X_DH ** -0.5)
    p = jax.nn.softmax(s, axis=-1).astype(h.dtype)
    o = jnp.einsum('bhtm,bmhd->bthd', p, mv.astype(h.dtype)).reshape(B, T, X_HEADS * X_DH)
    return o @ wo


def swiglu(h, w_gate, w_up, w_down):
    return (jax.nn.silu(h @ w_gate) * (h @ w_up)) @ w_down


def block(x, lp, mk, mv, C0, n0, m0, S0, conv0):
    (g_mix, w_in, b_i, b_f, m_norm, conv_w, A_log, dt_bias, g_norm, w_out,
     g_x, wq, wo, g_ffn, w_gate, w_up, w_down) = lp
    mix, st = parallel_mixer(rmsnorm(x, g_mix), w_in, b_i, b_f, m_norm, conv_w, A_log, dt_bias, g_norm, w_out,
                             C0, n0, m0, S0, conv0)
    x = x + mix
    x = x + cross_attn(rmsnorm(x, g_x), mk, mv, wq, wo)
    x = x + swiglu(rmsnorm(x, g_ffn), w_gate, w_up, w_down)
    return x, st


def setup_inputs(seed: int = 0) -> dict:
    key = jax.random.key(seed)
    ks = iter(jax.random.split(key, 48))
    f32 = jnp.float32

    def nrm(shape, scale):
        return scale * jax.random.normal(next(ks), shape, f32)

    def gain(shape):
        return 1.0 + nrm(shape, 0.02)

    d = {}
    d['x_prompt'] = nrm((BATCH, SEQ, D_MODEL), 1.0)
    d['x_sample'] = nrm((DEC_BATCH, DEC_SEQ, D_MODEL), 1.0)
    d['mem_prompt'] = nrm((BATCH, N_MEM, D_MODEL), 1.0)
    d['state_mlstm_C'] = nrm((DEPTH, DEC_BATCH, M_HEADS, M_DK, M_DV), 0.1)
    d['state_mlstm_n'] = nrm((DEPTH, DEC_BATCH, M_HEADS, M_DK), 0.1)
    d['state_mlstm_m'] = nrm((DEPTH, DEC_BATCH, M_HEADS), 1.0)
    d['state_gdn_S'] = nrm((DEPTH, DEC_BATCH, G_HEADS, G_DK, G_DV), 0.3)
    d['state_gdn_conv'] = nrm((DEPTH, DEC_BATCH, CONV_W - 1, G_CONV_CH), 1.0)
    d['cache_mem_k'] = nrm((DEPTH, DEC_BATCH, N_MEM, X_HEADS, X_DH), 1.0)
    d['cache_mem_v'] = nrm((DEPTH, DEC_BATCH, N_MEM, X_HEADS, X_DH), 1.0)
    d['norm_mix'] = gain((DEPTH, D_MODEL))
    d['w_in'] = nrm((DEPTH, D_MODEL, N_IN), D_MODEL ** -0.5)
    d['mlstm_b_i'] = nrm((DEPTH, M_HEADS), 0.1)
    d['mlstm_b_f'] = jnp.linspace(3.0, 6.0, M_HEADS, dtype=f32)[None, :] + nrm((DEPTH, M_HEADS), 0.1)
    d['mlstm_norm'] = gain((DEPTH, M_HEADS, M_DV))
    d['gdn_conv_w'] = nrm((DEPTH, CONV_W, G_CONV_CH), CONV_W ** -0.5)
    d['gdn_A_log'] = jnp.log(jax.random.uniform(next(ks), (DEPTH, G_HEADS), f32, 1.0, 16.0))
    dt = jnp.exp(jax.random.uniform(next(ks), (DEPTH, G_HEADS), f32, math.log(1e-3), math.log(1e-1)))
    d['gdn_dt_bias'] = dt + jnp.log(-jnp.expm1(-dt))
    d['gdn_norm'] = gain((DEPTH, G_HEADS, G_DV))
    d['w_out'] = nrm((DEPTH, D_MIX, D_MODEL), D_MIX ** -0.5)
    d['norm_xattn'] = gain((DEPTH, D_MODEL))
    d['norm_mem'] = gain((DEPTH, D_MODEL))
    d['xattn_wq'] = nrm((DEPTH, D_MODEL, X_HEADS * X_DH), D_MODEL ** -0.5)
    d['xattn_wk'] = nrm((DEPTH, D_MODEL, X_HEADS * X_DH), D_MODEL ** -0.5)
    d['xattn_wv'] = nrm((DEPTH, D_MODEL, X_HEADS * X_DH), D_MODEL ** -0.5)
    d['xattn_wo'] = nrm((DEPTH, X_HEADS * X_DH, D_MODEL), (X_HEADS * X_DH) ** -0.5)
    d['norm_ffn'] = gain((DEPTH, D_MODEL))
    d['ffn_w_gate'] = nrm((DEPTH, D_MODEL, D_FF), D_MODEL ** -0.5)
    d['ffn_w_up'] = nrm((DEPTH, D_MODEL, D_FF), D_MODEL ** -0.5)
    d['ffn_w_down'] = nrm((DEPTH, D_FF, D_MODEL), D_FF ** -0.5)
    d['norm_final'] = gain((D_MODEL,))
    return d


def reference(x_prompt, x_sample, mem_prompt, state_mlstm_C, state_mlstm_n, state_mlstm_m, state_gdn_S,
              state_gdn_conv, cache_mem_k, cache_mem_v, norm_mix, w_in, mlstm_b_i, mlstm_b_f, mlstm_norm,
              gdn_conv_w, gdn_A_log, gdn_dt_bias, gdn_norm, w_out, norm_xattn, norm_mem, xattn_wq, xattn_wk,
              xattn_wv, xattn_wo, norm_ffn, ffn_w_gate, ffn_w_up, ffn_w_down, norm_final):
    f32 = jnp.float32
    B = x_prompt.shape[0]
    yp, ys = x_prompt, x_sample
    pC, pn, pm, pS, pconv, pk, pv = [], [], [], [], [], [], []
    sC, sn, sm, sS, sconv = [], [], [], [], []
    for l in range(DEPTH):
        lp = (norm_mix[l], w_in[l], mlstm_b_i[l], mlstm_b_f[l], mlstm_norm[l], gdn_conv_w[l], gdn_A_log[l],
              gdn_dt_bias[l], gdn_norm[l], w_out[l], norm_xattn[l], xattn_wq[l], xattn_wo[l], norm_ffn[l],
              ffn_w_gate[l], ffn_w_up[l], ffn_w_down[l])
        mk_p, mv_p = mem_kv(mem_prompt, norm_mem[l], xattn_wk[l], xattn_wv[l])
        yp, (C, n, m, S, cv) = block(
            yp, lp, mk_p, mv_p,
            jnp.zeros((B, M_HEADS, M_DK, M_DV), f32), jnp.zeros((B, M_HEADS, M_DK), f32),
            jnp.zeros((B, M_HEADS), f32), jnp.zeros((B, G_HEADS, G_DK, G_DV), f32),
            jnp.zeros((B, CONV_W - 1, G_CONV_CH), x_prompt.dtype))
        pC.append(C); pn.append(n); pm.append(m); pS.append(S); pconv.append(cv); pk.append(mk_p); pv.append(mv_p)
        ys, (C, n, m, S, cv) = block(
            ys, lp, cache_mem_k[l], cache_mem_v[l], state_mlstm_C[l], state_mlstm_n[l], state_mlstm_m[l],
            state_gdn_S[l], state_gdn_conv[l])
        sC.append(C); sn.append(n); sm.append(m); sS.append(S); sconv.append(cv)
    y_prompt = rmsnorm(yp, norm_final)
    y_sample = rmsnorm(ys, norm_final)
    return (y_prompt, y_sample,
            jnp.stack(pC), jnp.stack(pn), jnp.stack(pm), jnp.stack(pS), jnp.stack(pconv), jnp.stack(pk), jnp.stack(pv),
            jnp.stack(sC), jnp.stack(sn), jnp.stack(sm), jnp.stack(sS), jnp.stack(sconv))
```

```python
from contextlib import ExitStack
import numpy as np
import concourse.bass as bass
import concourse.mybir as mybir
from concourse.bass_utils import run_bass_kernel_spmd

F32 = mybir.dt.float32
BF16 = mybir.dt.bfloat16
ALU = mybir.AluOpType
AF = mybir.ActivationFunctionType
AX = mybir.AxisListType

D = 2048
NCH = 16
DEPTH = 4
SEQ = 2048
TILE = 512
NCK = TILE // 128
NSMP = 16
DFF = 5632
NIN = 7192
EPS = 1e-6
NEG = -30000.0

C_ID = 0
C_NI = 128
C_MS = 256
C_SEL = 384
C_ONE = 384
C_E16 = 512
C_MK = 768
CW = 768 + 7 * 128


class Buf:
    __slots__ = ("w", "r")

    def __init__(self):
        self.w = None
        self.r = {}


class Sched:
    def __init__(self, nc, es):
        self.nc = nc
        self.es = es
        self.eng = {"pe": nc.tensor, "act": nc.scalar, "dve": nc.vector, "pool": nc.gpsimd, "sp": nc.sync}
        self.sem = {}
        self.cnt = {}
        self.waited = {e: {} for e in self.eng}
        for e in self.eng:
            self.sem[e] = es.enter_context(nc.semaphore("s_" + e))
            self.cnt[e] = 0

    def _deps(self, e, r, w):
        deps = {}

        def add(s, v):
            if deps.get(s, 0) < v:
                deps[s] = v

        for b in r:
            if b.w is not None:
                add(*b.w)
        for b in w:
            if b.w is not None:
                add(*b.w)
            for s, v in b.r.items():
                add(s, v)
        for s, v in deps.items():
            if e == "pe" and s == "pe":
                continue
            if self.waited[e].get(s, 0) >= v:
                continue
            self.eng[e].wait_ge(self.sem[s], v)
            self.waited[e][s] = v

    def _upd(self, tok, r, w):
        for b in w:
            b.w = tok
            b.r = {}
        s, v = tok
        for b in r:
            if b in w:
                continue
            if b.r.get(s, 0) < v:
                b.r[s] = v

    def op(self, e, fn, r=(), w=()):
        self._deps(e, r, w)
        ins = fn(self.eng[e])
        self.cnt[e] += 1
        ins.then_inc(self.sem[e], 1)
        tok = (e, self.cnt[e])
        self._upd(tok, r, w)
        return tok

    def dma(self, q, stream, out, in_, r=(), w=()):
        if stream not in self.sem:
            self.sem[stream] = self.es.enter_context(self.nc.semaphore("d_" + stream))
            self.cnt[stream] = 0
        self._deps(q, r, w)
        ins = self.eng[q].dma_start(out=out, in_=in_)
        self.cnt[stream] += 16
        ins.then_inc(self.sem[stream], 16)
        tok = (stream, self.cnt[stream])
        self._upd(tok, r, w)
        return tok

    def finish(self, q="sp"):
        for s, v in self.cnt.items():
            if v > 0 and self.waited[q].get(s, 0) < v:
                self.eng[q].wait_ge(self.sem[s], v)
                self.waited[q][s] = v


def weight_blocks():
    bl = []
    bl.append(("w_in", 0, 16, 3072, 8))
    for h in range(4):
        bl.append(("w_in", 0, 16, h * 128, 128))
        bl.append(("w_in", 0, 16, 512 + h * 128, 128))
        for j in range(2):
            bl.append(("w_in", 0, 16, 1024 + h * 256 + j * 128, 128))
        for j in range(2):
            bl.append(("w_in", 0, 16, 2048 + h * 256 + j * 128, 128))
    bl.append(("w_in", 0, 16, 7176, 16))
    for h in range(8):
        bl.append(("w_in", 0, 16, 3080 + h * 128, 128))
        bl.append(("w_in", 0, 16, 4104 + h * 128, 128))
        bl.append(("w_in", 0, 16, 5128 + h * 128, 128))
        bl.append(("w_in", 0, 16, 6152 + h * 128, 128))
    for j in range(16):
        bl.append(("w_out", 0, 16, j * 128, 128))
    for j in range(4):
        bl.append(("xattn_wk", 0, 16, j * 128, 128))
    for j in range(4):
        bl.append(("xattn_wv", 0, 16, j * 128, 128))
    for j in range(4):
        bl.append(("xattn_wq", 0, 16, j * 128, 128))
    for j in range(16):
        bl.append(("xattn_wo", 0, 4, j * 128, 128))
    for g in range(4):
        for c in range(11):
            bl.append(("ffn_w_gate", 0, 16, (g * 11 + c) * 128, 128))
            bl.append(("ffn_w_up", 0, 16, (g * 11 + c) * 128, 128))
        for j in range(16):
            bl.append(("ffn_w_down", g * 11, 11, j * 128, 128))
    return bl


WBLOCKS = weight_blocks()
WOFF = np.cumsum([0] + [b[2] * b[4] for b in WBLOCKS]).tolist()
WTOT = WOFF[-1]


def build(NL=DEPTH, NT=4, stage=99, with_sample=True):
    nc = bass.Bass("TRN2", target_bir_lowering=False)
    es = ExitStack()
    S = Sched(nc, es)

    def din(name, shape):
        return nc.dram_tensor(name, list(shape), F32, kind="ExternalInput").ap()

    def dout(name, shape):
        return nc.dram_tensor(name, list(shape), F32, kind="ExternalOutput").ap()

    def dscr(name, shape):
        return nc.dram_tensor(name, list(shape), F32).ap()

    def sb(name, shape, dt=F32):
        return es.enter_context(nc.sbuf_tensor("sb_" + name, list(shape), dt))

    d_xT = din("xT", [D, SEQ])
    d_xsT = din("xsT", [D, NSMP])
    d_memT = din("memT", [D, 256])
    d_w = din("wts", [NL, 128, WTOT])
    d_cst = din("cst", [128, CW])
    d_gain = din("gains", [128, (NL * 4 + 1) * 16])
    d_gb = din("gateb", [8, NL * 4])
    d_mn = din("mnorm", [NL, 4 * 256])
    d_gn = din("gnorm", [NL, 8 * 128])
    d_cw = din("convw", [128, NL * 24 * 4])
    d_sC = din("sC", [NL, NSMP, 4, 128, 256])
    d_sn = din("sn", [NL, NSMP, 4 * 128])
    d_sm = din("sm", [NL, NSMP, 4])
    d_sS = din("sS", [NL, NSMP, 8, 128, 128])
    d_sconv = din("sconv", [NL, NSMP, 3, 3072])
    d_ck = din("ck", [NL, NSMP, 256, 512])
    d_cv = din("cv", [NL, NSMP, 256, 512])

    o_yT = dout("o_yT", [D, SEQ])
    o_ysT = dout("o_ysT", [D, NSMP])
    o_pC = dout("o_pC", [NL, 4, 128, 256])
    o_pn = dout("o_pn", [NL, 4, 128])
    o_pm = dout("o_pm", [NL, 4])
    o_pS = dout("o_pS", [NL, 8, 128, 128])
    o_pconvT = dout("o_pconvT", [NL, 3072, 3])
    o_pkT = dout("o_pkT", [NL, 512, 256])
    o_pv = dout("o_pv", [NL, 256, 512])
    o_sC = dout("o_sC", [NL, NSMP, 4, 128, 256])
    o_sn = dout("o_sn", [NL, NSMP, 4 * 128])
    o_sm = dout("o_sm", [NL, NSMP, 4])
    o_sS = dout("o_sS", [NL, NSMP, 8, 128, 128])
    o_sconv = dout("o_sconv", [NL, NSMP, 3, 3072])

    cst = sb("cst", [128, CW])
    b_cst = Buf()
    S.dma("sp", "c0", cst[:], d_cst, w=[b_cst])
    identb = sb("identb", [128, 128], BF16)
    onesb = sb("onesb", [128, 128], BF16)
    selb = sb("selb", [8, 1024], BF16)
    b_cb = Buf()
    S.op("pool", lambda e: e.tensor_copy(out=identb[:], in_=cst[:, C_ID:C_ID + 128]), r=[b_cst], w=[b_cb])
    S.op("pool", lambda e: e.tensor_copy(out=onesb[:], in_=cst[:, C_ONE:C_ONE + 128]), r=[b_cst], w=[b_cb])
    d_sel = din("sel", [8, 1024])
    self32 = sb("self32", [8, 1024])
    S.dma("sp", "c0", self32[:], d_sel, w=[b_cst])
    S.op("pool", lambda e: e.tensor_copy(out=selb[:], in_=self32[:]), r=[b_cst], w=[b_cb])
    identf = cst[:, C_ID:C_ID + 128]
    negi = cst[:, C_NI:C_NI + 128]
    mstrict = cst[:, C_MS:C_MS + 128]
    CB = [b_cst, b_cb]

    gains = sb("gains", [128, (NL * 4 + 1) * 16])
    gateb = sb("gateb", [8, NL * 4])
    convw = sb("convw", [128, NL * 24 * 4])
    b_par = Buf()
    S.dma("sp", "c1", gains[:], d_gain, w=[b_par])
    S.dma("sp", "c2", gateb[:], d_gb, w=[b_par])
    S.dma("sp", "c3", convw[:], d_cw, w=[b_par])
    epsc = sb("epsc", [128, 1])
    S.op("pool", lambda e: e.memset(epsc[:], EPS), w=[b_par])

    TTM = TILE + NSMP
    xT = sb("xT", [128, NCH, TTM])
    b_x = Buf()
    nT = sb("nT", [128, NCH, TTM], BF16)
    b_n = Buf()
    mixT = sb("mixT", [128, NCH, TTM], BF16)
    b_mix = Buf()
    NWS = 2
    NWB = 2
    wst = [sb("wst%d" % i, [128, 8 * 128]) for i in range(NWS)]
    b_wst = [Buf() for _ in range(NWS)]
    wbf = [sb("wbf%d" % i, [128, 16 * 128], BF16) for i in range(NWB)]
    b_wbf = [Buf() for _ in range(NWB)]
    NPS = 8
    PS = [es.enter_context(nc.psum_tensor("ps%d" % i, [128, 512], F32)) for i in range(NPS)]
    BPS = [Buf() for _ in range(NPS)]
    st = {"ps": 0, "w": 0, "layer": 0, "wi": 0, "ws": 0}

    pinned = set()

    def psum(pin=False):
        while True:
            i = st["ps"]
            st["ps"] = (i + 1) % NPS
            if i not in pinned:
                break
        if pin:
            pinned.add(i)
        return PS[i], BPS[i]

    def unpin(bps):
        pinned.discard(BPS.index(bps))

    def getw():
        i = st["wi"]
        st["wi"] += 1
        mat, k0, nk, c0, ncw = WBLOCKS[i]
        sz = nk * ncw
        n = st["w"]
        st["w"] += 1
        s2 = n % NWB
        hs = ((nk + 1) // 2) * ncw
        for hf, (lo, hi) in enumerate(((0, hs), (hs, sz))):
            if hi <= lo:
                continue
            s1 = st["ws"] % NWS
            st["ws"] += 1
            S.dma("sp", "w%d" % s1, wst[s1][:, 0:hi - lo], d_w[st["layer"], :, WOFF[i] + lo:WOFF[i] + hi], w=[b_wst[s1]])
            S.op("pool", lambda e, s1=s1, lo=lo, hi=hi: e.tensor_copy(out=wbf[s2][:, lo:hi], in_=wst[s1][:, 0:hi - lo]), r=[b_wst[s1]], w=[b_wbf[s2]])
        return wbf[s2][:, 0:sz].rearrange("p (k c) -> p k c", k=nk), b_wbf[s2], (mat, k0, nk, c0, ncw)

    def groups(ti):
        g = [(c0, 512) for c0 in range(0, TILE, 512)]
        if ti == 0 and with_sample:
            g.append((TILE, NSMP))
        return g

    def proj_F(src, bsrc, ti, evac, nk=16, m=None, koff=0):
        wv, bw, desc = getw()
        mm = desc[4] if m is None else m
        for gi, (c0, n) in enumerate(groups(ti)):
            ps, bps = psum()
            for k in range(nk):
                S.op("pe", lambda e, k=k: e.matmul(ps[0:mm, 0:n], lhsT=wv[:, k, 0:mm], rhs=src[:, koff + k, c0:c0 + n],
                                                   start=(k == 0), stop=(k == nk - 1)), r=[bw, bsrc], w=[bps])
            evac(ps, bps, mm, c0, n, gi)
        return desc

    def rmsnorm_F(ti, gcol0, out, bout, src=None, bsrc=None, ncols=None):
        src = xT if src is None else src
        bsrc = b_x if bsrc is None else bsrc
        grp = groups(ti) if ncols is None else [(0, ncols)]
        for (c0, n) in grp:
            S.op("act", lambda e: e.activation(out=out[:, :, c0:c0 + n], in_=src[:, :, c0:c0 + n], func=AF.Square), r=[bsrc], w=[bout])
            ps, bps = psum()
            for k in range(NCH):
                S.op("pe", lambda e, k=k: e.matmul(ps[:, 0:n], lhsT=onesb[:], rhs=out[:, k, c0:c0 + n], start=(k == 0), stop=(k == NCH - 1)),
                     r=[bout] + CB, w=[bps])
            rstd = sb_rstd
            S.op("act", lambda e: e.activation(out=rstd[:, 0:n], in_=ps[:, 0:n], func=AF.Ln, bias=epsc[:], scale=1.0 / D), r=[bps, b_par], w=[b_rstd])
            S.op("act", lambda e: e.activation(out=rstd[:, 0:n], in_=rstd[:, 0:n], func=AF.Exp, scale=-0.5), r=[b_rstd], w=[b_rstd])
            for k in range(NCH):
                S.op("dve", lambda e, k=k: e.scalar_tensor_tensor(out=out[:, k, c0:c0 + n], in0=src[:, k, c0:c0 + n], scalar=gains[:, gcol0 + k:gcol0 + k + 1],
                                                                  in1=rstd[:, 0:n], op0=ALU.mult, op1=ALU.mult), r=[bsrc, b_rstd, b_par], w=[bout])

    sb_rstd = sb("rstd", [128, 512])
    b_rstd = Buf()

    def resid_evac(j):
        def ev(ps, bps, m, c0, n, gi, si=0):
            S.op("dve", lambda e: e.tensor_tensor(out=xT[:, j, c0:c0 + n], in0=xT[:, j, c0:c0 + n], in1=ps[:, 0:n], op=ALU.add), r=[bps, b_x], w=[b_x])
        return ev

    def proj(src, bsrc, ti, evac, subs=None, nk=16, koff=0, grp=None):
        wv, bw, desc = getw()
        subs = [(0, desc[4])] if subs is None else subs
        for gi, (c0, n) in enumerate(groups(ti) if grp is None else grp):
            for si, (lo, hi) in enumerate(subs):
                ps, bps = psum()
                for k in range(nk):
                    S.op("pe", lambda e, k=k: e.matmul(ps[0:hi - lo, 0:n], lhsT=wv[:, k, lo:hi], rhs=src[:, koff + k, c0:c0 + n],
                                                       start=(k == 0), stop=(k == nk - 1)), r=[bw, bsrc], w=[bps])
                evac(ps, bps, hi - lo, c0, n, gi, si)
        return wv, bw

    def projT(wv, bw, src, bsrc, ti, evac, evac_s=None, nk=16, ntt=NCK):
        ncols = wv.shape[2]
        for b0 in range(0, ntt, 4):
            ps, bps = psum()
            nj = min(4, ntt - b0)
            for jj in range(nj):
                j = b0 + jj
                for k in range(nk):
                    S.op("pe", lambda e, k=k, j=j, jj=jj: e.matmul(ps[:, jj * ncols:(jj + 1) * ncols], lhsT=src[:, k, j * 128:(j + 1) * 128], rhs=wv[:, k, :],
                                                                   start=(k == 0), stop=(k == nk - 1)), r=[bw, bsrc], w=[bps])
            evac(ps[:, 0:nj * ncols].rearrange("p (j c) -> p j c", j=nj), bps, b0, nj)
        if evac_s is not None and ti == 0 and with_sample:
            ps, bps = psum()
            for k in range(nk):
                S.op("pe", lambda e, k=k: e.matmul(ps[0:NSMP, 0:ncols], lhsT=src[:, k, TILE:TILE + NSMP], rhs=wv[:, k, :],
                                                   start=(k == 0), stop=(k == nk - 1)), r=[bw, bsrc], w=[bps])
            evac_s(ps[0:NSMP, 0:ncols], bps)

    def split3(src, bsrc, np_, n, pieces, bpc, tmp, btmp, npc=3):
        cur = src
        bcur = bsrc
        for i in range(npc):
            S.op("dve", lambda e, i=i, cur=cur: e.tensor_copy(out=pieces[i][0:np_, 0:n], in_=cur[0:np_, 0:n]), r=[bcur], w=[bpc])
            if i < npc - 1:
                S.op("dve", lambda e, i=i, cur=cur: e.tensor_tensor(out=tmp[0:np_, 0:n], in0=cur[0:np_, 0:n], in1=pieces[i][0:np_, 0:n], op=ALU.subtract),
                     r=[bcur, bpc], w=[btmp])
                cur = tmp
                bcur = btmp

    def bcast_rows(pieces, bpc, np_, h, n, dst, bdst, npc=3):
        for c0 in range(0, n, 512):
            nn = min(512, n - c0)
            ps, bps = psum()
            for i in range(npc):
                S.op("pe", lambda e, i=i: e.matmul(ps[:, 0:nn], lhsT=selb[0:np_, h * 128:(h + 1) * 128], rhs=pieces[i][0:np_, c0:c0 + nn],
                                                   start=(i == 0), stop=(i == npc - 1)), r=[bpc] + CB, w=[bps])
            S.op("act", lambda e: e.copy(out=dst[:, c0:c0 + nn], in_=ps[:, 0:nn]), r=[bps], w=[bdst])

    def rows_to_cols(rowlist, brows, np_, dst, bdst):
        nq = len(rowlist)
        ps, bps = psum()
        for c in range(NCK):
            for qi, rt in enumerate(rowlist):
                o = (c * nq + qi) * np_
                S.op("pe", lambda e, rt=rt, c=c, o=o: e.transpose(out=ps[:, o:o + np_], in_=rt[0:np_, c * 128:(c + 1) * 128], identity=cst[0:np_, C_ID:C_ID + np_]),
                     r=brows + CB, w=[bps])
        S.op("dve", lambda e: e.tensor_copy(out=dst[:, 0:NCK * nq * np_], in_=ps[:, 0:NCK * nq * np_]), r=[bps], w=[bdst])

    R = TILE + NSMP
    rg = sb("rg", [8, R]); rf = sb("rf", [8, R]); rF = sb("rF", [8, TILE]); rm = sb("rm", [8, R])
    rux = sb("rux", [8, (TILE + 1)]); rw = sb("rw", [8, TILE]); ra = sb("ra", [8, TILE]); rem = sb("rem", [8, TILE])
    rnu = sb("rnu", [8, (TILE + 1)])
    rz = sb("rz", [8, TILE])
    b_rows = Buf()
    S.op("pool", lambda e: e.memset(rz[:], 0.0), w=[b_rows])
    pcs = [sb("pcs%d" % i, [8, (TILE + 1)], BF16) for i in range(3)]
    b_pcs = Buf()
    ptmp = sb("ptmp", [8, (TILE + 1)]); b_ptmp = Buf()
    gb15 = sb("gb15", [8, 4]); b_gb15 = Buf()
    UBx = sb("UBx", [128, (TILE + 1)]); b_UB = Buf()
    mcols = sb("mcols", [128, NCK * 3 * 8]); b_mcols = Buf()
    qTb = sb("qTb", [128, R], BF16); b_qT = Buf()
    kTb = sb("kTb", [128, R], BF16); b_kT = Buf()
    ktok = sb("ktok", [128, NCK, 128], BF16); b_ktok = Buf()
    vtok = sb("vtok", [128, NCK, 257], BF16); b_vtok = Buf()
    gtok = sb("gtok", [128, NCK, 256], BF16); b_gtok = Buf()
    S.op("pool", lambda e: e.memset(vtok[:], 1.0), w=[b_vtok])
    Caug = sb("Caug", [128, 257]); Cb = sb("Cb", [128, 257], BF16); b_C = Buf(); b_Cb = Buf()
    Sst = sb("Sst", [128, 128]); Sb = sb("Sb", [128, 128], BF16); b_S = Buf(); b_Sb = Buf()
    pre = sb("pre", [128, 3 + R]); b_pre = Buf()
    cvy = sb("cvy", [128, R]); b_cvy = Buf()
    k2f = sb("k2f", [128, TILE]); b_k2f = Buf()
    ctail = sb("ctail", [128, NL * 24 * 3]); b_ctail = Buf()
    S.op("pool", lambda e: e.memset(ctail[:], 0.0), w=[b_ctail])
    mnb = sb("mnb", [128, 1024]); gnb = mnb; b_nb = Buf()
    cX = sb("cX", [128, 128]); cE = sb("cE", [128, 128]); cW = sb("cW", [128, 128], BF16); b_cX = Buf(); b_cE = Buf(); b_cW = Buf()
    cnumA = sb("cnumA", [128, 257]); cnum = sb("cnum", [128, 257]); b_numA = Buf(); b_num = Buf()
    csm = sb("csm", [128, 16]); b_csm = Buf()
    chm = sb("chm", [128, 256]); b_chm = Buf()
    cjunk = sb("cjunk", [128, 256]); b_junk = Buf()
    ckw = sb("ckw", [128, 128], BF16); b_kw = Buf()
    cAT = sb("cAT", [128, 128]); b_AT = Buf()
    cQK = sb("cQK", [128, 128], BF16); b_QK = Buf()
    cP = [sb("cP%d" % i, [128, 128], BF16) for i in range(2)]; b_P = [Buf(), Buf()]
    cPT = [sb("cPT%d" % i, [128, 128], BF16) for i in range(2)]; b_PT = [Buf(), Buf()]
    cTT = [sb("cTT%d" % i, [128, 128], BF16) for i in range(2)]; b_TT = [Buf(), Buf()]
    cBk = [sb("cBk%d" % i, [128, 128], BF16) for i in range(6)]; b_Bk = [Buf() for _ in range(6)]
    crhs = sb("crhs", [128, 128], BF16); b_rhs = Buf()
    cwu = sb("cwu", [128, 128], BF16); b_wu = Buf()
    co1 = sb("co1", [128, 128]); b_o1 = Buf()
    d_scrC = dscr("scrC", [NL, 4, 128, 257])
    d_scrS = dscr("scrS", [NL, 8, 128, 128])
    b_scr = Buf()
    mcar = sb("mcar", [8, NL]); b_mcar = Buf()
    S.op("pool", lambda e: e.memset(mcar[:], 0.0), w=[b_mcar])
    s_k = sb("s_k", [NSMP, 128]); s_v = sb("s_v", [NSMP, 257]); s_g = sb("s_g", [NSMP, 256]); b_sk = Buf(); b_sv = Buf(); b_sg = Buf()
    S.op("pool", lambda e: e.memset(s_v[:], 1.0), w=[b_sv])
    s_q = sb("s_q", [128, NSMP]); b_sq = Buf()
    s_kf = sb("s_kf", [128, NSMP]); b_skf = Buf()

    def gated_norm_to_mix(src_ap, ncol, nrm_row, gate_ap, bgate, chunk_ids, tcol0, ntok, brsrc):
        S.op("act", lambda e: e.activation(out=cjunk[0:ntok, 0:ncol], in_=src_ap, func=AF.Square, accum_out=csm[0:ntok, 4:5]), r=brsrc, w=[b_junk, b_csm])
        S.op("act", lambda e: e.activation(out=csm[0:ntok, 5:6], in_=csm[0:ntok, 4:5], func=AF.Ln, bias=epsc[0:ntok, :], scale=1.0 / ncol), r=[b_csm, b_par], w=[b_csm])
        S.op("act", lambda e: e.activation(out=csm[0:ntok, 5:6], in_=csm[0:ntok, 5:6], func=AF.Exp, scale=-0.5), r=[b_csm], w=[b_csm])
        S.op("dve", lambda e: e.scalar_tensor_tensor(out=chm[0:ntok, 0:ncol], in0=src_ap, scalar=csm[0:ntok, 5:6], in1=nrm_row, op0=ALU.mult, op1=ALU.mult),
             r=brsrc + [b_csm, b_nb], w=[b_chm])
        S.op("dve", lambda e: e.tensor_tensor(out=chm[0:ntok, 0:ncol], in0=chm[0:ntok, 0:ncol], in1=gate_ap, op=ALU.mult), r=[b_chm, bgate], w=[b_chm])
        for i, ch in enumerate(chunk_ids):
            ps, bps = psum()
            S.op("pe", lambda e, i=i: e.transpose(out=ps[:, 0:ntok], in_=chm[0:ntok, i * 128:(i + 1) * 128], identity=cst[0:ntok, C_ID:C_ID + ntok]),
                 r=[b_chm] + CB, w=[bps])
            S.op("act", lambda e, ch=ch: e.copy(out=mixT[:, ch, tcol0:tcol0 + ntok], in_=ps[:, 0:ntok]), r=[bps], w=[b_mix])

    def mlstm(l, ti):
        smp = (ti == 0 and with_sample)
        ncol = R if smp else TILE
        S.op("dve", lambda e: e.tensor_scalar(out=gb15[0:4, 0:2], in0=gateb[0:4, l * 4:l * 4 + 2], scalar1=1.0 / 15.0, scalar2=None, op0=ALU.mult), r=[b_par], w=[b_gb15])

        def ev_g(ps, bps, m, c0, n, gi, si):
            dst = rg if si == 0 else rf
            S.op("act", lambda e: e.activation(out=dst[0:4, c0:c0 + n], in_=ps[0:4, 0:n], func=AF.Tanh, bias=gb15[0:4, si:si + 1], scale=1.0 / 15.0),
                 r=[bps, b_gb15], w=[b_rows])
        proj(nT, b_n, ti, ev_g, subs=[(0, 4), (4, 8)])
        S.op("dve", lambda e: e.tensor_scalar(out=rg[0:4, 0:ncol], in0=rg[0:4, 0:ncol], scalar1=15.0, scalar2=None, op0=ALU.mult), r=[b_rows], w=[b_rows])
        S.op("act", lambda e: e.activation(out=rf[0:4, 0:ncol], in_=rf[0:4, 0:ncol], func=AF.Exp, scale=-15.0), r=[b_rows], w=[b_rows])
        S.op("act", lambda e: e.activation(out=rf[0:4, 0:ncol], in_=rf[0:4, 0:ncol], func=AF.Ln, bias=1.0), r=[b_rows], w=[b_rows])
        S.op("dve", lambda e: e.tensor_scalar(out=rf[0:4, 0:ncol], in0=rf[0:4, 0:ncol], scalar1=-1.0, scalar2=None, op0=ALU.mult), r=[b_rows], w=[b_rows])
        S.op("dve", lambda e: e.tensor_tensor_scan(out=rF[0:4, :], data0=rf[0:4, 0:TILE], data1=rz[0:4, :], initial=0.0, op0=ALU.add, op1=ALU.add), r=[b_rows], w=[b_rows])
        S.op("dve", lambda e: e.tensor_tensor_scan(out=rm[0:4, 0:TILE], data0=rf[0:4, 0:TILE], data1=rg[0:4, 0:TILE], initial=mcar[0:4, l:l + 1], op0=ALU.add, op1=ALU.max),
             r=[b_rows, b_mcar], w=[b_rows])
        S.op("dve", lambda e: e.tensor_scalar(out=rux[0:4, 0:1], in0=mcar[0:4, l:l + 1], scalar1=-1.0, scalar2=None, op0=ALU.mult), r=[b_mcar], w=[b_rows])
        S.op("dve", lambda e: e.tensor_tensor(out=rux[0:4, 1:(TILE + 1)], in0=rF[0:4, :], in1=rm[0:4, 0:TILE], op=ALU.subtract), r=[b_rows], w=[b_rows])
        S.op("dve", lambda e: e.tensor_scalar(out=rnu[0:4, :], in0=rux[0:4, :], scalar1=-1.0, scalar2=None, op0=ALU.mult), r=[b_rows], w=[b_rows])
        S.op("dve", lambda e: e.tensor_tensor(out=rw[0:4, :], in0=rg[0:4, 0:TILE], in1=rF[0:4, :], op=ALU.subtract), r=[b_rows], w=[b_rows])
        S.op("act", lambda e: e.activation(out=rem[0:4, :], in_=rm[0:4, 0:TILE], func=AF.Exp, scale=-1.0), r=[b_rows], w=[b_rows])
        for c in range(NCK):
            S.op("act", lambda e, c=c: e.activation(out=ra[0:4, c * 128:(c + 1) * 128], in_=rux[0:4, 1 + c * 128:1 + (c + 1) * 128], func=AF.Exp,
                                                    bias=rnu[0:4, c * 128:c * 128 + 1], scale=1.0), r=[b_rows], w=[b_rows])
        S.op("dve", lambda e: e.tensor_copy(out=mcar[0:4, l:l + 1], in_=rm[0:4, (TILE - 1):TILE]), r=[b_rows], w=[b_mcar])
        if ti == NT - 1:
            S.dma("sp", "o_pm", o_pm[l].rearrange("(h o) -> h o", o=1), rm[0:4, (TILE - 1):TILE], r=[b_rows])
        rows_to_cols([rw, ra, rem], [b_rows], 4, mcols, b_mcols)
        split3(rux, b_rows, 4, (TILE + 1), pcs, b_pcs, ptmp, b_ptmp)
        S.dma("sp", "nb", mnb[:], d_mn[l].partition_broadcast(128), w=[b_nb])

        def mcol(c, qi, h):
            o = (c * 3 + qi) * 4 + h
            return mcols[:, o:o + 1]

        for h in range(4):
            bcast_rows(pcs, b_pcs, 4, h, (TILE + 1), UBx, b_UB)
            def ev_q(ps, bps, m, c0, n, gi, si):
                if c0 == TILE:
                    S.op("dve", lambda e: e.tensor_copy(out=s_q[:, :], in_=ps[:, 0:n]), r=[bps], w=[b_sq])
                    S.op("act", lambda e: e.copy(out=qTb[:, c0:c0 + n], in_=s_q[:, :]), r=[b_sq], w=[b_qT])
                else:
                    S.op("act", lambda e: e.copy(out=qTb[:, c0:c0 + n], in_=ps[:, 0:n]), r=[bps], w=[b_qT])
            proj(nT, b_n, ti, ev_q)

            def ev_k(ps, bps, m, c0, n, gi, si):
                if c0 == TILE:
                    S.op("dve", lambda e: e.tensor_scalar(out=s_kf[:, :], in0=ps[:, 0:n], scalar1=128.0 ** -0.5, scalar2=None, op0=ALU.mult), r=[bps], w=[b_skf])
                    S.op("act", lambda e: e.copy(out=kTb[:, c0:c0 + n], in_=s_kf[:, :]), r=[b_skf], w=[b_kT])
                else:
                    S.op("act", lambda e: e.activation(out=kTb[:, c0:c0 + n], in_=ps[:, 0:n], func=AF.Copy, scale=128.0 ** -0.5), r=[bps], w=[b_kT])
            wv, bw = proj(nT, b_n, ti, ev_k)

            def ev_kt(ps3, bps, j0, nj):
                S.op("act", lambda e: e.activation(out=ktok[:, j0:j0 + nj, :], in_=ps3, func=AF.Copy, scale=128.0 ** -0.5), r=[bps], w=[b_ktok])

            def ev_kts(ps, bps):
                S.op("act", lambda e: e.activation(out=s_k[:, :], in_=ps, func=AF.Copy, scale=128.0 ** -0.5), r=[bps], w=[b_sk])
            projT(wv, bw, nT, b_n, ti, ev_kt, ev_kts)
            for j in range(2):
                wv, bw, _ = getw()

                def ev_v(ps3, bps, j0, nj, j=j):
                    S.op("dve", lambda e: e.tensor_copy(out=vtok[:, j0:j0 + nj, j * 128:(j + 1) * 128], in_=ps3), r=[bps], w=[b_vtok])

                def ev_vs(ps, bps, j=j):
                    S.op("dve", lambda e: e.tensor_copy(out=s_v[:, j * 128:(j + 1) * 128], in_=ps), r=[bps], w=[b_sv])
                projT(wv, bw, nT, b_n, ti, ev_v, ev_vs)
            for j in range(2):
                wv, bw, _ = getw()

                def ev_o(ps3, bps, j0, nj, j=j):
                    S.op("act", lambda e: e.activation(out=gtok[:, j0:j0 + nj, j * 128:(j + 1) * 128], in_=ps3, func=AF.Sigmoid), r=[bps], w=[b_gtok])

                def ev_os(ps, bps, j=j):
                    S.op("act", lambda e: e.activation(out=s_g[:, j * 128:(j + 1) * 128], in_=ps, func=AF.Sigmoid), r=[bps], w=[b_sg])
                projT(wv, bw, nT, b_n, ti, ev_o, ev_os)
            if ti == 0:
                S.op("dve", lambda e: e.memset(Caug[:], 0.0), w=[b_C])
            else:
                S.dma("sp", "scrC_r", Caug[:], d_scrC[l, h], r=[b_scr], w=[b_C])
            S.op("act", lambda e: e.copy(out=Cb[:], in_=Caug[:]), r=[b_C], w=[b_Cb])
            for c in range(NCK):
                t0 = c * 128
                ps_s, bps_s = psum()
                S.op("pe", lambda e: e.matmul(ps_s[:, 0:128], lhsT=kTb[:, t0:t0 + 128], rhs=qTb[:, t0:t0 + 128], start=True, stop=True), r=[b_kT, b_qT], w=[bps_s])
                S.op("dve", lambda e: e.scalar_tensor_tensor(out=cX[:], in0=UBx[:, 1 + t0:1 + t0 + 128], scalar=mcol(c, 0, h), in1=negi, op0=ALU.add, op1=ALU.add),
                     r=[b_UB, b_mcols] + CB, w=[b_cX])
                S.op("act", lambda e: e.activation(out=cE[:], in_=cX[:], func=AF.Exp), r=[b_cX], w=[b_cE])
                S.op("dve", lambda e: e.tensor_tensor(out=cW[:], in0=cE[:], in1=ps_s[:, 0:128], op=ALU.mult), r=[b_cE, bps_s], w=[b_cW])
                psA, bpsA = psum()
                S.op("pe", lambda e: e.matmul(psA[:, 0:257], lhsT=qTb[:, t0:t0 + 128], rhs=Cb[:], start=True, stop=True), r=[b_qT, b_Cb], w=[bpsA])
                psB, bpsB = psum()
                S.op("pe", lambda e: e.matmul(psB[:, 0:257], lhsT=cW[:], rhs=vtok[:, c, :], start=True, stop=True), r=[b_cW, b_vtok], w=[bpsB])
                S.op("act", lambda e: e.activation(out=cnumA[:], in_=psA[:, 0:257], func=AF.Copy, scale=mcol(c, 1, h)), r=[bpsA, b_mcols], w=[b_numA])
                S.op("dve", lambda e: e.tensor_tensor(out=cnum[:], in0=cnumA[:], in1=psB[:, 0:257], op=ALU.add), r=[b_numA, bpsB], w=[b_num])
                S.op("act", lambda e: e.activation(out=csm[:, 0:1], in_=cnum[:, 256:257], func=AF.Abs), r=[b_num], w=[b_csm])
                S.op("dve", lambda e: e.tensor_tensor(out=csm[:, 0:1], in0=csm[:, 0:1], in1=mcol(c, 2, h), op=ALU.max), r=[b_csm, b_mcols], w=[b_csm])
                S.op("dve", lambda e: e.reciprocal(out=csm[:, 1:2], in_=csm[:, 0:1]), r=[b_csm], w=[b_csm])
                S.op("dve", lambda e: e.tensor_scalar(out=cnum[:, 0:256], in0=cnum[:, 0:256], scalar1=csm[:, 1:2], scalar2=None, op0=ALU.mult), r=[b_num, b_csm], w=[b_num])
                gated_norm_to_mix(cnum[:, 0:256], 256, mnb[:, h * 256:(h + 1) * 256], gtok[:, c, :], b_gtok, [2 * h, 2 * h + 1], t0, 128, [b_num])
                S.op("act", lambda e: e.activation(out=csm[:, 2:3], in_=mcol(c, 0, h), func=AF.Exp, bias=UBx[:, t0 + 128:t0 + 129], scale=1.0), r=[b_mcols, b_UB], w=[b_csm])
                S.op("dve", lambda e: e.tensor_scalar(out=ckw[:], in0=ktok[:, c, :], scalar1=csm[:, 2:3], scalar2=None, op0=ALU.mult), r=[b_ktok, b_csm], w=[b_kw])
                psC, bpsC = psum()
                S.op("pe", lambda e: e.matmul(psC[:, 0:257], lhsT=ckw[:], rhs=vtok[:, c, :], start=True, stop=True), r=[b_kw, b_vtok], w=[bpsC])
                S.op("dve", lambda e: e.tensor_tensor(out=csm[:, 3:4], in0=UBx[:, t0 + 128:t0 + 129], in1=UBx[:, t0:t0 + 1], op=ALU.subtract), r=[b_UB], w=[b_csm])
                S.op("act", lambda e: e.activation(out=csm[:, 3:4], in_=csm[:, 3:4], func=AF.Exp), r=[b_csm], w=[b_csm])
                S.op("dve", lambda e: e.scalar_tensor_tensor(out=Caug[:], in0=Caug[:], scalar=csm[:, 3:4], in1=psC[:, 0:257], op0=ALU.mult, op1=ALU.add),
                     r=[b_C, b_csm, bpsC], w=[b_C])
                S.op("act", lambda e: e.copy(out=Cb[:], in_=Caug[:]), r=[b_C], w=[b_Cb])
            if ti < NT - 1:
                S.dma("sp", "scrC_w", d_scrC[l, h], Caug[:], r=[b_C], w=[b_scr])
            else:
                S.dma("sp", "o_pC", o_pC[l, h], Caug[:, 0:256], r=[b_C])
                S.dma("sp", "o_pn", o_pn[l, h].rearrange("(k o) -> k o", o=1), Caug[:, 256:257], r=[b_C])
            if smp:
                mlstm_sample(l, h)

    eye3 = cst[:, C_E16:C_E16 + 256].rearrange("p (a b) -> p a b", a=16)
    eyeP = cst[0:NSMP, C_ID:C_ID + NSMP]
    s_rows = sb("s_rows", [8, 8 * NSMP]); b_srows = Buf()
    s_cols = sb("s_cols", [NSMP, 64]); b_scols = Buf()
    s_rep = sb("s_rep", [128, 8 * NSMP]); b_srep = Buf()
    s_pc = [sb("s_pc%d" % i, [8, NSMP], BF16) for i in range(2)]; b_spc = Buf()
    s_pt = sb("s_pt", [8, NSMP]); b_spt = Buf()
    s_mtok = sb("s_mtok", [NSMP, 8]); b_smtok = Buf()
    Qm = sb("Qm", [128, NSMP, NSMP], BF16); b_Qm = Buf()
    Km = sb("Km", [NSMP, NSMP, 128], BF16); b_Km = Buf()
    s_t1 = sb("s_t1", [NSMP, 257]); b_st1 = Buf()
    s_t2 = sb("s_t2", [NSMP, 257]); b_st2 = Buf()
    s_vb = sb("s_vb", [NSMP, 257], BF16); b_svb = Buf()
    s_f1 = sb("s_f1", [128, NSMP]); b_sf1 = Buf()
    s_fb = sb("s_fb", [128, NSMP], BF16); b_sfb = Buf()
    NRB = 2
    rowf = [sb("rowf%d" % i, [128, 257]) for i in range(NRB)]; b_rowf = [Buf() for _ in range(NRB)]
    rowb = [sb("rowb%d" % i, [128, 257], BF16) for i in range(NRB)]; b_rowb = [Buf() for _ in range(NRB)]
    st2 = {"rb": 0}

    def rep_rows(src_rows, np_, qslot):
        S.op("dve", lambda e: e.tensor_copy(out=s_pc[0][0:np_, :], in_=src_rows), r=[b_srows], w=[b_spc])
        S.op("dve", lambda e: e.tensor_tensor(out=s_pt[0:np_, :], in0=src_rows, in1=s_pc[0][0:np_, :], op=ALU.subtract), r=[b_srows, b_spc], w=[b_spt])
        S.op("dve", lambda e: e.tensor_copy(out=s_pc[1][0:np_, :], in_=s_pt[0:np_, :]), r=[b_spt], w=[b_spc])
        ps, bps = psum()
        for h in range(np_):
            for i in range(2):
                S.op("pe", lambda e, h=h, i=i: e.matmul(ps[:, h * NSMP:(h + 1) * NSMP], lhsT=selb[0:np_, h * 128:(h + 1) * 128], rhs=s_pc[i][0:np_, :],
                                                        start=(i == 0), stop=(i == 1)), r=[b_spc] + CB, w=[bps])
        S.op("act", lambda e: e.copy(out=s_rep[:, 0:np_ * NSMP], in_=ps[:, 0:np_ * NSMP]), r=[bps], w=[b_srep])

    def rows_to_tok(np_, nq):
        ps, bps = psum()
        for qi in range(nq):
            S.op("pe", lambda e, qi=qi: e.transpose(out=ps[0:NSMP, qi * np_:(qi + 1) * np_], in_=s_rows[0:np_, qi * NSMP:(qi + 1) * NSMP], identity=cst[0:np_, C_ID:C_ID + np_]),
                 r=[b_srows] + CB, w=[bps])
        S.op("dve", lambda e: e.tensor_copy(out=s_cols[:, 0:nq * np_], in_=ps[0:NSMP, 0:nq * np_]), r=[bps], w=[b_scols])

    def mlstm_sample_prep(l):
        S.dma("sp", "s_m", s_mtok[:, 0:4], d_sm[l], w=[b_smtok])
        ps, bps = psum()
        S.op("pe", lambda e: e.transpose(out=ps[0:4, 0:NSMP], in_=s_mtok[:, 0:4], identity=eyeP), r=[b_smtok] + CB, w=[bps])
        fi = rf[0:4, TILE:TILE + NSMP]
        ii = rg[0:4, TILE:TILE + NSMP]
        sl = lambda q: s_rows[0:4, q * NSMP:(q + 1) * NSMP]
        S.op("dve", lambda e: e.tensor_tensor(out=sl(4), in0=ps[0:4, 0:NSMP], in1=fi, op=ALU.add), r=[bps, b_rows], w=[b_srows])
        S.op("dve", lambda e: e.tensor_tensor(out=sl(0), in0=sl(4), in1=ii, op=ALU.max), r=[b_srows, b_rows], w=[b_srows])
        S.op("dve", lambda e: e.tensor_tensor(out=sl(1), in0=sl(4), in1=sl(0), op=ALU.subtract), r=[b_srows], w=[b_srows])
        S.op("act", lambda e: e.activation(out=sl(1), in_=sl(1), func=AF.Exp), r=[b_srows], w=[b_srows])
        S.op("dve", lambda e: e.tensor_tensor(out=sl(2), in0=ii, in1=sl(0), op=ALU.subtract), r=[b_srows, b_rows], w=[b_srows])
        S.op("act", lambda e: e.activation(out=sl(2), in_=sl(2), func=AF.Exp), r=[b_srows], w=[b_srows])
        S.op("act", lambda e: e.activation(out=sl(3), in_=sl(0), func=AF.Exp, scale=-1.0), r=[b_srows], w=[b_srows])
        rows_to_tok(4, 4)
        rep_rows(sl(1), 4, 0)
        S.op("dve", lambda e: e.tensor_copy(out=s_mtok[:, 4:8], in_=s_cols[:, 0:4]), r=[b_scols], w=[b_smtok])
        S.dma("sp", "o_sm", o_sm[l], s_mtok[:, 4:8], r=[b_smtok])

    def masked_q(src_f32, bsrc):
        S.op("dve", lambda e: e.tensor_tensor(out=Qm[:], in0=src_f32.unsqueeze(1).to_broadcast([128, NSMP, NSMP]), in1=eye3, op=ALU.mult), r=[bsrc] + CB, w=[b_Qm])

    def masked_k(src_tok, bsrc, scale_col=None, bscale=None):
        if scale_col is not None:
            S.op("dve", lambda e: e.tensor_scalar(out=s_t2[:, 0:128], in0=src_tok, scalar1=scale_col, scalar2=None, op0=ALU.mult), r=[bsrc, bscale], w=[b_st2])
            src_tok, bsrc = s_t2[:, 0:128], b_st2
        S.op("dve", lambda e: e.tensor_tensor(out=Km[:], in0=src_tok.unsqueeze(1).to_broadcast([NSMP, NSMP, 128]), in1=eyeP.unsqueeze(2).to_broadcast([NSMP, NSMP, 128]), op=ALU.mult),
             r=[bsrc] + CB, w=[b_Km])

    def dot_rows(a_f, ba, b_f, bb, out_col):
        S.op("dve", lambda e: e.tensor_tensor(out=s_fb[:], in0=a_f, in1=b_f, op=ALU.mult), r=[ba, bb], w=[b_sfb])
        ps, bps = psum()
        S.op("pe", lambda e: e.matmul(ps[0:NSMP, 0:1], lhsT=s_fb[:], rhs=onesb[:, 0:1], start=True, stop=True), r=[b_sfb] + CB, w=[bps])
        S.op("dve", lambda e: e.tensor_copy(out=out_col, in_=ps[0:NSMP, 0:1]), r=[bps], w=[b_scols])

    def mlstm_sample(l, h):
        if h == 0:
            mlstm_sample_prep(l)
        col = lambda q: s_cols[:, q * 4 + h:q * 4 + h + 1]
        masked_q(s_q[:], b_sq)
        masked_k(s_k[:], b_sk, scale_col=col(2), bscale=b_scols)
        S.op("act", lambda e: e.copy(out=s_vb[:], in_=s_v[:]), r=[b_sv], w=[b_svb])
        psA, bpsA = psum(pin=True)
        for r_ in range(NSMP):
            i = st2["rb"] % NRB
            st2["rb"] += 1
            S.dma("sp", "s_row%d" % i, rowf[i][:, 0:256], d_sC[l, r_, h], w=[b_rowf[i]])
            S.dma("sp", "s_row%d" % i, rowf[i][:, 256:257], d_sn[l, r_, h * 128:(h + 1) * 128].rearrange("(k o) -> k o", o=1), w=[b_rowf[i]])
            S.op("act", lambda e, i=i: e.copy(out=rowb[i][:], in_=rowf[i][:]), r=[b_rowf[i]], w=[b_rowb[i]])
            S.op("pe", lambda e, i=i, r_=r_: e.matmul(psA[0:NSMP, 0:257], lhsT=Qm[:, r_, :], rhs=rowb[i][:], start=(r_ == 0), stop=(r_ == NSMP - 1)), r=[b_Qm, b_rowb[i]], w=[bpsA])
            psC, bpsC = psum()
            S.op("pe", lambda e, r_=r_: e.matmul(psC[:, 0:257], lhsT=Km[:, r_, :], rhs=s_vb[:], start=True, stop=True), r=[b_Km, b_svb], w=[bpsC])
            S.op("dve", lambda e, i=i, r_=r_: e.scalar_tensor_tensor(out=rowf[i][:], in0=rowf[i][:], scalar=s_rep[:, h * NSMP + r_:h * NSMP + r_ + 1], in1=psC[:, 0:257],
                                                                     op0=ALU.mult, op1=ALU.add), r=[b_rowf[i], b_srep, bpsC], w=[b_rowf[i]])
            S.dma("sp", "o_sC%d" % i, o_sC[l, r_, h], rowf[i][:, 0:256], r=[b_rowf[i]])
            S.dma("sp", "o_sn%d" % i, o_sn[l, r_, h * 128:(h + 1) * 128].rearrange("(k o) -> k o", o=1), rowf[i][:, 256:257], r=[b_rowf[i]])
        dot_rows(s_q[:], b_sq, s_kf[:], b_skf, s_cols[:, 60:61])
        S.op("act", lambda e: e.activation(out=s_t1[:], in_=psA[0:NSMP, 0:257], func=AF.Copy, scale=col(1)), r=[bpsA, b_scols], w=[b_st1])
        unpin(bpsA)
        S.op("dve", lambda e: e.tensor_tensor(out=s_cols[:, 61:62], in0=s_cols[:, 60:61], in1=col(2), op=ALU.mult), r=[b_scols], w=[b_scols])
        S.op("dve", lambda e: e.scalar_tensor_tensor(out=s_t1[:], in0=s_v[:], scalar=s_cols[:, 61:62], in1=s_t1[:], op0=ALU.mult, op1=ALU.add), r=[b_sv, b_scols, b_st1], w=[b_st1])
        S.op("act", lambda e: e.activation(out=s_cols[:, 62:63], in_=s_t1[:, 256:257], func=AF.Abs), r=[b_st1], w=[b_scols])
        S.op("dve", lambda e: e.tensor_tensor(out=s_cols[:, 62:63], in0=s_cols[:, 62:63], in1=col(3), op=ALU.max), r=[b_scols], w=[b_scols])
        S.op("dve", lambda e: e.reciprocal(out=s_cols[:, 62:63], in_=s_cols[:, 62:63]), r=[b_scols], w=[b_scols])
        S.op("dve", lambda e: e.tensor_scalar(out=s_t1[:, 0:256], in0=s_t1[:, 0:256], scalar1=s_cols[:, 62:63], scalar2=None, op0=ALU.mult), r=[b_st1, b_scols], w=[b_st1])
        gated_norm_to_mix(s_t1[:, 0:256], 256, mnb[0:NSMP, h * 256:(h + 1) * 256], s_g[:, :], b_sg, [2 * h, 2 * h + 1], TILE, NSMP, [b_st1])

    s_cst = sb("s_cst", [NSMP, 3, 128]); b_scst = Buf()
    s_cf = sb("s_cf", [128, 3, NSMP]); b_scf = Buf()
    s_y = sb("s_y", [128, 3, NSMP]); b_sy = Buf()
    s_k2t = sb("s_k2t", [NSMP, 128]); b_sk2t = Buf()
    s_v2t = sb("s_v2t", [NSMP, 128]); b_sv2t = Buf()
    s_nt = sb("s_nt", [NSMP, 128]); b_snt = Buf()

    def gdn_sample_conv(l, h, qi):
        cc = qi * 8 + h
        if h == 0 and qi == 0:
            S.dma("sp", "o_scv", o_sconv[l, :, 0:2, :], d_sconv[l, :, 1:3, :])
        S.dma("sp", "s_cst", s_cst[:], d_sconv[l, :, :, cc * 128:(cc + 1) * 128], w=[b_scst])
        ps, bps = psum()
        for j in range(3):
            S.op("pe", lambda e, j=j: e.transpose(out=ps[:, j * NSMP:(j + 1) * NSMP], in_=s_cst[:, j, :], identity=eyeP), r=[b_scst] + CB, w=[bps])
        S.op("act", lambda e: e.copy(out=s_cf[:], in_=ps[:, 0:3 * NSMP].rearrange("p (a b) -> p a b", a=3)), r=[bps], w=[b_scf])
        wb = (l * 24 + cc) * 4
        S.op("dve", lambda e: e.tensor_scalar(out=s_y[:, qi, :], in0=s_pre[:, qi, :], scalar1=convw[:, wb + 3:wb + 4], scalar2=None, op0=ALU.mult), r=[b_spre, b_par], w=[b_sy])
        for j in range(3):
            S.op("dve", lambda e, j=j: e.scalar_tensor_tensor(out=s_y[:, qi, :], in0=s_cf[:, j, :], scalar=convw[:, wb + j:wb + j + 1], in1=s_y[:, qi, :], op0=ALU.mult, op1=ALU.add),
                 r=[b_scf, b_par, b_sy], w=[b_sy])
        S.op("act", lambda e: e.activation(out=s_y[:, qi, :], in_=s_y[:, qi, :], func=AF.Silu), r=[b_sy], w=[b_sy])
        ps2, bps2 = psum()
        S.op("pe", lambda e: e.transpose(out=ps2[0:NSMP, 0:128], in_=s_pre[:, qi, :], identity=identf), r=[b_spre] + CB, w=[bps2])
        S.op("dve", lambda e: e.tensor_copy(out=s_nt[:], in_=ps2[0:NSMP, 0:128]), r=[bps2], w=[b_snt])
        S.dma("sp", "o_scv2", o_sconv[l, :, 2, cc * 128:(cc + 1) * 128], s_nt[:], r=[b_snt])
        if qi < 2:
            S.op("act", lambda e: e.activation(out=s_fb[:], in_=s_y[:, qi, :], func=AF.Square), r=[b_sy], w=[b_sfb])
            ps3, bps3 = psum()
            S.op("pe", lambda e: e.matmul(ps3[:, 0:NSMP], lhsT=onesb[:], rhs=s_fb[:], start=True, stop=True), r=[b_sfb] + CB, w=[bps3])
            S.op("act", lambda e: e.activation(out=s_f1[:], in_=ps3[:, 0:NSMP], func=AF.Ln, bias=epsc[:], scale=1.0), r=[bps3, b_par], w=[b_sf1])
            S.op("act", lambda e: e.activation(out=s_f1[:], in_=s_f1[:], func=AF.Exp, scale=-0.5), r=[b_sf1], w=[b_sf1])
            sc = (128.0 ** -0.5) if qi == 0 else 1.0
            S.op("dve", lambda e: e.scalar_tensor_tensor(out=s_y[:, qi, :], in0=s_y[:, qi, :], scalar=sc, in1=s_f1[:], op0=ALU.mult, op1=ALU.mult), r=[b_sy, b_sf1], w=[b_sy])
        if qi >= 1:
            dst, bdst = (s_k2t, b_sk2t) if qi == 1 else (s_v2t, b_sv2t)
            ps4, bps4 = psum()
            S.op("pe", lambda e: e.transpose(out=ps4[0:NSMP, 0:128], in_=s_y[:, qi, :], identity=identf), r=[b_sy] + CB, w=[bps4])
            S.op("dve", lambda e: e.tensor_copy(out=dst[:], in_=ps4[0:NSMP, 0:128]), r=[bps4], w=[bdst])

    def gdn_sample_prep(l):
        sl = lambda q: s_rows[0:8, q * NSMP:(q + 1) * NSMP]
        S.op("act", lambda e: e.activation(out=sl(0), in_=rga[0:8, TILE:TILE + NSMP], func=AF.Exp), r=[b_rows], w=[b_srows])
        S.op("dve", lambda e: e.tensor_copy(out=sl(1), in_=rgb[0:8, TILE:TILE + NSMP]), r=[b_rows], w=[b_srows])
        rows_to_tok(8, 2)
        rep_rows(sl(0), 8, 0)

    def gdn_sample(l, h):
        if h == 0:
            gdn_sample_prep(l)
        col = lambda q: s_cols[:, q * 8 + h:q * 8 + h + 1]
        masked_q(s_y[:, 1, :], b_sy)
        psK, bpsK = psum(pin=True)
        rows_loaded = []
        for r_ in range(NSMP):
            i = st2["rb"] % NRB
            st2["rb"] += 1
            S.dma("sp", "s_row%d" % i, rowf[i][:, 0:128], d_sS[l, r_, h], w=[b_rowf[i]])
            S.op("act", lambda e, i=i: e.copy(out=rowb[i][:, 0:128], in_=rowf[i][:, 0:128]), r=[b_rowf[i]], w=[b_rowb[i]])
            S.op("pe", lambda e, i=i, r_=r_: e.matmul(psK[0:NSMP, 0:128], lhsT=Qm[:, r_, :], rhs=rowb[i][:, 0:128], start=(r_ == 0), stop=(r_ == NSMP - 1)), r=[b_Qm, b_rowb[i]], w=[bpsK])
        S.op("dve", lambda e: e.tensor_scalar(out=s_cols[:, 56:57], in0=col(0), scalar1=-1.0, scalar2=None, op0=ALU.mult), r=[b_scols], w=[b_scols])
        S.op("dve", lambda e: e.scalar_tensor_tensor(out=s_t1[:, 0:128], in0=psK[0:NSMP, 0:128], scalar=s_cols[:, 56:57], in1=s_v2t[:], op0=ALU.mult, op1=ALU.add),
             r=[bpsK, b_scols, b_sv2t], w=[b_st1])
        unpin(bpsK)
        S.op("dve", lambda e: e.tensor_scalar(out=s_t1[:, 0:128], in0=s_t1[:, 0:128], scalar1=col(1), scalar2=None, op0=ALU.mult), r=[b_st1, b_scols], w=[b_st1])
        S.op("act", lambda e: e.copy(out=s_vb[:, 0:128], in_=s_t1[:, 0:128]), r=[b_st1], w=[b_svb])
        masked_q(s_y[:, 0, :], b_sy)
        masked_k(s_k2t[:], b_sk2t)
        psQ, bpsQ = psum(pin=True)
        for r_ in range(NSMP):
            i = st2["rb"] % NRB
            st2["rb"] += 1
            S.dma("sp", "s_row%d" % i, rowf[i][:, 0:128], d_sS[l, r_, h], w=[b_rowf[i]])
            S.op("act", lambda e, i=i: e.copy(out=rowb[i][:, 0:128], in_=rowf[i][:, 0:128]), r=[b_rowf[i]], w=[b_rowb[i]])
            S.op("pe", lambda e, i=i, r_=r_: e.matmul(psQ[0:NSMP, 0:128], lhsT=Qm[:, r_, :], rhs=rowb[i][:, 0:128], start=(r_ == 0), stop=(r_ == NSMP - 1)), r=[b_Qm, b_rowb[i]], w=[bpsQ])
            psD, bpsD = psum()
            S.op("pe", lambda e, r_=r_: e.matmul(psD[:, 0:128], lhsT=Km[:, r_, :], rhs=s_vb[:, 0:128], start=True, stop=True), r=[b_Km, b_svb], w=[bpsD])
            S.op("dve", lambda e, i=i, r_=r_: e.scalar_tensor_tensor(out=rowf[i][:, 0:128], in0=rowf[i][:, 0:128], scalar=s_rep[:, h * NSMP + r_:h * NSMP + r_ + 1], in1=psD[:, 0:128],
                                                                     op0=ALU.mult, op1=ALU.add), r=[b_rowf[i], b_srep, bpsD], w=[b_rowf[i]])
            S.dma("sp", "o_sS%d" % i, o_sS[l, r_, h], rowf[i][:, 0:128], r=[b_rowf[i]])
        dot_rows(s_y[:, 0, :], b_sy, s_y[:, 1, :], b_sy, s_cols[:, 57:58])
        S.op("act", lambda e: e.activation(out=s_t2[:, 0:128], in_=psQ[0:NSMP, 0:128], func=AF.Copy, scale=col(0)), r=[bpsQ, b_scols], w=[b_st2])
        unpin(bpsQ)
        S.op("dve", lambda e: e.scalar_tensor_tensor(out=s_t2[:, 0:128], in0=s_t1[:, 0:128], scalar=s_cols[:, 57:58], in1=s_t2[:, 0:128], op0=ALU.mult, op1=ALU.add),
             r=[b_st1, b_scols, b_st2], w=[b_st2])
        gated_norm_to_mix(s_t2[:, 0:128], 128, gnb[0:NSMP, h * 128:(h + 1) * 128], s_z[:, :], b_sz, [8 + h], TILE, NSMP, [b_st2])

    kvf = sb("kvf", [128, 512]); b_kvf = Buf()
    kvb = sb("kvb", [128, 512], BF16); b_kvb = Buf()
    qtok = sb("qtok", [NSMP, 512], BF16); b_qtok = Buf()
    oh1 = sb("oh1", [NSMP, 128], BF16); b_ohR = Buf()
    sc_all = sb("sc_all", [128, 2, NSMP * 4]); b_sc = Buf()
    sm_p = sb("sm_p", [128, 256]); b_smp = Buf()
    pTs = sb("pTs", [128, 2, 64], BF16); b_pTs = Buf()

    def xattn_sample(l):
        ps, bps = psum()
        for hh in range(4):
            S.op("pe", lambda e, hh=hh: e.transpose(out=ps[0:NSMP, hh * 128:(hh + 1) * 128], in_=qxs[:, hh, :], identity=identf), r=[b_qxs] + CB, w=[bps])
        S.op("act", lambda e: e.copy(out=qtok[:], in_=ps[0:NSMP, 0:512]), r=[bps], w=[b_qtok])
        for r_ in range(NSMP):
            psb, bpsb = psum()
            S.op("dve", lambda e, r_=r_: e.tensor_copy(out=oh1[:], in_=eyeP[:, r_:r_ + 1].to_broadcast([NSMP, 128])), r=CB, w=[b_ohR])
            S.op("pe", lambda e, r_=r_: e.matmul(psb[:, 0:512], lhsT=oh1[:], rhs=qtok[:], start=True, stop=True), r=[b_ohR, b_qtok], w=[bpsb])
            for mt in range(2):
                S.dma("sp", "s_kv", kvf[:], d_ck[l, r_, mt * 128:(mt + 1) * 128, :], w=[b_kvf])
                S.op("dve", lambda e: e.tensor_tensor(out=kvf[:], in0=kvf[:], in1=psb[:, 0:512], op=ALU.mult), r=[b_kvf, bpsb], w=[b_kvf])
                S.op("dve", lambda e, r_=r_, mt=mt: e.tensor_reduce(out=sc_all[:, mt, r_ * 4:(r_ + 1) * 4], in_=kvf[:].rearrange("p (h d) -> p h d", h=4), axis=AX.X, op=ALU.add),
                     r=[b_kvf], w=[b_sc])
        ps2, bps2 = psum()
        for mt in range(2):
            S.op("pe", lambda e, mt=mt: e.transpose(out=ps2[0:64, mt * 128:(mt + 1) * 128], in_=sc_all[:, mt, :], identity=identf), r=[b_sc] + CB, w=[bps2])
        S.op("dve", lambda e: e.reduce_max(out=csm[0:64, 12:13], in_=ps2[0:64, 0:256], axis=AX.X), r=[bps2], w=[b_csm])
        S.op("dve", lambda e: e.tensor_scalar(out=csm[0:64, 12:13], in0=csm[0:64, 12:13], scalar1=-(128.0 ** -0.5), scalar2=None, op0=ALU.mult), r=[b_csm], w=[b_csm])
        S.op("act", lambda e: e.activation(out=sm_p[0:64, :], in_=ps2[0:64, 0:256], func=AF.Exp, bias=csm[0:64, 12:13], scale=128.0 ** -0.5, accum_out=csm[0:64, 13:14]),
             r=[bps2, b_csm], w=[b_smp, b_csm])
        S.op("dve", lambda e: e.reciprocal(out=csm[0:64, 14:15], in_=csm[0:64, 13:14]), r=[b_csm], w=[b_csm])
        S.op("dve", lambda e: e.tensor_scalar(out=sm_p[0:64, :], in0=sm_p[0:64, :], scalar1=csm[0:64, 14:15], scalar2=None, op0=ALU.mult), r=[b_smp, b_csm], w=[b_smp])
        ps3, bps3 = psum()
        for mt in range(2):
            S.op("pe", lambda e, mt=mt: e.transpose(out=ps3[:, mt * 64:(mt + 1) * 64], in_=sm_p[0:64, mt * 128:(mt + 1) * 128], identity=cst[0:64, C_ID:C_ID + 64]), r=[b_smp] + CB, w=[bps3])
        S.op("act", lambda e: e.copy(out=pTs[:], in_=ps3[:, 0:128].rearrange("p (a b) -> p a b", a=2)), r=[bps3], w=[b_pTs])
        pso, bpso = psum(pin=True)
        for r_ in range(NSMP):
            for mt in range(2):
                S.dma("sp", "s_kv", kvf[:], d_cv[l, r_, mt * 128:(mt + 1) * 128, :], w=[b_kvf])
                S.op("act", lambda e: e.copy(out=kvb[:], in_=kvf[:]), r=[b_kvf], w=[b_kvb])
                for hh in range(4):
                    S.op("pe", lambda e, hh=hh, mt=mt, r_=r_: e.matmul(pso[:, mt * 64 + hh * NSMP + r_:mt * 64 + hh * NSMP + r_ + 1], lhsT=kvb[:, hh * 128:(hh + 1) * 128],
                                                                      rhs=pTs[:, mt, r_ * 4 + hh:r_ * 4 + hh + 1], start=True, stop=True), r=[b_kvb, b_pTs], w=[bpso])
        S.op("act", lambda e: e.copy(out=sm_p[:, 0:64], in_=pso[:, 0:64]), r=[bpso], w=[b_smp])
        S.op("dve", lambda e: e.tensor_tensor(out=mixT[:, 0:4, TILE:TILE + NSMP], in0=sm_p[:, 0:64].rearrange("p (a b) -> p a b", a=4),
                                              in1=pso[:, 64:128].rearrange("p (a b) -> p a b", a=4), op=ALU.add), r=[bpso, b_smp], w=[b_mix])
        unpin(bpso)

    rga = rg; rgb = rf; rgc = rux; reg = ra; rngc = rnu
    rt1 = rm
    gcols = sb("gcols", [128, NCK * 3 * 8]); b_gcols = Buf()
    GBx = UBx
    b_GB = b_UB
    q2T = qTb
    b_q2 = b_qT
    k2T = kTb
    b_k2 = b_kT
    nega = sb("nega", [8, 2]); b_nega = Buf()
    ztok = sb("ztok", [128, NCK, 128], BF16); b_ztok = Buf()
    s_z = sb("s_z", [NSMP, 128]); b_sz = Buf()
    s_pre = sb("s_pre", [128, 3, NSMP]); b_spre = Buf()

    def gdn(l, ti):
        smp = (ti == 0 and with_sample)
        ncol = R if smp else TILE

        def ev_g(ps, bps, m, c0, n, gi, si):
            dst = rga if si == 0 else rgb
            if si == 0:
                S.op("dve", lambda e: e.tensor_scalar(out=dst[0:8, c0:c0 + n], in0=ps[0:8, 0:n], scalar1=gateb[0:8, l * 4 + 3:l * 4 + 4], scalar2=None, op0=ALU.add),
                     r=[bps, b_par], w=[b_rows])
            else:
                S.op("act", lambda e: e.activation(out=dst[0:8, c0:c0 + n], in_=ps[0:8, 0:n], func=AF.Sigmoid), r=[bps], w=[b_rows])
        proj(nT, b_n, ti, ev_g, subs=[(0, 8), (8, 16)])
        S.op("act", lambda e: e.activation(out=rt1[0:8, 0:ncol], in_=rga[0:8, 0:ncol], func=AF.Abs), r=[b_rows], w=[b_rows])
        S.op("act", lambda e: e.activation(out=rt1[0:8, 0:ncol], in_=rt1[0:8, 0:ncol], func=AF.Exp, scale=-1.0), r=[b_rows], w=[b_rows])
        S.op("act", lambda e: e.activation(out=rt1[0:8, 0:ncol], in_=rt1[0:8, 0:ncol], func=AF.Ln, bias=1.0), r=[b_rows], w=[b_rows])
        S.op("dve", lambda e: e.scalar_tensor_tensor(out=rt1[0:8, 0:ncol], in0=rga[0:8, 0:ncol], scalar=0.0, in1=rt1[0:8, 0:ncol], op0=ALU.max, op1=ALU.add), r=[b_rows], w=[b_rows])
        S.op("act", lambda e: e.activation(out=nega[0:8, 0:1], in_=gateb[0:8, l * 4 + 2:l * 4 + 3], func=AF.Exp), r=[b_par], w=[b_nega])
        S.op("dve", lambda e: e.tensor_scalar(out=nega[0:8, 0:1], in0=nega[0:8, 0:1], scalar1=-1.0, scalar2=None, op0=ALU.mult), r=[b_nega], w=[b_nega])
        S.op("dve", lambda e: e.tensor_scalar(out=rga[0:8, 0:ncol], in0=rt1[0:8, 0:ncol], scalar1=nega[0:8, 0:1], scalar2=None, op0=ALU.mult), r=[b_rows, b_nega], w=[b_rows])
        S.op("dve", lambda e: e.memset(rgc[0:8, 0:1], 0.0), w=[b_rows])
        S.op("dve", lambda e: e.tensor_tensor_scan(out=rgc[0:8, 1:(TILE + 1)], data0=rga[0:8, 0:TILE], data1=rz[0:8, :], initial=0.0, op0=ALU.add, op1=ALU.add), r=[b_rows], w=[b_rows])
        S.op("dve", lambda e: e.tensor_scalar(out=rngc[0:8, :], in0=rgc[0:8, :], scalar1=-1.0, scalar2=None, op0=ALU.mult), r=[b_rows], w=[b_rows])
        for c in range(NCK):
            S.op("act", lambda e, c=c: e.activation(out=reg[0:8, c * 128:(c + 1) * 128], in_=rgc[0:8, 1 + c * 128:1 + (c + 1) * 128], func=AF.Exp,
                                                    bias=rngc[0:8, c * 128:c * 128 + 1], scale=1.0), r=[b_rows], w=[b_rows])
        rows_to_cols([rgc[:, 1:(TILE + 1)], reg, rgb], [b_rows], 8, gcols, b_gcols)
        split3(rgc, b_rows, 8, (TILE + 1), pcs, b_pcs, ptmp, b_ptmp)
        S.dma("sp", "nb", gnb[:], d_gn[l].partition_broadcast(128), w=[b_nb])

        def gcol(c, qi, h):
            o = (c * 3 + qi) * 8 + h
            return gcols[:, o:o + 1]

        for h in range(8):
            bcast_rows(pcs, b_pcs, 8, h, (TILE + 1), GBx, b_GB)
            for qi in range(3):
                cc = qi * 8 + h
                tb = (l * 24 + cc) * 3
                S.op("pool", lambda e: e.tensor_copy(out=pre[:, 0:3], in_=ctail[:, tb:tb + 3]), r=[b_ctail], w=[b_pre])

                def ev_p(ps, bps, m, c0, n, gi, si):
                    if c0 == TILE:
                        S.op("dve", lambda e: e.tensor_copy(out=s_pre[:, qi, :], in_=ps[:, 0:n]), r=[bps], w=[b_spre])
                    else:
                        S.op("act", lambda e: e.copy(out=pre[:, 3 + c0:3 + c0 + n], in_=ps[:, 0:n]), r=[bps], w=[b_pre])
                proj(nT, b_n, ti, ev_p)
                S.op("pool", lambda e: e.tensor_copy(out=ctail[:, tb:tb + 3], in_=pre[:, TILE:(TILE + 3)]), r=[b_pre], w=[b_ctail])
                if ti == NT - 1:
                    S.dma("sp", "o_pcv", o_pconvT[l, cc * 128:(cc + 1) * 128, :], pre[:, TILE:(TILE + 3)], r=[b_pre])
                wb = (l * 24 + cc) * 4
                S.op("dve", lambda e: e.tensor_scalar(out=cvy[:, 0:TILE], in0=pre[:, 0:TILE], scalar1=convw[:, wb:wb + 1], scalar2=None, op0=ALU.mult), r=[b_pre, b_par], w=[b_cvy])
                for j in range(1, 4):
                    S.op("dve", lambda e, j=j: e.scalar_tensor_tensor(out=cvy[:, 0:TILE], in0=pre[:, j:j + TILE], scalar=convw[:, wb + j:wb + j + 1], in1=cvy[:, 0:TILE],
                                                                      op0=ALU.mult, op1=ALU.add), r=[b_pre, b_par, b_cvy], w=[b_cvy])
                S.op("act", lambda e: e.activation(out=cvy[:, 0:TILE], in_=cvy[:, 0:TILE], func=AF.Silu), r=[b_cvy], w=[b_cvy])
                if qi < 2:
                    dstb, bdst = (q2T, b_q2) if qi == 0 else (k2T, b_k2)
                    sc = (128.0 ** -0.5) if qi == 0 else 1.0
                    for c0 in range(0, TILE, 512):
                        S.op("act", lambda e: e.activation(out=dstb[:, c0:c0 + 512], in_=cvy[:, c0:c0 + 512], func=AF.Square), r=[b_cvy], w=[bdst])
                        ps, bps = psum()
                        S.op("pe", lambda e: e.matmul(ps[:, 0:512], lhsT=onesb[:], rhs=dstb[:, c0:c0 + 512], start=True, stop=True), r=[bdst] + CB, w=[bps])
                        S.op("act", lambda e: e.activation(out=sb_rstd[:, 0:512], in_=ps[:, 0:512], func=AF.Ln, bias=epsc[:], scale=1.0), r=[bps, b_par], w=[b_rstd])
                        S.op("act", lambda e: e.activation(out=sb_rstd[:, 0:512], in_=sb_rstd[:, 0:512], func=AF.Exp, scale=-0.5), r=[b_rstd], w=[b_rstd])
                        if qi == 0:
                            S.op("dve", lambda e: e.scalar_tensor_tensor(out=dstb[:, c0:c0 + 512], in0=cvy[:, c0:c0 + 512], scalar=sc, in1=sb_rstd[:, 0:512], op0=ALU.mult, op1=ALU.mult),
                                 r=[b_cvy, b_rstd], w=[bdst])
                        else:
                            S.op("dve", lambda e: e.tensor_tensor(out=k2f[:, c0:c0 + 512], in0=cvy[:, c0:c0 + 512], in1=sb_rstd[:, 0:512], op=ALU.mult), r=[b_cvy, b_rstd], w=[b_k2f])
                            S.op("act", lambda e: e.copy(out=dstb[:, c0:c0 + 512], in_=k2f[:, c0:c0 + 512]), r=[b_k2f], w=[bdst])
                    if qi == 1:
                        for b0 in range(0, NCK, 4):
                            ps, bps = psum()
                            for jj in range(4):
                                S.op("pe", lambda e, jj=jj: e.transpose(out=ps[:, jj * 128:(jj + 1) * 128], in_=k2f[:, (b0 + jj) * 128:(b0 + jj + 1) * 128], identity=identf),
                                     r=[b_k2f] + CB, w=[bps])
                            S.op("act", lambda e: e.copy(out=ktok[:, b0:b0 + 4, :], in_=ps[:, 0:512].rearrange("p (j c) -> p j c", j=4)), r=[bps], w=[b_ktok])
                else:
                    for b0 in range(0, NCK, 4):
                        ps, bps = psum()
                        for jj in range(4):
                            S.op("pe", lambda e, jj=jj: e.transpose(out=ps[:, jj * 128:(jj + 1) * 128], in_=cvy[:, (b0 + jj) * 128:(b0 + jj + 1) * 128], identity=identf),
                                 r=[b_cvy] + CB, w=[bps])
                        S.op("act", lambda e: e.copy(out=vtok[:, b0:b0 + 4, 0:128], in_=ps[:, 0:512].rearrange("p (j c) -> p j c", j=4)), r=[bps], w=[b_vtok])
                if smp:
                    gdn_sample_conv(l, h, qi)
            wv, bw, _ = getw()

            def ev_z(ps3, bps, j0, nj):
                S.op("act", lambda e: e.activation(out=ztok[:, j0:j0 + nj, :], in_=ps3, func=AF.Silu), r=[bps], w=[b_ztok])

            def ev_zs(ps, bps):
                S.op("act", lambda e: e.activation(out=s_z[:, :], in_=ps, func=AF.Silu), r=[bps], w=[b_sz])
            projT(wv, bw, nT, b_n, ti, ev_z, ev_zs)
            if ti == 0:
                S.op("dve", lambda e: e.memset(Sst[:], 0.0), w=[b_S])
            else:
                S.dma("sp", "scrS_r", Sst[:], d_scrS[l, h], r=[b_scr], w=[b_S])
            S.op("act", lambda e: e.copy(out=Sb[:], in_=Sst[:]), r=[b_S], w=[b_Sb])
            for c in range(NCK):
                t0 = c * 128
                psa, bpsa = psum()
                S.op("pe", lambda e: e.matmul(psa[:, 0:128], lhsT=k2T[:, t0:t0 + 128], rhs=k2T[:, t0:t0 + 128], start=True, stop=True), r=[b_k2], w=[bpsa])
                psq, bpsq = psum()
                S.op("pe", lambda e: e.matmul(psq[:, 0:128], lhsT=k2T[:, t0:t0 + 128], rhs=q2T[:, t0:t0 + 128], start=True, stop=True), r=[b_k2, b_q2], w=[bpsq])
                S.op("dve", lambda e: e.scalar_tensor_tensor(out=cX[:], in0=GBx[:, 1 + t0:1 + t0 + 128], scalar=gcol(c, 0, h), in1=negi, op0=ALU.subtract, op1=ALU.add),
                     r=[b_GB, b_gcols] + CB, w=[b_cX])
                S.op("act", lambda e: e.activation(out=cE[:], in_=cX[:], func=AF.Exp), r=[b_cX], w=[b_cE])
                S.op("dve", lambda e: e.tensor_tensor(out=cQK[:], in0=cE[:], in1=psq[:, 0:128], op=ALU.mult), r=[b_cE, bpsq], w=[b_QK])
                S.op("dve", lambda e: e.tensor_tensor(out=cAT[:], in0=cE[:], in1=psa[:, 0:128], op=ALU.mult), r=[b_cE, bpsa], w=[b_AT])
                S.op("dve", lambda e: e.scalar_tensor_tensor(out=cAT[:], in0=cAT[:], scalar=gcol(c, 2, h), in1=mstrict, op0=ALU.mult, op1=ALU.mult), r=[b_AT, b_gcols] + CB, w=[b_AT])
                S.op("dve", lambda e: e.tensor_tensor(out=cX[:], in0=cAT[:], in1=cst[:, C_MK:C_MK + 128], op=ALU.mult), r=[b_AT] + CB, w=[b_cX])
                S.op("dve", lambda e: e.tensor_tensor(out=cX[:], in0=identf, in1=cX[:], op=ALU.subtract), r=[b_cX] + CB, w=[b_cX])
                S.op("act", lambda e: e.copy(out=cPT[0][:], in_=cX[:]), r=[b_cX], w=[b_PT[0]])
                pst, bpst = psum()
                S.op("pe", lambda e: e.transpose(out=pst[:, 0:128], in_=cX[:], identity=identf), r=[b_cX] + CB, w=[bpst])
                S.op("act", lambda e: e.copy(out=cP[0][:], in_=pst[:, 0:128]), r=[bpst], w=[b_P[0]])
                for lv in range(6):
                    S.op("pool", lambda e, lv=lv: e.tensor_tensor(out=cBk[lv][:], in0=cAT[:], in1=cst[:, C_MK + (lv + 1) * 128:C_MK + (lv + 2) * 128], op=ALU.mult),
                         r=[b_AT] + CB, w=[b_Bk[lv]])
                pi = 0
                for lv in range(6):
                    po = 1 - pi
                    psw, bpsw = psum()
                    S.op("pe", lambda e, lv=lv, pi=pi: e.matmul(psw[:, 0:128], lhsT=cBk[lv][:], rhs=cP[pi][:], start=True, stop=True), r=[b_Bk[lv], b_P[pi]], w=[bpsw])
                    S.op("act", lambda e: e.copy(out=cTT[0][:], in_=psw[:, 0:128]), r=[bpsw], w=[b_TT[0]])
                    ps1, bps1 = psum()
                    S.op("pe", lambda e, pi=pi: e.matmul(ps1[:, 0:128], lhsT=cTT[0][:], rhs=cPT[pi][:], start=True, stop=True), r=[b_TT[0], b_PT[pi]], w=[bps1])
                    if lv < 5:
                        ps2, bps2 = psum()
                        S.op("pe", lambda e, pi=pi: e.matmul(ps2[:, 0:128], lhsT=cPT[pi][:], rhs=cTT[0][:], start=True, stop=True), r=[b_TT[0], b_PT[pi]], w=[bps2])
                    S.op("dve", lambda e, pi=pi, po=po: e.tensor_tensor(out=cPT[po][:], in0=cPT[pi][:], in1=ps1[:, 0:128], op=ALU.subtract), r=[b_PT[pi], bps1], w=[b_PT[po]])
                    if lv < 5:
                        S.op("dve", lambda e, pi=pi, po=po: e.tensor_tensor(out=cP[po][:], in0=cP[pi][:], in1=ps2[:, 0:128], op=ALU.subtract), r=[b_P[pi], bps2], w=[b_P[po]])
                    pi = po
                TTf, bTTf = cPT[pi], b_PT[pi]
                psk, bpsk = psum()
                S.op("pe", lambda e: e.matmul(psk[:, 0:128], lhsT=k2T[:, t0:t0 + 128], rhs=Sb[:], start=True, stop=True), r=[b_k2, b_Sb], w=[bpsk])
                psqs, bpsqs = psum()
                S.op("pe", lambda e: e.matmul(psqs[:, 0:128], lhsT=q2T[:, t0:t0 + 128], rhs=Sb[:], start=True, stop=True), r=[b_q2, b_Sb], w=[bpsqs])
                S.op("dve", lambda e: e.tensor_scalar(out=csm[:, 6:7], in0=gcol(c, 1, h), scalar1=-1.0, scalar2=None, op0=ALU.mult), r=[b_gcols], w=[b_csm])
                S.op("dve", lambda e: e.scalar_tensor_tensor(out=crhs[:], in0=psk[:, 0:128], scalar=csm[:, 6:7], in1=vtok[:, c, 0:128], op0=ALU.mult, op1=ALU.add),
                     r=[bpsk, b_csm, b_vtok], w=[b_rhs])
                psu, bpsu = psum()
                S.op("pe", lambda e: e.matmul(psu[:, 0:128], lhsT=TTf[:], rhs=crhs[:], start=True, stop=True), r=[bTTf, b_rhs], w=[bpsu])
                S.op("act", lambda e: e.activation(out=cwu[:], in_=psu[:, 0:128], func=AF.Copy, scale=gcol(c, 2, h)), r=[bpsu, b_gcols], w=[b_wu])
                S.op("act", lambda e: e.activation(out=co1[:], in_=psqs[:, 0:128], func=AF.Copy, scale=gcol(c, 1, h)), r=[bpsqs, b_gcols], w=[b_o1])
                pso, bpso = psum()
                S.op("pe", lambda e: e.matmul(pso[:, 0:128], lhsT=cQK[:], rhs=cwu[:], start=True, stop=True), r=[b_QK, b_wu], w=[bpso])
                S.op("dve", lambda e: e.tensor_tensor(out=cnum[:, 0:128], in0=co1[:], in1=pso[:, 0:128], op=ALU.add), r=[b_o1, bpso], w=[b_num])
                gated_norm_to_mix(cnum[:, 0:128], 128, gnb[:, h * 128:(h + 1) * 128], ztok[:, c, :], b_ztok, [8 + h], t0, 128, [b_num])
                S.op("act", lambda e: e.activation(out=csm[:, 7:8], in_=gcol(c, 0, h), func=AF.Exp, bias=GBx[:, t0 + 128:t0 + 129], scale=-1.0), r=[b_gcols, b_GB], w=[b_csm])
                S.op("dve", lambda e: e.tensor_scalar(out=ckw[:], in0=ktok[:, c, :], scalar1=csm[:, 7:8], scalar2=None, op0=ALU.mult), r=[b_ktok, b_csm], w=[b_kw])
                psd, bpsd = psum()
                S.op("pe", lambda e: e.matmul(psd[:, 0:128], lhsT=ckw[:], rhs=cwu[:], start=True, stop=True), r=[b_kw, b_wu], w=[bpsd])
                S.op("dve", lambda e: e.tensor_tensor(out=csm[:, 8:9], in0=GBx[:, t0 + 128:t0 + 129], in1=GBx[:, t0:t0 + 1], op=ALU.subtract), r=[b_GB], w=[b_csm])
                S.op("act", lambda e: e.activation(out=csm[:, 8:9], in_=csm[:, 8:9], func=AF.Exp), r=[b_csm], w=[b_csm])
                S.op("dve", lambda e: e.scalar_tensor_tensor(out=Sst[:], in0=Sst[:], scalar=csm[:, 8:9], in1=psd[:, 0:128], op0=ALU.mult, op1=ALU.add), r=[b_S, b_csm, bpsd], w=[b_S])
                S.op("act", lambda e: e.copy(out=Sb[:], in_=Sst[:]), r=[b_S], w=[b_Sb])
            if ti < NT - 1:
                S.dma("sp", "scrS_w", d_scrS[l, h], Sst[:], r=[b_S], w=[b_scr])
            else:
                S.dma("sp", "o_pS", o_pS[l, h], Sst[:], r=[b_S])
            if smp:
                gdn_sample(l, h)
    memh = sb("memh", [128, 8, 256]); b_memh = Buf()
    d_memv = d_memT.rearrange("(c p) m -> p c m", p=128)

    def mem_norm(l):
        gcol0 = (l * 4 + 3) * 16
        ps, bps = psum()
        for hf in range(2):
            S.dma("sp", "mem", memh[:], d_memv[:, hf * 8:(hf + 1) * 8, :], w=[b_memh])
            S.op("act", lambda e, hf=hf: e.activation(out=mhT[:, hf * 8:(hf + 1) * 8, :], in_=memh[:], func=AF.Square), r=[b_memh], w=[b_mh])
            for k in range(8):
                kk = hf * 8 + k
                S.op("pe", lambda e, kk=kk: e.matmul(ps[:, 0:256], lhsT=onesb[:], rhs=mhT[:, kk, :], start=(kk == 0), stop=(kk == 15)), r=[b_mh] + CB, w=[bps])
        S.op("act", lambda e: e.activation(out=sb_rstd[:, 0:256], in_=ps[:, 0:256], func=AF.Ln, bias=epsc[:], scale=1.0 / D), r=[bps, b_par], w=[b_rstd])
        S.op("act", lambda e: e.activation(out=sb_rstd[:, 0:256], in_=sb_rstd[:, 0:256], func=AF.Exp, scale=-0.5), r=[b_rstd], w=[b_rstd])
        for hf in range(2):
            S.dma("sp", "mem", memh[:], d_memv[:, hf * 8:(hf + 1) * 8, :], w=[b_memh])
            for k in range(8):
                kk = hf * 8 + k
                S.op("dve", lambda e, k=k, kk=kk: e.scalar_tensor_tensor(out=mhT[:, kk, :], in0=memh[:, k, :], scalar=gains[:, gcol0 + kk:gcol0 + kk + 1], in1=sb_rstd[:, 0:256],
                                                                          op0=ALU.mult, op1=ALU.mult), r=[b_memh, b_rstd, b_par], w=[b_mh])
    mhT = mixT[:, :, :].rearrange("p c t -> p (c t)")[:, 0:4096].rearrange("p (c m) -> p c m", c=16)
    b_mh = b_mix
    KT = sb("KT", [128, 4, 256], BF16); b_KT = Buf()
    KTf = sb("KTf", [128, 256]); b_KTf = Buf()
    Vm = sb("Vm", [128, 2, 512], BF16); b_Vm = Buf()
    Vf = sb("Vf", [128, 128]); b_Vf = Buf()
    qx = sb("qx", [128, 4, R], BF16); b_qx = Buf()
    qxs = sb("qxs", [128, 4, NSMP]); b_qxs = Buf()
    pex = sb("pex", [128, 256]); b_pex = Buf()
    pTb = sb("pTb", [128, 2, 128], BF16); b_pT = Buf()

    def xattn(l, ti):
        smp = (ti == 0 and with_sample)
        mem_norm(l)
        if stage == 5.1:
            return
        for j in range(4):
            def ev_k(ps, bps, m, c0, n, gi, si, j=j):
                S.op("dve", lambda e: e.tensor_copy(out=KTf[:], in_=ps[:, 0:256]), r=[bps], w=[b_KTf])
                S.op("act", lambda e: e.copy(out=KT[:, j, :], in_=KTf[:]), r=[b_KTf], w=[b_KT])
                if ti == 0:
                    S.dma("sp", "o_pk", o_pkT[l, j * 128:(j + 1) * 128, :], KTf[:], r=[b_KTf])
            proj(mhT, b_mh, ti, ev_k, grp=[(0, 256)])
        if stage == 5.2:
            return
        for j in range(4):
            wv, bw, _ = getw()

            def ev_v(ps3, bps, j0, nj, j=j):
                for mt in range(2):
                    S.op("dve", lambda e, mt=mt: e.tensor_copy(out=Vf[:, 0:128], in_=ps3[:, mt, :]), r=[bps], w=[b_Vf])
                    S.op("act", lambda e, mt=mt: e.copy(out=Vm[:, mt, j * 128:(j + 1) * 128], in_=Vf[:, 0:128]), r=[b_Vf], w=[b_Vm])
                    if ti == 0:
                        S.dma("sp", "o_pv", o_pv[l, mt * 128:(mt + 1) * 128, j * 128:(j + 1) * 128], Vf[:, 0:128], r=[b_Vf])
            projT(wv, bw, mhT, b_mh, 1, ev_v, None, ntt=2)
        if stage == 5.3:
            return
        rmsnorm_F(ti, (l * 4 + 1) * 16, nT, b_n)
        for j in range(4):
            def ev_q(ps, bps, m, c0, n, gi, si, j=j):
                if c0 == TILE:
                    S.op("dve", lambda e: e.tensor_copy(out=qxs[:, j, :], in_=ps[:, 0:n]), r=[bps], w=[b_qxs])
                    S.op("act", lambda e: e.copy(out=qx[:, j, c0:c0 + n], in_=qxs[:, j, :]), r=[b_qxs], w=[b_qx])
                else:
                    S.op("act", lambda e: e.copy(out=qx[:, j, c0:c0 + n], in_=ps[:, 0:n]), r=[bps], w=[b_qx])
            proj(nT, b_n, ti, ev_q)
        if stage == 5.4:
            return
        for tt in range(NCK):
            t0 = tt * 128
            for hh in range(4):
                ps, bps = psum()
                S.op("pe", lambda e: e.matmul(ps[:, 0:256], lhsT=qx[:, hh, t0:t0 + 128], rhs=KT[:, hh, :], start=True, stop=True), r=[b_qx, b_KT], w=[bps])
                S.op("dve", lambda e: e.reduce_max(out=csm[:, 9:10], in_=ps[:, 0:256], axis=AX.X), r=[bps], w=[b_csm])
                S.op("dve", lambda e: e.tensor_scalar(out=csm[:, 9:10], in0=csm[:, 9:10], scalar1=-(128.0 ** -0.5), scalar2=None, op0=ALU.mult), r=[b_csm], w=[b_csm])
                S.op("act", lambda e: e.activation(out=pex[:], in_=ps[:, 0:256], func=AF.Exp, bias=csm[:, 9:10], scale=128.0 ** -0.5, accum_out=csm[:, 10:11]),
                     r=[bps, b_csm], w=[b_pex, b_csm])
                S.op("dve", lambda e: e.reciprocal(out=csm[:, 11:12], in_=csm[:, 10:11]), r=[b_csm], w=[b_csm])
                S.op("dve", lambda e: e.tensor_scalar(out=pex[:], in0=pex[:], scalar1=csm[:, 11:12], scalar2=None, op0=ALU.mult), r=[b_pex, b_csm], w=[b_pex])
                ps2, bps2 = psum()
                for mt in range(2):
                    S.op("pe", lambda e, mt=mt: e.transpose(out=ps2[:, mt * 128:(mt + 1) * 128], in_=pex[:, mt * 128:(mt + 1) * 128], identity=identf), r=[b_pex] + CB, w=[bps2])
                S.op("act", lambda e: e.copy(out=pTb[:], in_=ps2[:, 0:256].rearrange("p (a b) -> p a b", a=2)), r=[bps2], w=[b_pT])
                ps3, bps3 = psum()
                for mt in range(2):
                    S.op("pe", lambda e, mt=mt: e.matmul(ps3[:, 0:128], lhsT=Vm[:, mt, hh * 128:(hh + 1) * 128], rhs=pTb[:, mt, :], start=(mt == 0), stop=(mt == 1)),
                         r=[b_Vm, b_pT], w=[bps3])
                S.op("act", lambda e: e.copy(out=mixT[:, hh, t0:t0 + 128], in_=ps3[:, 0:128]), r=[bps3], w=[b_mix])
        if stage == 5.5:
            return
        if smp:
            xattn_sample(l)
        for j in range(16):
            proj(mixT, b_mix, ti, resid_evac(j), nk=4)

    hg = sb_rstd
    b_hg = b_rstd

    def ffn(l, ti):
        rmsnorm_F(ti, (l * 4 + 2) * 16, nT, b_n)
        for g in range(4):
            for c in range(11):
                wg, bwg, _ = getw()
                wu_, bwu, _ = getw()
                for (c0, n) in groups(ti):
                    psg, bpsg = psum()
                    psu, bpsu = psum()
                    for k in range(16):
                        S.op("pe", lambda e, k=k: e.matmul(psg[:, 0:n], lhsT=wg[:, k, :], rhs=nT[:, k, c0:c0 + n], start=(k == 0), stop=(k == 15)), r=[bwg, b_n], w=[bpsg])
                    for k in range(16):
                        S.op("pe", lambda e, k=k: e.matmul(psu[:, 0:n], lhsT=wu_[:, k, :], rhs=nT[:, k, c0:c0 + n], start=(k == 0), stop=(k == 15)), r=[bwu, b_n], w=[bpsu])
                    S.op("act", lambda e: e.activation(out=hg[:, 0:n], in_=psg[:, 0:n], func=AF.Silu), r=[bpsg], w=[b_hg])
                    S.op("dve", lambda e: e.tensor_tensor(out=mixT[:, c, c0:c0 + n], in0=hg[:, 0:n], in1=psu[:, 0:n], op=ALU.mult), r=[b_hg, bpsu], w=[b_mix])
            for j in range(16):
                proj(mixT, b_mix, ti, resid_evac(j), nk=11)

    def mixer(l, ti):
        rmsnorm_F(ti, (l * 4 + 0) * 16, nT, b_n)
        mlstm(l, ti)
        gdn(l, ti)
        for j in range(16):
            proj(mixT, b_mix, ti, resid_evac(j))

    for ti in range(NT):
        S.dma("sp", "x", xT[:, :, 0:TILE], d_xT[:, ti * TILE:(ti + 1) * TILE].rearrange("(c p) t -> p c t", p=128), w=[b_x])
        if ti == 0 and with_sample:
            S.dma("sp", "x", xT[:, :, TILE:TILE + NSMP], d_xsT.rearrange("(c p) t -> p c t", p=128), w=[b_x])
        for l in range(NL):
            st["layer"] = l
            st["wi"] = 0
            if stage >= 6:
                mixer(l, ti)
                xattn(l, ti)
                ffn(l, ti)
                assert st["wi"] == len(WBLOCKS), (st["wi"], len(WBLOCKS))
            else:
                rmsnorm_F(ti, (l * 4 + 0) * 16, nT, b_n)
                if stage >= 2:
                    mlstm(l, ti)
                if stage >= 3:
                    gdn(l, ti)
                if stage >= 4:
                    for j in range(16):
                        proj(mixT, b_mix, ti, resid_evac(j))
                if stage >= 5:
                    xattn(l, ti)
        for (c0, n) in groups(ti):
            S.op("act", lambda e: e.activation(out=nT[:, :, c0:c0 + n], in_=xT[:, :, c0:c0 + n], func=AF.Square), r=[b_x], w=[b_n])
            ps, bps = psum()
            for k in range(NCH):
                S.op("pe", lambda e, k=k: e.matmul(ps[:, 0:n], lhsT=onesb[:], rhs=nT[:, k, c0:c0 + n], start=(k == 0), stop=(k == NCH - 1)), r=[b_n] + CB, w=[bps])
            S.op("act", lambda e: e.activation(out=sb_rstd[:, 0:n], in_=ps[:, 0:n], func=AF.Ln, bias=epsc[:], scale=1.0 / D), r=[bps, b_par], w=[b_rstd])
            S.op("act", lambda e: e.activation(out=sb_rstd[:, 0:n], in_=sb_rstd[:, 0:n], func=AF.Exp, scale=-0.5), r=[b_rstd], w=[b_rstd])
            gc = NL * 4 * 16
            for k in range(NCH):
                S.op("dve", lambda e, k=k: e.scalar_tensor_tensor(out=xT[:, k, c0:c0 + n], in0=xT[:, k, c0:c0 + n], scalar=gains[:, gc + k:gc + k + 1],
                                                                  in1=sb_rstd[:, 0:n], op0=ALU.mult, op1=ALU.mult), r=[b_x, b_rstd, b_par], w=[b_x])
        S.dma("sp", "oy", o_yT[:, ti * TILE:(ti + 1) * TILE].rearrange("(c p) t -> p c t", p=128), xT[:, :, 0:TILE], r=[b_x])
        if ti == 0 and with_sample:
            S.dma("sp", "oy", o_ysT.rearrange("(c p) t -> p c t", p=128), xT[:, :, TILE:TILE + NSMP], r=[b_x])
    S.finish()
    print('sbuf bytes remaining', nc.sbuf_bytes_remaining, 'instr counts', dict(S.cnt))
    return nc, es


def _consts():
    c = np.zeros((128, CW), np.float32)
    c[:, C_ID:C_ID + 128] = np.eye(128, dtype=np.float32)
    s = np.arange(128)[:, None]
    t = np.arange(128)[None, :]
    c[:, C_NI:C_NI + 128] = np.where(s <= t, 0.0, NEG)
    c[:, C_MS:C_MS + 128] = (s < t).astype(np.float32)
    c[:, C_ONE:C_ONE + 128] = 1.0
    c[:, C_MK:C_MK + 128] = (s // 2 == t // 2)
    bs = 2
    for lv in range(6):
        c[:, C_MK + (lv + 1) * 128:C_MK + (lv + 2) * 128] = (s // (2 * bs) == t // (2 * bs)) & (s // bs != t // bs)
        bs *= 2
    c[:, C_E16:C_E16 + 256] = np.eye(16, dtype=np.float32).reshape(1, 256)
    return c


def _pack_weights(inp, NL):
    w = np.empty((NL, 128, WTOT), np.float32)
    for i, (mat, k0, nk, c0, ncw) in enumerate(WBLOCKS):
        src = inp[mat]
        for l in range(NL):
            blk = src[l, k0 * 128:(k0 + nk) * 128, c0:c0 + ncw].reshape(nk, 128, ncw)
            w[l, :, WOFF[i]:WOFF[i + 1]] = blk.transpose(1, 0, 2).reshape(128, nk * ncw)
    return w


def _fm(v):
    return np.ascontiguousarray(v.reshape(16, 128).T)


def run(inp, NL=DEPTH, NT=SEQ // TILE, with_sample=True, trace=False, stage=99, ncores=8):
    inp = {k: np.asarray(v) for k, v in inp.items()}
    nc, es = build(NL=NL, NT=NT, with_sample=with_sample, stage=stage)
    wts = _pack_weights(inp, NL)
    cst = _consts()
    gains = np.zeros((128, (NL * 4 + 1) * 16), np.float32)
    for l in range(NL):
        for wi, nm in enumerate(("norm_mix", "norm_xattn", "norm_ffn", "norm_mem")):
            gains[:, (l * 4 + wi) * 16:(l * 4 + wi + 1) * 16] = _fm(inp[nm][l])
    gains[:, NL * 64:NL * 64 + 16] = _fm(inp["norm_final"])
    gateb = np.zeros((8, NL * 4), np.float32)
    for l in range(NL):
        gateb[0:4, l * 4 + 0] = inp["mlstm_b_i"][l]
        gateb[0:4, l * 4 + 1] = inp["mlstm_b_f"][l]
        gateb[0:8, l * 4 + 2] = inp["gdn_A_log"][l]
        gateb[0:8, l * 4 + 3] = inp["gdn_dt_bias"][l]
    mnorm = np.ascontiguousarray(inp["mlstm_norm"][:NL].reshape(NL, 1024))
    gnorm = np.ascontiguousarray(inp["gdn_norm"][:NL].reshape(NL, 1024))
    convw = np.zeros((128, NL * 24 * 4), np.float32)
    for l in range(NL):
        cw = inp["gdn_conv_w"][l]
        convw[:, l * 96:(l + 1) * 96] = cw.reshape(4, 24, 128).transpose(2, 1, 0).reshape(128, 96)
    in_maps = []
    sel = np.zeros((8, 1024), np.float32)
    for h in range(8):
        sel[h, h * 128:(h + 1) * 128] = 1.0
    for c in range(8):
        b = c % 4
        r0 = c * NSMP
        m = {
            "xT": np.ascontiguousarray(inp["x_prompt"][b].T),
            "xsT": np.ascontiguousarray(inp["x_sample"][r0:r0 + NSMP, 0, :].T),
            "memT": np.ascontiguousarray(inp["mem_prompt"][b].T),
            "wts": wts, "cst": cst, "sel": sel, "gains": gains, "gateb": gateb, "mnorm": mnorm, "gnorm": gnorm, "convw": convw,
            "sC": np.ascontiguousarray(inp["state_mlstm_C"][:NL, r0:r0 + NSMP]),
            "sn": np.ascontiguousarray(inp["state_mlstm_n"][:NL, r0:r0 + NSMP].reshape(NL, NSMP, 512)),
            "sm": np.ascontiguousarray(inp["state_mlstm_m"][:NL, r0:r0 + NSMP]),
            "sS": np.ascontiguousarray(inp["state_gdn_S"][:NL, r0:r0 + NSMP]),
            "sconv": np.ascontiguousarray(inp["state_gdn_conv"][:NL, r0:r0 + NSMP]),
            "ck": np.ascontiguousarray(inp["cache_mem_k"][:NL, r0:r0 + NSMP].reshape(NL, NSMP, 256, 512)),
            "cv": np.ascontiguousarray(inp["cache_mem_v"][:NL, r0:r0 + NSMP].reshape(NL, NSMP, 256, 512)),
        }
        in_maps.append(m)
    res = run_bass_kernel_spmd(nc, in_maps[:ncores], core_ids=list(range(ncores)), trace=trace)
    R_ = list(res.results) + [res.results[0]] * (8 - ncores)
    B = 4
    y_prompt = np.stack([R_[b]["o_yT"].T for b in range(B)])
    y_sample = np.concatenate([R_[c]["o_ysT"].T for c in range(8)], 0)[:, None, :]
    pC = np.stack([R_[b]["o_pC"] for b in range(B)], 1)
    pn = np.stack([R_[b]["o_pn"] for b in range(B)], 1)
    pm = np.stack([R_[b]["o_pm"] for b in range(B)], 1)
    pS = np.stack([R_[b]["o_pS"] for b in range(B)], 1)
    pconv = np.stack([R_[b]["o_pconvT"].transpose(0, 2, 1) for b in range(B)], 1)
    pk = np.stack([R_[b]["o_pkT"].transpose(0, 2, 1).reshape(NL, 256, 4, 128) for b in range(B)], 1)
    pv = np.stack([R_[b]["o_pv"].reshape(NL, 256, 4, 128) for b in range(B)], 1)
    sC = np.concatenate([R_[c]["o_sC"] for c in range(8)], 1)
    sn = np.concatenate([R_[c]["o_sn"].reshape(NL, NSMP, 4, 128) for c in range(8)], 1)
    sm = np.concatenate([R_[c]["o_sm"] for c in range(8)], 1)
    sS = np.concatenate([R_[c]["o_sS"] for c in range(8)], 1)
    sconv = np.concatenate([R_[c]["o_sconv"] for c in range(8)], 1)
    outs = (y_prompt, y_sample, pC, pn, pm, pS, pconv, pk, pv, sC, sn, sm, sS, sconv)
    outs = tuple(np.ascontiguousarray(o, dtype=np.float32) for o in outs)
    if trace:
        return outs, res
    return outs


def kernel(**inputs):
    return run(inputs)
```

```python
from contextlib import ExitStack
import numpy as np
import concourse.bass as bass
import concourse.mybir as mybir
from concourse.bass_utils import run_bass_kernel_spmd

F32 = mybir.dt.float32
BF16 = mybir.dt.bfloat16
ALU = mybir.AluOpType
AF = mybir.ActivationFunctionType
AX = mybir.AxisListType

D = 2048
NCH = 16
DEPTH = 4
SEQ = 2048
TILE = 512
NCK = TILE // 128
NSMP = 16
DFF = 5632
NIN = 7192
EPS = 1e-6
NEG = -30000.0

C_ID = 0
C_NI = 128
C_MS = 256
C_SEL = 384
C_ONE = 384
C_E16 = 512
C_MK = 768
CW = 768 + 7 * 128


class Buf:
    __slots__ = ("w", "r")

    def __init__(self):
        self.w = None
        self.r = {}


class Sched:
    def __init__(self, nc, es):
        self.nc = nc
        self.es = es
        self.eng = {"pe": nc.tensor, "act": nc.scalar, "dve": nc.vector, "pool": nc.gpsimd, "sp": nc.sync}
        self.sem = {}
        self.cnt = {}
        self.waited = {e: {} for e in self.eng}
        for e in self.eng:
            self.sem[e] = es.enter_context(nc.semaphore("s_" + e))
            self.cnt[e] = 0

    def _deps(self, e, r, w):
        deps = {}

        def add(s, v):
            if deps.get(s, 0) < v:
                deps[s] = v

        for b in r:
            if b.w is not None:
                add(*b.w)
        for b in w:
            if b.w is not None:
                add(*b.w)
            for s, v in b.r.items():
                add(s, v)
        for s, v in deps.items():
            if e == "pe" and s == "pe":
                continue
            if self.waited[e].get(s, 0) >= v:
                continue
            self.eng[e].wait_ge(self.sem[s], v)
            self.waited[e][s] = v

    def _upd(self, tok, r, w):
        for b in w:
            b.w = tok
            b.r = {}
        s, v = tok
        for b in r:
            if b in w:
                continue
            if b.r.get(s, 0) < v:
                b.r[s] = v

    def op(self, e, fn, r=(), w=()):
        self._deps(e, r, w)
        ins = fn(self.eng[e])
        self.cnt[e] += 1
        ins.then_inc(self.sem[e], 1)
        tok = (e, self.cnt[e])
        self._upd(tok, r, w)
        return tok

    def dma(self, q, stream, out, in_, r=(), w=()):
        if stream not in self.sem:
            self.sem[stream] = self.es.enter_context(self.nc.semaphore("d_" + stream))
            self.cnt[stream] = 0
        self._deps(q, r, w)
        ins = self.eng[q].dma_start(out=out, in_=in_)
        self.cnt[stream] += 16
        ins.then_inc(self.sem[stream], 16)
        tok = (stream, self.cnt[stream])
        self._upd(tok, r, w)
        return tok

    def finish(self, q="sp"):
        for s, v in self.cnt.items():
            if v > 0 and self.waited[q].get(s, 0) < v:
                self.eng[q].wait_ge(self.sem[s], v)
                self.waited[q][s] = v


def weight_blocks():
    bl = []
    bl.append(("w_in", 0, 16, 3072, 8))
    for h in range(4):
        bl.append(("w_in", 0, 16, h * 128, 128))
        bl.append(("w_in", 0, 16, 512 + h * 128, 128))
        for j in range(2):
            bl.append(("w_in", 0, 16, 1024 + h * 256 + j * 128, 128))
        for j in range(2):
            bl.append(("w_in", 0, 16, 2048 + h * 256 + j * 128, 128))
    bl.append(("w_in", 0, 16, 7176, 16))
    for h in range(8):
        bl.append(("w_in", 0, 16, 3080 + h * 128, 128))
        bl.append(("w_in", 0, 16, 4104 + h * 128, 128))
        bl.append(("w_in", 0, 16, 5128 + h * 128, 128))
        bl.append(("w_in", 0, 16, 6152 + h * 128, 128))
    for j in range(16):
        bl.append(("w_out", 0, 16, j * 128, 128))
    for j in range(4):
        bl.append(("xattn_wk", 0, 16, j * 128, 128))
    for j in range(4):
        bl.append(("xattn_wv", 0, 16, j * 128, 128))
    for j in range(4):
        bl.append(("xattn_wq", 0, 16, j * 128, 128))
    for j in range(16):
        bl.append(("xattn_wo", 0, 4, j * 128, 128))
    for g in range(4):
        for c in range(11):
            bl.append(("ffn_w_gate", 0, 16, (g * 11 + c) * 128, 128))
            bl.append(("ffn_w_up", 0, 16, (g * 11 + c) * 128, 128))
        for j in range(16):
            bl.append(("ffn_w_down", g * 11, 11, j * 128, 128))
    return bl


WBLOCKS = weight_blocks()
WOFF = np.cumsum([0] + [b[2] * b[4] for b in WBLOCKS]).tolist()
WTOT = WOFF[-1]


def build(NL=DEPTH, NT=4, stage=99, with_sample=True):
    nc = bass.Bass("TRN2", target_bir_lowering=False)
    es = ExitStack()
    S = Sched(nc, es)

    def din(name, shape):
        return nc.dram_tensor(name, list(shape), F32, kind="ExternalInput").ap()

    def dout(name, shape):
        return nc.dram_tensor(name, list(shape), F32, kind="ExternalOutput").ap()

    def dscr(name, shape):
        return nc.dram_tensor(name, list(shape), F32).ap()

    def sb(name, shape, dt=F32):
        return es.enter_context(nc.sbuf_tensor("sb_" + name, list(shape), dt))

    d_xT = din("xT", [D, SEQ])
    d_xsT = din("xsT", [D, NSMP])
    d_memT = din("memT", [D, 256])
    d_w = din("wts", [NL, 128, WTOT])
    d_cst = din("cst", [128, CW])
    d_gain = din("gains", [128, (NL * 4 + 1) * 16])
    d_gb = din("gateb", [8, NL * 4])
    d_mn = din("mnorm", [NL, 4 * 256])
    d_gn = din("gnorm", [NL, 8 * 128])
    d_cw = din("convw", [128, NL * 24 * 4])
    d_sC = din("sC", [NL, NSMP, 4, 128, 256])
    d_sn = din("sn", [NL, NSMP, 4 * 128])
    d_sm = din("sm", [NL, NSMP, 4])
    d_sS = din("sS", [NL, NSMP, 8, 128, 128])
    d_sconv = din("sconv", [NL, NSMP, 3, 3072])
    d_ck = din("ck", [NL, NSMP, 256, 512])
    d_cv = din("cv", [NL, NSMP, 256, 512])

    o_yT = dout("o_yT", [D, SEQ])
    o_ysT = dout("o_ysT", [D, NSMP])
    o_pC = dout("o_pC", [NL, 4, 128, 256])
    o_pn = dout("o_pn", [NL, 4, 128])
    o_pm = dout("o_pm", [NL, 4])
    o_pS = dout("o_pS", [NL, 8, 128, 128])
    o_pconvT = dout("o_pconvT", [NL, 3072, 3])
    o_pkT = dout("o_pkT", [NL, 512, 256])
    o_pv = dout("o_pv", [NL, 256, 512])
    o_sC = dout("o_sC", [NL, NSMP, 4, 128, 256])
    o_sn = dout("o_sn", [NL, NSMP, 4 * 128])
    o_sm = dout("o_sm", [NL, NSMP, 4])
    o_sS = dout("o_sS", [NL, NSMP, 8, 128, 128])
    o_sconv = dout("o_sconv", [NL, NSMP, 3, 3072])

    cst = sb("cst", [128, CW])
    b_cst = Buf()
    S.dma("sp", "c0", cst[:], d_cst, w=[b_cst])
    identb = sb("identb", [128, 128], BF16)
    onesb = sb("onesb", [128, 128], BF16)
    selb = sb("selb", [8, 1024], BF16)
    b_cb = Buf()
    S.op("pool", lambda e: e.tensor_copy(out=identb[:], in_=cst[:, C_ID:C_ID + 128]), r=[b_cst], w=[b_cb])
    S.op("pool", lambda e: e.tensor_copy(out=onesb[:], in_=cst[:, C_ONE:C_ONE + 128]), r=[b_cst], w=[b_cb])
    d_sel = din("sel", [8, 1024])
    self32 = sb("self32", [8, 1024])
    S.dma("sp", "c0", self32[:], d_sel, w=[b_cst])
    S.op("pool", lambda e: e.tensor_copy(out=selb[:], in_=self32[:]), r=[b_cst], w=[b_cb])
    identf = cst[:, C_ID:C_ID + 128]
    negi = cst[:, C_NI:C_NI + 128]
    mstrict = cst[:, C_MS:C_MS + 128]
    CB = [b_cst, b_cb]

    gains = sb("gains", [128, (NL * 4 + 1) * 16])
    gateb = sb("gateb", [8, NL * 4])
    convw = sb("convw", [128, NL * 24 * 4])
    b_par = Buf()
    S.dma("sp", "c1", gains[:], d_gain, w=[b_par])
    S.dma("sp", "c2", gateb[:], d_gb, w=[b_par])
    S.dma("sp", "c3", convw[:], d_cw, w=[b_par])
    epsc = sb("epsc", [128, 1])
    S.op("pool", lambda e: e.memset(epsc[:], EPS), w=[b_par])

    TTM = TILE + NSMP
    xT = sb("xT", [128, NCH, TTM])
    b_x = Buf()
    nT = sb("nT", [128, NCH, TTM], BF16)
    b_n = Buf()
    mixT = sb("mixT", [128, NCH, TTM], BF16)
    b_mix = Buf()
    NWB = 4
    wbf = [sb("wbf%d" % i, [128, 16 * 128], BF16) for i in range(NWB)]
    b_wbf = [Buf() for _ in range(NWB)]
    NPS = 8
    PS = [es.enter_context(nc.psum_tensor("ps%d" % i, [128, 512], F32)) for i in range(NPS)]
    BPS = [Buf() for _ in range(NPS)]
    st = {"ps": 0, "w": 0, "layer": 0, "wi": 0, "ws": 0}

    pinned = set()

    def psum(pin=False):
        while True:
            i = st["ps"]
            st["ps"] = (i + 1) % NPS
            if i not in pinned:
                break
        if pin:
            pinned.add(i)
        return PS[i], BPS[i]

    def unpin(bps):
        pinned.discard(BPS.index(bps))

    def getw():
        i = st["wi"]
        st["wi"] += 1
        mat, k0, nk, c0, ncw = WBLOCKS[i]
        sz = nk * ncw
        n = st["w"]
        st["w"] += 1
        s2 = n % NWB
        S.dma("pool", "w%d" % s2, wbf[s2][:, 0:sz], d_w[st["layer"], :, WOFF[i]:WOFF[i] + sz], w=[b_wbf[s2]])
        return wbf[s2][:, 0:sz].rearrange("p (k c) -> p k c", k=nk), b_wbf[s2], (mat, k0, nk, c0, ncw)

    def groups(ti):
        g = [(c0, 512) for c0 in range(0, TILE, 512)]
        if ti == 0 and with_sample:
            g.append((TILE, NSMP))
        return g

    def proj_F(src, bsrc, ti, evac, nk=16, m=None, koff=0):
        wv, bw, desc = getw()
        mm = desc[4] if m is None else m
        for gi, (c0, n) in enumerate(groups(ti)):
            ps, bps = psum()
            for k in range(nk):
                S.op("pe", lambda e, k=k: e.matmul(ps[0:mm, 0:n], lhsT=wv[:, k, 0:mm], rhs=src[:, koff + k, c0:c0 + n],
                                                   start=(k == 0), stop=(k == nk - 1)), r=[bw, bsrc], w=[bps])
            evac(ps, bps, mm, c0, n, gi)
        return desc

    def rmsnorm_F(ti, gcol0, out, bout, src=None, bsrc=None, ncols=None):
        src = xT if src is None else src
        bsrc = b_x if bsrc is None else bsrc
        grp = groups(ti) if ncols is None else [(0, ncols)]
        for (c0, n) in grp:
            S.op("act", lambda e: e.activation(out=out[:, :, c0:c0 + n], in_=src[:, :, c0:c0 + n], func=AF.Square), r=[bsrc], w=[bout])
            ps, bps = psum()
            for k in range(NCH):
                S.op("pe", lambda e, k=k: e.matmul(ps[:, 0:n], lhsT=onesb[:], rhs=out[:, k, c0:c0 + n], start=(k == 0), stop=(k == NCH - 1)),
                     r=[bout] + CB, w=[bps])
            rstd = sb_rstd
            S.op("act", lambda e: e.activation(out=rstd[:, 0:n], in_=ps[:, 0:n], func=AF.Ln, bias=epsc[:], scale=1.0 / D), r=[bps, b_par], w=[b_rstd])
            S.op("act", lambda e: e.activation(out=rstd[:, 0:n], in_=rstd[:, 0:n], func=AF.Exp, scale=-0.5), r=[b_rstd], w=[b_rstd])
            for k in range(NCH):
                S.op("dve", lambda e, k=k: e.scalar_tensor_tensor(out=out[:, k, c0:c0 + n], in0=src[:, k, c0:c0 + n], scalar=gains[:, gcol0 + k:gcol0 + k + 1],
                                                                  in1=rstd[:, 0:n], op0=ALU.mult, op1=ALU.mult), r=[bsrc, b_rstd, b_par], w=[bout])

    sb_rstd = sb("rstd", [128, 512])
    b_rstd = Buf()

    def resid_evac(j):
        def ev(ps, bps, m, c0, n, gi, si=0):
            S.op("dve", lambda e: e.tensor_tensor(out=xT[:, j, c0:c0 + n], in0=xT[:, j, c0:c0 + n], in1=ps[:, 0:n], op=ALU.add), r=[bps, b_x], w=[b_x])
        return ev

    def proj(src, bsrc, ti, evac, subs=None, nk=16, koff=0, grp=None):
        wv, bw, desc = getw()
        subs = [(0, desc[4])] if subs is None else subs
        for gi, (c0, n) in enumerate(groups(ti) if grp is None else grp):
            for si, (lo, hi) in enumerate(subs):
                ps, bps = psum()
                for k in range(nk):
                    S.op("pe", lambda e, k=k: e.matmul(ps[0:hi - lo, 0:n], lhsT=wv[:, k, lo:hi], rhs=src[:, koff + k, c0:c0 + n],
                                                       start=(k == 0), stop=(k == nk - 1)), r=[bw, bsrc], w=[bps])
                evac(ps, bps, hi - lo, c0, n, gi, si)
        return wv, bw

    def projT(wv, bw, src, bsrc, ti, evac, evac_s=None, nk=16, ntt=NCK):
        ncols = wv.shape[2]
        for b0 in range(0, ntt, 4):
            ps, bps = psum()
            nj = min(4, ntt - b0)
            for jj in range(nj):
                j = b0 + jj
                for k in range(nk):
                    S.op("pe", lambda e, k=k, j=j, jj=jj: e.matmul(ps[:, jj * ncols:(jj + 1) * ncols], lhsT=src[:, k, j * 128:(j + 1) * 128], rhs=wv[:, k, :],
                                                                   start=(k == 0), stop=(k == nk - 1)), r=[bw, bsrc], w=[bps])
            evac(ps[:, 0:nj * ncols].rearrange("p (j c) -> p j c", j=nj), bps, b0, nj)
        if evac_s is not None and ti == 0 and with_sample:
            ps, bps = psum()
            for k in range(nk):
                S.op("pe", lambda e, k=k: e.matmul(ps[0:NSMP, 0:ncols], lhsT=src[:, k, TILE:TILE + NSMP], rhs=wv[:, k, :],
                                                   start=(k == 0), stop=(k == nk - 1)), r=[bw, bsrc], w=[bps])
            evac_s(ps[0:NSMP, 0:ncols], bps)

    def split3(src, bsrc, np_, n, pieces, bpc, tmp, btmp, npc=3):
        cur = src
        bcur = bsrc
        for i in range(npc):
            S.op("dve", lambda e, i=i, cur=cur: e.tensor_copy(out=pieces[i][0:np_, 0:n], in_=cur[0:np_, 0:n]), r=[bcur], w=[bpc])
            if i < npc - 1:
                S.op("dve", lambda e, i=i, cur=cur: e.tensor_tensor(out=tmp[0:np_, 0:n], in0=cur[0:np_, 0:n], in1=pieces[i][0:np_, 0:n], op=ALU.subtract),
                     r=[bcur, bpc], w=[btmp])
                cur = tmp
                bcur = btmp

    def bcast_rows(pieces, bpc, np_, h, n, dst, bdst, npc=3):
        for c0 in range(0, n, 512):
            nn = min(512, n - c0)
            ps, bps = psum()
            for i in range(npc):
                S.op("pe", lambda e, i=i: e.matmul(ps[:, 0:nn], lhsT=selb[0:np_, h * 128:(h + 1) * 128], rhs=pieces[i][0:np_, c0:c0 + nn],
                                                   start=(i == 0), stop=(i == npc - 1)), r=[bpc] + CB, w=[bps])
            S.op("act", lambda e: e.copy(out=dst[:, c0:c0 + nn], in_=ps[:, 0:nn]), r=[bps], w=[bdst])

    def rows_to_cols(rowlist, brows, np_, dst, bdst):
        nq = len(rowlist)
        ps, bps = psum()
        for c in range(NCK):
            for qi, rt in enumerate(rowlist):
                o = (c * nq + qi) * np_
                S.op("pe", lambda e, rt=rt, c=c, o=o: e.transpose(out=ps[:, o:o + np_], in_=rt[0:np_, c * 128:(c + 1) * 128], identity=cst[0:np_, C_ID:C_ID + np_]),
                     r=brows + CB, w=[bps])
        S.op("dve", lambda e: e.tensor_copy(out=dst[:, 0:NCK * nq * np_], in_=ps[:, 0:NCK * nq * np_]), r=[bps], w=[bdst])

    R = TILE + NSMP
    rg = sb("rg", [8, R]); rf = sb("rf", [8, R]); rF = sb("rF", [8, TILE]); rm = sb("rm", [8, R])
    rux = sb("rux", [8, (TILE + 1)]); rw = sb("rw", [8, TILE]); ra = sb("ra", [8, TILE]); rem = sb("rem", [8, TILE])
    rnu = sb("rnu", [8, (TILE + 1)])
    rz = sb("rz", [8, TILE])
    b_rows = Buf()
    S.op("pool", lambda e: e.memset(rz[:], 0.0), w=[b_rows])
    pcs = [sb("pcs%d" % i, [8, (TILE + 1)], BF16) for i in range(3)]
    b_pcs = Buf()
    ptmp = sb("ptmp", [8, (TILE + 1)]); b_ptmp = Buf()
    gb15 = sb("gb15", [8, 4]); b_gb15 = Buf()
    UBx = sb("UBx", [128, (TILE + 1)]); b_UB = Buf()
    mcols = sb("mcols", [128, NCK * 3 * 8]); b_mcols = Buf()
    qTb = sb("qTb", [128, R], BF16); b_qT = Buf()
    kTb = sb("kTb", [128, R], BF16); b_kT = Buf()
    ktok = sb("ktok", [128, NCK, 128], BF16); b_ktok = Buf()
    vtok = sb("vtok", [128, NCK, 257], BF16); b_vtok = Buf()
    gtok = sb("gtok", [128, NCK, 256], BF16); b_gtok = Buf()
    S.op("pool", lambda e: e.memset(vtok[:], 1.0), w=[b_vtok])
    Caug = sb("Caug", [128, 257]); Cb = sb("Cb", [128, 257], BF16); b_C = Buf(); b_Cb = Buf()
    Sst = sb("Sst", [128, 128]); Sb = sb("Sb", [128, 128], BF16); b_S = Buf(); b_Sb = Buf()
    pre = sb("pre", [128, 3 + R]); b_pre = Buf()
    cvy = sb("cvy", [128, R]); b_cvy = Buf()
    k2f = sb("k2f", [128, TILE]); b_k2f = Buf()
    ctail = sb("ctail", [128, NL * 24 * 3]); b_ctail = Buf()
    S.op("pool", lambda e: e.memset(ctail[:], 0.0), w=[b_ctail])
    mnb = sb("mnb", [128, 1024]); gnb = mnb; b_nb = Buf()
    cX = sb("cX", [128, 128]); cE = sb("cE", [128, 128]); cW = sb("cW", [128, 128], BF16); b_cX = Buf(); b_cE = Buf(); b_cW = Buf()
    cnumA = sb("cnumA", [128, 257]); cnum = sb("cnum", [128, 257]); b_numA = Buf(); b_num = Buf()
    csm = sb("csm", [128, 16]); b_csm = Buf()
    chm = sb("chm", [128, 256]); b_chm = Buf()
    cjunk = sb("cjunk", [128, 256]); b_junk = Buf()
    ckw = sb("ckw", [128, 128], BF16); b_kw = Buf()
    cAT = sb("cAT", [128, 128]); b_AT = Buf()
    cQK = sb("cQK", [128, 128], BF16); b_QK = Buf()
    cP = [sb("cP%d" % i, [128, 128], BF16) for i in range(2)]; b_P = [Buf(), Buf()]
    cPT = [sb("cPT%d" % i, [128, 128], BF16) for i in range(2)]; b_PT = [Buf(), Buf()]
    cTT = [sb("cTT%d" % i, [128, 128], BF16) for i in range(2)]; b_TT = [Buf(), Buf()]
    cBk = [sb("cBk%d" % i, [128, 128], BF16) for i in range(6)]; b_Bk = [Buf() for _ in range(6)]
    crhs = sb("crhs", [128, 128], BF16); b_rhs = Buf()
    cwu = sb("cwu", [128, 128], BF16); b_wu = Buf()
    co1 = sb("co1", [128, 128]); b_o1 = Buf()
    d_scrC = dscr("scrC", [NL, 4, 128, 257])
    d_scrS = dscr("scrS", [NL, 8, 128, 128])
    b_scr = Buf()
    mcar = sb("mcar", [8, NL]); b_mcar = Buf()
    S.op("pool", lambda e: e.memset(mcar[:], 0.0), w=[b_mcar])
    s_k = sb("s_k", [NSMP, 128]); s_v = sb("s_v", [NSMP, 257]); s_g = sb("s_g", [NSMP, 256]); b_sk = Buf(); b_sv = Buf(); b_sg = Buf()
    S.op("pool", lambda e: e.memset(s_v[:], 1.0), w=[b_sv])
    s_q = sb("s_q", [128, NSMP]); b_sq = Buf()
    s_kf = sb("s_kf", [128, NSMP]); b_skf = Buf()

    def gated_norm_to_mix(src_ap, ncol, nrm_row, gate_ap, bgate, chunk_ids, tcol0, ntok, brsrc):
        S.op("act", lambda e: e.activation(out=cjunk[0:ntok, 0:ncol], in_=src_ap, func=AF.Square, accum_out=csm[0:ntok, 4:5]), r=brsrc, w=[b_junk, b_csm])
        S.op("act", lambda e: e.activation(out=csm[0:ntok, 5:6], in_=csm[0:ntok, 4:5], func=AF.Ln, bias=epsc[0:ntok, :], scale=1.0 / ncol), r=[b_csm, b_par], w=[b_csm])
        S.op("act", lambda e: e.activation(out=csm[0:ntok, 5:6], in_=csm[0:ntok, 5:6], func=AF.Exp, scale=-0.5), r=[b_csm], w=[b_csm])
        S.op("dve", lambda e: e.scalar_tensor_tensor(out=chm[0:ntok, 0:ncol], in0=src_ap, scalar=csm[0:ntok, 5:6], in1=nrm_row, op0=ALU.mult, op1=ALU.mult),
             r=brsrc + [b_csm, b_nb], w=[b_chm])
        S.op("dve", lambda e: e.tensor_tensor(out=chm[0:ntok, 0:ncol], in0=chm[0:ntok, 0:ncol], in1=gate_ap, op=ALU.mult), r=[b_chm, bgate], w=[b_chm])
        for i, ch in enumerate(chunk_ids):
            ps, bps = psum()
            S.op("pe", lambda e, i=i: e.transpose(out=ps[:, 0:ntok], in_=chm[0:ntok, i * 128:(i + 1) * 128], identity=cst[0:ntok, C_ID:C_ID + ntok]),
                 r=[b_chm] + CB, w=[bps])
            S.op("act", lambda e, ch=ch: e.copy(out=mixT[:, ch, tcol0:tcol0 + ntok], in_=ps[:, 0:ntok]), r=[bps], w=[b_mix])

    def mlstm(l, ti):
        smp = (ti == 0 and with_sample)
        ncol = R if smp else TILE
        S.op("dve", lambda e: e.tensor_scalar(out=gb15[0:4, 0:2], in0=gateb[0:4, l * 4:l * 4 + 2], scalar1=1.0 / 15.0, scalar2=None, op0=ALU.mult), r=[b_par], w=[b_gb15])

        def ev_g(ps, bps, m, c0, n, gi, si):
            dst = rg if si == 0 else rf
            S.op("act", lambda e: e.activation(out=dst[0:4, c0:c0 + n], in_=ps[0:4, 0:n], func=AF.Tanh, bias=gb15[0:4, si:si + 1], scale=1.0 / 15.0),
                 r=[bps, b_gb15], w=[b_rows])
        proj(nT, b_n, ti, ev_g, subs=[(0, 4), (4, 8)])
        S.op("dve", lambda e: e.tensor_scalar(out=rg[0:4, 0:ncol], in0=rg[0:4, 0:ncol], scalar1=15.0, scalar2=None, op0=ALU.mult), r=[b_rows], w=[b_rows])
        S.op("act", lambda e: e.activation(out=rf[0:4, 0:ncol], in_=rf[0:4, 0:ncol], func=AF.Exp, scale=-15.0), r=[b_rows], w=[b_rows])
        S.op("act", lambda e: e.activation(out=rf[0:4, 0:ncol], in_=rf[0:4, 0:ncol], func=AF.Ln, bias=1.0), r=[b_rows], w=[b_rows])
        S.op("dve", lambda e: e.tensor_scalar(out=rf[0:4, 0:ncol], in0=rf[0:4, 0:ncol], scalar1=-1.0, scalar2=None, op0=ALU.mult), r=[b_rows], w=[b_rows])
        S.op("dve", lambda e: e.tensor_tensor_scan(out=rF[0:4, :], data0=rf[0:4, 0:TILE], data1=rz[0:4, :], initial=0.0, op0=ALU.add, op1=ALU.add), r=[b_rows], w=[b_rows])
        S.op("dve", lambda e: e.tensor_tensor_scan(out=rm[0:4, 0:TILE], data0=rf[0:4, 0:TILE], data1=rg[0:4, 0:TILE], initial=mcar[0:4, l:l + 1], op0=ALU.add, op1=ALU.max),
             r=[b_rows, b_mcar], w=[b_rows])
        S.op("dve", lambda e: e.tensor_scalar(out=rux[0:4, 0:1], in0=mcar[0:4, l:l + 1], scalar1=-1.0, scalar2=None, op0=ALU.mult), r=[b_mcar], w=[b_rows])
        S.op("dve", lambda e: e.tensor_tensor(out=rux[0:4, 1:(TILE + 1)], in0=rF[0:4, :], in1=rm[0:4, 0:TILE], op=ALU.subtract), r=[b_rows], w=[b_rows])
        S.op("dve", lambda e: e.tensor_scalar(out=rnu[0:4, :], in0=rux[0:4, :], scalar1=-1.0, scalar2=None, op0=ALU.mult), r=[b_rows], w=[b_rows])
        S.op("dve", lambda e: e.tensor_tensor(out=rw[0:4, :], in0=rg[0:4, 0:TILE], in1=rF[0:4, :], op=ALU.subtract), r=[b_rows], w=[b_rows])
        S.op("act", lambda e: e.activation(out=rem[0:4, :], in_=rm[0:4, 0:TILE], func=AF.Exp, scale=-1.0), r=[b_rows], w=[b_rows])
        for c in range(NCK):
            S.op("act", lambda e, c=c: e.activation(out=ra[0:4, c * 128:(c + 1) * 128], in_=rux[0:4, 1 + c * 128:1 + (c + 1) * 128], func=AF.Exp,
                                                    bias=rnu[0:4, c * 128:c * 128 + 1], scale=1.0), r=[b_rows], w=[b_rows])
        S.op("dve", lambda e: e.tensor_copy(out=mcar[0:4, l:l + 1], in_=rm[0:4, (TILE - 1):TILE]), r=[b_rows], w=[b_mcar])
        if ti == NT - 1:
            S.dma("sp", "o_pm", o_pm[l].rearrange("(h o) -> h o", o=1), rm[0:4, (TILE - 1):TILE], r=[b_rows])
        rows_to_cols([rw, ra, rem], [b_rows], 4, mcols, b_mcols)
        split3(rux, b_rows, 4, (TILE + 1), pcs, b_pcs, ptmp, b_ptmp)
        S.dma("sp", "nb", mnb[:], d_mn[l].partition_broadcast(128), w=[b_nb])

        def mcol(c, qi, h):
            o = (c * 3 + qi) * 4 + h
            return mcols[:, o:o + 1]

        for h in range(4):
            bcast_rows(pcs, b_pcs, 4, h, (TILE + 1), UBx, b_UB)
            def ev_q(ps, bps, m, c0, n, gi, si):
                if c0 == TILE:
                    S.op("dve", lambda e: e.tensor_copy(out=s_q[:, :], in_=ps[:, 0:n]), r=[bps], w=[b_sq])
                    S.op("act", lambda e: e.copy(out=qTb[:, c0:c0 + n], in_=s_q[:, :]), r=[b_sq], w=[b_qT])
                else:
                    S.op("act", lambda e: e.copy(out=qTb[:, c0:c0 + n], in_=ps[:, 0:n]), r=[bps], w=[b_qT])
            proj(nT, b_n, ti, ev_q)

            def ev_k(ps, bps, m, c0, n, gi, si):
                if c0 == TILE:
                    S.op("dve", lambda e: e.tensor_scalar(out=s_kf[:, :], in0=ps[:, 0:n], scalar1=128.0 ** -0.5, scalar2=None, op0=ALU.mult), r=[bps], w=[b_skf])
                    S.op("act", lambda e: e.copy(out=kTb[:, c0:c0 + n], in_=s_kf[:, :]), r=[b_skf], w=[b_kT])
                else:
                    S.op("act", lambda e: e.activation(out=kTb[:, c0:c0 + n], in_=ps[:, 0:n], func=AF.Copy, scale=128.0 ** -0.5), r=[bps], w=[b_kT])
            wv, bw = proj(nT, b_n, ti, ev_k)

            def ev_kt(ps3, bps, j0, nj):
                S.op("act", lambda e: e.activation(out=ktok[:, j0:j0 + nj, :], in_=ps3, func=AF.Copy, scale=128.0 ** -0.5), r=[bps], w=[b_ktok])

            def ev_kts(ps, bps):
                S.op("act", lambda e: e.activation(out=s_k[:, :], in_=ps, func=AF.Copy, scale=128.0 ** -0.5), r=[bps], w=[b_sk])
            projT(wv, bw, nT, b_n, ti, ev_kt, ev_kts)
            for j in range(2):
                wv, bw, _ = getw()

                def ev_v(ps3, bps, j0, nj, j=j):
                    S.op("dve", lambda e: e.tensor_copy(out=vtok[:, j0:j0 + nj, j * 128:(j + 1) * 128], in_=ps3), r=[bps], w=[b_vtok])

                def ev_vs(ps, bps, j=j):
                    S.op("dve", lambda e: e.tensor_copy(out=s_v[:, j * 128:(j + 1) * 128], in_=ps), r=[bps], w=[b_sv])
                projT(wv, bw, nT, b_n, ti, ev_v, ev_vs)
            for j in range(2):
                wv, bw, _ = getw()

                def ev_o(ps3, bps, j0, nj, j=j):
                    S.op("act", lambda e: e.activation(out=gtok[:, j0:j0 + nj, j * 128:(j + 1) * 128], in_=ps3, func=AF.Sigmoid), r=[bps], w=[b_gtok])

                def ev_os(ps, bps, j=j):
                    S.op("act", lambda e: e.activation(out=s_g[:, j * 128:(j + 1) * 128], in_=ps, func=AF.Sigmoid), r=[bps], w=[b_sg])
                projT(wv, bw, nT, b_n, ti, ev_o, ev_os)
            if ti == 0:
                S.op("dve", lambda e: e.memset(Caug[:], 0.0), w=[b_C])
            else:
                S.dma("sp", "scrC_r", Caug[:], d_scrC[l, h], r=[b_scr], w=[b_C])
            S.op("act", lambda e: e.copy(out=Cb[:], in_=Caug[:]), r=[b_C], w=[b_Cb])
            for c in range(NCK):
                t0 = c * 128
                ps_s, bps_s = psum()
                S.op("pe", lambda e: e.matmul(ps_s[:, 0:128], lhsT=kTb[:, t0:t0 + 128], rhs=qTb[:, t0:t0 + 128], start=True, stop=True), r=[b_kT, b_qT], w=[bps_s])
                S.op("dve", lambda e: e.scalar_tensor_tensor(out=cX[:], in0=UBx[:, 1 + t0:1 + t0 + 128], scalar=mcol(c, 0, h), in1=negi, op0=ALU.add, op1=ALU.add),
                     r=[b_UB, b_mcols] + CB, w=[b_cX])
                S.op("act", lambda e: e.activation(out=cE[:], in_=cX[:], func=AF.Exp), r=[b_cX], w=[b_cE])
                S.op("dve", lambda e: e.tensor_tensor(out=cW[:], in0=cE[:], in1=ps_s[:, 0:128], op=ALU.mult), r=[b_cE, bps_s], w=[b_cW])
                psA, bpsA = psum()
                S.op("pe", lambda e: e.matmul(psA[:, 0:257], lhsT=qTb[:, t0:t0 + 128], rhs=Cb[:], start=True, stop=True), r=[b_qT, b_Cb], w=[bpsA])
                psB, bpsB = psum()
                S.op("pe", lambda e: e.matmul(psB[:, 0:257], lhsT=cW[:], rhs=vtok[:, c, :], start=True, stop=True), r=[b_cW, b_vtok], w=[bpsB])
                S.op("act", lambda e: e.activation(out=cnumA[:], in_=psA[:, 0:257], func=AF.Copy, scale=mcol(c, 1, h)), r=[bpsA, b_mcols], w=[b_numA])
                S.op("dve", lambda e: e.tensor_tensor(out=cnum[:], in0=cnumA[:], in1=psB[:, 0:257], op=ALU.add), r=[b_numA, bpsB], w=[b_num])
                S.op("act", lambda e: e.activation(out=csm[:, 0:1], in_=cnum[:, 256:257], func=AF.Abs), r=[b_num], w=[b_csm])
                S.op("dve", lambda e: e.tensor_tensor(out=csm[:, 0:1], in0=csm[:, 0:1], in1=mcol(c, 2, h), op=ALU.max), r=[b_csm, b_mcols], w=[b_csm])
                S.op("dve", lambda e: e.reciprocal(out=csm[:, 1:2], in_=csm[:, 0:1]), r=[b_csm], w=[b_csm])
                S.op("dve", lambda e: e.tensor_scalar(out=cnum[:, 0:256], in0=cnum[:, 0:256], scalar1=csm[:, 1:2], scalar2=None, op0=ALU.mult), r=[b_num, b_csm], w=[b_num])
                gated_norm_to_mix(cnum[:, 0:256], 256, mnb[:, h * 256:(h + 1) * 256], gtok[:, c, :], b_gtok, [2 * h, 2 * h + 1], t0, 128, [b_num])
                S.op("act", lambda e: e.activation(out=csm[:, 2:3], in_=mcol(c, 0, h), func=AF.Exp, bias=UBx[:, t0 + 128:t0 + 129], scale=1.0), r=[b_mcols, b_UB], w=[b_csm])
                S.op("dve", lambda e: e.tensor_scalar(out=ckw[:], in0=ktok[:, c, :], scalar1=csm[:, 2:3], scalar2=None, op0=ALU.mult), r=[b_ktok, b_csm], w=[b_kw])
                psC, bpsC = psum()
                S.op("pe", lambda e: e.matmul(psC[:, 0:257], lhsT=ckw[:], rhs=vtok[:, c, :], start=True, stop=True), r=[b_kw, b_vtok], w=[bpsC])
                S.op("dve", lambda e: e.tensor_tensor(out=csm[:, 3:4], in0=UBx[:, t0 + 128:t0 + 129], in1=UBx[:, t0:t0 + 1], op=ALU.subtract), r=[b_UB], w=[b_csm])
                S.op("act", lambda e: e.activation(out=csm[:, 3:4], in_=csm[:, 3:4], func=AF.Exp), r=[b_csm], w=[b_csm])
                S.op("dve", lambda e: e.scalar_tensor_tensor(out=Caug[:], in0=Caug[:], scalar=csm[:, 3:4], in1=psC[:, 0:257], op0=ALU.mult, op1=ALU.add),
                     r=[b_C, b_csm, bpsC], w=[b_C])
                S.op("act", lambda e: e.copy(out=Cb[:], in_=Caug[:]), r=[b_C], w=[b_Cb])
            if ti < NT - 1:
                S.dma("sp", "scrC_w", d_scrC[l, h], Caug[:], r=[b_C], w=[b_scr])
            else:
                S.dma("sp", "o_pC", o_pC[l, h], Caug[:, 0:256], r=[b_C])
                S.dma("sp", "o_pn", o_pn[l, h].rearrange("(k o) -> k o", o=1), Caug[:, 256:257], r=[b_C])
            if smp:
                mlstm_sample(l, h)

    eye3 = cst[:, C_E16:C_E16 + 256].rearrange("p (a b) -> p a b", a=16)
    eyeP = cst[0:NSMP, C_ID:C_ID + NSMP]
    s_rows = sb("s_rows", [8, 8 * NSMP]); b_srows = Buf()
    s_cols = sb("s_cols", [NSMP, 64]); b_scols = Buf()
    s_rep = sb("s_rep", [128, 8 * NSMP]); b_srep = Buf()
    s_pc = [sb("s_pc%d" % i, [8, NSMP], BF16) for i in range(2)]; b_spc = Buf()
    s_pt = sb("s_pt", [8, NSMP]); b_spt = Buf()
    s_mtok = sb("s_mtok", [NSMP, 8]); b_smtok = Buf()
    Qm = sb("Qm", [128, NSMP, NSMP], BF16); b_Qm = Buf()
    Km = sb("Km", [NSMP, NSMP, 128], BF16); b_Km = Buf()
    s_t1 = sb("s_t1", [NSMP, 257]); b_st1 = Buf()
    s_t2 = sb("s_t2", [NSMP, 257]); b_st2 = Buf()
    s_vb = sb("s_vb", [NSMP, 257], BF16); b_svb = Buf()
    s_f1 = sb("s_f1", [128, NSMP]); b_sf1 = Buf()
    s_fb = sb("s_fb", [128, NSMP], BF16); b_sfb = Buf()
    NRB = 2
    rowf = [sb("rowf%d" % i, [128, 257]) for i in range(NRB)]; b_rowf = [Buf() for _ in range(NRB)]
    rowb = [sb("rowb%d" % i, [128, 257], BF16) for i in range(NRB)]; b_rowb = [Buf() for _ in range(NRB)]
    st2 = {"rb": 0}

    def rep_rows(src_rows, np_, qslot):
        S.op("dve", lambda e: e.tensor_copy(out=s_pc[0][0:np_, :], in_=src_rows), r=[b_srows], w=[b_spc])
        S.op("dve", lambda e: e.tensor_tensor(out=s_pt[0:np_, :], in0=src_rows, in1=s_pc[0][0:np_, :], op=ALU.subtract), r=[b_srows, b_spc], w=[b_spt])
        S.op("dve", lambda e: e.tensor_copy(out=s_pc[1][0:np_, :], in_=s_pt[0:np_, :]), r=[b_spt], w=[b_spc])
        ps, bps = psum()
        for h in range(np_):
            for i in range(2):
                S.op("pe", lambda e, h=h, i=i: e.matmul(ps[:, h * NSMP:(h + 1) * NSMP], lhsT=selb[0:np_, h * 128:(h + 1) * 128], rhs=s_pc[i][0:np_, :],
                                                        start=(i == 0), stop=(i == 1)), r=[b_spc] + CB, w=[bps])
        S.op("act", lambda e: e.copy(out=s_rep[:, 0:np_ * NSMP], in_=ps[:, 0:np_ * NSMP]), r=[bps], w=[b_srep])

    def rows_to_tok(np_, nq):
        ps, bps = psum()
        for qi in range(nq):
            S.op("pe", lambda e, qi=qi: e.transpose(out=ps[0:NSMP, qi * np_:(qi + 1) * np_], in_=s_rows[0:np_, qi * NSMP:(qi + 1) * NSMP], identity=cst[0:np_, C_ID:C_ID + np_]),
                 r=[b_srows] + CB, w=[bps])
        S.op("dve", lambda e: e.tensor_copy(out=s_cols[:, 0:nq * np_], in_=ps[0:NSMP, 0:nq * np_]), r=[bps], w=[b_scols])

    def mlstm_sample_prep(l):
        S.dma("sp", "s_m", s_mtok[:, 0:4], d_sm[l], w=[b_smtok])
        ps, bps = psum()
        S.op("pe", lambda e: e.transpose(out=ps[0:4, 0:NSMP], in_=s_mtok[:, 0:4], identity=eyeP), r=[b_smtok] + CB, w=[bps])
        fi = rf[0:4, TILE:TILE + NSMP]
        ii = rg[0:4, TILE:TILE + NSMP]
        sl = lambda q: s_rows[0:4, q * NSMP:(q + 1) * NSMP]
        S.op("dve", lambda e: e.tensor_tensor(out=sl(4), in0=ps[0:4, 0:NSMP], in1=fi, op=ALU.add), r=[bps, b_rows], w=[b_srows])
        S.op("dve", lambda e: e.tensor_tensor(out=sl(0), in0=sl(4), in1=ii, op=ALU.max), r=[b_srows, b_rows], w=[b_srows])
        S.op("dve", lambda e: e.tensor_tensor(out=sl(1), in0=sl(4), in1=sl(0), op=ALU.subtract), r=[b_srows], w=[b_srows])
        S.op("act", lambda e: e.activation(out=sl(1), in_=sl(1), func=AF.Exp), r=[b_srows], w=[b_srows])
        S.op("dve", lambda e: e.tensor_tensor(out=sl(2), in0=ii, in1=sl(0), op=ALU.subtract), r=[b_srows, b_rows], w=[b_srows])
        S.op("act", lambda e: e.activation(out=sl(2), in_=sl(2), func=AF.Exp), r=[b_srows], w=[b_srows])
        S.op("act", lambda e: e.activation(out=sl(3), in_=sl(0), func=AF.Exp, scale=-1.0), r=[b_srows], w=[b_srows])
        rows_to_tok(4, 4)
        rep_rows(sl(1), 4, 0)
        S.op("dve", lambda e: e.tensor_copy(out=s_mtok[:, 4:8], in_=s_cols[:, 0:4]), r=[b_scols], w=[b_smtok])
        S.dma("sp", "o_sm", o_sm[l], s_mtok[:, 4:8], r=[b_smtok])

    def masked_q(src_f32, bsrc):
        S.op("dve", lambda e: e.tensor_tensor(out=Qm[:], in0=src_f32.unsqueeze(1).to_broadcast([128, NSMP, NSMP]), in1=eye3, op=ALU.mult), r=[bsrc] + CB, w=[b_Qm])

    def masked_k(src_tok, bsrc, scale_col=None, bscale=None):
        if scale_col is not None:
            S.op("dve", lambda e: e.tensor_scalar(out=s_t2[:, 0:128], in0=src_tok, scalar1=scale_col, scalar2=None, op0=ALU.mult), r=[bsrc, bscale], w=[b_st2])
            src_tok, bsrc = s_t2[:, 0:128], b_st2
        S.op("dve", lambda e: e.tensor_tensor(out=Km[:], in0=src_tok.unsqueeze(1).to_broadcast([NSMP, NSMP, 128]), in1=eyeP.unsqueeze(2).to_broadcast([NSMP, NSMP, 128]), op=ALU.mult),
             r=[bsrc] + CB, w=[b_Km])

    def dot_rows(a_f, ba, b_f, bb, out_col):
        S.op("dve", lambda e: e.tensor_tensor(out=s_fb[:], in0=a_f, in1=b_f, op=ALU.mult), r=[ba, bb], w=[b_sfb])
        ps, bps = psum()
        S.op("pe", lambda e: e.matmul(ps[0:NSMP, 0:1], lhsT=s_fb[:], rhs=onesb[:, 0:1], start=True, stop=True), r=[b_sfb] + CB, w=[bps])
        S.op("dve", lambda e: e.tensor_copy(out=out_col, in_=ps[0:NSMP, 0:1]), r=[bps], w=[b_scols])

    def mlstm_sample(l, h):
        if h == 0:
            mlstm_sample_prep(l)
        col = lambda q: s_cols[:, q * 4 + h:q * 4 + h + 1]
        masked_q(s_q[:], b_sq)
        masked_k(s_k[:], b_sk, scale_col=col(2), bscale=b_scols)
        S.op("act", lambda e: e.copy(out=s_vb[:], in_=s_v[:]), r=[b_sv], w=[b_svb])
        psA, bpsA = psum(pin=True)
        for r_ in range(NSMP):
            i = st2["rb"] % NRB
            st2["rb"] += 1
            S.dma("sp", "s_row%d" % i, rowf[i][:, 0:256], d_sC[l, r_, h], w=[b_rowf[i]])
            S.dma("sp", "s_row%d" % i, rowf[i][:, 256:257], d_sn[l, r_, h * 128:(h + 1) * 128].rearrange("(k o) -> k o", o=1), w=[b_rowf[i]])
            S.op("act", lambda e, i=i: e.copy(out=rowb[i][:], in_=rowf[i][:]), r=[b_rowf[i]], w=[b_rowb[i]])
            S.op("pe", lambda e, i=i, r_=r_: e.matmul(psA[0:NSMP, 0:257], lhsT=Qm[:, r_, :], rhs=rowb[i][:], start=(r_ == 0), stop=(r_ == NSMP - 1)), r=[b_Qm, b_rowb[i]], w=[bpsA])
            psC, bpsC = psum()
            S.op("pe", lambda e, r_=r_: e.matmul(psC[:, 0:257], lhsT=Km[:, r_, :], rhs=s_vb[:], start=True, stop=True), r=[b_Km, b_svb], w=[bpsC])
            S.op("dve", lambda e, i=i, r_=r_: e.scalar_tensor_tensor(out=rowf[i][:], in0=rowf[i][:], scalar=s_rep[:, h * NSMP + r_:h * NSMP + r_ + 1], in1=psC[:, 0:257],
                                                                     op0=ALU.mult, op1=ALU.add), r=[b_rowf[i], b_srep, bpsC], w=[b_rowf[i]])
            S.dma("sp", "o_sC%d" % i, o_sC[l, r_, h], rowf[i][:, 0:256], r=[b_rowf[i]])
            S.dma("sp", "o_sn%d" % i, o_sn[l, r_, h * 128:(h + 1) * 128].rearrange("(k o) -> k o", o=1), rowf[i][:, 256:257], r=[b_rowf[i]])
        dot_rows(s_q[:], b_sq, s_kf[:], b_skf, s_cols[:, 60:61])
        S.op("act", lambda e: e.activation(out=s_t1[:], in_=psA[0:NSMP, 0:257], func=AF.Copy, scale=col(1)), r=[bpsA, b_scols], w=[b_st1])
        unpin(bpsA)
        S.op("dve", lambda e: e.tensor_tensor(out=s_cols[:, 61:62], in0=s_cols[:, 60:61], in1=col(2), op=ALU.mult), r=[b_scols], w=[b_scols])
        S.op("dve", lambda e: e.scalar_tensor_tensor(out=s_t1[:], in0=s_v[:], scalar=s_cols[:, 61:62], in1=s_t1[:], op0=ALU.mult, op1=ALU.add), r=[b_sv, b_scols, b_st1], w=[b_st1])
        S.op("act", lambda e: e.activation(out=s_cols[:, 62:63], in_=s_t1[:, 256:257], func=AF.Abs), r=[b_st1], w=[b_scols])
        S.op("dve", lambda e: e.tensor_tensor(out=s_cols[:, 62:63], in0=s_cols[:, 62:63], in1=col(3), op=ALU.max), r=[b_scols], w=[b_scols])
        S.op("dve", lambda e: e.reciprocal(out=s_cols[:, 62:63], in_=s_cols[:, 62:63]), r=[b_scols], w=[b_scols])
        S.op("dve", lambda e: e.tensor_scalar(out=s_t1[:, 0:256], in0=s_t1[:, 0:256], scalar1=s_cols[:, 62:63], scalar2=None, op0=ALU.mult), r=[b_st1, b_scols], w=[b_st1])
        gated_norm_to_mix(s_t1[:, 0:256], 256, mnb[0:NSMP, h * 256:(h + 1) * 256], s_g[:, :], b_sg, [2 * h, 2 * h + 1], TILE, NSMP, [b_st1])

    s_cst = sb("s_cst", [NSMP, 3, 128]); b_scst = Buf()
    s_cf = sb("s_cf", [128, 3, NSMP]); b_scf = Buf()
    s_y = sb("s_y", [128, 3, NSMP]); b_sy = Buf()
    s_k2t = sb("s_k2t", [NSMP, 128]); b_sk2t = Buf()
    s_v2t = sb("s_v2t", [NSMP, 128]); b_sv2t = Buf()
    s_nt = sb("s_nt", [NSMP, 128]); b_snt = Buf()

    def gdn_sample_conv(l, h, qi):
        cc = qi * 8 + h
        if h == 0 and qi == 0:
            S.dma("sp", "o_scv", o_sconv[l, :, 0:2, :], d_sconv[l, :, 1:3, :])
        S.dma("sp", "s_cst", s_cst[:], d_sconv[l, :, :, cc * 128:(cc + 1) * 128], w=[b_scst])
        ps, bps = psum()
        for j in range(3):
            S.op("pe", lambda e, j=j: e.transpose(out=ps[:, j * NSMP:(j + 1) * NSMP], in_=s_cst[:, j, :], identity=eyeP), r=[b_scst] + CB, w=[bps])
        S.op("act", lambda e: e.copy(out=s_cf[:], in_=ps[:, 0:3 * NSMP].rearrange("p (a b) -> p a b", a=3)), r=[bps], w=[b_scf])
        wb = (l * 24 + cc) * 4
        S.op("dve", lambda e: e.tensor_scalar(out=s_y[:, qi, :], in0=s_pre[:, qi, :], scalar1=convw[:, wb + 3:wb + 4], scalar2=None, op0=ALU.mult), r=[b_spre, b_par], w=[b_sy])
        for j in range(3):
            S.op("dve", lambda e, j=j: e.scalar_tensor_tensor(out=s_y[:, qi, :], in0=s_cf[:, j, :], scalar=convw[:, wb + j:wb + j + 1], in1=s_y[:, qi, :], op0=ALU.mult, op1=ALU.add),
                 r=[b_scf, b_par, b_sy], w=[b_sy])
        S.op("act", lambda e: e.activation(out=s_y[:, qi, :], in_=s_y[:, qi, :], func=AF.Silu), r=[b_sy], w=[b_sy])
        ps2, bps2 = psum()
        S.op("pe", lambda e: e.transpose(out=ps2[0:NSMP, 0:128], in_=s_pre[:, qi, :], identity=identf), r=[b_spre] + CB, w=[bps2])
        S.op("dve", lambda e: e.tensor_copy(out=s_nt[:], in_=ps2[0:NSMP, 0:128]), r=[bps2], w=[b_snt])
        S.dma("sp", "o_scv2", o_sconv[l, :, 2, cc * 128:(cc + 1) * 128], s_nt[:], r=[b_snt])
        if qi < 2:
            S.op("act", lambda e: e.activation(out=s_fb[:], in_=s_y[:, qi, :], func=AF.Square), r=[b_sy], w=[b_sfb])
            ps3, bps3 = psum()
            S.op("pe", lambda e: e.matmul(ps3[:, 0:NSMP], lhsT=onesb[:], rhs=s_fb[:], start=True, stop=True), r=[b_sfb] + CB, w=[bps3])
            S.op("act", lambda e: e.activation(out=s_f1[:], in_=ps3[:, 0:NSMP], func=AF.Ln, bias=epsc[:], scale=1.0), r=[bps3, b_par], w=[b_sf1])
            S.op("act", lambda e: e.activation(out=s_f1[:], in_=s_f1[:], func=AF.Exp, scale=-0.5), r=[b_sf1], w=[b_sf1])
            sc = (128.0 ** -0.5) if qi == 0 else 1.0
            S.op("dve", lambda e: e.scalar_tensor_tensor(out=s_y[:, qi, :], in0=s_y[:, qi, :], scalar=sc, in1=s_f1[:], op0=ALU.mult, op1=ALU.mult), r=[b_sy, b_sf1], w=[b_sy])
        if qi >= 1:
            dst, bdst = (s_k2t, b_sk2t) if qi == 1 else (s_v2t, b_sv2t)
            ps4, bps4 = psum()
            S.op("pe", lambda e: e.transpose(out=ps4[0:NSMP, 0:128], in_=s_y[:, qi, :], identity=identf), r=[b_sy] + CB, w=[bps4])
            S.op("dve", lambda e: e.tensor_copy(out=dst[:], in_=ps4[0:NSMP, 0:128]), r=[bps4], w=[bdst])

    def gdn_sample_prep(l):
        sl = lambda q: s_rows[0:8, q * NSMP:(q + 1) * NSMP]
        S.op("act", lambda e: e.activation(out=sl(0), in_=rga[0:8, TILE:TILE + NSMP], func=AF.Exp), r=[b_rows], w=[b_srows])
        S.op("dve", lambda e: e.tensor_copy(out=sl(1), in_=rgb[0:8, TILE:TILE + NSMP]), r=[b_rows], w=[b_srows])
        rows_to_tok(8, 2)
        rep_rows(sl(0), 8, 0)

    def gdn_sample(l, h):
        if h == 0:
            gdn_sample_prep(l)
        col = lambda q: s_cols[:, q * 8 + h:q * 8 + h + 1]
        masked_q(s_y[:, 1, :], b_sy)
        psK, bpsK = psum(pin=True)
        rows_loaded = []
        for r_ in range(NSMP):
            i = st2["rb"] % NRB
            st2["rb"] += 1
            S.dma("sp", "s_row%d" % i, rowf[i][:, 0:128], d_sS[l, r_, h], w=[b_rowf[i]])
            S.op("act", lambda e, i=i: e.copy(out=rowb[i][:, 0:128], in_=rowf[i][:, 0:128]), r=[b_rowf[i]], w=[b_rowb[i]])
            S.op("pe", lambda e, i=i, r_=r_: e.matmul(psK[0:NSMP, 0:128], lhsT=Qm[:, r_, :], rhs=rowb[i][:, 0:128], start=(r_ == 0), stop=(r_ == NSMP - 1)), r=[b_Qm, b_rowb[i]], w=[bpsK])
        S.op("dve", lambda e: e.tensor_scalar(out=s_cols[:, 56:57], in0=col(0), scalar1=-1.0, scalar2=None, op0=ALU.mult), r=[b_scols], w=[b_scols])
        S.op("dve", lambda e: e.scalar_tensor_tensor(out=s_t1[:, 0:128], in0=psK[0:NSMP, 0:128], scalar=s_cols[:, 56:57], in1=s_v2t[:], op0=ALU.mult, op1=ALU.add),
             r=[bpsK, b_scols, b_sv2t], w=[b_st1])
        unpin(bpsK)
        S.op("dve", lambda e: e.tensor_scalar(out=s_t1[:, 0:128], in0=s_t1[:, 0:128], scalar1=col(1), scalar2=None, op0=ALU.mult), r=[b_st1, b_scols], w=[b_st1])
        S.op("act", lambda e: e.copy(out=s_vb[:, 0:128], in_=s_t1[:, 0:128]), r=[b_st1], w=[b_svb])
        masked_q(s_y[:, 0, :], b_sy)
        masked_k(s_k2t[:], b_sk2t)
        psQ, bpsQ = psum(pin=True)
        for r_ in range(NSMP):
            i = st2["rb"] % NRB
            st2["rb"] += 1
            S.dma("sp", "s_row%d" % i, rowf[i][:, 0:128], d_sS[l, r_, h], w=[b_rowf[i]])
            S.op("act", lambda e, i=i: e.copy(out=rowb[i][:, 0:128], in_=rowf[i][:, 0:128]), r=[b_rowf[i]], w=[b_rowb[i]])
            S.op("pe", lambda e, i=i, r_=r_: e.matmul(psQ[0:NSMP, 0:128], lhsT=Qm[:, r_, :], rhs=rowb[i][:, 0:128], start=(r_ == 0), stop=(r_ == NSMP - 1)), r=[b_Qm, b_rowb[i]], w=[bpsQ])
            psD, bpsD = psum()
            S.op("pe", lambda e, r_=r_: e.matmul(psD[:, 0:128], lhsT=Km[:, r_, :], rhs=s_vb[:, 0:128], start=True, stop=True), r=[b_Km, b_svb], w=[bpsD])
            S.op("dve", lambda e, i=i, r_=r_: e.scalar_tensor_tensor(out=rowf[i][:, 0:128], in0=rowf[i][:, 0:128], scalar=s_rep[:, h * NSMP + r_:h * NSMP + r_ + 1], in1=psD[:, 0:128],
                                                                     op0=ALU.mult, op1=ALU.add), r=[b_rowf[i], b_srep, bpsD], w=[b_rowf[i]])
            S.dma("sp", "o_sS%d" % i, o_sS[l, r_, h], rowf[i][:, 0:128], r=[b_rowf[i]])
        dot_rows(s_y[:, 0, :], b_sy, s_y[:, 1, :], b_sy, s_cols[:, 57:58])
        S.op("act", lambda e: e.activation(out=s_t2[:, 0:128], in_=psQ[0:NSMP, 0:128], func=AF.Copy, scale=col(0)), r=[bpsQ, b_scols], w=[b_st2])
        unpin(bpsQ)
        S.op("dve", lambda e: e.scalar_tensor_tensor(out=s_t2[:, 0:128], in0=s_t1[:, 0:128], scalar=s_cols[:, 57:58], in1=s_t2[:, 0:128], op0=ALU.mult, op1=ALU.add),
             r=[b_st1, b_scols, b_st2], w=[b_st2])
        gated_norm_to_mix(s_t2[:, 0:128], 128, gnb[0:NSMP, h * 128:(h + 1) * 128], s_z[:, :], b_sz, [8 + h], TILE, NSMP, [b_st2])

    kvf = sb("kvf", [128, 512]); b_kvf = Buf()
    kvb = sb("kvb", [128, 512], BF16); b_kvb = Buf()
    qtok = sb("qtok", [NSMP, 512], BF16); b_qtok = Buf()
    oh1 = sb("oh1", [NSMP, 128], BF16); b_ohR = Buf()
    sc_all = sb("sc_all", [128, 2, NSMP * 4]); b_sc = Buf()
    sm_p = sb("sm_p", [128, 256]); b_smp = Buf()
    pTs = sb("pTs", [128, 2, 64], BF16); b_pTs = Buf()

    def xattn_sample(l):
        ps, bps = psum()
        for hh in range(4):
            S.op("pe", lambda e, hh=hh: e.transpose(out=ps[0:NSMP, hh * 128:(hh + 1) * 128], in_=qxs[:, hh, :], identity=identf), r=[b_qxs] + CB, w=[bps])
        S.op("act", lambda e: e.copy(out=qtok[:], in_=ps[0:NSMP, 0:512]), r=[bps], w=[b_qtok])
        for r_ in range(NSMP):
            psb, bpsb = psum()
            S.op("dve", lambda e, r_=r_: e.tensor_copy(out=oh1[:], in_=eyeP[:, r_:r_ + 1].to_broadcast([NSMP, 128])), r=CB, w=[b_ohR])
            S.op("pe", lambda e, r_=r_: e.matmul(psb[:, 0:512], lhsT=oh1[:], rhs=qtok[:], start=True, stop=True), r=[b_ohR, b_qtok], w=[bpsb])
            for mt in range(2):
                S.dma("sp", "s_kv", kvf[:], d_ck[l, r_, mt * 128:(mt + 1) * 128, :], w=[b_kvf])
                S.op("dve", lambda e: e.tensor_tensor(out=kvf[:], in0=kvf[:], in1=psb[:, 0:512], op=ALU.mult), r=[b_kvf, bpsb], w=[b_kvf])
                S.op("dve", lambda e, r_=r_, mt=mt: e.tensor_reduce(out=sc_all[:, mt, r_ * 4:(r_ + 1) * 4], in_=kvf[:].rearrange("p (h d) -> p h d", h=4), axis=AX.X, op=ALU.add),
                     r=[b_kvf], w=[b_sc])
        ps2, bps2 = psum()
        for mt in range(2):
            S.op("pe", lambda e, mt=mt: e.transpose(out=ps2[0:64, mt * 128:(mt + 1) * 128], in_=sc_all[:, mt, :], identity=identf), r=[b_sc] + CB, w=[bps2])
        S.op("dve", lambda e: e.reduce_max(out=csm[0:64, 12:13], in_=ps2[0:64, 0:256], axis=AX.X), r=[bps2], w=[b_csm])
        S.op("dve", lambda e: e.tensor_scalar(out=csm[0:64, 12:13], in0=csm[0:64, 12:13], scalar1=-(128.0 ** -0.5), scalar2=None, op0=ALU.mult), r=[b_csm], w=[b_csm])
        S.op("act", lambda e: e.activation(out=sm_p[0:64, :], in_=ps2[0:64, 0:256], func=AF.Exp, bias=csm[0:64, 12:13], scale=128.0 ** -0.5, accum_out=csm[0:64, 13:14]),
             r=[bps2, b_csm], w=[b_smp, b_csm])
        S.op("dve", lambda e: e.reciprocal(out=csm[0:64, 14:15], in_=csm[0:64, 13:14]), r=[b_csm], w=[b_csm])
        S.op("dve", lambda e: e.tensor_scalar(out=sm_p[0:64, :], in0=sm_p[0:64, :], scalar1=csm[0:64, 14:15], scalar2=None, op0=ALU.mult), r=[b_smp, b_csm], w=[b_smp])
        ps3, bps3 = psum()
        for mt in range(2):
            S.op("pe", lambda e, mt=mt: e.transpose(out=ps3[:, mt * 64:(mt + 1) * 64], in_=sm_p[0:64, mt * 128:(mt + 1) * 128], identity=cst[0:64, C_ID:C_ID + 64]), r=[b_smp] + CB, w=[bps3])
        S.op("act", lambda e: e.copy(out=pTs[:], in_=ps3[:, 0:128].rearrange("p (a b) -> p a b", a=2)), r=[bps3], w=[b_pTs])
        pso, bpso = psum(pin=True)
        for r_ in range(NSMP):
            for mt in range(2):
                S.dma("sp", "s_kv", kvf[:], d_cv[l, r_, mt * 128:(mt + 1) * 128, :], w=[b_kvf])
                S.op("act", lambda e: e.copy(out=kvb[:], in_=kvf[:]), r=[b_kvf], w=[b_kvb])
                for hh in range(4):
                    S.op("pe", lambda e, hh=hh, mt=mt, r_=r_: e.matmul(pso[:, mt * 64 + hh * NSMP + r_:mt * 64 + hh * NSMP + r_ + 1], lhsT=kvb[:, hh * 128:(hh + 1) * 128],
                                                                      rhs=pTs[:, mt, r_ * 4 + hh:r_ * 4 + hh + 1], start=True, stop=True), r=[b_kvb, b_pTs], w=[bpso])
        S.op("act", lambda e: e.copy(out=sm_p[:, 0:64], in_=pso[:, 0:64]), r=[bpso], w=[b_smp])
        S.op("dve", lambda e: e.tensor_tensor(out=mixT[:, 0:4, TILE:TILE + NSMP], in0=sm_p[:, 0:64].rearrange("p (a b) -> p a b", a=4),
                                              in1=pso[:, 64:128].rearrange("p (a b) -> p a b", a=4), op=ALU.add), r=[bpso, b_smp], w=[b_mix])
        unpin(bpso)

    rga = rg; rgb = rf; rgc = rux; reg = ra; rngc = rnu
    rt1 = rm
    gcols = sb("gcols", [128, NCK * 3 * 8]); b_gcols = Buf()
    GBx = UBx
    b_GB = b_UB
    q2T = qTb
    b_q2 = b_qT
    k2T = kTb
    b_k2 = b_kT
    nega = sb("nega", [8, 2]); b_nega = Buf()
    ztok = sb("ztok", [128, NCK, 128], BF16); b_ztok = Buf()
    s_z = sb("s_z", [NSMP, 128]); b_sz = Buf()
    s_pre = sb("s_pre", [128, 3, NSMP]); b_spre = Buf()

    def gdn(l, ti):
        smp = (ti == 0 and with_sample)
        ncol = R if smp else TILE

        def ev_g(ps, bps, m, c0, n, gi, si):
            dst = rga if si == 0 else rgb
            if si == 0:
                S.op("dve", lambda e: e.tensor_scalar(out=dst[0:8, c0:c0 + n], in0=ps[0:8, 0:n], scalar1=gateb[0:8, l * 4 + 3:l * 4 + 4], scalar2=None, op0=ALU.add),
                     r=[bps, b_par], w=[b_rows])
            else:
                S.op("act", lambda e: e.activation(out=dst[0:8, c0:c0 + n], in_=ps[0:8, 0:n], func=AF.Sigmoid), r=[bps], w=[b_rows])
        proj(nT, b_n, ti, ev_g, subs=[(0, 8), (8, 16)])
        S.op("act", lambda e: e.activation(out=rt1[0:8, 0:ncol], in_=rga[0:8, 0:ncol], func=AF.Abs), r=[b_rows], w=[b_rows])
        S.op("act", lambda e: e.activation(out=rt1[0:8, 0:ncol], in_=rt1[0:8, 0:ncol], func=AF.Exp, scale=-1.0), r=[b_rows], w=[b_rows])
        S.op("act", lambda e: e.activation(out=rt1[0:8, 0:ncol], in_=rt1[0:8, 0:ncol], func=AF.Ln, bias=1.0), r=[b_rows], w=[b_rows])
        S.op("dve", lambda e: e.scalar_tensor_tensor(out=rt1[0:8, 0:ncol], in0=rga[0:8, 0:ncol], scalar=0.0, in1=rt1[0:8, 0:ncol], op0=ALU.max, op1=ALU.add), r=[b_rows], w=[b_rows])
        S.op("act", lambda e: e.activation(out=nega[0:8, 0:1], in_=gateb[0:8, l * 4 + 2:l * 4 + 3], func=AF.Exp), r=[b_par], w=[b_nega])
        S.op("dve", lambda e: e.tensor_scalar(out=nega[0:8, 0:1], in0=nega[0:8, 0:1], scalar1=-1.0, scalar2=None, op0=ALU.mult), r=[b_nega], w=[b_nega])
        S.op("dve", lambda e: e.tensor_scalar(out=rga[0:8, 0:ncol], in0=rt1[0:8, 0:ncol], scalar1=nega[0:8, 0:1], scalar2=None, op0=ALU.mult), r=[b_rows, b_nega], w=[b_rows])
        S.op("dve", lambda e: e.memset(rgc[0:8, 0:1], 0.0), w=[b_rows])
        S.op("dve", lambda e: e.tensor_tensor_scan(out=rgc[0:8, 1:(TILE + 1)], data0=rga[0:8, 0:TILE], data1=rz[0:8, :], initial=0.0, op0=ALU.add, op1=ALU.add), r=[b_rows], w=[b_rows])
        S.op("dve", lambda e: e.tensor_scalar(out=rngc[0:8, :], in0=rgc[0:8, :], scalar1=-1.0, scalar2=None, op0=ALU.mult), r=[b_rows], w=[b_rows])
        for c in range(NCK):
            S.op("act", lambda e, c=c: e.activation(out=reg[0:8, c * 128:(c + 1) * 128], in_=rgc[0:8, 1 + c * 128:1 + (c + 1) * 128], func=AF.Exp,
                                                    bias=rngc[0:8, c * 128:c * 128 + 1], scale=1.0), r=[b_rows], w=[b_rows])
        rows_to_cols([rgc[:, 1:(TILE + 1)], reg, rgb], [b_rows], 8, gcols, b_gcols)
        split3(rgc, b_rows, 8, (TILE + 1), pcs, b_pcs, ptmp, b_ptmp)
        S.dma("sp", "nb", gnb[:], d_gn[l].partition_broadcast(128), w=[b_nb])

        def gcol(c, qi, h):
            o = (c * 3 + qi) * 8 + h
            return gcols[:, o:o + 1]

        for h in range(8):
            bcast_rows(pcs, b_pcs, 8, h, (TILE + 1), GBx, b_GB)
            for qi in range(3):
                cc = qi * 8 + h
                tb = (l * 24 + cc) * 3
                S.op("pool", lambda e: e.tensor_copy(out=pre[:, 0:3], in_=ctail[:, tb:tb + 3]), r=[b_ctail], w=[b_pre])

                def ev_p(ps, bps, m, c0, n, gi, si):
                    if c0 == TILE:
                        S.op("dve", lambda e: e.tensor_copy(out=s_pre[:, qi, :], in_=ps[:, 0:n]), r=[bps], w=[b_spre])
                    else:
                        S.op("act", lambda e: e.copy(out=pre[:, 3 + c0:3 + c0 + n], in_=ps[:, 0:n]), r=[bps], w=[b_pre])
                proj(nT, b_n, ti, ev_p)
                S.op("pool", lambda e: e.tensor_copy(out=ctail[:, tb:tb + 3], in_=pre[:, TILE:(TILE + 3)]), r=[b_pre], w=[b_ctail])
                if ti == NT - 1:
                    S.dma("sp", "o_pcv", o_pconvT[l, cc * 128:(cc + 1) * 128, :], pre[:, TILE:(TILE + 3)], r=[b_pre])
                wb = (l * 24 + cc) * 4
                S.op("dve", lambda e: e.tensor_scalar(out=cvy[:, 0:TILE], in0=pre[:, 0:TILE], scalar1=convw[:, wb:wb + 1], scalar2=None, op0=ALU.mult), r=[b_pre, b_par], w=[b_cvy])
                for j in range(1, 4):
                    S.op("dve", lambda e, j=j: e.scalar_tensor_tensor(out=cvy[:, 0:TILE], in0=pre[:, j:j + TILE], scalar=convw[:, wb + j:wb + j + 1], in1=cvy[:, 0:TILE],
                                                                      op0=ALU.mult, op1=ALU.add), r=[b_pre, b_par, b_cvy], w=[b_cvy])
                S.op("act", lambda e: e.activation(out=cvy[:, 0:TILE], in_=cvy[:, 0:TILE], func=AF.Silu), r=[b_cvy], w=[b_cvy])
                if qi < 2:
                    dstb, bdst = (q2T, b_q2) if qi == 0 else (k2T, b_k2)
                    sc = (128.0 ** -0.5) if qi == 0 else 1.0
                    for c0 in range(0, TILE, 512):
                        S.op("act", lambda e: e.activation(out=dstb[:, c0:c0 + 512], in_=cvy[:, c0:c0 + 512], func=AF.Square), r=[b_cvy], w=[bdst])
                        ps, bps = psum()
                        S.op("pe", lambda e: e.matmul(ps[:, 0:512], lhsT=onesb[:], rhs=dstb[:, c0:c0 + 512], start=True, stop=True), r=[bdst] + CB, w=[bps])
                        S.op("act", lambda e: e.activation(out=sb_rstd[:, 0:512], in_=ps[:, 0:512], func=AF.Ln, bias=epsc[:], scale=1.0), r=[bps, b_par], w=[b_rstd])
                        S.op("act", lambda e: e.activation(out=sb_rstd[:, 0:512], in_=sb_rstd[:, 0:512], func=AF.Exp, scale=-0.5), r=[b_rstd], w=[b_rstd])
                        if qi == 0:
                            S.op("dve", lambda e: e.scalar_tensor_tensor(out=dstb[:, c0:c0 + 512], in0=cvy[:, c0:c0 + 512], scalar=sc, in1=sb_rstd[:, 0:512], op0=ALU.mult, op1=ALU.mult),
                                 r=[b_cvy, b_rstd], w=[bdst])
                        else:
                            S.op("dve", lambda e: e.tensor_tensor(out=k2f[:, c0:c0 + 512], in0=cvy[:, c0:c0 + 512], in1=sb_rstd[:, 0:512], op=ALU.mult), r=[b_cvy, b_rstd], w=[b_k2f])
                            S.op("act", lambda e: e.copy(out=dstb[:, c0:c0 + 512], in_=k2f[:, c0:c0 + 512]), r=[b_k2f], w=[bdst])
                    if qi == 1:
                        for b0 in range(0, NCK, 4):
                            ps, bps = psum()
                            for jj in range(4):
                                S.op("pe", lambda e, jj=jj: e.transpose(out=ps[:, jj * 128:(jj + 1) * 128], in_=k2f[:, (b0 + jj) * 128:(b0 + jj + 1) * 128], identity=identf),
                                     r=[b_k2f] + CB, w=[bps])
                            S.op("act", lambda e: e.copy(out=ktok[:, b0:b0 + 4, :], in_=ps[:, 0:512].rearrange("p (j c) -> p j c", j=4)), r=[bps], w=[b_ktok])
                else:
                    for b0 in range(0, NCK, 4):
                        ps, bps = psum()
                        for jj in range(4):
                            S.op("pe", lambda e, jj=jj: e.transpose(out=ps[:, jj * 128:(jj + 1) * 128], in_=cvy[:, (b0 + jj) * 128:(b0 + jj + 1) * 128], identity=identf),
                                 r=[b_cvy] + CB, w=[bps])
                        S.op("act", lambda e: e.copy(out=vtok[:, b0:b0 + 4, 0:128], in_=ps[:, 0:512].rearrange("p (j c) -> p j c", j=4)), r=[bps], w=[b_vtok])
                if smp:
                    gdn_sample_conv(l, h, qi)
            wv, bw, _ = getw()

            def ev_z(ps3, bps, j0, nj):
                S.op("act", lambda e: e.activation(out=ztok[:, j0:j0 + nj, :], in_=ps3, func=AF.Silu), r=[bps], w=[b_ztok])

            def ev_zs(ps, bps):
                S.op("act", lambda e: e.activation(out=s_z[:, :], in_=ps, func=AF.Silu), r=[bps], w=[b_sz])
            projT(wv, bw, nT, b_n, ti, ev_z, ev_zs)
            if ti == 0:
                S.op("dve", lambda e: e.memset(Sst[:], 0.0), w=[b_S])
            else:
                S.dma("sp", "scrS_r", Sst[:], d_scrS[l, h], r=[b_scr], w=[b_S])
            S.op("act", lambda e: e.copy(out=Sb[:], in_=Sst[:]), r=[b_S], w=[b_Sb])
            for c in range(NCK):
                t0 = c * 128
                psa, bpsa = psum()
                S.op("pe", lambda e: e.matmul(psa[:, 0:128], lhsT=k2T[:, t0:t0 + 128], rhs=k2T[:, t0:t0 + 128], start=True, stop=True), r=[b_k2], w=[bpsa])
                psq, bpsq = psum()
                S.op("pe", lambda e: e.matmul(psq[:, 0:128], lhsT=k2T[:, t0:t0 + 128], rhs=q2T[:, t0:t0 + 128], start=True, stop=True), r=[b_k2, b_q2], w=[bpsq])
                S.op("dve", lambda e: e.scalar_tensor_tensor(out=cX[:], in0=GBx[:, 1 + t0:1 + t0 + 128], scalar=gcol(c, 0, h), in1=negi, op0=ALU.subtract, op1=ALU.add),
                     r=[b_GB, b_gcols] + CB, w=[b_cX])
                S.op("act", lambda e: e.activation(out=cE[:], in_=cX[:], func=AF.Exp), r=[b_cX], w=[b_cE])
                S.op("dve", lambda e: e.tensor_tensor(out=cQK[:], in0=cE[:], in1=psq[:, 0:128], op=ALU.mult), r=[b_cE, bpsq], w=[b_QK])
                S.op("dve", lambda e: e.tensor_tensor(out=cAT[:], in0=cE[:], in1=psa[:, 0:128], op=ALU.mult), r=[b_cE, bpsa], w=[b_AT])
                S.op("dve", lambda e: e.scalar_tensor_tensor(out=cAT[:], in0=cAT[:], scalar=gcol(c, 2, h), in1=mstrict, op0=ALU.mult, op1=ALU.mult), r=[b_AT, b_gcols] + CB, w=[b_AT])
                S.op("dve", lambda e: e.tensor_tensor(out=cX[:], in0=cAT[:], in1=cst[:, C_MK:C_MK + 128], op=ALU.mult), r=[b_AT] + CB, w=[b_cX])
                S.op("dve", lambda e: e.tensor_tensor(out=cX[:], in0=identf, in1=cX[:], op=ALU.subtract), r=[b_cX] + CB, w=[b_cX])
                S.op("act", lambda e: e.copy(out=cPT[0][:], in_=cX[:]), r=[b_cX], w=[b_PT[0]])
                pst, bpst = psum()
                S.op("pe", lambda e: e.transpose(out=pst[:, 0:128], in_=cX[:], identity=identf), r=[b_cX] + CB, w=[bpst])
                S.op("act", lambda e: e.copy(out=cP[0][:], in_=pst[:, 0:128]), r=[bpst], w=[b_P[0]])
                for lv in range(6):
                    S.op("dve", lambda e, lv=lv: e.tensor_tensor(out=cBk[lv][:], in0=cAT[:], in1=cst[:, C_MK + (lv + 1) * 128:C_MK + (lv + 2) * 128], op=ALU.mult),
                         r=[b_AT] + CB, w=[b_Bk[lv]])
                pi = 0
                for lv in range(6):
                    po = 1 - pi
                    psw, bpsw = psum()
                    S.op("pe", lambda e, lv=lv, pi=pi: e.matmul(psw[:, 0:128], lhsT=cBk[lv][:], rhs=cP[pi][:], start=True, stop=True), r=[b_Bk[lv], b_P[pi]], w=[bpsw])
                    S.op("act", lambda e: e.copy(out=cTT[0][:], in_=psw[:, 0:128]), r=[bpsw], w=[b_TT[0]])
                    ps1, bps1 = psum()
                    S.op("pe", lambda e, pi=pi: e.matmul(ps1[:, 0:128], lhsT=cTT[0][:], rhs=cPT[pi][:], start=True, stop=True), r=[b_TT[0], b_PT[pi]], w=[bps1])
                    if lv < 5:
                        ps2, bps2 = psum()
                        S.op("pe", lambda e, pi=pi: e.matmul(ps2[:, 0:128], lhsT=cPT[pi][:], rhs=cTT[0][:], start=True, stop=True), r=[b_TT[0], b_PT[pi]], w=[bps2])
                    S.op("dve", lambda e, pi=pi, po=po: e.tensor_tensor(out=cPT[po][:], in0=cPT[pi][:], in1=ps1[:, 0:128], op=ALU.subtract), r=[b_PT[pi], bps1], w=[b_PT[po]])
                    if lv < 5:
                        S.op("dve", lambda e, pi=pi, po=po: e.tensor_tensor(out=cP[po][:], in0=cP[pi][:], in1=ps2[:, 0:128], op=ALU.subtract), r=[b_P[pi], bps2], w=[b_P[po]])
                    pi = po
                TTf, bTTf = cPT[pi], b_PT[pi]
                psk, bpsk = psum()
                S.op("pe", lambda e: e.matmul(psk[:, 0:128], lhsT=k2T[:, t0:t0 + 128], rhs=Sb[:], start=True, stop=True), r=[b_k2, b_Sb], w=[bpsk])
                psqs, bpsqs = psum()
                S.op("pe", lambda e: e.matmul(psqs[:, 0:128], lhsT=q2T[:, t0:t0 + 128], rhs=Sb[:], start=True, stop=True), r=[b_q2, b_Sb], w=[bpsqs])
                S.op("dve", lambda e: e.tensor_scalar(out=csm[:, 6:7], in0=gcol(c, 1, h), scalar1=-1.0, scalar2=None, op0=ALU.mult), r=[b_gcols], w=[b_csm])
                S.op("dve", lambda e: e.scalar_tensor_tensor(out=crhs[:], in0=psk[:, 0:128], scalar=csm[:, 6:7], in1=vtok[:, c, 0:128], op0=ALU.mult, op1=ALU.add),
                     r=[bpsk, b_csm, b_vtok], w=[b_rhs])
                psu, bpsu = psum()
                S.op("pe", lambda e: e.matmul(psu[:, 0:128], lhsT=TTf[:], rhs=crhs[:], start=True, stop=True), r=[bTTf, b_rhs], w=[bpsu])
                S.op("act", lambda e: e.activation(out=cwu[:], in_=psu[:, 0:128], func=AF.Copy, scale=gcol(c, 2, h)), r=[bpsu, b_gcols], w=[b_wu])
                S.op("act", lambda e: e.activation(out=co1[:], in_=psqs[:, 0:128], func=AF.Copy, scale=gcol(c, 1, h)), r=[bpsqs, b_gcols], w=[b_o1])
                pso, bpso = psum()
                S.op("pe", lambda e: e.matmul(pso[:, 0:128], lhsT=cQK[:], rhs=cwu[:], start=True, stop=True), r=[b_QK, b_wu], w=[bpso])
                S.op("dve", lambda e: e.tensor_tensor(out=cnum[:, 0:128], in0=co1[:], in1=pso[:, 0:128], op=ALU.add), r=[b_o1, bpso], w=[b_num])
                gated_norm_to_mix(cnum[:, 0:128], 128, gnb[:, h * 128:(h + 1) * 128], ztok[:, c, :], b_ztok, [8 + h], t0, 128, [b_num])
                S.op("act", lambda e: e.activation(out=csm[:, 7:8], in_=gcol(c, 0, h), func=AF.Exp, bias=GBx[:, t0 + 128:t0 + 129], scale=-1.0), r=[b_gcols, b_GB], w=[b_csm])
                S.op("dve", lambda e: e.tensor_scalar(out=ckw[:], in0=ktok[:, c, :], scalar1=csm[:, 7:8], scalar2=None, op0=ALU.mult), r=[b_ktok, b_csm], w=[b_kw])
                psd, bpsd = psum()
                S.op("pe", lambda e: e.matmul(psd[:, 0:128], lhsT=ckw[:], rhs=cwu[:], start=True, stop=True), r=[b_kw, b_wu], w=[bpsd])
                S.op("dve", lambda e: e.tensor_tensor(out=csm[:, 8:9], in0=GBx[:, t0 + 128:t0 + 129], in1=GBx[:, t0:t0 + 1], op=ALU.subtract), r=[b_GB], w=[b_csm])
                S.op("act", lambda e: e.activation(out=csm[:, 8:9], in_=csm[:, 8:9], func=AF.Exp), r=[b_csm], w=[b_csm])
                S.op("dve", lambda e: e.scalar_tensor_tensor(out=Sst[:], in0=Sst[:], scalar=csm[:, 8:9], in1=psd[:, 0:128], op0=ALU.mult, op1=ALU.add), r=[b_S, b_csm, bpsd], w=[b_S])
                S.op("act", lambda e: e.copy(out=Sb[:], in_=Sst[:]), r=[b_S], w=[b_Sb])
            if ti < NT - 1:
                S.dma("sp", "scrS_w", d_scrS[l, h], Sst[:], r=[b_S], w=[b_scr])
            else:
                S.dma("sp", "o_pS", o_pS[l, h], Sst[:], r=[b_S])
            if smp:
                gdn_sample(l, h)
    memh = sb("memh", [128, 8, 256]); b_memh = Buf()
    d_memv = d_memT.rearrange("(c p) m -> p c m", p=128)

    def mem_norm(l):
        gcol0 = (l * 4 + 3) * 16
        ps, bps = psum()
        for hf in range(2):
            S.dma("sp", "mem", memh[:], d_memv[:, hf * 8:(hf + 1) * 8, :], w=[b_memh])
            S.op("act", lambda e, hf=hf: e.activation(out=mhT[:, hf * 8:(hf + 1) * 8, :], in_=memh[:], func=AF.Square), r=[b_memh], w=[b_mh])
            for k in range(8):
                kk = hf * 8 + k
                S.op("pe", lambda e, kk=kk: e.matmul(ps[:, 0:256], lhsT=onesb[:], rhs=mhT[:, kk, :], start=(kk == 0), stop=(kk == 15)), r=[b_mh] + CB, w=[bps])
        S.op("act", lambda e: e.activation(out=sb_rstd[:, 0:256], in_=ps[:, 0:256], func=AF.Ln, bias=epsc[:], scale=1.0 / D), r=[bps, b_par], w=[b_rstd])
        S.op("act", lambda e: e.activation(out=sb_rstd[:, 0:256], in_=sb_rstd[:, 0:256], func=AF.Exp, scale=-0.5), r=[b_rstd], w=[b_rstd])
        for hf in range(2):
            S.dma("sp", "mem", memh[:], d_memv[:, hf * 8:(hf + 1) * 8, :], w=[b_memh])
            for k in range(8):
                kk = hf * 8 + k
                S.op("dve", lambda e, k=k, kk=kk: e.scalar_tensor_tensor(out=mhT[:, kk, :], in0=memh[:, k, :], scalar=gains[:, gcol0 + kk:gcol0 + kk + 1], in1=sb_rstd[:, 0:256],
                                                                          op0=ALU.mult, op1=ALU.mult), r=[b_memh, b_rstd, b_par], w=[b_mh])
    mhT = mixT[:, :, :].rearrange("p c t -> p (c t)")[:, 0:4096].rearrange("p (c m) -> p c m", c=16)
    b_mh = b_mix
    KT = sb("KT", [128, 4, 256], BF16); b_KT = Buf()
    KTf = sb("KTf", [128, 256]); b_KTf = Buf()
    Vm = sb("Vm", [128, 2, 512], BF16); b_Vm = Buf()
    Vf = sb("Vf", [128, 128]); b_Vf = Buf()
    qx = sb("qx", [128, 4, R], BF16); b_qx = Buf()
    qxs = sb("qxs", [128, 4, NSMP]); b_qxs = Buf()
    pex = sb("pex", [128, 256]); b_pex = Buf()
    pTb = sb("pTb", [128, 2, 128], BF16); b_pT = Buf()

    def xattn(l, ti):
        smp = (ti == 0 and with_sample)
        mem_norm(l)
        if stage == 5.1:
            return
        for j in range(4):
            def ev_k(ps, bps, m, c0, n, gi, si, j=j):
                S.op("dve", lambda e: e.tensor_copy(out=KTf[:], in_=ps[:, 0:256]), r=[bps], w=[b_KTf])
                S.op("act", lambda e: e.copy(out=KT[:, j, :], in_=KTf[:]), r=[b_KTf], w=[b_KT])
                if ti == 0:
                    S.dma("sp", "o_pk", o_pkT[l, j * 128:(j + 1) * 128, :], KTf[:], r=[b_KTf])
            proj(mhT, b_mh, ti, ev_k, grp=[(0, 256)])
        if stage == 5.2:
            return
        for j in range(4):
            wv, bw, _ = getw()

            def ev_v(ps3, bps, j0, nj, j=j):
                for mt in range(2):
                    S.op("dve", lambda e, mt=mt: e.tensor_copy(out=Vf[:, 0:128], in_=ps3[:, mt, :]), r=[bps], w=[b_Vf])
                    S.op("act", lambda e, mt=mt: e.copy(out=Vm[:, mt, j * 128:(j + 1) * 128], in_=Vf[:, 0:128]), r=[b_Vf], w=[b_Vm])
                    if ti == 0:
                        S.dma("sp", "o_pv", o_pv[l, mt * 128:(mt + 1) * 128, j * 128:(j + 1) * 128], Vf[:, 0:128], r=[b_Vf])
            projT(wv, bw, mhT, b_mh, 1, ev_v, None, ntt=2)
        if stage == 5.3:
            return
        rmsnorm_F(ti, (l * 4 + 1) * 16, nT, b_n)
        for j in range(4):
            def ev_q(ps, bps, m, c0, n, gi, si, j=j):
                if c0 == TILE:
                    S.op("dve", lambda e: e.tensor_copy(out=qxs[:, j, :], in_=ps[:, 0:n]), r=[bps], w=[b_qxs])
                    S.op("act", lambda e: e.copy(out=qx[:, j, c0:c0 + n], in_=qxs[:, j, :]), r=[b_qxs], w=[b_qx])
                else:
                    S.op("act", lambda e: e.copy(out=qx[:, j, c0:c0 + n], in_=ps[:, 0:n]), r=[bps], w=[b_qx])
            proj(nT, b_n, ti, ev_q)
        if stage == 5.4:
            return
        for tt in range(NCK):
            t0 = tt * 128
            for hh in range(4):
                ps, bps = psum()
                S.op("pe", lambda e: e.matmul(ps[:, 0:256], lhsT=qx[:, hh, t0:t0 + 128], rhs=KT[:, hh, :], start=True, stop=True), r=[b_qx, b_KT], w=[bps])
                S.op("dve", lambda e: e.reduce_max(out=csm[:, 9:10], in_=ps[:, 0:256], axis=AX.X), r=[bps], w=[b_csm])
                S.op("dve", lambda e: e.tensor_scalar(out=csm[:, 9:10], in0=csm[:, 9:10], scalar1=-(128.0 ** -0.5), scalar2=None, op0=ALU.mult), r=[b_csm], w=[b_csm])
                S.op("act", lambda e: e.activation(out=pex[:], in_=ps[:, 0:256], func=AF.Exp, bias=csm[:, 9:10], scale=128.0 ** -0.5, accum_out=csm[:, 10:11]),
                     r=[bps, b_csm], w=[b_pex, b_csm])
                S.op("dve", lambda e: e.reciprocal(out=csm[:, 11:12], in_=csm[:, 10:11]), r=[b_csm], w=[b_csm])
                S.op("dve", lambda e: e.tensor_scalar(out=pex[:], in0=pex[:], scalar1=csm[:, 11:12], scalar2=None, op0=ALU.mult), r=[b_pex, b_csm], w=[b_pex])
                ps2, bps2 = psum()
                for mt in range(2):
                    S.op("pe", lambda e, mt=mt: e.transpose(out=ps2[:, mt * 128:(mt + 1) * 128], in_=pex[:, mt * 128:(mt + 1) * 128], identity=identf), r=[b_pex] + CB, w=[bps2])
                S.op("act", lambda e: e.copy(out=pTb[:], in_=ps2[:, 0:256].rearrange("p (a b) -> p a b", a=2)), r=[bps2], w=[b_pT])
                ps3, bps3 = psum()
                for mt in range(2):
                    S.op("pe", lambda e, mt=mt: e.matmul(ps3[:, 0:128], lhsT=Vm[:, mt, hh * 128:(hh + 1) * 128], rhs=pTb[:, mt, :], start=(mt == 0), stop=(mt == 1)),
                         r=[b_Vm, b_pT], w=[bps3])
                S.op("act", lambda e: e.copy(out=mixT[:, hh, t0:t0 + 128], in_=ps3[:, 0:128]), r=[bps3], w=[b_mix])
        if stage == 5.5:
            return
        if smp:
            xattn_sample(l)
        for j in range(16):
            proj(mixT, b_mix, ti, resid_evac(j), nk=4)

    hg = sb_rstd
    b_hg = b_rstd

    def ffn(l, ti):
        rmsnorm_F(ti, (l * 4 + 2) * 16, nT, b_n)
        for g in range(4):
            for c in range(11):
                wg, bwg, _ = getw()
                wu_, bwu, _ = getw()
                for (c0, n) in groups(ti):
                    psg, bpsg = psum()
                    psu, bpsu = psum()
                    for k in range(16):
                        S.op("pe", lambda e, k=k: e.matmul(psg[:, 0:n], lhsT=wg[:, k, :], rhs=nT[:, k, c0:c0 + n], start=(k == 0), stop=(k == 15)), r=[bwg, b_n], w=[bpsg])
                    for k in range(16):
                        S.op("pe", lambda e, k=k: e.matmul(psu[:, 0:n], lhsT=wu_[:, k, :], rhs=nT[:, k, c0:c0 + n], start=(k == 0), stop=(k == 15)), r=[bwu, b_n], w=[bpsu])
                    S.op("act", lambda e: e.activation(out=hg[:, 0:n], in_=psg[:, 0:n], func=AF.Silu), r=[bpsg], w=[b_hg])
                    S.op("dve", lambda e: e.tensor_tensor(out=mixT[:, c, c0:c0 + n], in0=hg[:, 0:n], in1=psu[:, 0:n], op=ALU.mult), r=[b_hg, bpsu], w=[b_mix])
            for j in range(16):
                proj(mixT, b_mix, ti, resid_evac(j), nk=11)

    def mixer(l, ti):
        rmsnorm_F(ti, (l * 4 + 0) * 16, nT, b_n)
        mlstm(l, ti)
        gdn(l, ti)
        for j in range(16):
            proj(mixT, b_mix, ti, resid_evac(j))

    for ti in range(NT):
        S.dma("sp", "x", xT[:, :, 0:TILE], d_xT[:, ti * TILE:(ti + 1) * TILE].rearrange("(c p) t -> p c t", p=128), w=[b_x])
        if ti == 0 and with_sample:
            S.dma("sp", "x", xT[:, :, TILE:TILE + NSMP], d_xsT.rearrange("(c p) t -> p c t", p=128), w=[b_x])
        for l in range(NL):
            st["layer"] = l
            st["wi"] = 0
            if stage >= 6:
                mixer(l, ti)
                xattn(l, ti)
                ffn(l, ti)
                assert st["wi"] == len(WBLOCKS), (st["wi"], len(WBLOCKS))
            else:
                rmsnorm_F(ti, (l * 4 + 0) * 16, nT, b_n)
                if stage >= 2:
                    mlstm(l, ti)
                if stage >= 3:
                    gdn(l, ti)
                if stage >= 4:
                    for j in range(16):
                        proj(mixT, b_mix, ti, resid_evac(j))
                if stage >= 5:
                    xattn(l, ti)
        for (c0, n) in groups(ti):
            S.op("act", lambda e: e.activation(out=nT[:, :, c0:c0 + n], in_=xT[:, :, c0:c0 + n], func=AF.Square), r=[b_x], w=[b_n])
            ps, bps = psum()
            for k in range(NCH):
                S.op("pe", lambda e, k=k: e.matmul(ps[:, 0:n], lhsT=onesb[:], rhs=nT[:, k, c0:c0 + n], start=(k == 0), stop=(k == NCH - 1)), r=[b_n] + CB, w=[bps])
            S.op("act", lambda e: e.activation(out=sb_rstd[:, 0:n], in_=ps[:, 0:n], func=AF.Ln, bias=epsc[:], scale=1.0 / D), r=[bps, b_par], w=[b_rstd])
            S.op("act", lambda e: e.activation(out=sb_rstd[:, 0:n], in_=sb_rstd[:, 0:n], func=AF.Exp, scale=-0.5), r=[b_rstd], w=[b_rstd])
            gc = NL * 4 * 16
            for k in range(NCH):
                S.op("dve", lambda e, k=k: e.scalar_tensor_tensor(out=xT[:, k, c0:c0 + n], in0=xT[:, k, c0:c0 + n], scalar=gains[:, gc + k:gc + k + 1],
                                                                  in1=sb_rstd[:, 0:n], op0=ALU.mult, op1=ALU.mult), r=[b_x, b_rstd, b_par], w=[b_x])
        S.dma("sp", "oy", o_yT[:, ti * TILE:(ti + 1) * TILE].rearrange("(c p) t -> p c t", p=128), xT[:, :, 0:TILE], r=[b_x])
        if ti == 0 and with_sample:
            S.dma("sp", "oy", o_ysT.rearrange("(c p) t -> p c t", p=128), xT[:, :, TILE:TILE + NSMP], r=[b_x])
    S.finish()
    print('sbuf bytes remaining', nc.sbuf_bytes_remaining, 'instr counts', dict(S.cnt))
    return nc, es


def _consts():
    c = np.zeros((128, CW), np.float32)
    c[:, C_ID:C_ID + 128] = np.eye(128, dtype=np.float32)
    s = np.arange(128)[:, None]
    t = np.arange(128)[None, :]
    c[:, C_NI:C_NI + 128] = np.where(s <= t, 0.0, NEG)
    c[:, C_MS:C_MS + 128] = (s < t).astype(np.float32)
    c[:, C_ONE:C_ONE + 128] = 1.0
    c[:, C_MK:C_MK + 128] = (s // 2 == t // 2)
    bs = 2
    for lv in range(6):
        c[:, C_MK + (lv + 1) * 128:C_MK + (lv + 2) * 128] = (s // (2 * bs) == t // (2 * bs)) & (s // bs != t // bs)
        bs *= 2
    c[:, C_E16:C_E16 + 256] = np.eye(16, dtype=np.float32).reshape(1, 256)
    return c


def _pack_weights(inp, NL):
    w = np.empty((NL, 128, WTOT), np.float32)
    for i, (mat, k0, nk, c0, ncw) in enumerate(WBLOCKS):
        src = inp[mat]
        for l in range(NL):
            blk = src[l, k0 * 128:(k0 + nk) * 128, c0:c0 + ncw].reshape(nk, 128, ncw)
            w[l, :, WOFF[i]:WOFF[i + 1]] = blk.transpose(1, 0, 2).reshape(128, nk * ncw)
    return w


def _fm(v):
    return np.ascontiguousarray(v.reshape(16, 128).T)


def run(inp, NL=DEPTH, NT=SEQ // TILE, with_sample=True, trace=False, stage=99, ncores=8):
    inp = {k: np.asarray(v) for k, v in inp.items()}
    nc, es = build(NL=NL, NT=NT, with_sample=with_sample, stage=stage)
    wts = _pack_weights(inp, NL)
    cst = _consts()
    gains = np.zeros((128, (NL * 4 + 1) * 16), np.float32)
    for l in range(NL):
        for wi, nm in enumerate(("norm_mix", "norm_xattn", "norm_ffn", "norm_mem")):
            gains[:, (l * 4 + wi) * 16:(l * 4 + wi + 1) * 16] = _fm(inp[nm][l])
    gains[:, NL * 64:NL * 64 + 16] = _fm(inp["norm_final"])
    gateb = np.zeros((8, NL * 4), np.float32)
    for l in range(NL):
        gateb[0:4, l * 4 + 0] = inp["mlstm_b_i"][l]
        gateb[0:4, l * 4 + 1] = inp["mlstm_b_f"][l]
        gateb[0:8, l * 4 + 2] = inp["gdn_A_log"][l]
        gateb[0:8, l * 4 + 3] = inp["gdn_dt_bias"][l]
    mnorm = np.ascontiguousarray(inp["mlstm_norm"][:NL].reshape(NL, 1024))
    gnorm = np.ascontiguousarray(inp["gdn_norm"][:NL].reshape(NL, 1024))
    convw = np.zeros((128, NL * 24 * 4), np.float32)
    for l in range(NL):
        cw = inp["gdn_conv_w"][l]
        convw[:, l * 96:(l + 1) * 96] = cw.reshape(4, 24, 128).transpose(2, 1, 0).reshape(128, 96)
    in_maps = []
    sel = np.zeros((8, 1024), np.float32)
    for h in range(8):
        sel[h, h * 128:(h + 1) * 128] = 1.0
    for c in range(8):
        b = c % 4
        r0 = c * NSMP
        m = {
            "xT": np.ascontiguousarray(inp["x_prompt"][b].T),
            "xsT": np.ascontiguousarray(inp["x_sample"][r0:r0 + NSMP, 0, :].T),
            "memT": np.ascontiguousarray(inp["mem_prompt"][b].T),
            "wts": wts, "cst": cst, "sel": sel, "gains": gains, "gateb": gateb, "mnorm": mnorm, "gnorm": gnorm, "convw": convw,
            "sC": np.ascontiguousarray(inp["state_mlstm_C"][:NL, r0:r0 + NSMP]),
            "sn": np.ascontiguousarray(inp["state_mlstm_n"][:NL, r0:r0 + NSMP].reshape(NL, NSMP, 512)),
            "sm": np.ascontiguousarray(inp["state_mlstm_m"][:NL, r0:r0 + NSMP]),
            "sS": np.ascontiguousarray(inp["state_gdn_S"][:NL, r0:r0 + NSMP]),
            "sconv": np.ascontiguousarray(inp["state_gdn_conv"][:NL, r0:r0 + NSMP]),
            "ck": np.ascontiguousarray(inp["cache_mem_k"][:NL, r0:r0 + NSMP].reshape(NL, NSMP, 256, 512)),
            "cv": np.ascontiguousarray(inp["cache_mem_v"][:NL, r0:r0 + NSMP].reshape(NL, NSMP, 256, 512)),
        }
        in_maps.append(m)
    res = run_bass_kernel_spmd(nc, in_maps[:ncores], core_ids=list(range(ncores)), trace=trace)
    R_ = list(res.results) + [res.results[0]] * (8 - ncores)
    B = 4
    y_prompt = np.stack([R_[b]["o_yT"].T for b in range(B)])
    y_sample = np.concatenate([R_[c]["o_ysT"].T for c in range(8)], 0)[:, None, :]
    pC = np.stack([R_[b]["o_pC"] for b in range(B)], 1)
    pn = np.stack([R_[b]["o_pn"] for b in range(B)], 1)
    pm = np.stack([R_[b]["o_pm"] for b in range(B)], 1)
    pS = np.stack([R_[b]["o_pS"] for b in range(B)], 1)
    pconv = np.stack([R_[b]["o_pconvT"].transpose(0, 2, 1) for b in range(B)], 1)
    pk = np.stack([R_[b]["o_pkT"].transpose(0, 2, 1).reshape(NL, 256, 4, 128) for b in range(B)], 1)
    pv = np.stack([R_[b]["o_pv"].reshape(NL, 256, 4, 128) for b in range(B)], 1)
    sC = np.concatenate([R_[c]["o_sC"] for c in range(8)], 1)
    sn = np.concatenate([R_[c]["o_sn"].reshape(NL, NSMP, 4, 128) for c in range(8)], 1)
    sm = np.concatenate([R_[c]["o_sm"] for c in range(8)], 1)
    sS = np.concatenate([R_[c]["o_sS"] for c in range(8)], 1)
    sconv = np.concatenate([R_[c]["o_sconv"] for c in range(8)], 1)
    outs = (y_prompt, y_sample, pC, pn, pm, pS, pconv, pk, pv, sC, sn, sm, sS, sconv)
    outs = tuple(np.ascontiguousarray(o, dtype=np.float32) for o in outs)
    if trace:
        return outs, res
    return outs


def kernel(**inputs):
    return run(inputs)
```

```python
from contextlib import ExitStack
import numpy as np
import concourse.bass as bass
import concourse.mybir as mybir
from concourse.bass_utils import run_bass_kernel_spmd

F32 = mybir.dt.float32
BF16 = mybir.dt.bfloat16
ALU = mybir.AluOpType
AF = mybir.ActivationFunctionType
AX = mybir.AxisListType

D = 2048
NCH = 16
DEPTH = 4
SEQ = 2048
TILE = 512
NCK = TILE // 128
NSMP = 16
DFF = 5632
NIN = 7192
EPS = 1e-6
NEG = -30000.0

C_ID = 0
C_NI = 128
C_MS = 256
C_SEL = 384
C_ONE = 384
C_E16 = 512
C_MK = 768
CW = 768 + 7 * 128


class Buf:
    __slots__ = ("w", "r")

    def __init__(self):
        self.w = None
        self.r = {}


class Sched:
    def __init__(self, nc, es):
        self.nc = nc
        self.es = es
        self.eng = {"pe": nc.tensor, "act": nc.scalar, "dve": nc.vector, "pool": nc.gpsimd, "sp": nc.sync}
        self.sem = {}
        self.cnt = {}
        self.waited = {e: {} for e in self.eng}
        for e in self.eng:
            self.sem[e] = es.enter_context(nc.semaphore("s_" + e))
            self.cnt[e] = 0

    def _deps(self, e, r, w):
        deps = {}

        def add(s, v):
            if deps.get(s, 0) < v:
                deps[s] = v

        for b in r:
            if b.w is not None:
                add(*b.w)
        for b in w:
            if b.w is not None:
                add(*b.w)
            for s, v in b.r.items():
                add(s, v)
        for s, v in deps.items():
            if e == "pe" and s == "pe":
                continue
            if self.waited[e].get(s, 0) >= v:
                continue
            self.eng[e].wait_ge(self.sem[s], v)
            self.waited[e][s] = v

    def _upd(self, tok, r, w):
        for b in w:
            b.w = tok
            b.r = {}
        s, v = tok
        for b in r:
            if b in w:
                continue
            if b.r.get(s, 0) < v:
                b.r[s] = v

    def op(self, e, fn, r=(), w=()):
        self._deps(e, r, w)
        ins = fn(self.eng[e])
        self.cnt[e] += 1
        ins.then_inc(self.sem[e], 1)
        tok = (e, self.cnt[e])
        self._upd(tok, r, w)
        return tok

    def dma(self, q, stream, out, in_, r=(), w=()):
        if stream not in self.sem:
            self.sem[stream] = self.es.enter_context(self.nc.semaphore("d_" + stream))
            self.cnt[stream] = 0
        self._deps(q, r, w)
        ins = self.eng[q].dma_start(out=out, in_=in_)
        self.cnt[stream] += 16
        ins.then_inc(self.sem[stream], 16)
        tok = (stream, self.cnt[stream])
        self._upd(tok, r, w)
        return tok

    def finish(self, q="sp"):
        for s, v in self.cnt.items():
            if v > 0 and self.waited[q].get(s, 0) < v:
                self.eng[q].wait_ge(self.sem[s], v)
                self.waited[q][s] = v


def weight_blocks():
    bl = []
    bl.append(("w_in", 0, 16, 3072, 8))
    for h in range(4):
        bl.append(("w_in", 0, 16, h * 128, 128))
        bl.append(("w_in", 0, 16, 512 + h * 128, 128))
        for j in range(2):
            bl.append(("w_in", 0, 16, 1024 + h * 256 + j * 128, 128))
        for j in range(2):
            bl.append(("w_in", 0, 16, 2048 + h * 256 + j * 128, 128))
    bl.append(("w_in", 0, 16, 7176, 16))
    for h in range(8):
        bl.append(("w_in", 0, 16, 3080 + h * 128, 128))
        bl.append(("w_in", 0, 16, 4104 + h * 128, 128))
        bl.append(("w_in", 0, 16, 5128 + h * 128, 128))
        bl.append(("w_in", 0, 16, 6152 + h * 128, 128))
    for j in range(16):
        bl.append(("w_out", 0, 16, j * 128, 128))
    for j in range(4):
        bl.append(("xattn_wk", 0, 16, j * 128, 128))
    for j in range(4):
        bl.append(("xattn_wv", 0, 16, j * 128, 128))
    for j in range(4):
        bl.append(("xattn_wq", 0, 16, j * 128, 128))
    for j in range(16):
        bl.append(("xattn_wo", 0, 4, j * 128, 128))
    for g in range(4):
        for c in range(11):
            bl.append(("ffn_w_gate", 0, 16, (g * 11 + c) * 128, 128))
            bl.append(("ffn_w_up", 0, 16, (g * 11 + c) * 128, 128))
        for j in range(16):
            bl.append(("ffn_w_down", g * 11, 11, j * 128, 128))
    return bl


WBLOCKS = weight_blocks()
WOFF = np.cumsum([0] + [b[2] * b[4] for b in WBLOCKS]).tolist()
WTOT = WOFF[-1]


def build(NL=DEPTH, NT=4, stage=99, with_sample=True):
    nc = bass.Bass("TRN2", target_bir_lowering=False)
    es = ExitStack()
    S = Sched(nc, es)

    def din(name, shape):
        return nc.dram_tensor(name, list(shape), F32, kind="ExternalInput").ap()

    def dout(name, shape):
        return nc.dram_tensor(name, list(shape), F32, kind="ExternalOutput").ap()

    def dscr(name, shape):
        return nc.dram_tensor(name, list(shape), F32).ap()

    def sb(name, shape, dt=F32):
        return es.enter_context(nc.sbuf_tensor("sb_" + name, list(shape), dt))

    d_xT = din("xT", [D, SEQ])
    d_xsT = din("xsT", [D, NSMP])
    d_memT = din("memT", [D, 256])
    d_w = din("wts", [NL, 128, WTOT])
    d_cst = din("cst", [128, CW])
    d_gain = din("gains", [128, (NL * 4 + 1) * 16])
    d_gb = din("gateb", [8, NL * 4])
    d_mn = din("mnorm", [NL, 4 * 256])
    d_gn = din("gnorm", [NL, 8 * 128])
    d_cw = din("convw", [128, NL * 24 * 4])
    d_sC = din("sC", [NL, NSMP, 4, 128, 256])
    d_sn = din("sn", [NL, NSMP, 4 * 128])
    d_sm = din("sm", [NL, NSMP, 4])
    d_sS = din("sS", [NL, NSMP, 8, 128, 128])
    d_sconv = din("sconv", [NL, NSMP, 3, 3072])
    d_ck = din("ck", [NL, NSMP, 256, 512])
    d_cv = din("cv", [NL, NSMP, 256, 512])

    o_yT = dout("o_yT", [D, SEQ])
    o_ysT = dout("o_ysT", [D, NSMP])
    o_pC = dout("o_pC", [NL, 4, 128, 256])
    o_pn = dout("o_pn", [NL, 4, 128])
    o_pm = dout("o_pm", [NL, 4])
    o_pS = dout("o_pS", [NL, 8, 128, 128])
    o_pconvT = dout("o_pconvT", [NL, 3072, 3])
    o_pkT = dout("o_pkT", [NL, 512, 256])
    o_pv = dout("o_pv", [NL, 256, 512])
    o_sC = dout("o_sC", [NL, NSMP, 4, 128, 256])
    o_sn = dout("o_sn", [NL, NSMP, 4 * 128])
    o_sm = dout("o_sm", [NL, NSMP, 4])
    o_sS = dout("o_sS", [NL, NSMP, 8, 128, 128])
    o_sconv = dout("o_sconv", [NL, NSMP, 3, 3072])

    cst = sb("cst", [128, CW])
    b_cst = Buf()
    S.dma("sp", "c0", cst[:], d_cst, w=[b_cst])
    identb = sb("identb", [128, 128], BF16)
    onesb = sb("onesb", [128, 128], BF16)
    selb = sb("selb", [8, 1024], BF16)
    b_cb = Buf()
    S.op("pool", lambda e: e.tensor_copy(out=identb[:], in_=cst[:, C_ID:C_ID + 128]), r=[b_cst], w=[b_cb])
    S.op("pool", lambda e: e.tensor_copy(out=onesb[:], in_=cst[:, C_ONE:C_ONE + 128]), r=[b_cst], w=[b_cb])
    d_sel = din("sel", [8, 1024])
    identf = cst[:, C_ID:C_ID + 128]
    negi = cst[:, C_NI:C_NI + 128]
    mstrict = cst[:, C_MS:C_MS + 128]
    CB = [b_cst, b_cb]

    gains = sb("gains", [128, (NL * 4 + 1) * 16])
    gateb = sb("gateb", [8, NL * 4])
    convw = sb("convw", [128, NL * 24 * 4])
    b_par = Buf()
    S.dma("sp", "c1", gains[:], d_gain, w=[b_par])
    S.dma("sp", "c2", gateb[:], d_gb, w=[b_par])
    S.dma("sp", "c3", convw[:], d_cw, w=[b_par])
    epsc = sb("epsc", [128, 1])
    S.op("pool", lambda e: e.memset(epsc[:], EPS), w=[b_par])

    TTM = TILE + NSMP
    xT = sb("xT", [128, NCH, TTM])
    b_x = Buf()
    nT = sb("nT", [128, NCH, TTM], BF16)
    b_n = Buf()
    mixT = sb("mixT", [128, NCH, TTM], BF16)
    b_mix = Buf()
    NWB = 4
    wbf = [sb("wbf%d" % i, [128, 16 * 128], BF16) for i in range(NWB)]
    b_wbf = [Buf() for _ in range(NWB)]
    NPS = 8
    PS = [es.enter_context(nc.psum_tensor("ps%d" % i, [128, 512], F32)) for i in range(NPS)]
    BPS = [Buf() for _ in range(NPS)]
    st = {"ps": 0, "w": 0, "layer": 0, "wi": 0, "ws": 0}

    pinned = set()

    def psum(pin=False):
        while True:
            i = st["ps"]
            st["ps"] = (i + 1) % NPS
            if i not in pinned:
                break
        if pin:
            pinned.add(i)
        return PS[i], BPS[i]

    def unpin(bps):
        pinned.discard(BPS.index(bps))

    def getw():
        i = st["wi"]
        st["wi"] += 1
        mat, k0, nk, c0, ncw = WBLOCKS[i]
        sz = nk * ncw
        n = st["w"]
        st["w"] += 1
        s2 = n % NWB
        S.dma("pool", "w%d" % s2, wbf[s2][:, 0:sz], d_w[st["layer"], :, WOFF[i]:WOFF[i] + sz], w=[b_wbf[s2]])
        return wbf[s2][:, 0:sz].rearrange("p (k c) -> p k c", k=nk), b_wbf[s2], (mat, k0, nk, c0, ncw)

    def groups(ti):
        g = [(c0, 512) for c0 in range(0, TILE, 512)]
        if ti == 0 and with_sample:
            g.append((TILE, NSMP))
        return g

    def proj_F(src, bsrc, ti, evac, nk=16, m=None, koff=0):
        wv, bw, desc = getw()
        mm = desc[4] if m is None else m
        for gi, (c0, n) in enumerate(groups(ti)):
            ps, bps = psum()
            for k in range(nk):
                S.op("pe", lambda e, k=k: e.matmul(ps[0:mm, 0:n], lhsT=wv[:, k, 0:mm], rhs=src[:, koff + k, c0:c0 + n],
                                                   start=(k == 0), stop=(k == nk - 1)), r=[bw, bsrc], w=[bps])
            evac(ps, bps, mm, c0, n, gi)
        return desc

    def rmsnorm_F(ti, gcol0, out, bout, src=None, bsrc=None, ncols=None):
        src = xT if src is None else src
        bsrc = b_x if bsrc is None else bsrc
        grp = groups(ti) if ncols is None else [(0, ncols)]
        for (c0, n) in grp:
            S.op("act", lambda e: e.activation(out=out[:, :, c0:c0 + n], in_=src[:, :, c0:c0 + n], func=AF.Square), r=[bsrc], w=[bout])
            ps, bps = psum()
            for k in range(NCH):
                S.op("pe", lambda e, k=k: e.matmul(ps[:, 0:n], lhsT=onesb[:], rhs=out[:, k, c0:c0 + n], start=(k == 0), stop=(k == NCH - 1)),
                     r=[bout] + CB, w=[bps])
            rstd = sb_rstd
            S.op("act", lambda e: e.activation(out=rstd[:, 0:n], in_=ps[:, 0:n], func=AF.Ln, bias=epsc[:], scale=1.0 / D), r=[bps, b_par], w=[b_rstd])
            S.op("act", lambda e: e.activation(out=rstd[:, 0:n], in_=rstd[:, 0:n], func=AF.Exp, scale=-0.5), r=[b_rstd], w=[b_rstd])
            for k in range(NCH):
                S.op("dve", lambda e, k=k: e.scalar_tensor_tensor(out=out[:, k, c0:c0 + n], in0=src[:, k, c0:c0 + n], scalar=gains[:, gcol0 + k:gcol0 + k + 1],
                                                                  in1=rstd[:, 0:n], op0=ALU.mult, op1=ALU.mult), r=[bsrc, b_rstd, b_par], w=[bout])

    sb_rstd = sb("rstd", [128, 512])
    b_rstd = Buf()

    def resid_evac(j):
        def ev(ps, bps, m, c0, n, gi, si=0):
            S.op("dve", lambda e: e.tensor_tensor(out=xT[:, j, c0:c0 + n], in0=xT[:, j, c0:c0 + n], in1=ps[:, 0:n], op=ALU.add), r=[bps, b_x], w=[b_x])
        return ev

    def proj(src, bsrc, ti, evac, subs=None, nk=16, koff=0, grp=None):
        wv, bw, desc = getw()
        subs = [(0, desc[4])] if subs is None else subs
        for gi, (c0, n) in enumerate(groups(ti) if grp is None else grp):
            for si, (lo, hi) in enumerate(subs):
                ps, bps = psum()
                for k in range(nk):
                    S.op("pe", lambda e, k=k: e.matmul(ps[0:hi - lo, 0:n], lhsT=wv[:, k, lo:hi], rhs=src[:, koff + k, c0:c0 + n],
                                                       start=(k == 0), stop=(k == nk - 1)), r=[bw, bsrc], w=[bps])
                evac(ps, bps, hi - lo, c0, n, gi, si)
        return wv, bw

    def projT(wv, bw, src, bsrc, ti, evac, evac_s=None, nk=16, ntt=NCK):
        ncols = wv.shape[2]
        for b0 in range(0, ntt, 4):
            ps, bps = psum()
            nj = min(4, ntt - b0)
            for jj in range(nj):
                j = b0 + jj
                for k in range(nk):
                    S.op("pe", lambda e, k=k, j=j, jj=jj: e.matmul(ps[:, jj * ncols:(jj + 1) * ncols], lhsT=src[:, k, j * 128:(j + 1) * 128], rhs=wv[:, k, :],
                                                                   start=(k == 0), stop=(k == nk - 1)), r=[bw, bsrc], w=[bps])
            evac(ps[:, 0:nj * ncols].rearrange("p (j c) -> p j c", j=nj), bps, b0, nj)
        if evac_s is not None and ti == 0 and with_sample:
            ps, bps = psum()
            for k in range(nk):
                S.op("pe", lambda e, k=k: e.matmul(ps[0:NSMP, 0:ncols], lhsT=src[:, k, TILE:TILE + NSMP], rhs=wv[:, k, :],
                                                   start=(k == 0), stop=(k == nk - 1)), r=[bw, bsrc], w=[bps])
            evac_s(ps[0:NSMP, 0:ncols], bps)

    def split3(src, bsrc, np_, n, pieces, bpc, tmp, btmp, npc=3):
        cur = src
        bcur = bsrc
        for i in range(npc):
            S.op("dve", lambda e, i=i, cur=cur: e.tensor_copy(out=pieces[i][0:np_, 0:n], in_=cur[0:np_, 0:n]), r=[bcur], w=[bpc])
            if i < npc - 1:
                S.op("dve", lambda e, i=i, cur=cur: e.tensor_tensor(out=tmp[0:np_, 0:n], in0=cur[0:np_, 0:n], in1=pieces[i][0:np_, 0:n], op=ALU.subtract),
                     r=[bcur, bpc], w=[btmp])
                cur = tmp
                bcur = btmp

    def bcast_rows(pieces, bpc, np_, h, n, dst, bdst, npc=3):
        for c0 in range(0, n, 512):
            nn = min(512, n - c0)
            ps, bps = psum()
            for i in range(npc):
                S.op("pe", lambda e, i=i: e.matmul(ps[:, 0:nn], lhsT=selb[0:np_, h * 128:(h + 1) * 128], rhs=pieces[i][0:np_, c0:c0 + nn],
                                                   start=(i == 0), stop=(i == npc - 1)), r=[bpc] + CB, w=[bps])
            S.op("act", lambda e: e.copy(out=dst[:, c0:c0 + nn], in_=ps[:, 0:nn]), r=[bps], w=[bdst])

    def rows_to_cols(rowlist, brows, np_, dst, bdst):
        nq = len(rowlist)
        ps, bps = psum()
        for c in range(NCK):
            for qi, rt in enumerate(rowlist):
                o = (c * nq + qi) * np_
                S.op("pe", lambda e, rt=rt, c=c, o=o: e.transpose(out=ps[:, o:o + np_], in_=rt[0:np_, c * 128:(c + 1) * 128], identity=cst[0:np_, C_ID:C_ID + np_]),
                     r=brows + CB, w=[bps])
        S.op("dve", lambda e: e.tensor_copy(out=dst[:, 0:NCK * nq * np_], in_=ps[:, 0:NCK * nq * np_]), r=[bps], w=[bdst])

    R = TILE + NSMP
    _rsz = [R, R, TILE, R, TILE + 1, TILE, TILE, TILE, TILE + 1]
    arena = sb("arena", [128, sum(_rsz)])
    _ro = np.cumsum([0] + _rsz).tolist()
    rg, rf, rF, rm, rux, rw, ra, rem, rnu = [arena[0:8, _ro[i]:_ro[i + 1]] for i in range(9)]
    rz = sb("rz", [8, TILE])
    b_rows = Buf()
    S.op("pool", lambda e: e.memset(rz[:], 0.0), w=[b_rows])
    S.dma("sp", "c0", arena[0:8, 0:1024], d_sel, w=[b_rows])
    S.op("pool", lambda e: e.tensor_copy(out=selb[:], in_=arena[0:8, 0:1024]), r=[b_rows], w=[b_cb])
    pcs = [sb("pcs%d" % i, [8, (TILE + 1)], BF16) for i in range(3)]
    b_pcs = Buf()
    ptmp = sb("ptmp", [8, (TILE + 1)]); b_ptmp = Buf()
    gb15 = sb("gb15", [8, 4]); b_gb15 = Buf()
    UBx = sb("UBx", [128, (TILE + 1)]); b_UB = Buf()
    mcols = sb("mcols", [128, NCK * 3 * 8]); b_mcols = Buf()
    qTb = sb("qTb", [128, R], BF16); b_qT = Buf()
    kTb = sb("kTb", [128, R], BF16); b_kT = Buf()
    ktok = sb("ktok", [128, NCK, 128], BF16); b_ktok = Buf()
    vtok = sb("vtok", [128, NCK, 257], BF16); b_vtok = Buf()
    gtok = sb("gtok", [128, NCK, 256], BF16); b_gtok = Buf()
    S.op("pool", lambda e: e.memset(vtok[:], 1.0), w=[b_vtok])
    Caug = sb("Caug", [128, 257]); Cb = sb("Cb", [128, 257], BF16); b_C = Buf(); b_Cb = Buf()
    Sst = sb("Sst", [128, 128]); Sb = sb("Sb", [128, 128], BF16); b_S = Buf(); b_Sb = Buf()
    pre = sb("pre", [128, 3 + R]); b_pre = Buf()
    cvy = sb("cvy", [128, R]); b_cvy = Buf()
    k2f = sb("k2f", [128, TILE]); b_k2f = Buf()
    ctail = sb("ctail", [128, NL * 24 * 3]); b_ctail = Buf()
    S.op("pool", lambda e: e.memset(ctail[:], 0.0), w=[b_ctail])
    mnb = sb("mnb", [128, 1024]); gnb = mnb; b_nb = Buf()
    gX = [sb("gX%d" % c, [128, 128]) for c in range(NCK)]; b_gX = [Buf() for _ in range(NCK)]
    gE = [sb("gE%d" % c, [128, 128]) for c in range(NCK)]; b_gE = [Buf() for _ in range(NCK)]
    gAT = [sb("gAT%d" % c, [128, 128]) for c in range(NCK)]; b_gAT = [Buf() for _ in range(NCK)]
    gQK = [sb("gQK%d" % c, [128, 128], BF16) for c in range(NCK)]; b_gQK = [Buf() for _ in range(NCK)]
    gBk = [[sb("gBk%d_%d" % (c, i), [128, 128], BF16) for i in range(6)] for c in range(NCK)]; b_gBk = [[Buf() for _ in range(6)] for _ in range(NCK)]
    gP = [[sb("gP%d_%d" % (c, i), [128, 128], BF16) for i in range(2)] for c in range(NCK)]; b_gP = [[Buf(), Buf()] for _ in range(NCK)]
    gPT = [[sb("gPT%d_%d" % (c, i), [128, 128], BF16) for i in range(2)] for c in range(NCK)]; b_gPT = [[Buf(), Buf()] for _ in range(NCK)]
    gW = [sb("gW%d" % c, [128, 128], BF16) for c in range(NCK)]; b_gW = [Buf() for _ in range(NCK)]
    cX = gX[0]; cE = gE[0]; b_cX = b_gX[0]; b_cE = b_gE[0]
    cW = sb("cW", [128, 128], BF16); b_cW = Buf()
    cnumA = sb("cnumA", [128, 257]); cnum = sb("cnum", [128, 257]); b_numA = Buf(); b_num = Buf()
    csm = sb("csm", [128, 16]); b_csm = Buf()
    chm = sb("chm", [128, 256]); b_chm = Buf()
    cjunk = sb("cjunk", [128, 256]); b_junk = Buf()
    ckw = sb("ckw", [128, 128], BF16); b_kw = Buf()
    crhs = sb("crhs", [128, 128], BF16); b_rhs = Buf()
    cwu = sb("cwu", [128, 128], BF16); b_wu = Buf()
    co1 = sb("co1", [128, 128]); b_o1 = Buf()
    d_scrC = dscr("scrC", [NL, 4, 128, 257])
    d_scrS = dscr("scrS", [NL, 8, 128, 128])
    b_scr = Buf()
    mcar = sb("mcar", [8, NL]); b_mcar = Buf()
    S.op("pool", lambda e: e.memset(mcar[:], 0.0), w=[b_mcar])
    s_k = sb("s_k", [NSMP, 128]); s_v = sb("s_v", [NSMP, 257]); s_g = sb("s_g", [NSMP, 256]); b_sk = Buf(); b_sv = Buf(); b_sg = Buf()
    S.op("pool", lambda e: e.memset(s_v[:], 1.0), w=[b_sv])
    s_q = sb("s_q", [128, NSMP]); b_sq = Buf()
    s_kf = sb("s_kf", [128, NSMP]); b_skf = Buf()

    def gated_norm_to_mix(src_ap, ncol, nrm_row, gate_ap, bgate, chunk_ids, tcol0, ntok, brsrc):
        S.op("act", lambda e: e.activation(out=cjunk[0:ntok, 0:ncol], in_=src_ap, func=AF.Square, accum_out=csm[0:ntok, 4:5]), r=brsrc, w=[b_junk, b_csm])
        S.op("act", lambda e: e.activation(out=csm[0:ntok, 5:6], in_=csm[0:ntok, 4:5], func=AF.Ln, bias=epsc[0:ntok, :], scale=1.0 / ncol), r=[b_csm, b_par], w=[b_csm])
        S.op("act", lambda e: e.activation(out=csm[0:ntok, 5:6], in_=csm[0:ntok, 5:6], func=AF.Exp, scale=-0.5), r=[b_csm], w=[b_csm])
        S.op("dve", lambda e: e.scalar_tensor_tensor(out=chm[0:ntok, 0:ncol], in0=src_ap, scalar=csm[0:ntok, 5:6], in1=nrm_row, op0=ALU.mult, op1=ALU.mult),
             r=brsrc + [b_csm, b_nb], w=[b_chm])
        S.op("dve", lambda e: e.tensor_tensor(out=chm[0:ntok, 0:ncol], in0=chm[0:ntok, 0:ncol], in1=gate_ap, op=ALU.mult), r=[b_chm, bgate], w=[b_chm])
        for i, ch in enumerate(chunk_ids):
            ps, bps = psum()
            S.op("pe", lambda e, i=i: e.transpose(out=ps[:, 0:ntok], in_=chm[0:ntok, i * 128:(i + 1) * 128], identity=cst[0:ntok, C_ID:C_ID + ntok]),
                 r=[b_chm] + CB, w=[bps])
            S.op("act", lambda e, ch=ch: e.copy(out=mixT[:, ch, tcol0:tcol0 + ntok], in_=ps[:, 0:ntok]), r=[bps], w=[b_mix])

    def mlstm(l, ti):
        smp = (ti == 0 and with_sample)
        ncol = R if smp else TILE
        S.op("dve", lambda e: e.tensor_scalar(out=gb15[0:4, 0:2], in0=gateb[0:4, l * 4:l * 4 + 2], scalar1=1.0 / 15.0, scalar2=None, op0=ALU.mult), r=[b_par], w=[b_gb15])

        def ev_g(ps, bps, m, c0, n, gi, si):
            dst = rg if si == 0 else rf
            S.op("act", lambda e: e.activation(out=dst[0:4, c0:c0 + n], in_=ps[0:4, 0:n], func=AF.Tanh, bias=gb15[0:4, si:si + 1], scale=1.0 / 15.0),
                 r=[bps, b_gb15], w=[b_rows])
        proj(nT, b_n, ti, ev_g, subs=[(0, 4), (4, 8)])
        S.op("dve", lambda e: e.tensor_scalar(out=rg[0:4, 0:ncol], in0=rg[0:4, 0:ncol], scalar1=15.0, scalar2=None, op0=ALU.mult), r=[b_rows], w=[b_rows])
        S.op("act", lambda e: e.activation(out=rf[0:4, 0:ncol], in_=rf[0:4, 0:ncol], func=AF.Exp, scale=-15.0), r=[b_rows], w=[b_rows])
        S.op("act", lambda e: e.activation(out=rf[0:4, 0:ncol], in_=rf[0:4, 0:ncol], func=AF.Ln, bias=1.0), r=[b_rows], w=[b_rows])
        S.op("dve", lambda e: e.tensor_scalar(out=rf[0:4, 0:ncol], in0=rf[0:4, 0:ncol], scalar1=-1.0, scalar2=None, op0=ALU.mult), r=[b_rows], w=[b_rows])
        S.op("dve", lambda e: e.tensor_tensor_scan(out=rF[0:4, :], data0=rf[0:4, 0:TILE], data1=rz[0:4, :], initial=0.0, op0=ALU.add, op1=ALU.add), r=[b_rows], w=[b_rows])
        S.op("dve", lambda e: e.tensor_tensor_scan(out=rm[0:4, 0:TILE], data0=rf[0:4, 0:TILE], data1=rg[0:4, 0:TILE], initial=mcar[0:4, l:l + 1], op0=ALU.add, op1=ALU.max),
             r=[b_rows, b_mcar], w=[b_rows])
        S.op("dve", lambda e: e.tensor_scalar(out=rux[0:4, 0:1], in0=mcar[0:4, l:l + 1], scalar1=-1.0, scalar2=None, op0=ALU.mult), r=[b_mcar], w=[b_rows])
        S.op("dve", lambda e: e.tensor_tensor(out=rux[0:4, 1:(TILE + 1)], in0=rF[0:4, :], in1=rm[0:4, 0:TILE], op=ALU.subtract), r=[b_rows], w=[b_rows])
        S.op("dve", lambda e: e.tensor_scalar(out=rnu[0:4, :], in0=rux[0:4, :], scalar1=-1.0, scalar2=None, op0=ALU.mult), r=[b_rows], w=[b_rows])
        S.op("dve", lambda e: e.tensor_tensor(out=rw[0:4, :], in0=rg[0:4, 0:TILE], in1=rF[0:4, :], op=ALU.subtract), r=[b_rows], w=[b_rows])
        S.op("act", lambda e: e.activation(out=rem[0:4, :], in_=rm[0:4, 0:TILE], func=AF.Exp, scale=-1.0), r=[b_rows], w=[b_rows])
        for c in range(NCK):
            S.op("act", lambda e, c=c: e.activation(out=ra[0:4, c * 128:(c + 1) * 128], in_=rux[0:4, 1 + c * 128:1 + (c + 1) * 128], func=AF.Exp,
                                                    bias=rnu[0:4, c * 128:c * 128 + 1], scale=1.0), r=[b_rows], w=[b_rows])
        S.op("dve", lambda e: e.tensor_copy(out=mcar[0:4, l:l + 1], in_=rm[0:4, (TILE - 1):TILE]), r=[b_rows], w=[b_mcar])
        if ti == NT - 1:
            S.dma("sp", "o_pm", o_pm[l].rearrange("(h o) -> h o", o=1), rm[0:4, (TILE - 1):TILE], r=[b_rows])
        rows_to_cols([rw, ra, rem], [b_rows], 4, mcols, b_mcols)
        split3(rux, b_rows, 4, (TILE + 1), pcs, b_pcs, ptmp, b_ptmp)
        S.dma("sp", "nb", mnb[:], d_mn[l].partition_broadcast(128), w=[b_nb])

        def mcol(c, qi, h):
            o = (c * 3 + qi) * 4 + h
            return mcols[:, o:o + 1]

        for h in range(4):
            bcast_rows(pcs, b_pcs, 4, h, (TILE + 1), UBx, b_UB)
            def ev_q(ps, bps, m, c0, n, gi, si):
                if c0 == TILE:
                    S.op("dve", lambda e: e.tensor_copy(out=s_q[:, :], in_=ps[:, 0:n]), r=[bps], w=[b_sq])
                    S.op("act", lambda e: e.copy(out=qTb[:, c0:c0 + n], in_=s_q[:, :]), r=[b_sq], w=[b_qT])
                else:
                    S.op("act", lambda e: e.copy(out=qTb[:, c0:c0 + n], in_=ps[:, 0:n]), r=[bps], w=[b_qT])
            proj(nT, b_n, ti, ev_q)

            def ev_k(ps, bps, m, c0, n, gi, si):
                if c0 == TILE:
                    S.op("dve", lambda e: e.tensor_scalar(out=s_kf[:, :], in0=ps[:, 0:n], scalar1=128.0 ** -0.5, scalar2=None, op0=ALU.mult), r=[bps], w=[b_skf])
                    S.op("act", lambda e: e.copy(out=kTb[:, c0:c0 + n], in_=s_kf[:, :]), r=[b_skf], w=[b_kT])
                else:
                    S.op("act", lambda e: e.activation(out=kTb[:, c0:c0 + n], in_=ps[:, 0:n], func=AF.Copy, scale=128.0 ** -0.5), r=[bps], w=[b_kT])
            wv, bw = proj(nT, b_n, ti, ev_k)

            def ev_kt(ps3, bps, j0, nj):
                S.op("act", lambda e: e.activation(out=ktok[:, j0:j0 + nj, :], in_=ps3, func=AF.Copy, scale=128.0 ** -0.5), r=[bps], w=[b_ktok])

            def ev_kts(ps, bps):
                S.op("act", lambda e: e.activation(out=s_k[:, :], in_=ps, func=AF.Copy, scale=128.0 ** -0.5), r=[bps], w=[b_sk])
            projT(wv, bw, nT, b_n, ti, ev_kt, ev_kts)
            for j in range(2):
                wv, bw, _ = getw()

                def ev_v(ps3, bps, j0, nj, j=j):
                    S.op("dve", lambda e: e.tensor_copy(out=vtok[:, j0:j0 + nj, j * 128:(j + 1) * 128], in_=ps3), r=[bps], w=[b_vtok])

                def ev_vs(ps, bps, j=j):
                    S.op("dve", lambda e: e.tensor_copy(out=s_v[:, j * 128:(j + 1) * 128], in_=ps), r=[bps], w=[b_sv])
                projT(wv, bw, nT, b_n, ti, ev_v, ev_vs)
            for j in range(2):
                wv, bw, _ = getw()

                def ev_o(ps3, bps, j0, nj, j=j):
                    S.op("act", lambda e: e.activation(out=gtok[:, j0:j0 + nj, j * 128:(j + 1) * 128], in_=ps3, func=AF.Sigmoid), r=[bps], w=[b_gtok])

                def ev_os(ps, bps, j=j):
                    S.op("act", lambda e: e.activation(out=s_g[:, j * 128:(j + 1) * 128], in_=ps, func=AF.Sigmoid), r=[bps], w=[b_sg])
                projT(wv, bw, nT, b_n, ti, ev_o, ev_os)
            if ti == 0:
                S.op("dve", lambda e: e.memset(Caug[:], 0.0), w=[b_C])
            else:
                S.dma("sp", "scrC_r", Caug[:], d_scrC[l, h], r=[b_scr], w=[b_C])
            S.op("act", lambda e: e.copy(out=Cb[:], in_=Caug[:]), r=[b_C], w=[b_Cb])
            for c in range(NCK):
                t0 = c * 128
                ps_s, bps_s = psum()
                S.op("pe", lambda e: e.matmul(ps_s[:, 0:128], lhsT=kTb[:, t0:t0 + 128], rhs=qTb[:, t0:t0 + 128], start=True, stop=True), r=[b_kT, b_qT], w=[bps_s])
                S.op("dve", lambda e: e.scalar_tensor_tensor(out=cX[:], in0=UBx[:, 1 + t0:1 + t0 + 128], scalar=mcol(c, 0, h), in1=negi, op0=ALU.add, op1=ALU.add),
                     r=[b_UB, b_mcols] + CB, w=[b_cX])
                S.op("act", lambda e: e.activation(out=cE[:], in_=cX[:], func=AF.Exp), r=[b_cX], w=[b_cE])
                S.op("dve", lambda e: e.tensor_tensor(out=cW[:], in0=cE[:], in1=ps_s[:, 0:128], op=ALU.mult), r=[b_cE, bps_s], w=[b_cW])
                psA, bpsA = psum()
                S.op("pe", lambda e: e.matmul(psA[:, 0:257], lhsT=qTb[:, t0:t0 + 128], rhs=Cb[:], start=True, stop=True), r=[b_qT, b_Cb], w=[bpsA])
                psB, bpsB = psum()
                S.op("pe", lambda e: e.matmul(psB[:, 0:257], lhsT=cW[:], rhs=vtok[:, c, :], start=True, stop=True), r=[b_cW, b_vtok], w=[bpsB])
                S.op("act", lambda e: e.activation(out=cnumA[:], in_=psA[:, 0:257], func=AF.Copy, scale=mcol(c, 1, h)), r=[bpsA, b_mcols], w=[b_numA])
                S.op("dve", lambda e: e.tensor_tensor(out=cnum[:], in0=cnumA[:], in1=psB[:, 0:257], op=ALU.add), r=[b_numA, bpsB], w=[b_num])
                S.op("act", lambda e: e.activation(out=csm[:, 0:1], in_=cnum[:, 256:257], func=AF.Abs), r=[b_num], w=[b_csm])
                S.op("dve", lambda e: e.tensor_tensor(out=csm[:, 0:1], in0=csm[:, 0:1], in1=mcol(c, 2, h), op=ALU.max), r=[b_csm, b_mcols], w=[b_csm])
                S.op("dve", lambda e: e.reciprocal(out=csm[:, 1:2], in_=csm[:, 0:1]), r=[b_csm], w=[b_csm])
                S.op("dve", lambda e: e.tensor_scalar(out=cnum[:, 0:256], in0=cnum[:, 0:256], scalar1=csm[:, 1:2], scalar2=None, op0=ALU.mult), r=[b_num, b_csm], w=[b_num])
                gated_norm_to_mix(cnum[:, 0:256], 256, mnb[:, h * 256:(h + 1) * 256], gtok[:, c, :], b_gtok, [2 * h, 2 * h + 1], t0, 128, [b_num])
                S.op("act", lambda e: e.activation(out=csm[:, 2:3], in_=mcol(c, 0, h), func=AF.Exp, bias=UBx[:, t0 + 128:t0 + 129], scale=1.0), r=[b_mcols, b_UB], w=[b_csm])
                S.op("dve", lambda e: e.tensor_scalar(out=ckw[:], in0=ktok[:, c, :], scalar1=csm[:, 2:3], scalar2=None, op0=ALU.mult), r=[b_ktok, b_csm], w=[b_kw])
                psC, bpsC = psum()
                S.op("pe", lambda e: e.matmul(psC[:, 0:257], lhsT=ckw[:], rhs=vtok[:, c, :], start=True, stop=True), r=[b_kw, b_vtok], w=[bpsC])
                S.op("dve", lambda e: e.tensor_tensor(out=csm[:, 3:4], in0=UBx[:, t0 + 128:t0 + 129], in1=UBx[:, t0:t0 + 1], op=ALU.subtract), r=[b_UB], w=[b_csm])
                S.op("act", lambda e: e.activation(out=csm[:, 3:4], in_=csm[:, 3:4], func=AF.Exp), r=[b_csm], w=[b_csm])
                S.op("dve", lambda e: e.scalar_tensor_tensor(out=Caug[:], in0=Caug[:], scalar=csm[:, 3:4], in1=psC[:, 0:257], op0=ALU.mult, op1=ALU.add),
                     r=[b_C, b_csm, bpsC], w=[b_C])
                S.op("act", lambda e: e.copy(out=Cb[:], in_=Caug[:]), r=[b_C], w=[b_Cb])
            if ti < NT - 1:
                S.dma("sp", "scrC_w", d_scrC[l, h], Caug[:], r=[b_C], w=[b_scr])
            else:
                S.dma("sp", "o_pC", o_pC[l, h], Caug[:, 0:256], r=[b_C])
                S.dma("sp", "o_pn", o_pn[l, h].rearrange("(k o) -> k o", o=1), Caug[:, 256:257], r=[b_C])
            if smp:
                mlstm_sample(l, h)

    eye3 = cst[:, C_E16:C_E16 + 256].rearrange("p (a b) -> p a b", a=16)
    eyeP = cst[0:NSMP, C_ID:C_ID + NSMP]
    s_rows = sb("s_rows", [8, 8 * NSMP]); b_srows = Buf()
    s_cols = sb("s_cols", [NSMP, 64]); b_scols = Buf()
    s_rep = sb("s_rep", [128, 8 * NSMP]); b_srep = Buf()
    s_pc = [sb("s_pc%d" % i, [8, NSMP], BF16) for i in range(2)]; b_spc = Buf()
    s_pt = sb("s_pt", [8, NSMP]); b_spt = Buf()
    s_mtok = sb("s_mtok", [NSMP, 8]); b_smtok = Buf()
    Qm = sb("Qm", [128, NSMP, NSMP], BF16); b_Qm = Buf()
    Km = sb("Km", [NSMP, NSMP, 128], BF16); b_Km = Buf()
    s_t1 = sb("s_t1", [NSMP, 257]); b_st1 = Buf()
    s_t2 = sb("s_t2", [NSMP, 257]); b_st2 = Buf()
    s_vb = sb("s_vb", [NSMP, 257], BF16); b_svb = Buf()
    s_f1 = sb("s_f1", [128, NSMP]); b_sf1 = Buf()
    s_fb = sb("s_fb", [128, NSMP], BF16); b_sfb = Buf()
    NRB = 2
    rowf = [sb("rowf%d" % i, [128, 257]) for i in range(NRB)]; b_rowf = [Buf() for _ in range(NRB)]
    rowb = [sb("rowb%d" % i, [128, 257], BF16) for i in range(NRB)]; b_rowb = [Buf() for _ in range(NRB)]
    st2 = {"rb": 0}

    def rep_rows(src_rows, np_, qslot):
        S.op("dve", lambda e: e.tensor_copy(out=s_pc[0][0:np_, :], in_=src_rows), r=[b_srows], w=[b_spc])
        S.op("dve", lambda e: e.tensor_tensor(out=s_pt[0:np_, :], in0=src_rows, in1=s_pc[0][0:np_, :], op=ALU.subtract), r=[b_srows, b_spc], w=[b_spt])
        S.op("dve", lambda e: e.tensor_copy(out=s_pc[1][0:np_, :], in_=s_pt[0:np_, :]), r=[b_spt], w=[b_spc])
        ps, bps = psum()
        for h in range(np_):
            for i in range(2):
                S.op("pe", lambda e, h=h, i=i: e.matmul(ps[:, h * NSMP:(h + 1) * NSMP], lhsT=selb[0:np_, h * 128:(h + 1) * 128], rhs=s_pc[i][0:np_, :],
                                                        start=(i == 0), stop=(i == 1)), r=[b_spc] + CB, w=[bps])
        S.op("act", lambda e: e.copy(out=s_rep[:, 0:np_ * NSMP], in_=ps[:, 0:np_ * NSMP]), r=[bps], w=[b_srep])

    def rows_to_tok(np_, nq):
        ps, bps = psum()
        for qi in range(nq):
            S.op("pe", lambda e, qi=qi: e.transpose(out=ps[0:NSMP, qi * np_:(qi + 1) * np_], in_=s_rows[0:np_, qi * NSMP:(qi + 1) * NSMP], identity=cst[0:np_, C_ID:C_ID + np_]),
                 r=[b_srows] + CB, w=[bps])
        S.op("dve", lambda e: e.tensor_copy(out=s_cols[:, 0:nq * np_], in_=ps[0:NSMP, 0:nq * np_]), r=[bps], w=[b_scols])

    def mlstm_sample_prep(l):
        S.dma("sp", "s_m", s_mtok[:, 0:4], d_sm[l], w=[b_smtok])
        ps, bps = psum()
        S.op("pe", lambda e: e.transpose(out=ps[0:4, 0:NSMP], in_=s_mtok[:, 0:4], identity=eyeP), r=[b_smtok] + CB, w=[bps])
        fi = rf[0:4, TILE:TILE + NSMP]
        ii = rg[0:4, TILE:TILE + NSMP]
        sl = lambda q: s_rows[0:4, q * NSMP:(q + 1) * NSMP]
        S.op("dve", lambda e: e.tensor_tensor(out=sl(4), in0=ps[0:4, 0:NSMP], in1=fi, op=ALU.add), r=[bps, b_rows], w=[b_srows])
        S.op("dve", lambda e: e.tensor_tensor(out=sl(0), in0=sl(4), in1=ii, op=ALU.max), r=[b_srows, b_rows], w=[b_srows])
        S.op("dve", lambda e: e.tensor_tensor(out=sl(1), in0=sl(4), in1=sl(0), op=ALU.subtract), r=[b_srows], w=[b_srows])
        S.op("act", lambda e: e.activation(out=sl(1), in_=sl(1), func=AF.Exp), r=[b_srows], w=[b_srows])
        S.op("dve", lambda e: e.tensor_tensor(out=sl(2), in0=ii, in1=sl(0), op=ALU.subtract), r=[b_srows, b_rows], w=[b_srows])
        S.op("act", lambda e: e.activation(out=sl(2), in_=sl(2), func=AF.Exp), r=[b_srows], w=[b_srows])
        S.op("act", lambda e: e.activation(out=sl(3), in_=sl(0), func=AF.Exp, scale=-1.0), r=[b_srows], w=[b_srows])
        rows_to_tok(4, 4)
        rep_rows(sl(1), 4, 0)
        S.op("dve", lambda e: e.tensor_copy(out=s_mtok[:, 4:8], in_=s_cols[:, 0:4]), r=[b_scols], w=[b_smtok])
        S.dma("sp", "o_sm", o_sm[l], s_mtok[:, 4:8], r=[b_smtok])

    def masked_q(src_f32, bsrc):
        S.op("dve", lambda e: e.tensor_tensor(out=Qm[:], in0=src_f32.unsqueeze(1).to_broadcast([128, NSMP, NSMP]), in1=eye3, op=ALU.mult), r=[bsrc] + CB, w=[b_Qm])

    def masked_k(src_tok, bsrc, scale_col=None, bscale=None):
        if scale_col is not None:
            S.op("dve", lambda e: e.tensor_scalar(out=s_t2[:, 0:128], in0=src_tok, scalar1=scale_col, scalar2=None, op0=ALU.mult), r=[bsrc, bscale], w=[b_st2])
            src_tok, bsrc = s_t2[:, 0:128], b_st2
        S.op("dve", lambda e: e.tensor_tensor(out=Km[:], in0=src_tok.unsqueeze(1).to_broadcast([NSMP, NSMP, 128]), in1=eyeP.unsqueeze(2).to_broadcast([NSMP, NSMP, 128]), op=ALU.mult),
             r=[bsrc] + CB, w=[b_Km])

    def dot_rows(a_f, ba, b_f, bb, out_col):
        S.op("dve", lambda e: e.tensor_tensor(out=s_fb[:], in0=a_f, in1=b_f, op=ALU.mult), r=[ba, bb], w=[b_sfb])
        ps, bps = psum()
        S.op("pe", lambda e: e.matmul(ps[0:NSMP, 0:1], lhsT=s_fb[:], rhs=onesb[:, 0:1], start=True, stop=True), r=[b_sfb] + CB, w=[bps])
        S.op("dve", lambda e: e.tensor_copy(out=out_col, in_=ps[0:NSMP, 0:1]), r=[bps], w=[b_scols])

    def mlstm_sample(l, h):
        if h == 0:
            mlstm_sample_prep(l)
        col = lambda q: s_cols[:, q * 4 + h:q * 4 + h + 1]
        masked_q(s_q[:], b_sq)
        masked_k(s_k[:], b_sk, scale_col=col(2), bscale=b_scols)
        S.op("act", lambda e: e.copy(out=s_vb[:], in_=s_v[:]), r=[b_sv], w=[b_svb])
        psA, bpsA = psum(pin=True)
        for r_ in range(NSMP):
            i = st2["rb"] % NRB
            st2["rb"] += 1
            S.dma("sp", "s_row%d" % i, rowf[i][:, 0:256], d_sC[l, r_, h], w=[b_rowf[i]])
            S.dma("sp", "s_row%d" % i, rowf[i][:, 256:257], d_sn[l, r_, h * 128:(h + 1) * 128].rearrange("(k o) -> k o", o=1), w=[b_rowf[i]])
            S.op("act", lambda e, i=i: e.copy(out=rowb[i][:], in_=rowf[i][:]), r=[b_rowf[i]], w=[b_rowb[i]])
            S.op("pe", lambda e, i=i, r_=r_: e.matmul(psA[0:NSMP, 0:257], lhsT=Qm[:, r_, :], rhs=rowb[i][:], start=(r_ == 0), stop=(r_ == NSMP - 1)), r=[b_Qm, b_rowb[i]], w=[bpsA])
            psC, bpsC = psum()
            S.op("pe", lambda e, r_=r_: e.matmul(psC[:, 0:257], lhsT=Km[:, r_, :], rhs=s_vb[:], start=True, stop=True), r=[b_Km, b_svb], w=[bpsC])
            S.op("dve", lambda e, i=i, r_=r_: e.scalar_tensor_tensor(out=rowf[i][:], in0=rowf[i][:], scalar=s_rep[:, h * NSMP + r_:h * NSMP + r_ + 1], in1=psC[:, 0:257],
                                                                     op0=ALU.mult, op1=ALU.add), r=[b_rowf[i], b_srep, bpsC], w=[b_rowf[i]])
            S.dma("sp", "o_sC%d" % i, o_sC[l, r_, h], rowf[i][:, 0:256], r=[b_rowf[i]])
            S.dma("sp", "o_sn%d" % i, o_sn[l, r_, h * 128:(h + 1) * 128].rearrange("(k o) -> k o", o=1), rowf[i][:, 256:257], r=[b_rowf[i]])
        dot_rows(s_q[:], b_sq, s_kf[:], b_skf, s_cols[:, 60:61])
        S.op("act", lambda e: e.activation(out=s_t1[:], in_=psA[0:NSMP, 0:257], func=AF.Copy, scale=col(1)), r=[bpsA, b_scols], w=[b_st1])
        unpin(bpsA)
        S.op("dve", lambda e: e.tensor_tensor(out=s_cols[:, 61:62], in0=s_cols[:, 60:61], in1=col(2), op=ALU.mult), r=[b_scols], w=[b_scols])
        S.op("dve", lambda e: e.scalar_tensor_tensor(out=s_t1[:], in0=s_v[:], scalar=s_cols[:, 61:62], in1=s_t1[:], op0=ALU.mult, op1=ALU.add), r=[b_sv, b_scols, b_st1], w=[b_st1])
        S.op("act", lambda e: e.activation(out=s_cols[:, 62:63], in_=s_t1[:, 256:257], func=AF.Abs), r=[b_st1], w=[b_scols])
        S.op("dve", lambda e: e.tensor_tensor(out=s_cols[:, 62:63], in0=s_cols[:, 62:63], in1=col(3), op=ALU.max), r=[b_scols], w=[b_scols])
        S.op("dve", lambda e: e.reciprocal(out=s_cols[:, 62:63], in_=s_cols[:, 62:63]), r=[b_scols], w=[b_scols])
        S.op("dve", lambda e: e.tensor_scalar(out=s_t1[:, 0:256], in0=s_t1[:, 0:256], scalar1=s_cols[:, 62:63], scalar2=None, op0=ALU.mult), r=[b_st1, b_scols], w=[b_st1])
        gated_norm_to_mix(s_t1[:, 0:256], 256, mnb[0:NSMP, h * 256:(h + 1) * 256], s_g[:, :], b_sg, [2 * h, 2 * h + 1], TILE, NSMP, [b_st1])

    s_cst = sb("s_cst", [NSMP, 3, 128]); b_scst = Buf()
    s_cf = sb("s_cf", [128, 3, NSMP]); b_scf = Buf()
    s_y = sb("s_y", [128, 3, NSMP]); b_sy = Buf()
    s_k2t = sb("s_k2t", [NSMP, 128]); b_sk2t = Buf()
    s_v2t = sb("s_v2t", [NSMP, 128]); b_sv2t = Buf()
    s_nt = sb("s_nt", [NSMP, 128]); b_snt = Buf()

    def gdn_sample_conv(l, h, qi):
        cc = qi * 8 + h
        if h == 0 and qi == 0:
            S.dma("sp", "o_scv", o_sconv[l, :, 0:2, :], d_sconv[l, :, 1:3, :])
        S.dma("sp", "s_cst", s_cst[:], d_sconv[l, :, :, cc * 128:(cc + 1) * 128], w=[b_scst])
        ps, bps = psum()
        for j in range(3):
            S.op("pe", lambda e, j=j: e.transpose(out=ps[:, j * NSMP:(j + 1) * NSMP], in_=s_cst[:, j, :], identity=eyeP), r=[b_scst] + CB, w=[bps])
        S.op("act", lambda e: e.copy(out=s_cf[:], in_=ps[:, 0:3 * NSMP].rearrange("p (a b) -> p a b", a=3)), r=[bps], w=[b_scf])
        wb = (l * 24 + cc) * 4
        S.op("dve", lambda e: e.tensor_scalar(out=s_y[:, qi, :], in0=s_pre[:, qi, :], scalar1=convw[:, wb + 3:wb + 4], scalar2=None, op0=ALU.mult), r=[b_spre, b_par], w=[b_sy])
        for j in range(3):
            S.op("dve", lambda e, j=j: e.scalar_tensor_tensor(out=s_y[:, qi, :], in0=s_cf[:, j, :], scalar=convw[:, wb + j:wb + j + 1], in1=s_y[:, qi, :], op0=ALU.mult, op1=ALU.add),
                 r=[b_scf, b_par, b_sy], w=[b_sy])
        S.op("act", lambda e: e.activation(out=s_y[:, qi, :], in_=s_y[:, qi, :], func=AF.Silu), r=[b_sy], w=[b_sy])
        ps2, bps2 = psum()
        S.op("pe", lambda e: e.transpose(out=ps2[0:NSMP, 0:128], in_=s_pre[:, qi, :], identity=identf), r=[b_spre] + CB, w=[bps2])
        S.op("dve", lambda e: e.tensor_copy(out=s_nt[:], in_=ps2[0:NSMP, 0:128]), r=[bps2], w=[b_snt])
        S.dma("sp", "o_scv2", o_sconv[l, :, 2, cc * 128:(cc + 1) * 128], s_nt[:], r=[b_snt])
        if qi < 2:
            S.op("act", lambda e: e.activation(out=s_fb[:], in_=s_y[:, qi, :], func=AF.Square), r=[b_sy], w=[b_sfb])
            ps3, bps3 = psum()
            S.op("pe", lambda e: e.matmul(ps3[:, 0:NSMP], lhsT=onesb[:], rhs=s_fb[:], start=True, stop=True), r=[b_sfb] + CB, w=[bps3])
            S.op("act", lambda e: e.activation(out=s_f1[:], in_=ps3[:, 0:NSMP], func=AF.Ln, bias=epsc[:], scale=1.0), r=[bps3, b_par], w=[b_sf1])
            S.op("act", lambda e: e.activation(out=s_f1[:], in_=s_f1[:], func=AF.Exp, scale=-0.5), r=[b_sf1], w=[b_sf1])
            sc = (128.0 ** -0.5) if qi == 0 else 1.0
            S.op("dve", lambda e: e.scalar_tensor_tensor(out=s_y[:, qi, :], in0=s_y[:, qi, :], scalar=sc, in1=s_f1[:], op0=ALU.mult, op1=ALU.mult), r=[b_sy, b_sf1], w=[b_sy])
        if qi >= 1:
            dst, bdst = (s_k2t, b_sk2t) if qi == 1 else (s_v2t, b_sv2t)
            ps4, bps4 = psum()
            S.op("pe", lambda e: e.transpose(out=ps4[0:NSMP, 0:128], in_=s_y[:, qi, :], identity=identf), r=[b_sy] + CB, w=[bps4])
            S.op("dve", lambda e: e.tensor_copy(out=dst[:], in_=ps4[0:NSMP, 0:128]), r=[bps4], w=[bdst])

    def gdn_sample_prep(l):
        sl = lambda q: s_rows[0:8, q * NSMP:(q + 1) * NSMP]
        S.op("act", lambda e: e.activation(out=sl(0), in_=rga[0:8, TILE:TILE + NSMP], func=AF.Exp), r=[b_rows], w=[b_srows])
        S.op("dve", lambda e: e.tensor_copy(out=sl(1), in_=rgb[0:8, TILE:TILE + NSMP]), r=[b_rows], w=[b_srows])
        rows_to_tok(8, 2)
        rep_rows(sl(0), 8, 0)

    def gdn_sample(l, h):
        if h == 0:
            gdn_sample_prep(l)
        col = lambda q: s_cols[:, q * 8 + h:q * 8 + h + 1]
        masked_q(s_y[:, 1, :], b_sy)
        psK, bpsK = psum(pin=True)
        rows_loaded = []
        for r_ in range(NSMP):
            i = st2["rb"] % NRB
            st2["rb"] += 1
            S.dma("sp", "s_row%d" % i, rowf[i][:, 0:128], d_sS[l, r_, h], w=[b_rowf[i]])
            S.op("act", lambda e, i=i: e.copy(out=rowb[i][:, 0:128], in_=rowf[i][:, 0:128]), r=[b_rowf[i]], w=[b_rowb[i]])
            S.op("pe", lambda e, i=i, r_=r_: e.matmul(psK[0:NSMP, 0:128], lhsT=Qm[:, r_, :], rhs=rowb[i][:, 0:128], start=(r_ == 0), stop=(r_ == NSMP - 1)), r=[b_Qm, b_rowb[i]], w=[bpsK])
        S.op("dve", lambda e: e.tensor_scalar(out=s_cols[:, 56:57], in0=col(0), scalar1=-1.0, scalar2=None, op0=ALU.mult), r=[b_scols], w=[b_scols])
        S.op("dve", lambda e: e.scalar_tensor_tensor(out=s_t1[:, 0:128], in0=psK[0:NSMP, 0:128], scalar=s_cols[:, 56:57], in1=s_v2t[:], op0=ALU.mult, op1=ALU.add),
             r=[bpsK, b_scols, b_sv2t], w=[b_st1])
        unpin(bpsK)
        S.op("dve", lambda e: e.tensor_scalar(out=s_t1[:, 0:128], in0=s_t1[:, 0:128], scalar1=col(1), scalar2=None, op0=ALU.mult), r=[b_st1, b_scols], w=[b_st1])
        S.op("act", lambda e: e.copy(out=s_vb[:, 0:128], in_=s_t1[:, 0:128]), r=[b_st1], w=[b_svb])
        masked_q(s_y[:, 0, :], b_sy)
        masked_k(s_k2t[:], b_sk2t)
        psQ, bpsQ = psum(pin=True)
        for r_ in range(NSMP):
            i = st2["rb"] % NRB
            st2["rb"] += 1
            S.dma("sp", "s_row%d" % i, rowf[i][:, 0:128], d_sS[l, r_, h], w=[b_rowf[i]])
            S.op("act", lambda e, i=i: e.copy(out=rowb[i][:, 0:128], in_=rowf[i][:, 0:128]), r=[b_rowf[i]], w=[b_rowb[i]])
            S.op("pe", lambda e, i=i, r_=r_: e.matmul(psQ[0:NSMP, 0:128], lhsT=Qm[:, r_, :], rhs=rowb[i][:, 0:128], start=(r_ == 0), stop=(r_ == NSMP - 1)), r=[b_Qm, b_rowb[i]], w=[bpsQ])
            psD, bpsD = psum()
            S.op("pe", lambda e, r_=r_: e.matmul(psD[:, 0:128], lhsT=Km[:, r_, :], rhs=s_vb[:, 0:128], start=True, stop=True), r=[b_Km, b_svb], w=[bpsD])
            S.op("dve", lambda e, i=i, r_=r_: e.scalar_tensor_tensor(out=rowf[i][:, 0:128], in0=rowf[i][:, 0:128], scalar=s_rep[:, h * NSMP + r_:h * NSMP + r_ + 1], in1=psD[:, 0:128],
                                                                     op0=ALU.mult, op1=ALU.add), r=[b_rowf[i], b_srep, bpsD], w=[b_rowf[i]])
            S.dma("sp", "o_sS%d" % i, o_sS[l, r_, h], rowf[i][:, 0:128], r=[b_rowf[i]])
        dot_rows(s_y[:, 0, :], b_sy, s_y[:, 1, :], b_sy, s_cols[:, 57:58])
        S.op("act", lambda e: e.activation(out=s_t2[:, 0:128], in_=psQ[0:NSMP, 0:128], func=AF.Copy, scale=col(0)), r=[bpsQ, b_scols], w=[b_st2])
        unpin(bpsQ)
        S.op("dve", lambda e: e.scalar_tensor_tensor(out=s_t2[:, 0:128], in0=s_t1[:, 0:128], scalar=s_cols[:, 57:58], in1=s_t2[:, 0:128], op0=ALU.mult, op1=ALU.add),
             r=[b_st1, b_scols, b_st2], w=[b_st2])
        gated_norm_to_mix(s_t2[:, 0:128], 128, gnb[0:NSMP, h * 128:(h + 1) * 128], s_z[:, :], b_sz, [8 + h], TILE, NSMP, [b_st2])

    kvf = sb("kvf", [128, 512]); b_kvf = Buf()
    kvb = sb("kvb", [128, 512], BF16); b_kvb = Buf()
    qtok = sb("qtok", [NSMP, 512], BF16); b_qtok = Buf()
    oh1 = sb("oh1", [NSMP, 128], BF16); b_ohR = Buf()
    sc_all = sb("sc_all", [128, 2, NSMP * 4]); b_sc = Buf()
    sm_p = sb("sm_p", [128, 256]); b_smp = Buf()
    pTs = sb("pTs", [128, 2, 64], BF16); b_pTs = Buf()

    def xattn_sample(l):
        ps, bps = psum()
        for hh in range(4):
            S.op("pe", lambda e, hh=hh: e.transpose(out=ps[0:NSMP, hh * 128:(hh + 1) * 128], in_=qxs[:, hh, :], identity=identf), r=[b_qxs] + CB, w=[bps])
        S.op("act", lambda e: e.copy(out=qtok[:], in_=ps[0:NSMP, 0:512]), r=[bps], w=[b_qtok])
        for r_ in range(NSMP):
            psb, bpsb = psum()
            S.op("dve", lambda e, r_=r_: e.tensor_copy(out=oh1[:], in_=eyeP[:, r_:r_ + 1].to_broadcast([NSMP, 128])), r=CB, w=[b_ohR])
            S.op("pe", lambda e, r_=r_: e.matmul(psb[:, 0:512], lhsT=oh1[:], rhs=qtok[:], start=True, stop=True), r=[b_ohR, b_qtok], w=[bpsb])
            for mt in range(2):
                S.dma("sp", "s_kv", kvf[:], d_ck[l, r_, mt * 128:(mt + 1) * 128, :], w=[b_kvf])
                S.op("dve", lambda e: e.tensor_tensor(out=kvf[:], in0=kvf[:], in1=psb[:, 0:512], op=ALU.mult), r=[b_kvf, bpsb], w=[b_kvf])
                S.op("dve", lambda e, r_=r_, mt=mt: e.tensor_reduce(out=sc_all[:, mt, r_ * 4:(r_ + 1) * 4], in_=kvf[:].rearrange("p (h d) -> p h d", h=4), axis=AX.X, op=ALU.add),
                     r=[b_kvf], w=[b_sc])
        ps2, bps2 = psum()
        for mt in range(2):
            S.op("pe", lambda e, mt=mt: e.transpose(out=ps2[0:64, mt * 128:(mt + 1) * 128], in_=sc_all[:, mt, :], identity=identf), r=[b_sc] + CB, w=[bps2])
        S.op("dve", lambda e: e.reduce_max(out=csm[0:64, 12:13], in_=ps2[0:64, 0:256], axis=AX.X), r=[bps2], w=[b_csm])
        S.op("dve", lambda e: e.tensor_scalar(out=csm[0:64, 12:13], in0=csm[0:64, 12:13], scalar1=-(128.0 ** -0.5), scalar2=None, op0=ALU.mult), r=[b_csm], w=[b_csm])
        S.op("act", lambda e: e.activation(out=sm_p[0:64, :], in_=ps2[0:64, 0:256], func=AF.Exp, bias=csm[0:64, 12:13], scale=128.0 ** -0.5, accum_out=csm[0:64, 13:14]),
             r=[bps2, b_csm], w=[b_smp, b_csm])
        S.op("dve", lambda e: e.reciprocal(out=csm[0:64, 14:15], in_=csm[0:64, 13:14]), r=[b_csm], w=[b_csm])
        S.op("dve", lambda e: e.tensor_scalar(out=sm_p[0:64, :], in0=sm_p[0:64, :], scalar1=csm[0:64, 14:15], scalar2=None, op0=ALU.mult), r=[b_smp, b_csm], w=[b_smp])
        ps3, bps3 = psum()
        for mt in range(2):
            S.op("pe", lambda e, mt=mt: e.transpose(out=ps3[:, mt * 64:(mt + 1) * 64], in_=sm_p[0:64, mt * 128:(mt + 1) * 128], identity=cst[0:64, C_ID:C_ID + 64]), r=[b_smp] + CB, w=[bps3])
        S.op("act", lambda e: e.copy(out=pTs[:], in_=ps3[:, 0:128].rearrange("p (a b) -> p a b", a=2)), r=[bps3], w=[b_pTs])
        pso, bpso = psum(pin=True)
        for r_ in range(NSMP):
            for mt in range(2):
                S.dma("sp", "s_kv", kvf[:], d_cv[l, r_, mt * 128:(mt + 1) * 128, :], w=[b_kvf])
                S.op("act", lambda e: e.copy(out=kvb[:], in_=kvf[:]), r=[b_kvf], w=[b_kvb])
                for hh in range(4):
                    S.op("pe", lambda e, hh=hh, mt=mt, r_=r_: e.matmul(pso[:, mt * 64 + hh * NSMP + r_:mt * 64 + hh * NSMP + r_ + 1], lhsT=kvb[:, hh * 128:(hh + 1) * 128],
                                                                      rhs=pTs[:, mt, r_ * 4 + hh:r_ * 4 + hh + 1], start=True, stop=True), r=[b_kvb, b_pTs], w=[bpso])
        S.op("act", lambda e: e.copy(out=sm_p[:, 0:64], in_=pso[:, 0:64]), r=[bpso], w=[b_smp])
        S.op("dve", lambda e: e.tensor_tensor(out=mixT[:, 0:4, TILE:TILE + NSMP], in0=sm_p[:, 0:64].rearrange("p (a b) -> p a b", a=4),
                                              in1=pso[:, 64:128].rearrange("p (a b) -> p a b", a=4), op=ALU.add), r=[bpso, b_smp], w=[b_mix])
        unpin(bpso)

    rga = rg; rgb = rf; rgc = rux; reg = ra; rngc = rnu
    rt1 = rm
    gcols = sb("gcols", [128, NCK * 3 * 8]); b_gcols = Buf()
    GBx = UBx
    b_GB = b_UB
    q2T = qTb
    b_q2 = b_qT
    k2T = kTb
    b_k2 = b_kT
    nega = sb("nega", [8, 2]); b_nega = Buf()
    ztok = sb("ztok", [128, NCK, 128], BF16); b_ztok = Buf()
    s_z = sb("s_z", [NSMP, 128]); b_sz = Buf()
    s_pre = sb("s_pre", [128, 3, NSMP]); b_spre = Buf()

    def gdn(l, ti):
        smp = (ti == 0 and with_sample)
        ncol = R if smp else TILE

        def ev_g(ps, bps, m, c0, n, gi, si):
            dst = rga if si == 0 else rgb
            if si == 0:
                S.op("dve", lambda e: e.tensor_scalar(out=dst[0:8, c0:c0 + n], in0=ps[0:8, 0:n], scalar1=gateb[0:8, l * 4 + 3:l * 4 + 4], scalar2=None, op0=ALU.add),
                     r=[bps, b_par], w=[b_rows])
            else:
                S.op("act", lambda e: e.activation(out=dst[0:8, c0:c0 + n], in_=ps[0:8, 0:n], func=AF.Sigmoid), r=[bps], w=[b_rows])
        proj(nT, b_n, ti, ev_g, subs=[(0, 8), (8, 16)])
        S.op("act", lambda e: e.activation(out=rt1[0:8, 0:ncol], in_=rga[0:8, 0:ncol], func=AF.Abs), r=[b_rows], w=[b_rows])
        S.op("act", lambda e: e.activation(out=rt1[0:8, 0:ncol], in_=rt1[0:8, 0:ncol], func=AF.Exp, scale=-1.0), r=[b_rows], w=[b_rows])
        S.op("act", lambda e: e.activation(out=rt1[0:8, 0:ncol], in_=rt1[0:8, 0:ncol], func=AF.Ln, bias=1.0), r=[b_rows], w=[b_rows])
        S.op("dve", lambda e: e.scalar_tensor_tensor(out=rt1[0:8, 0:ncol], in0=rga[0:8, 0:ncol], scalar=0.0, in1=rt1[0:8, 0:ncol], op0=ALU.max, op1=ALU.add), r=[b_rows], w=[b_rows])
        S.op("act", lambda e: e.activation(out=nega[0:8, 0:1], in_=gateb[0:8, l * 4 + 2:l * 4 + 3], func=AF.Exp), r=[b_par], w=[b_nega])
        S.op("dve", lambda e: e.tensor_scalar(out=nega[0:8, 0:1], in0=nega[0:8, 0:1], scalar1=-1.0, scalar2=None, op0=ALU.mult), r=[b_nega], w=[b_nega])
        S.op("dve", lambda e: e.tensor_scalar(out=rga[0:8, 0:ncol], in0=rt1[0:8, 0:ncol], scalar1=nega[0:8, 0:1], scalar2=None, op0=ALU.mult), r=[b_rows, b_nega], w=[b_rows])
        S.op("dve", lambda e: e.memset(rgc[0:8, 0:1], 0.0), w=[b_rows])
        S.op("dve", lambda e: e.tensor_tensor_scan(out=rgc[0:8, 1:(TILE + 1)], data0=rga[0:8, 0:TILE], data1=rz[0:8, :], initial=0.0, op0=ALU.add, op1=ALU.add), r=[b_rows], w=[b_rows])
        S.op("dve", lambda e: e.tensor_scalar(out=rngc[0:8, :], in0=rgc[0:8, :], scalar1=-1.0, scalar2=None, op0=ALU.mult), r=[b_rows], w=[b_rows])
        for c in range(NCK):
            S.op("act", lambda e, c=c: e.activation(out=reg[0:8, c * 128:(c + 1) * 128], in_=rgc[0:8, 1 + c * 128:1 + (c + 1) * 128], func=AF.Exp,
                                                    bias=rngc[0:8, c * 128:c * 128 + 1], scale=1.0), r=[b_rows], w=[b_rows])
        rows_to_cols([rgc[:, 1:(TILE + 1)], reg, rgb], [b_rows], 8, gcols, b_gcols)
        split3(rgc, b_rows, 8, (TILE + 1), pcs, b_pcs, ptmp, b_ptmp)
        S.dma("sp", "nb", gnb[:], d_gn[l].partition_broadcast(128), w=[b_nb])

        def gcol(c, qi, h):
            o = (c * 3 + qi) * 8 + h
            return gcols[:, o:o + 1]

        for h in range(8):
            bcast_rows(pcs, b_pcs, 8, h, (TILE + 1), GBx, b_GB)
            for qi in range(3):
                cc = qi * 8 + h
                tb = (l * 24 + cc) * 3
                S.op("pool", lambda e: e.tensor_copy(out=pre[:, 0:3], in_=ctail[:, tb:tb + 3]), r=[b_ctail], w=[b_pre])

                def ev_p(ps, bps, m, c0, n, gi, si):
                    if c0 == TILE:
                        S.op("dve", lambda e: e.tensor_copy(out=s_pre[:, qi, :], in_=ps[:, 0:n]), r=[bps], w=[b_spre])
                    else:
                        S.op("act", lambda e: e.copy(out=pre[:, 3 + c0:3 + c0 + n], in_=ps[:, 0:n]), r=[bps], w=[b_pre])
                proj(nT, b_n, ti, ev_p)
                S.op("pool", lambda e: e.tensor_copy(out=ctail[:, tb:tb + 3], in_=pre[:, TILE:(TILE + 3)]), r=[b_pre], w=[b_ctail])
                if ti == NT - 1:
                    S.dma("sp", "o_pcv", o_pconvT[l, cc * 128:(cc + 1) * 128, :], pre[:, TILE:(TILE + 3)], r=[b_pre])
                wb = (l * 24 + cc) * 4
                S.op("dve", lambda e: e.tensor_scalar(out=cvy[:, 0:TILE], in0=pre[:, 0:TILE], scalar1=convw[:, wb:wb + 1], scalar2=None, op0=ALU.mult), r=[b_pre, b_par], w=[b_cvy])
                for j in range(1, 4):
                    S.op("dve", lambda e, j=j: e.scalar_tensor_tensor(out=cvy[:, 0:TILE], in0=pre[:, j:j + TILE], scalar=convw[:, wb + j:wb + j + 1], in1=cvy[:, 0:TILE],
                                                                      op0=ALU.mult, op1=ALU.add), r=[b_pre, b_par, b_cvy], w=[b_cvy])
                S.op("act", lambda e: e.activation(out=cvy[:, 0:TILE], in_=cvy[:, 0:TILE], func=AF.Silu), r=[b_cvy], w=[b_cvy])
                if qi < 2:
                    dstb, bdst = (q2T, b_q2) if qi == 0 else (k2T, b_k2)
                    sc = (128.0 ** -0.5) if qi == 0 else 1.0
                    for c0 in range(0, TILE, 512):
                        S.op("act", lambda e: e.activation(out=dstb[:, c0:c0 + 512], in_=cvy[:, c0:c0 + 512], func=AF.Square), r=[b_cvy], w=[bdst])
                        ps, bps = psum()
                        S.op("pe", lambda e: e.matmul(ps[:, 0:512], lhsT=onesb[:], rhs=dstb[:, c0:c0 + 512], start=True, stop=True), r=[bdst] + CB, w=[bps])
                        S.op("act", lambda e: e.activation(out=sb_rstd[:, 0:512], in_=ps[:, 0:512], func=AF.Ln, bias=epsc[:], scale=1.0), r=[bps, b_par], w=[b_rstd])
                        S.op("act", lambda e: e.activation(out=sb_rstd[:, 0:512], in_=sb_rstd[:, 0:512], func=AF.Exp, scale=-0.5), r=[b_rstd], w=[b_rstd])
                        if qi == 0:
                            S.op("dve", lambda e: e.scalar_tensor_tensor(out=dstb[:, c0:c0 + 512], in0=cvy[:, c0:c0 + 512], scalar=sc, in1=sb_rstd[:, 0:512], op0=ALU.mult, op1=ALU.mult),
                                 r=[b_cvy, b_rstd], w=[bdst])
                        else:
                            S.op("dve", lambda e: e.tensor_tensor(out=k2f[:, c0:c0 + 512], in0=cvy[:, c0:c0 + 512], in1=sb_rstd[:, 0:512], op=ALU.mult), r=[b_cvy, b_rstd], w=[b_k2f])
                            S.op("act", lambda e: e.copy(out=dstb[:, c0:c0 + 512], in_=k2f[:, c0:c0 + 512]), r=[b_k2f], w=[bdst])
                    if qi == 1:
                        for b0 in range(0, NCK, 4):
                            ps, bps = psum()
                            for jj in range(4):
                                S.op("pe", lambda e, jj=jj: e.transpose(out=ps[:, jj * 128:(jj + 1) * 128], in_=k2f[:, (b0 + jj) * 128:(b0 + jj + 1) * 128], identity=identf),
                                     r=[b_k2f] + CB, w=[bps])
                            S.op("act", lambda e: e.copy(out=ktok[:, b0:b0 + 4, :], in_=ps[:, 0:512].rearrange("p (j c) -> p j c", j=4)), r=[bps], w=[b_ktok])
                else:
                    for b0 in range(0, NCK, 4):
                        ps, bps = psum()
                        for jj in range(4):
                            S.op("pe", lambda e, jj=jj: e.transpose(out=ps[:, jj * 128:(jj + 1) * 128], in_=cvy[:, (b0 + jj) * 128:(b0 + jj + 1) * 128], identity=identf),
                                 r=[b_cvy] + CB, w=[bps])
                        S.op("act", lambda e: e.copy(out=vtok[:, b0:b0 + 4, 0:128], in_=ps[:, 0:512].rearrange("p (j c) -> p j c", j=4)), r=[bps], w=[b_vtok])
                if smp:
                    gdn_sample_conv(l, h, qi)
            wv, bw, _ = getw()

            def ev_z(ps3, bps, j0, nj):
                S.op("act", lambda e: e.activation(out=ztok[:, j0:j0 + nj, :], in_=ps3, func=AF.Silu), r=[bps], w=[b_ztok])

            def ev_zs(ps, bps):
                S.op("act", lambda e: e.activation(out=s_z[:, :], in_=ps, func=AF.Silu), r=[bps], w=[b_sz])
            projT(wv, bw, nT, b_n, ti, ev_z, ev_zs)
            if ti == 0:
                S.op("dve", lambda e: e.memset(Sst[:], 0.0), w=[b_S])
            else:
                S.dma("sp", "scrS_r", Sst[:], d_scrS[l, h], r=[b_scr], w=[b_S])
            S.op("act", lambda e: e.copy(out=Sb[:], in_=Sst[:]), r=[b_S], w=[b_Sb])
            def phaseA(c):
                t0 = c * 128
                cX_, bX_, cE_, bE_, cAT, b_AT, cQK, b_QK = gX[c], b_gX[c], gE[c], b_gE[c], gAT[c], b_gAT[c], gQK[c], b_gQK[c]
                cBk, b_Bk, cP, b_P, cPT, b_PT, cWw, b_Ww = gBk[c], b_gBk[c], gP[c], b_gP[c], gPT[c], b_gPT[c], gW[c], b_gW[c]
                psa, bpsa = psum(pin=True)
                S.op("pe", lambda e: e.matmul(psa[:, 0:128], lhsT=k2T[:, t0:t0 + 128], rhs=k2T[:, t0:t0 + 128], start=True, stop=True), r=[b_k2], w=[bpsa])
                psq, bpsq = psum(pin=True)
                S.op("pe", lambda e: e.matmul(psq[:, 0:128], lhsT=k2T[:, t0:t0 + 128], rhs=q2T[:, t0:t0 + 128], start=True, stop=True), r=[b_k2, b_q2], w=[bpsq])
                S.op("dve", lambda e: e.scalar_tensor_tensor(out=cX_[:], in0=GBx[:, 1 + t0:1 + t0 + 128], scalar=gcol(c, 0, h), in1=negi, op0=ALU.subtract, op1=ALU.add),
                     r=[b_GB, b_gcols] + CB, w=[bX_])
                yield
                S.op("act", lambda e: e.activation(out=cE_[:], in_=cX_[:], func=AF.Exp), r=[bX_], w=[bE_])
                yield
                S.op("dve", lambda e: e.tensor_tensor(out=cQK[:], in0=cE_[:], in1=psq[:, 0:128], op=ALU.mult), r=[bE_, bpsq], w=[b_QK])
                S.op("dve", lambda e: e.tensor_tensor(out=cAT[:], in0=cE_[:], in1=psa[:, 0:128], op=ALU.mult), r=[bE_, bpsa], w=[b_AT])
                unpin(bpsa)
                unpin(bpsq)
                yield
                S.op("dve", lambda e: e.scalar_tensor_tensor(out=cAT[:], in0=cAT[:], scalar=gcol(c, 2, h), in1=mstrict, op0=ALU.mult, op1=ALU.mult), r=[b_AT, b_gcols] + CB, w=[b_AT])
                yield
                S.op("dve", lambda e: e.tensor_tensor(out=cX_[:], in0=cAT[:], in1=cst[:, C_MK:C_MK + 128], op=ALU.mult), r=[b_AT] + CB, w=[bX_])
                for lv in range(6):
                    S.op("dve", lambda e, lv=lv: e.tensor_tensor(out=cBk[lv][:], in0=cAT[:], in1=cst[:, C_MK + (lv + 1) * 128:C_MK + (lv + 2) * 128], op=ALU.mult),
                         r=[b_AT] + CB, w=[b_Bk[lv]])
                yield
                S.op("dve", lambda e: e.tensor_tensor(out=cX_[:], in0=identf, in1=cX_[:], op=ALU.subtract), r=[bX_] + CB, w=[bX_])
                yield
                S.op("act", lambda e: e.copy(out=cPT[0][:], in_=cX_[:]), r=[bX_], w=[b_PT[0]])
                pst, bpst = psum(pin=True)
                S.op("pe", lambda e: e.transpose(out=pst[:, 0:128], in_=cX_[:], identity=identf), r=[bX_] + CB, w=[bpst])
                yield
                S.op("act", lambda e: e.copy(out=cP[0][:], in_=pst[:, 0:128]), r=[bpst], w=[b_P[0]])
                unpin(bpst)
                yield
                pi = 0
                for lv in range(6):
                    po = 1 - pi
                    psw, bpsw = psum(pin=True)
                    S.op("pe", lambda e, lv=lv, pi=pi: e.matmul(psw[:, 0:128], lhsT=cBk[lv][:], rhs=cP[pi][:], start=True, stop=True), r=[b_Bk[lv], b_P[pi]], w=[bpsw])
                    yield
                    S.op("act", lambda e: e.copy(out=cWw[:], in_=psw[:, 0:128]), r=[bpsw], w=[b_Ww])
                    unpin(bpsw)
                    yield
                    ps1, bps1 = psum(pin=True)
                    S.op("pe", lambda e, pi=pi: e.matmul(ps1[:, 0:128], lhsT=cWw[:], rhs=cPT[pi][:], start=True, stop=True), r=[b_Ww, b_PT[pi]], w=[bps1])
                    if lv < 5:
                        ps2, bps2 = psum(pin=True)
                        S.op("pe", lambda e, pi=pi: e.matmul(ps2[:, 0:128], lhsT=cPT[pi][:], rhs=cWw[:], start=True, stop=True), r=[b_Ww, b_PT[pi]], w=[bps2])
                    yield
                    S.op("dve", lambda e, pi=pi, po=po: e.tensor_tensor(out=cPT[po][:], in0=cPT[pi][:], in1=ps1[:, 0:128], op=ALU.subtract), r=[b_PT[pi], bps1], w=[b_PT[po]])
                    unpin(bps1)
                    if lv < 5:
                        S.op("dve", lambda e, pi=pi, po=po: e.tensor_tensor(out=cP[po][:], in0=cP[pi][:], in1=ps2[:, 0:128], op=ALU.subtract), r=[b_P[pi], bps2], w=[b_P[po]])
                        unpin(bps2)
                    yield
                    pi = po
                TTres[c] = (cPT[pi], b_PT[pi])

            TTres = {}
            gens = [phaseA(c) for c in range(NCK)]
            while gens:
                for g_ in list(gens):
                    try:
                        next(g_)
                    except StopIteration:
                        gens.remove(g_)
            for c in range(NCK):
                t0 = c * 128
                TTf, bTTf = TTres[c]
                cQK, b_QK = gQK[c], b_gQK[c]
                psk, bpsk = psum()
                S.op("pe", lambda e: e.matmul(psk[:, 0:128], lhsT=k2T[:, t0:t0 + 128], rhs=Sb[:], start=True, stop=True), r=[b_k2, b_Sb], w=[bpsk])
                psqs, bpsqs = psum()
                S.op("pe", lambda e: e.matmul(psqs[:, 0:128], lhsT=q2T[:, t0:t0 + 128], rhs=Sb[:], start=True, stop=True), r=[b_q2, b_Sb], w=[bpsqs])
                S.op("dve", lambda e: e.tensor_scalar(out=csm[:, 6:7], in0=gcol(c, 1, h), scalar1=-1.0, scalar2=None, op0=ALU.mult), r=[b_gcols], w=[b_csm])
                S.op("dve", lambda e: e.scalar_tensor_tensor(out=crhs[:], in0=psk[:, 0:128], scalar=csm[:, 6:7], in1=vtok[:, c, 0:128], op0=ALU.mult, op1=ALU.add),
                     r=[bpsk, b_csm, b_vtok], w=[b_rhs])
                psu, bpsu = psum()
                S.op("pe", lambda e: e.matmul(psu[:, 0:128], lhsT=TTf[:], rhs=crhs[:], start=True, stop=True), r=[bTTf, b_rhs], w=[bpsu])
                S.op("act", lambda e: e.activation(out=cwu[:], in_=psu[:, 0:128], func=AF.Copy, scale=gcol(c, 2, h)), r=[bpsu, b_gcols], w=[b_wu])
                S.op("act", lambda e: e.activation(out=co1[:], in_=psqs[:, 0:128], func=AF.Copy, scale=gcol(c, 1, h)), r=[bpsqs, b_gcols], w=[b_o1])
                pso, bpso = psum()
                S.op("pe", lambda e: e.matmul(pso[:, 0:128], lhsT=cQK[:], rhs=cwu[:], start=True, stop=True), r=[b_QK, b_wu], w=[bpso])
                S.op("dve", lambda e: e.tensor_tensor(out=cnum[:, 0:128], in0=co1[:], in1=pso[:, 0:128], op=ALU.add), r=[b_o1, bpso], w=[b_num])
                gated_norm_to_mix(cnum[:, 0:128], 128, gnb[:, h * 128:(h + 1) * 128], ztok[:, c, :], b_ztok, [8 + h], t0, 128, [b_num])
                S.op("act", lambda e: e.activation(out=csm[:, 7:8], in_=gcol(c, 0, h), func=AF.Exp, bias=GBx[:, t0 + 128:t0 + 129], scale=-1.0), r=[b_gcols, b_GB], w=[b_csm])
                S.op("dve", lambda e: e.tensor_scalar(out=ckw[:], in0=ktok[:, c, :], scalar1=csm[:, 7:8], scalar2=None, op0=ALU.mult), r=[b_ktok, b_csm], w=[b_kw])
                psd, bpsd = psum()
                S.op("pe", lambda e: e.matmul(psd[:, 0:128], lhsT=ckw[:], rhs=cwu[:], start=True, stop=True), r=[b_kw, b_wu], w=[bpsd])
                S.op("dve", lambda e: e.tensor_tensor(out=csm[:, 8:9], in0=GBx[:, t0 + 128:t0 + 129], in1=GBx[:, t0:t0 + 1], op=ALU.subtract), r=[b_GB], w=[b_csm])
                S.op("act", lambda e: e.activation(out=csm[:, 8:9], in_=csm[:, 8:9], func=AF.Exp), r=[b_csm], w=[b_csm])
                S.op("dve", lambda e: e.scalar_tensor_tensor(out=Sst[:], in0=Sst[:], scalar=csm[:, 8:9], in1=psd[:, 0:128], op0=ALU.mult, op1=ALU.add), r=[b_S, b_csm, bpsd], w=[b_S])
                S.op("act", lambda e: e.copy(out=Sb[:], in_=Sst[:]), r=[b_S], w=[b_Sb])
            if ti < NT - 1:
                S.dma("sp", "scrS_w", d_scrS[l, h], Sst[:], r=[b_S], w=[b_scr])
            else:
                S.dma("sp", "o_pS", o_pS[l, h], Sst[:], r=[b_S])
            if smp:
                gdn_sample(l, h)
    memh = arena[:, 0:2048].rearrange("p (c m) -> p c m", c=8)
    b_memh = b_rows
    d_memv = d_memT.rearrange("(c p) m -> p c m", p=128)

    def mem_norm(l):
        gcol0 = (l * 4 + 3) * 16
        ps, bps = psum()
        for hf in range(2):
            S.dma("sp", "mem", memh, d_memv[:, hf * 8:(hf + 1) * 8, :], w=[b_memh])
            S.op("act", lambda e, hf=hf: e.activation(out=mhT[:, hf * 8:(hf + 1) * 8, :], in_=memh, func=AF.Square), r=[b_memh], w=[b_mh])
            for k in range(8):
                kk = hf * 8 + k
                S.op("pe", lambda e, kk=kk: e.matmul(ps[:, 0:256], lhsT=onesb[:], rhs=mhT[:, kk, :], start=(kk == 0), stop=(kk == 15)), r=[b_mh] + CB, w=[bps])
        S.op("act", lambda e: e.activation(out=sb_rstd[:, 0:256], in_=ps[:, 0:256], func=AF.Ln, bias=epsc[:], scale=1.0 / D), r=[bps, b_par], w=[b_rstd])
        S.op("act", lambda e: e.activation(out=sb_rstd[:, 0:256], in_=sb_rstd[:, 0:256], func=AF.Exp, scale=-0.5), r=[b_rstd], w=[b_rstd])
        for hf in range(2):
            S.dma("sp", "mem", memh, d_memv[:, hf * 8:(hf + 1) * 8, :], w=[b_memh])
            for k in range(8):
                kk = hf * 8 + k
                S.op("dve", lambda e, k=k, kk=kk: e.scalar_tensor_tensor(out=mhT[:, kk, :], in0=memh[:, k, :], scalar=gains[:, gcol0 + kk:gcol0 + kk + 1], in1=sb_rstd[:, 0:256],
                                                                          op0=ALU.mult, op1=ALU.mult), r=[b_memh, b_rstd, b_par], w=[b_mh])
    mhT = mixT[:, :, :].rearrange("p c t -> p (c t)")[:, 0:4096].rearrange("p (c m) -> p c m", c=16)
    b_mh = b_mix
    KT = sb("KT", [128, 4, 256], BF16); b_KT = Buf()
    KTf = sb("KTf", [128, 256]); b_KTf = Buf()
    Vm = sb("Vm", [128, 2, 512], BF16); b_Vm = Buf()
    Vf = sb("Vf", [128, 128]); b_Vf = Buf()
    qx = sb("qx", [128, 4, R], BF16); b_qx = Buf()
    qxs = sb("qxs", [128, 4, NSMP]); b_qxs = Buf()
    pex = sb("pex", [128, 256]); b_pex = Buf()
    pTb = sb("pTb", [128, 2, 128], BF16); b_pT = Buf()

    def xattn(l, ti):
        smp = (ti == 0 and with_sample)
        mem_norm(l)
        if stage == 5.1:
            return
        for j in range(4):
            def ev_k(ps, bps, m, c0, n, gi, si, j=j):
                S.op("dve", lambda e: e.tensor_copy(out=KTf[:], in_=ps[:, 0:256]), r=[bps], w=[b_KTf])
                S.op("act", lambda e: e.copy(out=KT[:, j, :], in_=KTf[:]), r=[b_KTf], w=[b_KT])
                if ti == 0:
                    S.dma("sp", "o_pk", o_pkT[l, j * 128:(j + 1) * 128, :], KTf[:], r=[b_KTf])
            proj(mhT, b_mh, ti, ev_k, grp=[(0, 256)])
        if stage == 5.2:
            return
        for j in range(4):
            wv, bw, _ = getw()

            def ev_v(ps3, bps, j0, nj, j=j):
                for mt in range(2):
                    S.op("dve", lambda e, mt=mt: e.tensor_copy(out=Vf[:, 0:128], in_=ps3[:, mt, :]), r=[bps], w=[b_Vf])
                    S.op("act", lambda e, mt=mt: e.copy(out=Vm[:, mt, j * 128:(j + 1) * 128], in_=Vf[:, 0:128]), r=[b_Vf], w=[b_Vm])
                    if ti == 0:
                        S.dma("sp", "o_pv", o_pv[l, mt * 128:(mt + 1) * 128, j * 128:(j + 1) * 128], Vf[:, 0:128], r=[b_Vf])
            projT(wv, bw, mhT, b_mh, 1, ev_v, None, ntt=2)
        if stage == 5.3:
            return
        rmsnorm_F(ti, (l * 4 + 1) * 16, nT, b_n)
        for j in range(4):
            def ev_q(ps, bps, m, c0, n, gi, si, j=j):
                if c0 == TILE:
                    S.op("dve", lambda e: e.tensor_copy(out=qxs[:, j, :], in_=ps[:, 0:n]), r=[bps], w=[b_qxs])
                    S.op("act", lambda e: e.copy(out=qx[:, j, c0:c0 + n], in_=qxs[:, j, :]), r=[b_qxs], w=[b_qx])
                else:
                    S.op("act", lambda e: e.copy(out=qx[:, j, c0:c0 + n], in_=ps[:, 0:n]), r=[bps], w=[b_qx])
            proj(nT, b_n, ti, ev_q)
        if stage == 5.4:
            return
        for tt in range(NCK):
            t0 = tt * 128
            for hh in range(4):
                ps, bps = psum()
                S.op("pe", lambda e: e.matmul(ps[:, 0:256], lhsT=qx[:, hh, t0:t0 + 128], rhs=KT[:, hh, :], start=True, stop=True), r=[b_qx, b_KT], w=[bps])
                S.op("dve", lambda e: e.reduce_max(out=csm[:, 9:10], in_=ps[:, 0:256], axis=AX.X), r=[bps], w=[b_csm])
                S.op("dve", lambda e: e.tensor_scalar(out=csm[:, 9:10], in0=csm[:, 9:10], scalar1=-(128.0 ** -0.5), scalar2=None, op0=ALU.mult), r=[b_csm], w=[b_csm])
                S.op("act", lambda e: e.activation(out=pex[:], in_=ps[:, 0:256], func=AF.Exp, bias=csm[:, 9:10], scale=128.0 ** -0.5, accum_out=csm[:, 10:11]),
                     r=[bps, b_csm], w=[b_pex, b_csm])
                S.op("dve", lambda e: e.reciprocal(out=csm[:, 11:12], in_=csm[:, 10:11]), r=[b_csm], w=[b_csm])
                S.op("dve", lambda e: e.tensor_scalar(out=pex[:], in0=pex[:], scalar1=csm[:, 11:12], scalar2=None, op0=ALU.mult), r=[b_pex, b_csm], w=[b_pex])
                ps2, bps2 = psum()
                for mt in range(2):
                    S.op("pe", lambda e, mt=mt: e.transpose(out=ps2[:, mt * 128:(mt + 1) * 128], in_=pex[:, mt * 128:(mt + 1) * 128], identity=identf), r=[b_pex] + CB, w=[bps2])
                S.op("act", lambda e: e.copy(out=pTb[:], in_=ps2[:, 0:256].rearrange("p (a b) -> p a b", a=2)), r=[bps2], w=[b_pT])
                ps3, bps3 = psum()
                for mt in range(2):
                    S.op("pe", lambda e, mt=mt: e.matmul(ps3[:, 0:128], lhsT=Vm[:, mt, hh * 128:(hh + 1) * 128], rhs=pTb[:, mt, :], start=(mt == 0), stop=(mt == 1)),
                         r=[b_Vm, b_pT], w=[bps3])
                S.op("act", lambda e: e.copy(out=mixT[:, hh, t0:t0 + 128], in_=ps3[:, 0:128]), r=[bps3], w=[b_mix])
        if stage == 5.5:
            return
        if smp:
            xattn_sample(l)
        for j in range(16):
            proj(mixT, b_mix, ti, resid_evac(j), nk=4)

    hg = sb_rstd
    b_hg = b_rstd

    def ffn(l, ti):
        rmsnorm_F(ti, (l * 4 + 2) * 16, nT, b_n)
        for g in range(4):
            for c in range(11):
                wg, bwg, _ = getw()
                wu_, bwu, _ = getw()
                for (c0, n) in groups(ti):
                    psg, bpsg = psum()
                    psu, bpsu = psum()
                    for k in range(16):
                        S.op("pe", lambda e, k=k: e.matmul(psg[:, 0:n], lhsT=wg[:, k, :], rhs=nT[:, k, c0:c0 + n], start=(k == 0), stop=(k == 15)), r=[bwg, b_n], w=[bpsg])
                    for k in range(16):
                        S.op("pe", lambda e, k=k: e.matmul(psu[:, 0:n], lhsT=wu_[:, k, :], rhs=nT[:, k, c0:c0 + n], start=(k == 0), stop=(k == 15)), r=[bwu, b_n], w=[bpsu])
                    S.op("act", lambda e: e.activation(out=hg[:, 0:n], in_=psg[:, 0:n], func=AF.Silu), r=[bpsg], w=[b_hg])
                    S.op("dve", lambda e: e.tensor_tensor(out=mixT[:, c, c0:c0 + n], in0=hg[:, 0:n], in1=psu[:, 0:n], op=ALU.mult), r=[b_hg, bpsu], w=[b_mix])
            for j in range(16):
                proj(mixT, b_mix, ti, resid_evac(j), nk=11)

    def mixer(l, ti):
        rmsnorm_F(ti, (l * 4 + 0) * 16, nT, b_n)
        mlstm(l, ti)
        gdn(l, ti)
        for j in range(16):
            proj(mixT, b_mix, ti, resid_evac(j))

    for ti in range(NT):
        S.dma("sp", "x", xT[:, :, 0:TILE], d_xT[:, ti * TILE:(ti + 1) * TILE].rearrange("(c p) t -> p c t", p=128), w=[b_x])
        if ti == 0 and with_sample:
            S.dma("sp", "x", xT[:, :, TILE:TILE + NSMP], d_xsT.rearrange("(c p) t -> p c t", p=128), w=[b_x])
        for l in range(NL):
            st["layer"] = l
            st["wi"] = 0
            if stage >= 6:
                mixer(l, ti)
                xattn(l, ti)
                ffn(l, ti)
                assert st["wi"] == len(WBLOCKS), (st["wi"], len(WBLOCKS))
            else:
                rmsnorm_F(ti, (l * 4 + 0) * 16, nT, b_n)
                if stage >= 2:
                    mlstm(l, ti)
                if stage >= 3:
                    gdn(l, ti)
                if stage >= 4:
                    for j in range(16):
                        proj(mixT, b_mix, ti, resid_evac(j))
                if stage >= 5:
                    xattn(l, ti)
        for (c0, n) in groups(ti):
            S.op("act", lambda e: e.activation(out=nT[:, :, c0:c0 + n], in_=xT[:, :, c0:c0 + n], func=AF.Square), r=[b_x], w=[b_n])
            ps, bps = psum()
            for k in range(NCH):
                S.op("pe", lambda e, k=k: e.matmul(ps[:, 0:n], lhsT=onesb[:], rhs=nT[:, k, c0:c0 + n], start=(k == 0), stop=(k == NCH - 1)), r=[b_n] + CB, w=[bps])
            S.op("act", lambda e: e.activation(out=sb_rstd[:, 0:n], in_=ps[:, 0:n], func=AF.Ln, bias=epsc[:], scale=1.0 / D), r=[bps, b_par], w=[b_rstd])
            S.op("act", lambda e: e.activation(out=sb_rstd[:, 0:n], in_=sb_rstd[:, 0:n], func=AF.Exp, scale=-0.5), r=[b_rstd], w=[b_rstd])
            gc = NL * 4 * 16
            for k in range(NCH):
                S.op("dve", lambda e, k=k: e.scalar_tensor_tensor(out=xT[:, k, c0:c0 + n], in0=xT[:, k, c0:c0 + n], scalar=gains[:, gc + k:gc + k + 1],
                                                                  in1=sb_rstd[:, 0:n], op0=ALU.mult, op1=ALU.mult), r=[b_x, b_rstd, b_par], w=[b_x])
        S.dma("sp", "oy", o_yT[:, ti * TILE:(ti + 1) * TILE].rearrange("(c p) t -> p c t", p=128), xT[:, :, 0:TILE], r=[b_x])
        if ti == 0 and with_sample:
            S.dma("sp", "oy", o_ysT.rearrange("(c p) t -> p c t", p=128), xT[:, :, TILE:TILE + NSMP], r=[b_x])
    S.finish()
    print('sbuf bytes remaining', nc.sbuf_bytes_remaining, 'instr counts', dict(S.cnt))
    return nc, es


def _consts():
    c = np.zeros((128, CW), np.float32)
    c[:, C_ID:C_ID + 128] = np.eye(128, dtype=np.float32)
    s = np.arange(128)[:, None]
    t = np.arange(128)[None, :]
    c[:, C_NI:C_NI + 128] = np.where(s <= t, 0.0, NEG)
    c[:, C_MS:C_MS + 128] = (s < t).astype(np.float32)
    c[:, C_ONE:C_ONE + 128] = 1.0
    c[:, C_MK:C_MK + 128] = (s // 2 == t // 2)
    bs = 2
    for lv in range(6):
        c[:, C_MK + (lv + 1) * 128:C_MK + (lv + 2) * 128] = (s // (2 * bs) == t // (2 * bs)) & (s // bs != t // bs)
        bs *= 2
    c[:, C_E16:C_E16 + 256] = np.eye(16, dtype=np.float32).reshape(1, 256)
    return c


def _pack_weights(inp, NL):
    w = np.empty((NL, 128, WTOT), np.float32)
    for i, (mat, k0, nk, c0, ncw) in enumerate(WBLOCKS):
        src = inp[mat]
        for l in range(NL):
            blk = src[l, k0 * 128:(k0 + nk) * 128, c0:c0 + ncw].reshape(nk, 128, ncw)
            w[l, :, WOFF[i]:WOFF[i + 1]] = blk.transpose(1, 0, 2).reshape(128, nk * ncw)
    return w


def _fm(v):
    return np.ascontiguousarray(v.reshape(16, 128).T)


def run(inp, NL=DEPTH, NT=SEQ // TILE, with_sample=True, trace=False, stage=99, ncores=8):
    inp = {k: np.asarray(v) for k, v in inp.items()}
    nc, es = build(NL=NL, NT=NT, with_sample=with_sample, stage=stage)
    wts = _pack_weights(inp, NL)
    cst = _consts()
    gains = np.zeros((128, (NL * 4 + 1) * 16), np.float32)
    for l in range(NL):
        for wi, nm in enumerate(("norm_mix", "norm_xattn", "norm_ffn", "norm_mem")):
            gains[:, (l * 4 + wi) * 16:(l * 4 + wi + 1) * 16] = _fm(inp[nm][l])
    gains[:, NL * 64:NL * 64 + 16] = _fm(inp["norm_final"])
    gateb = np.zeros((8, NL * 4), np.float32)
    for l in range(NL):
        gateb[0:4, l * 4 + 0] = inp["mlstm_b_i"][l]
        gateb[0:4, l * 4 + 1] = inp["mlstm_b_f"][l]
        gateb[0:8, l * 4 + 2] = inp["gdn_A_log"][l]
        gateb[0:8, l * 4 + 3] = inp["gdn_dt_bias"][l]
    mnorm = np.ascontiguousarray(inp["mlstm_norm"][:NL].reshape(NL, 1024))
    gnorm = np.ascontiguousarray(inp["gdn_norm"][:NL].reshape(NL, 1024))
    convw = np.zeros((128, NL * 24 * 4), np.float32)
    for l in range(NL):
        cw = inp["gdn_conv_w"][l]
        convw[:, l * 96:(l + 1) * 96] = cw.reshape(4, 24, 128).transpose(2, 1, 0).reshape(128, 96)
    in_maps = []
    sel = np.zeros((8, 1024), np.float32)
    for h in range(8):
        sel[h, h * 128:(h + 1) * 128] = 1.0
    for c in range(8):
        b = c % 4
        r0 = c * NSMP
        m = {
            "xT": np.ascontiguousarray(inp["x_prompt"][b].T),
            "xsT": np.ascontiguousarray(inp["x_sample"][r0:r0 + NSMP, 0, :].T),
            "memT": np.ascontiguousarray(inp["mem_prompt"][b].T),
            "wts": wts, "cst": cst, "sel": sel, "gains": gains, "gateb": gateb, "mnorm": mnorm, "gnorm": gnorm, "convw": convw,
            "sC": np.ascontiguousarray(inp["state_mlstm_C"][:NL, r0:r0 + NSMP]),
            "sn": np.ascontiguousarray(inp["state_mlstm_n"][:NL, r0:r0 + NSMP].reshape(NL, NSMP, 512)),
            "sm": np.ascontiguousarray(inp["state_mlstm_m"][:NL, r0:r0 + NSMP]),
            "sS": np.ascontiguousarray(inp["state_gdn_S"][:NL, r0:r0 + NSMP]),
            "sconv": np.ascontiguousarray(inp["state_gdn_conv"][:NL, r0:r0 + NSMP]),
            "ck": np.ascontiguousarray(inp["cache_mem_k"][:NL, r0:r0 + NSMP].reshape(NL, NSMP, 256, 512)),
            "cv": np.ascontiguousarray(inp["cache_mem_v"][:NL, r0:r0 + NSMP].reshape(NL, NSMP, 256, 512)),
        }
        in_maps.append(m)
    res = run_bass_kernel_spmd(nc, in_maps[:ncores], core_ids=list(range(ncores)), trace=trace)
    R_ = list(res.results) + [res.results[0]] * (8 - ncores)
    B = 4
    y_prompt = np.stack([R_[b]["o_yT"].T for b in range(B)])
    y_sample = np.concatenate([R_[c]["o_ysT"].T for c in range(8)], 0)[:, None, :]
    pC = np.stack([R_[b]["o_pC"] for b in range(B)], 1)
    pn = np.stack([R_[b]["o_pn"] for b in range(B)], 1)
    pm = np.stack([R_[b]["o_pm"] for b in range(B)], 1)
    pS = np.stack([R_[b]["o_pS"] for b in range(B)], 1)
    pconv = np.stack([R_[b]["o_pconvT"].transpose(0, 2, 1) for b in range(B)], 1)
    pk = np.stack([R_[b]["o_pkT"].transpose(0, 2, 1).reshape(NL, 256, 4, 128) for b in range(B)], 1)
    pv = np.stack([R_[b]["o_pv"].reshape(NL, 256, 4, 128) for b in range(B)], 1)
    sC = np.concatenate([R_[c]["o_sC"] for c in range(8)], 1)
    sn = np.concatenate([R_[c]["o_sn"].reshape(NL, NSMP, 4, 128) for c in range(8)], 1)
    sm = np.concatenate([R_[c]["o_sm"] for c in range(8)], 1)
    sS = np.concatenate([R_[c]["o_sS"] for c in range(8)], 1)
    sconv = np.concatenate([R_[c]["o_sconv"] for c in range(8)], 1)
    outs = (y_prompt, y_sample, pC, pn, pm, pS, pconv, pk, pv, sC, sn, sm, sS, sconv)
    outs = tuple(np.ascontiguousarray(o, dtype=np.float32) for o in outs)
    if trace:
        return outs, res
    return outs


def kernel(**inputs):
    return run(inputs)
```

```python
from contextlib import ExitStack
import numpy as np
import concourse.bass as bass
import concourse.mybir as mybir
from concourse.bass_utils import run_bass_kernel_spmd

F32 = mybir.dt.float32
BF16 = mybir.dt.bfloat16
ALU = mybir.AluOpType
AF = mybir.ActivationFunctionType
AX = mybir.AxisListType

D = 2048
NCH = 16
DEPTH = 4
SEQ = 2048
TILE = 512
NCK = TILE // 128
NSMP = 16
DFF = 5632
NIN = 7192
EPS = 1e-6
NEG = -30000.0

C_ID = 0
C_NI = 128
C_MS = 256
C_SEL = 384
C_ONE = 384
C_E16 = 512
C_MK = 768
CW = 768 + 7 * 128


class Buf:
    __slots__ = ("w", "r")

    def __init__(self):
        self.w = None
        self.r = {}


class Sched:
    def __init__(self, nc, es):
        self.nc = nc
        self.es = es
        self.eng = {"pe": nc.tensor, "act": nc.scalar, "dve": nc.vector, "pool": nc.gpsimd, "sp": nc.sync}
        self.sem = {}
        self.cnt = {}
        self.waited = {e: {} for e in self.eng}
        for e in self.eng:
            self.sem[e] = es.enter_context(nc.semaphore("s_" + e))
            self.cnt[e] = 0

    def _deps(self, e, r, w):
        deps = {}

        def add(s, v):
            if deps.get(s, 0) < v:
                deps[s] = v

        for b in r:
            if b.w is not None:
                add(*b.w)
        for b in w:
            if b.w is not None:
                add(*b.w)
            for s, v in b.r.items():
                add(s, v)
        for s, v in deps.items():
            if e == "pe" and s == "pe":
                continue
            if self.waited[e].get(s, 0) >= v:
                continue
            self.eng[e].wait_ge(self.sem[s], v)
            self.waited[e][s] = v

    def _upd(self, tok, r, w):
        for b in w:
            b.w = tok
            b.r = {}
        s, v = tok
        for b in r:
            if b in w:
                continue
            if b.r.get(s, 0) < v:
                b.r[s] = v

    def op(self, e, fn, r=(), w=()):
        self._deps(e, r, w)
        ins = fn(self.eng[e])
        self.cnt[e] += 1
        ins.then_inc(self.sem[e], 1)
        tok = (e, self.cnt[e])
        self._upd(tok, r, w)
        return tok

    def dma(self, q, stream, out, in_, r=(), w=()):
        if stream not in self.sem:
            self.sem[stream] = self.es.enter_context(self.nc.semaphore("d_" + stream))
            self.cnt[stream] = 0
        self._deps(q, r, w)
        ins = self.eng[q].dma_start(out=out, in_=in_)
        self.cnt[stream] += 16
        ins.then_inc(self.sem[stream], 16)
        tok = (stream, self.cnt[stream])
        self._upd(tok, r, w)
        return tok

    def finish(self, q="sp"):
        for s, v in self.cnt.items():
            if v > 0 and self.waited[q].get(s, 0) < v:
                self.eng[q].wait_ge(self.sem[s], v)
                self.waited[q][s] = v


def weight_blocks():
    bl = []
    bl.append(("w_in", 0, 16, 3072, 8))
    for h in range(4):
        bl.append(("w_in", 0, 16, h * 128, 128))
        bl.append(("w_in", 0, 16, 512 + h * 128, 128))
        for j in range(2):
            bl.append(("w_in", 0, 16, 1024 + h * 256 + j * 128, 128))
        for j in range(2):
            bl.append(("w_in", 0, 16, 2048 + h * 256 + j * 128, 128))
    bl.append(("w_in", 0, 16, 7176, 16))
    for h in range(8):
        bl.append(("w_in", 0, 16, 3080 + h * 128, 128))
        bl.append(("w_in", 0, 16, 4104 + h * 128, 128))
        bl.append(("w_in", 0, 16, 5128 + h * 128, 128))
        bl.append(("w_in", 0, 16, 6152 + h * 128, 128))
    for j in range(16):
        bl.append(("w_out", 0, 16, j * 128, 128))
    for j in range(4):
        bl.append(("xattn_wk", 0, 16, j * 128, 128))
    for j in range(4):
        bl.append(("xattn_wv", 0, 16, j * 128, 128))
    for j in range(4):
        bl.append(("xattn_wq", 0, 16, j * 128, 128))
    for j in range(16):
        bl.append(("xattn_wo", 0, 4, j * 128, 128))
    for g in range(4):
        for c in range(11):
            bl.append(("ffn_w_gate", 0, 16, (g * 11 + c) * 128, 128))
            bl.append(("ffn_w_up", 0, 16, (g * 11 + c) * 128, 128))
        for j in range(16):
            bl.append(("ffn_w_down", g * 11, 11, j * 128, 128))
    return bl


WBLOCKS = weight_blocks()
WOFF = np.cumsum([0] + [b[2] * b[4] for b in WBLOCKS]).tolist()
WTOT = WOFF[-1]


def build(NL=DEPTH, NT=4, stage=99, with_sample=True):
    nc = bass.Bass("TRN2", target_bir_lowering=False)
    es = ExitStack()
    S = Sched(nc, es)

    def din(name, shape):
        return nc.dram_tensor(name, list(shape), F32, kind="ExternalInput").ap()

    def dout(name, shape):
        return nc.dram_tensor(name, list(shape), F32, kind="ExternalOutput").ap()

    def dscr(name, shape):
        return nc.dram_tensor(name, list(shape), F32).ap()

    def sb(name, shape, dt=F32):
        return es.enter_context(nc.sbuf_tensor("sb_" + name, list(shape), dt))

    d_xT = din("xT", [D, SEQ])
    d_xsT = din("xsT", [D, NSMP])
    d_memT = din("memT", [D, 256])
    d_w = din("wts", [NL, 128, WTOT])
    d_cst = din("cst", [128, CW])
    d_gain = din("gains", [128, (NL * 4 + 1) * 16])
    d_gb = din("gateb", [8, NL * 4])
    d_mn = din("mnorm", [NL, 4 * 256])
    d_gn = din("gnorm", [NL, 8 * 128])
    d_cw = din("convw", [128, NL * 24 * 4])
    d_sC = din("sC", [NL, NSMP, 4, 128, 256])
    d_sn = din("sn", [NL, NSMP, 4 * 128])
    d_sm = din("sm", [NL, NSMP, 4])
    d_sS = din("sS", [NL, NSMP, 8, 128, 128])
    d_sconv = din("sconv", [NL, NSMP, 3, 3072])
    d_ck = din("ck", [NL, NSMP, 256, 512])
    d_cv = din("cv", [NL, NSMP, 256, 512])

    o_yT = dout("o_yT", [D, SEQ])
    o_ysT = dout("o_ysT", [D, NSMP])
    o_pC = dout("o_pC", [NL, 4, 128, 256])
    o_pn = dout("o_pn", [NL, 4, 128])
    o_pm = dout("o_pm", [NL, 4])
    o_pS = dout("o_pS", [NL, 8, 128, 128])
    o_pconvT = dout("o_pconvT", [NL, 3072, 3])
    o_pkT = dout("o_pkT", [NL, 512, 256])
    o_pv = dout("o_pv", [NL, 256, 512])
    o_sC = dout("o_sC", [NL, NSMP, 4, 128, 256])
    o_sn = dout("o_sn", [NL, NSMP, 4 * 128])
    o_sm = dout("o_sm", [NL, NSMP, 4])
    o_sS = dout("o_sS", [NL, NSMP, 8, 128, 128])
    o_sconv = dout("o_sconv", [NL, NSMP, 3, 3072])

    cst = sb("cst", [128, CW])
    b_cst = Buf()
    S.dma("sp", "c0", cst[:], d_cst, w=[b_cst])
    identb = sb("identb", [128, 128], BF16)
    onesb = sb("onesb", [128, 128], BF16)
    selb = sb("selb", [8, 1024], BF16)
    b_cb = Buf()
    S.op("pool", lambda e: e.tensor_copy(out=identb[:], in_=cst[:, C_ID:C_ID + 128]), r=[b_cst], w=[b_cb])
    S.op("pool", lambda e: e.tensor_copy(out=onesb[:], in_=cst[:, C_ONE:C_ONE + 128]), r=[b_cst], w=[b_cb])
    d_sel = din("sel", [8, 1024])
    identf = cst[:, C_ID:C_ID + 128]
    negi = cst[:, C_NI:C_NI + 128]
    mstrict = cst[:, C_MS:C_MS + 128]
    CB = [b_cst, b_cb]

    gains = sb("gains", [128, (NL * 4 + 1) * 16])
    gateb = sb("gateb", [8, NL * 4])
    convw = sb("convw", [128, NL * 24 * 4])
    b_par = Buf()
    S.dma("sp", "c1", gains[:], d_gain, w=[b_par])
    S.dma("sp", "c2", gateb[:], d_gb, w=[b_par])
    S.dma("sp", "c3", convw[:], d_cw, w=[b_par])
    epsc = sb("epsc", [128, 1])
    S.op("pool", lambda e: e.memset(epsc[:], EPS), w=[b_par])

    TTM = TILE + NSMP
    xT = sb("xT", [128, NCH, TTM])
    b_x = Buf()
    nT = sb("nT", [128, NCH, TTM], BF16)
    b_n = Buf()
    mixT = sb("mixT", [128, NCH, TTM], BF16)
    b_mix = Buf()
    NWB = 4
    wbf = [sb("wbf%d" % i, [128, 16 * 128], BF16) for i in range(NWB)]
    b_wbf = [Buf() for _ in range(NWB)]
    NPS = 8
    PS = [es.enter_context(nc.psum_tensor("ps%d" % i, [128, 512], F32)) for i in range(NPS)]
    BPS = [Buf() for _ in range(NPS)]
    st = {"ps": 0, "w": 0, "layer": 0, "wi": 0, "ws": 0}

    pinned = set()

    def psum(pin=False):
        while True:
            i = st["ps"]
            st["ps"] = (i + 1) % NPS
            if i not in pinned:
                break
        if pin:
            pinned.add(i)
        return PS[i], BPS[i]

    def unpin(bps):
        pinned.discard(BPS.index(bps))

    def getw():
        i = st["wi"]
        st["wi"] += 1
        mat, k0, nk, c0, ncw = WBLOCKS[i]
        sz = nk * ncw
        n = st["w"]
        st["w"] += 1
        s2 = n % NWB
        S.dma("pool", "w%d" % s2, wbf[s2][:, 0:sz], d_w[st["layer"], :, WOFF[i]:WOFF[i] + sz], w=[b_wbf[s2]])
        return wbf[s2][:, 0:sz].rearrange("p (k c) -> p k c", k=nk), b_wbf[s2], (mat, k0, nk, c0, ncw)

    def groups(ti):
        g = [(c0, 512) for c0 in range(0, TILE, 512)]
        if ti == 0 and with_sample:
            g.append((TILE, NSMP))
        return g

    def proj_F(src, bsrc, ti, evac, nk=16, m=None, koff=0):
        wv, bw, desc = getw()
        mm = desc[4] if m is None else m
        for gi, (c0, n) in enumerate(groups(ti)):
            ps, bps = psum()
            for k in range(nk):
                S.op("pe", lambda e, k=k: e.matmul(ps[0:mm, 0:n], lhsT=wv[:, k, 0:mm], rhs=src[:, koff + k, c0:c0 + n],
                                                   start=(k == 0), stop=(k == nk - 1)), r=[bw, bsrc], w=[bps])
            evac(ps, bps, mm, c0, n, gi)
        return desc

    def rmsnorm_F(ti, gcol0, out, bout, src=None, bsrc=None, ncols=None):
        src = xT if src is None else src
        bsrc = b_x if bsrc is None else bsrc
        grp = groups(ti) if ncols is None else [(0, ncols)]
        for (c0, n) in grp:
            S.op("act", lambda e: e.activation(out=out[:, :, c0:c0 + n], in_=src[:, :, c0:c0 + n], func=AF.Square), r=[bsrc], w=[bout])
            ps, bps = psum()
            for k in range(NCH):
                S.op("pe", lambda e, k=k: e.matmul(ps[:, 0:n], lhsT=onesb[:], rhs=out[:, k, c0:c0 + n], start=(k == 0), stop=(k == NCH - 1)),
                     r=[bout] + CB, w=[bps])
            rstd = sb_rstd
            S.op("act", lambda e: e.activation(out=rstd[:, 0:n], in_=ps[:, 0:n], func=AF.Ln, bias=epsc[:], scale=1.0 / D), r=[bps, b_par], w=[b_rstd])
            S.op("act", lambda e: e.activation(out=rstd[:, 0:n], in_=rstd[:, 0:n], func=AF.Exp, scale=-0.5), r=[b_rstd], w=[b_rstd])
            for k in range(NCH):
                S.op("dve", lambda e, k=k: e.scalar_tensor_tensor(out=out[:, k, c0:c0 + n], in0=src[:, k, c0:c0 + n], scalar=gains[:, gcol0 + k:gcol0 + k + 1],
                                                                  in1=rstd[:, 0:n], op0=ALU.mult, op1=ALU.mult), r=[bsrc, b_rstd, b_par], w=[bout])

    sb_rstd = sb("rstd", [128, 512])
    b_rstd = Buf()

    def resid_evac(j):
        def ev(ps, bps, m, c0, n, gi, si=0):
            S.op("dve", lambda e: e.tensor_tensor(out=xT[:, j, c0:c0 + n], in0=xT[:, j, c0:c0 + n], in1=ps[:, 0:n], op=ALU.add), r=[bps, b_x], w=[b_x])
        return ev

    def proj(src, bsrc, ti, evac, subs=None, nk=16, koff=0, grp=None):
        wv, bw, desc = getw()
        subs = [(0, desc[4])] if subs is None else subs
        for gi, (c0, n) in enumerate(groups(ti) if grp is None else grp):
            for si, (lo, hi) in enumerate(subs):
                ps, bps = psum()
                for k in range(nk):
                    S.op("pe", lambda e, k=k: e.matmul(ps[0:hi - lo, 0:n], lhsT=wv[:, k, lo:hi], rhs=src[:, koff + k, c0:c0 + n],
                                                       start=(k == 0), stop=(k == nk - 1)), r=[bw, bsrc], w=[bps])
                evac(ps, bps, hi - lo, c0, n, gi, si)
        return wv, bw

    def projT(wv, bw, src, bsrc, ti, evac, evac_s=None, nk=16, ntt=NCK):
        ncols = wv.shape[2]
        for b0 in range(0, ntt, 4):
            ps, bps = psum()
            nj = min(4, ntt - b0)
            for jj in range(nj):
                j = b0 + jj
                for k in range(nk):
                    S.op("pe", lambda e, k=k, j=j, jj=jj: e.matmul(ps[:, jj * ncols:(jj + 1) * ncols], lhsT=src[:, k, j * 128:(j + 1) * 128], rhs=wv[:, k, :],
                                                                   start=(k == 0), stop=(k == nk - 1)), r=[bw, bsrc], w=[bps])
            evac(ps[:, 0:nj * ncols].rearrange("p (j c) -> p j c", j=nj), bps, b0, nj)
        if evac_s is not None and ti == 0 and with_sample:
            ps, bps = psum()
            for k in range(nk):
                S.op("pe", lambda e, k=k: e.matmul(ps[0:NSMP, 0:ncols], lhsT=src[:, k, TILE:TILE + NSMP], rhs=wv[:, k, :],
                                                   start=(k == 0), stop=(k == nk - 1)), r=[bw, bsrc], w=[bps])
            evac_s(ps[0:NSMP, 0:ncols], bps)

    def split3(src, bsrc, np_, n, pieces, bpc, tmp, btmp, npc=3):
        cur = src
        bcur = bsrc
        for i in range(npc):
            S.op("dve", lambda e, i=i, cur=cur: e.tensor_copy(out=pieces[i][0:np_, 0:n], in_=cur[0:np_, 0:n]), r=[bcur], w=[bpc])
            if i < npc - 1:
                S.op("dve", lambda e, i=i, cur=cur: e.tensor_tensor(out=tmp[0:np_, 0:n], in0=cur[0:np_, 0:n], in1=pieces[i][0:np_, 0:n], op=ALU.subtract),
                     r=[bcur, bpc], w=[btmp])
                cur = tmp
                bcur = btmp

    def bcast_rows(pieces, bpc, np_, h, n, dst, bdst, npc=3):
        for c0 in range(0, n, 512):
            nn = min(512, n - c0)
            ps, bps = psum()
            for i in range(npc):
                S.op("pe", lambda e, i=i: e.matmul(ps[:, 0:nn], lhsT=selb[0:np_, h * 128:(h + 1) * 128], rhs=pieces[i][0:np_, c0:c0 + nn],
                                                   start=(i == 0), stop=(i == npc - 1)), r=[bpc] + CB, w=[bps])
            S.op("act", lambda e: e.copy(out=dst[:, c0:c0 + nn], in_=ps[:, 0:nn]), r=[bps], w=[bdst])

    def rows_to_cols(rowlist, brows, np_, dst, bdst):
        nq = len(rowlist)
        ps, bps = psum()
        for c in range(NCK):
            for qi, rt in enumerate(rowlist):
                o = (c * nq + qi) * np_
                S.op("pe", lambda e, rt=rt, c=c, o=o: e.transpose(out=ps[:, o:o + np_], in_=rt[0:np_, c * 128:(c + 1) * 128], identity=cst[0:np_, C_ID:C_ID + np_]),
                     r=brows + CB, w=[bps])
        S.op("dve", lambda e: e.tensor_copy(out=dst[:, 0:NCK * nq * np_], in_=ps[:, 0:NCK * nq * np_]), r=[bps], w=[bdst])

    R = TILE + NSMP
    _rsz = [R, R, TILE, R, TILE + 1, TILE, TILE, TILE, TILE + 1]
    arena = sb("arena", [128, sum(_rsz)])
    _ro = np.cumsum([0] + _rsz).tolist()
    rg, rf, rF, rm, rux, rw, ra, rem, rnu = [arena[0:8, _ro[i]:_ro[i + 1]] for i in range(9)]
    rz = sb("rz", [8, TILE])
    b_rows = Buf()
    S.op("pool", lambda e: e.memset(rz[:], 0.0), w=[b_rows])
    S.dma("sp", "c0", arena[0:8, 0:1024], d_sel, w=[b_rows])
    S.op("pool", lambda e: e.tensor_copy(out=selb[:], in_=arena[0:8, 0:1024]), r=[b_rows], w=[b_cb])
    pcs = [sb("pcs%d" % i, [8, (TILE + 1)], BF16) for i in range(3)]
    b_pcs = Buf()
    ptmp = sb("ptmp", [8, (TILE + 1)]); b_ptmp = Buf()
    gb15 = sb("gb15", [8, 4]); b_gb15 = Buf()
    UBx = sb("UBx", [128, (TILE + 1)]); b_UB = Buf()
    mcols = sb("mcols", [128, NCK * 3 * 8]); b_mcols = Buf()
    qTb = sb("qTb", [128, R], BF16); b_qT = Buf()
    kTb = sb("kTb", [128, R], BF16); b_kT = Buf()
    ktok = sb("ktok", [128, NCK, 128], BF16); b_ktok = Buf()
    vtok = sb("vtok", [128, NCK, 257], BF16); b_vtok = Buf()
    gtok = sb("gtok", [128, NCK, 256], BF16); b_gtok = Buf()
    S.op("pool", lambda e: e.memset(vtok[:], 1.0), w=[b_vtok])
    Caug = sb("Caug", [128, 257]); Cb = sb("Cb", [128, 257], BF16); b_C = Buf(); b_Cb = Buf()
    Sst = sb("Sst", [128, 128]); Sb = sb("Sb", [128, 128], BF16); b_S = Buf(); b_Sb = Buf()
    pre = sb("pre", [128, 3 + R]); b_pre = Buf()
    cvy = sb("cvy", [128, R]); b_cvy = Buf()
    k2f = sb("k2f", [128, TILE]); b_k2f = Buf()
    ctail = sb("ctail", [128, NL * 24 * 3]); b_ctail = Buf()
    S.op("pool", lambda e: e.memset(ctail[:], 0.0), w=[b_ctail])
    mnb = sb("mnb", [128, 1024]); gnb = mnb; b_nb = Buf()
    gX = [sb("gX%d" % c, [128, 128]) for c in range(NCK)]; b_gX = [Buf() for _ in range(NCK)]
    gE = [sb("gE%d" % c, [128, 128]) for c in range(NCK)]; b_gE = [Buf() for _ in range(NCK)]
    gAT = [sb("gAT%d" % c, [128, 128]) for c in range(NCK)]; b_gAT = [Buf() for _ in range(NCK)]
    gQK = [sb("gQK%d" % c, [128, 128], BF16) for c in range(NCK)]; b_gQK = [Buf() for _ in range(NCK)]
    gBk = [[sb("gBk%d_%d" % (c, i), [128, 128], BF16) for i in range(6)] for c in range(NCK)]; b_gBk = [[Buf() for _ in range(6)] for _ in range(NCK)]
    gP = [[sb("gP%d_%d" % (c, i), [128, 128], BF16) for i in range(2)] for c in range(NCK)]; b_gP = [[Buf(), Buf()] for _ in range(NCK)]
    gPT = [[sb("gPT%d_%d" % (c, i), [128, 128], BF16) for i in range(2)] for c in range(NCK)]; b_gPT = [[Buf(), Buf()] for _ in range(NCK)]
    gW = [sb("gW%d" % c, [128, 128], BF16) for c in range(NCK)]; b_gW = [Buf() for _ in range(NCK)]
    cX = gX[0]; cE = gE[0]; b_cX = b_gX[0]; b_cE = b_gE[0]
    cW = sb("cW", [128, 128], BF16); b_cW = Buf()
    cnumA = sb("cnumA", [128, 257]); cnum = sb("cnum", [128, 257]); b_numA = Buf(); b_num = Buf()
    csm = sb("csm", [128, 16]); b_csm = Buf()
    chm = sb("chm", [128, 256]); b_chm = Buf()
    cjunk = sb("cjunk", [128, 256]); b_junk = Buf()
    ckw = sb("ckw", [128, 128], BF16); b_kw = Buf()
    crhs = sb("crhs", [128, 128], BF16); b_rhs = Buf()
    cwu = sb("cwu", [128, 128], BF16); b_wu = Buf()
    co1 = sb("co1", [128, 128]); b_o1 = Buf()
    go = [sb("go%d" % i, [128, 128]) for i in range(2)]; b_go = [Buf(), Buf()]
    b_csm2 = Buf()
    d_scrC = dscr("scrC", [NL, 4, 128, 257])
    d_scrS = dscr("scrS", [NL, 8, 128, 128])
    b_scr = Buf()
    mcar = sb("mcar", [8, NL]); b_mcar = Buf()
    S.op("pool", lambda e: e.memset(mcar[:], 0.0), w=[b_mcar])
    s_k = sb("s_k", [NSMP, 128]); s_v = sb("s_v", [NSMP, 257]); s_g = sb("s_g", [NSMP, 256]); b_sk = Buf(); b_sv = Buf(); b_sg = Buf()
    S.op("pool", lambda e: e.memset(s_v[:], 1.0), w=[b_sv])
    s_q = sb("s_q", [128, NSMP]); b_sq = Buf()
    s_kf = sb("s_kf", [128, NSMP]); b_skf = Buf()

    def gated_norm_to_mix(src_ap, ncol, nrm_row, gate_ap, bgate, chunk_ids, tcol0, ntok, brsrc):
        S.op("act", lambda e: e.activation(out=cjunk[0:ntok, 0:ncol], in_=src_ap, func=AF.Square, accum_out=csm[0:ntok, 4:5]), r=brsrc, w=[b_junk, b_csm])
        S.op("act", lambda e: e.activation(out=csm[0:ntok, 5:6], in_=csm[0:ntok, 4:5], func=AF.Ln, bias=epsc[0:ntok, :], scale=1.0 / ncol), r=[b_csm, b_par], w=[b_csm])
        S.op("act", lambda e: e.activation(out=csm[0:ntok, 5:6], in_=csm[0:ntok, 5:6], func=AF.Exp, scale=-0.5), r=[b_csm], w=[b_csm])
        S.op("dve", lambda e: e.scalar_tensor_tensor(out=chm[0:ntok, 0:ncol], in0=src_ap, scalar=csm[0:ntok, 5:6], in1=nrm_row, op0=ALU.mult, op1=ALU.mult),
             r=brsrc + [b_csm, b_nb], w=[b_chm])
        S.op("dve", lambda e: e.tensor_tensor(out=chm[0:ntok, 0:ncol], in0=chm[0:ntok, 0:ncol], in1=gate_ap, op=ALU.mult), r=[b_chm, bgate], w=[b_chm])
        for i, ch in enumerate(chunk_ids):
            ps, bps = psum()
            S.op("pe", lambda e, i=i: e.transpose(out=ps[:, 0:ntok], in_=chm[0:ntok, i * 128:(i + 1) * 128], identity=cst[0:ntok, C_ID:C_ID + ntok]),
                 r=[b_chm] + CB, w=[bps])
            S.op("act", lambda e, ch=ch: e.copy(out=mixT[:, ch, tcol0:tcol0 + ntok], in_=ps[:, 0:ntok]), r=[bps], w=[b_mix])

    def mlstm(l, ti):
        smp = (ti == 0 and with_sample)
        ncol = R if smp else TILE
        S.op("dve", lambda e: e.tensor_scalar(out=gb15[0:4, 0:2], in0=gateb[0:4, l * 4:l * 4 + 2], scalar1=1.0 / 15.0, scalar2=None, op0=ALU.mult), r=[b_par], w=[b_gb15])

        def ev_g(ps, bps, m, c0, n, gi, si):
            dst = rg if si == 0 else rf
            S.op("act", lambda e: e.activation(out=dst[0:4, c0:c0 + n], in_=ps[0:4, 0:n], func=AF.Tanh, bias=gb15[0:4, si:si + 1], scale=1.0 / 15.0),
                 r=[bps, b_gb15], w=[b_rows])
        proj(nT, b_n, ti, ev_g, subs=[(0, 4), (4, 8)])
        S.op("dve", lambda e: e.tensor_scalar(out=rg[0:4, 0:ncol], in0=rg[0:4, 0:ncol], scalar1=15.0, scalar2=None, op0=ALU.mult), r=[b_rows], w=[b_rows])
        S.op("act", lambda e: e.activation(out=rf[0:4, 0:ncol], in_=rf[0:4, 0:ncol], func=AF.Exp, scale=-15.0), r=[b_rows], w=[b_rows])
        S.op("act", lambda e: e.activation(out=rf[0:4, 0:ncol], in_=rf[0:4, 0:ncol], func=AF.Ln, bias=1.0), r=[b_rows], w=[b_rows])
        S.op("dve", lambda e: e.tensor_scalar(out=rf[0:4, 0:ncol], in0=rf[0:4, 0:ncol], scalar1=-1.0, scalar2=None, op0=ALU.mult), r=[b_rows], w=[b_rows])
        S.op("dve", lambda e: e.tensor_tensor_scan(out=rF[0:4, :], data0=rf[0:4, 0:TILE], data1=rz[0:4, :], initial=0.0, op0=ALU.add, op1=ALU.add), r=[b_rows], w=[b_rows])
        S.op("dve", lambda e: e.tensor_tensor_scan(out=rm[0:4, 0:TILE], data0=rf[0:4, 0:TILE], data1=rg[0:4, 0:TILE], initial=mcar[0:4, l:l + 1], op0=ALU.add, op1=ALU.max),
             r=[b_rows, b_mcar], w=[b_rows])
        S.op("dve", lambda e: e.tensor_scalar(out=rux[0:4, 0:1], in0=mcar[0:4, l:l + 1], scalar1=-1.0, scalar2=None, op0=ALU.mult), r=[b_mcar], w=[b_rows])
        S.op("dve", lambda e: e.tensor_tensor(out=rux[0:4, 1:(TILE + 1)], in0=rF[0:4, :], in1=rm[0:4, 0:TILE], op=ALU.subtract), r=[b_rows], w=[b_rows])
        S.op("dve", lambda e: e.tensor_scalar(out=rnu[0:4, :], in0=rux[0:4, :], scalar1=-1.0, scalar2=None, op0=ALU.mult), r=[b_rows], w=[b_rows])
        S.op("dve", lambda e: e.tensor_tensor(out=rw[0:4, :], in0=rg[0:4, 0:TILE], in1=rF[0:4, :], op=ALU.subtract), r=[b_rows], w=[b_rows])
        S.op("act", lambda e: e.activation(out=rem[0:4, :], in_=rm[0:4, 0:TILE], func=AF.Exp, scale=-1.0), r=[b_rows], w=[b_rows])
        for c in range(NCK):
            S.op("act", lambda e, c=c: e.activation(out=ra[0:4, c * 128:(c + 1) * 128], in_=rux[0:4, 1 + c * 128:1 + (c + 1) * 128], func=AF.Exp,
                                                    bias=rnu[0:4, c * 128:c * 128 + 1], scale=1.0), r=[b_rows], w=[b_rows])
        S.op("dve", lambda e: e.tensor_copy(out=mcar[0:4, l:l + 1], in_=rm[0:4, (TILE - 1):TILE]), r=[b_rows], w=[b_mcar])
        if ti == NT - 1:
            S.dma("sp", "o_pm", o_pm[l].rearrange("(h o) -> h o", o=1), rm[0:4, (TILE - 1):TILE], r=[b_rows])
        rows_to_cols([rw, ra, rem], [b_rows], 4, mcols, b_mcols)
        split3(rux, b_rows, 4, (TILE + 1), pcs, b_pcs, ptmp, b_ptmp)
        S.dma("sp", "nb", mnb[:], d_mn[l].partition_broadcast(128), w=[b_nb])

        def mcol(c, qi, h):
            o = (c * 3 + qi) * 4 + h
            return mcols[:, o:o + 1]

        for h in range(4):
            bcast_rows(pcs, b_pcs, 4, h, (TILE + 1), UBx, b_UB)
            def ev_q(ps, bps, m, c0, n, gi, si):
                if c0 == TILE:
                    S.op("dve", lambda e: e.tensor_copy(out=s_q[:, :], in_=ps[:, 0:n]), r=[bps], w=[b_sq])
                    S.op("act", lambda e: e.copy(out=qTb[:, c0:c0 + n], in_=s_q[:, :]), r=[b_sq], w=[b_qT])
                else:
                    S.op("act", lambda e: e.copy(out=qTb[:, c0:c0 + n], in_=ps[:, 0:n]), r=[bps], w=[b_qT])
            proj(nT, b_n, ti, ev_q)

            def ev_k(ps, bps, m, c0, n, gi, si):
                if c0 == TILE:
                    S.op("dve", lambda e: e.tensor_scalar(out=s_kf[:, :], in0=ps[:, 0:n], scalar1=128.0 ** -0.5, scalar2=None, op0=ALU.mult), r=[bps], w=[b_skf])
                    S.op("act", lambda e: e.copy(out=kTb[:, c0:c0 + n], in_=s_kf[:, :]), r=[b_skf], w=[b_kT])
                else:
                    S.op("act", lambda e: e.activation(out=kTb[:, c0:c0 + n], in_=ps[:, 0:n], func=AF.Copy, scale=128.0 ** -0.5), r=[bps], w=[b_kT])
            wv, bw = proj(nT, b_n, ti, ev_k)

            def ev_kt(ps3, bps, j0, nj):
                S.op("act", lambda e: e.activation(out=ktok[:, j0:j0 + nj, :], in_=ps3, func=AF.Copy, scale=128.0 ** -0.5), r=[bps], w=[b_ktok])

            def ev_kts(ps, bps):
                S.op("act", lambda e: e.activation(out=s_k[:, :], in_=ps, func=AF.Copy, scale=128.0 ** -0.5), r=[bps], w=[b_sk])
            projT(wv, bw, nT, b_n, ti, ev_kt, ev_kts)
            for j in range(2):
                wv, bw, _ = getw()

                def ev_v(ps3, bps, j0, nj, j=j):
                    S.op("dve", lambda e: e.tensor_copy(out=vtok[:, j0:j0 + nj, j * 128:(j + 1) * 128], in_=ps3), r=[bps], w=[b_vtok])

                def ev_vs(ps, bps, j=j):
                    S.op("dve", lambda e: e.tensor_copy(out=s_v[:, j * 128:(j + 1) * 128], in_=ps), r=[bps], w=[b_sv])
                projT(wv, bw, nT, b_n, ti, ev_v, ev_vs)
            for j in range(2):
                wv, bw, _ = getw()

                def ev_o(ps3, bps, j0, nj, j=j):
                    S.op("act", lambda e: e.activation(out=gtok[:, j0:j0 + nj, j * 128:(j + 1) * 128], in_=ps3, func=AF.Sigmoid), r=[bps], w=[b_gtok])

                def ev_os(ps, bps, j=j):
                    S.op("act", lambda e: e.activation(out=s_g[:, j * 128:(j + 1) * 128], in_=ps, func=AF.Sigmoid), r=[bps], w=[b_sg])
                projT(wv, bw, nT, b_n, ti, ev_o, ev_os)
            if ti == 0:
                S.op("dve", lambda e: e.memset(Caug[:], 0.0), w=[b_C])
            else:
                S.dma("sp", "scrC_r", Caug[:], d_scrC[l, h], r=[b_scr], w=[b_C])
            S.op("act", lambda e: e.copy(out=Cb[:], in_=Caug[:]), r=[b_C], w=[b_Cb])
            for c in range(NCK):
                t0 = c * 128
                ps_s, bps_s = psum()
                S.op("pe", lambda e: e.matmul(ps_s[:, 0:128], lhsT=kTb[:, t0:t0 + 128], rhs=qTb[:, t0:t0 + 128], start=True, stop=True), r=[b_kT, b_qT], w=[bps_s])
                S.op("dve", lambda e: e.scalar_tensor_tensor(out=cX[:], in0=UBx[:, 1 + t0:1 + t0 + 128], scalar=mcol(c, 0, h), in1=negi, op0=ALU.add, op1=ALU.add),
                     r=[b_UB, b_mcols] + CB, w=[b_cX])
                S.op("act", lambda e: e.activation(out=cE[:], in_=cX[:], func=AF.Exp), r=[b_cX], w=[b_cE])
                S.op("dve", lambda e: e.tensor_tensor(out=cW[:], in0=cE[:], in1=ps_s[:, 0:128], op=ALU.mult), r=[b_cE, bps_s], w=[b_cW])
                psA, bpsA = psum()
                S.op("pe", lambda e: e.matmul(psA[:, 0:257], lhsT=qTb[:, t0:t0 + 128], rhs=Cb[:], start=True, stop=True), r=[b_qT, b_Cb], w=[bpsA])
                psB, bpsB = psum()
                S.op("pe", lambda e: e.matmul(psB[:, 0:257], lhsT=cW[:], rhs=vtok[:, c, :], start=True, stop=True), r=[b_cW, b_vtok], w=[bpsB])
                S.op("act", lambda e: e.activation(out=cnumA[:], in_=psA[:, 0:257], func=AF.Copy, scale=mcol(c, 1, h)), r=[bpsA, b_mcols], w=[b_numA])
                S.op("dve", lambda e: e.tensor_tensor(out=cnum[:], in0=cnumA[:], in1=psB[:, 0:257], op=ALU.add), r=[b_numA, bpsB], w=[b_num])
                S.op("act", lambda e: e.activation(out=csm[:, 0:1], in_=cnum[:, 256:257], func=AF.Abs), r=[b_num], w=[b_csm])
                S.op("dve", lambda e: e.tensor_tensor(out=csm[:, 0:1], in0=csm[:, 0:1], in1=mcol(c, 2, h), op=ALU.max), r=[b_csm, b_mcols], w=[b_csm])
                S.op("dve", lambda e: e.reciprocal(out=csm[:, 1:2], in_=csm[:, 0:1]), r=[b_csm], w=[b_csm])
                S.op("dve", lambda e: e.tensor_scalar(out=cnum[:, 0:256], in0=cnum[:, 0:256], scalar1=csm[:, 1:2], scalar2=None, op0=ALU.mult), r=[b_num, b_csm], w=[b_num])
                gated_norm_to_mix(cnum[:, 0:256], 256, mnb[:, h * 256:(h + 1) * 256], gtok[:, c, :], b_gtok, [2 * h, 2 * h + 1], t0, 128, [b_num])
                S.op("act", lambda e: e.activation(out=csm[:, 2:3], in_=mcol(c, 0, h), func=AF.Exp, bias=UBx[:, t0 + 128:t0 + 129], scale=1.0), r=[b_mcols, b_UB], w=[b_csm])
                S.op("dve", lambda e: e.tensor_scalar(out=ckw[:], in0=ktok[:, c, :], scalar1=csm[:, 2:3], scalar2=None, op0=ALU.mult), r=[b_ktok, b_csm], w=[b_kw])
                psC, bpsC = psum()
                S.op("pe", lambda e: e.matmul(psC[:, 0:257], lhsT=ckw[:], rhs=vtok[:, c, :], start=True, stop=True), r=[b_kw, b_vtok], w=[bpsC])
                S.op("dve", lambda e: e.tensor_tensor(out=csm[:, 3:4], in0=UBx[:, t0 + 128:t0 + 129], in1=UBx[:, t0:t0 + 1], op=ALU.subtract), r=[b_UB], w=[b_csm])
                S.op("act", lambda e: e.activation(out=csm[:, 3:4], in_=csm[:, 3:4], func=AF.Exp), r=[b_csm], w=[b_csm])
                S.op("dve", lambda e: e.scalar_tensor_tensor(out=Caug[:], in0=Caug[:], scalar=csm[:, 3:4], in1=psC[:, 0:257], op0=ALU.mult, op1=ALU.add),
                     r=[b_C, b_csm, bpsC], w=[b_C])
                S.op("act", lambda e: e.copy(out=Cb[:], in_=Caug[:]), r=[b_C], w=[b_Cb])
            if ti < NT - 1:
                S.dma("sp", "scrC_w", d_scrC[l, h], Caug[:], r=[b_C], w=[b_scr])
            else:
                S.dma("sp", "o_pC", o_pC[l, h], Caug[:, 0:256], r=[b_C])
                S.dma("sp", "o_pn", o_pn[l, h].rearrange("(k o) -> k o", o=1), Caug[:, 256:257], r=[b_C])
            if smp:
                mlstm_sample(l, h)

    eye3 = cst[:, C_E16:C_E16 + 256].rearrange("p (a b) -> p a b", a=16)
    eyeP = cst[0:NSMP, C_ID:C_ID + NSMP]
    s_rows = sb("s_rows", [8, 8 * NSMP]); b_srows = Buf()
    s_cols = sb("s_cols", [NSMP, 64]); b_scols = Buf()
    s_rep = sb("s_rep", [128, 8 * NSMP]); b_srep = Buf()
    s_pc = [sb("s_pc%d" % i, [8, NSMP], BF16) for i in range(2)]; b_spc = Buf()
    s_pt = sb("s_pt", [8, NSMP]); b_spt = Buf()
    s_mtok = sb("s_mtok", [NSMP, 8]); b_smtok = Buf()
    Qm = sb("Qm", [128, NSMP, NSMP], BF16); b_Qm = Buf()
    Km = sb("Km", [NSMP, NSMP, 128], BF16); b_Km = Buf()
    s_t1 = sb("s_t1", [NSMP, 257]); b_st1 = Buf()
    s_t2 = sb("s_t2", [NSMP, 257]); b_st2 = Buf()
    s_vb = sb("s_vb", [NSMP, 257], BF16); b_svb = Buf()
    s_f1 = sb("s_f1", [128, NSMP]); b_sf1 = Buf()
    s_fb = sb("s_fb", [128, NSMP], BF16); b_sfb = Buf()
    NRB = 2
    rowf = [sb("rowf%d" % i, [128, 257]) for i in range(NRB)]; b_rowf = [Buf() for _ in range(NRB)]
    rowb = [sb("rowb%d" % i, [128, 257], BF16) for i in range(NRB)]; b_rowb = [Buf() for _ in range(NRB)]
    st2 = {"rb": 0}

    def rep_rows(src_rows, np_, qslot):
        S.op("dve", lambda e: e.tensor_copy(out=s_pc[0][0:np_, :], in_=src_rows), r=[b_srows], w=[b_spc])
        S.op("dve", lambda e: e.tensor_tensor(out=s_pt[0:np_, :], in0=src_rows, in1=s_pc[0][0:np_, :], op=ALU.subtract), r=[b_srows, b_spc], w=[b_spt])
        S.op("dve", lambda e: e.tensor_copy(out=s_pc[1][0:np_, :], in_=s_pt[0:np_, :]), r=[b_spt], w=[b_spc])
        ps, bps = psum()
        for h in range(np_):
            for i in range(2):
                S.op("pe", lambda e, h=h, i=i: e.matmul(ps[:, h * NSMP:(h + 1) * NSMP], lhsT=selb[0:np_, h * 128:(h + 1) * 128], rhs=s_pc[i][0:np_, :],
                                                        start=(i == 0), stop=(i == 1)), r=[b_spc] + CB, w=[bps])
        S.op("act", lambda e: e.copy(out=s_rep[:, 0:np_ * NSMP], in_=ps[:, 0:np_ * NSMP]), r=[bps], w=[b_srep])

    def rows_to_tok(np_, nq):
        ps, bps = psum()
        for qi in range(nq):
            S.op("pe", lambda e, qi=qi: e.transpose(out=ps[0:NSMP, qi * np_:(qi + 1) * np_], in_=s_rows[0:np_, qi * NSMP:(qi + 1) * NSMP], identity=cst[0:np_, C_ID:C_ID + np_]),
                 r=[b_srows] + CB, w=[bps])
        S.op("dve", lambda e: e.tensor_copy(out=s_cols[:, 0:nq * np_], in_=ps[0:NSMP, 0:nq * np_]), r=[bps], w=[b_scols])

    def mlstm_sample_prep(l):
        S.dma("sp", "s_m", s_mtok[:, 0:4], d_sm[l], w=[b_smtok])
        ps, bps = psum()
        S.op("pe", lambda e: e.transpose(out=ps[0:4, 0:NSMP], in_=s_mtok[:, 0:4], identity=eyeP), r=[b_smtok] + CB, w=[bps])
        fi = rf[0:4, TILE:TILE + NSMP]
        ii = rg[0:4, TILE:TILE + NSMP]
        sl = lambda q: s_rows[0:4, q * NSMP:(q + 1) * NSMP]
        S.op("dve", lambda e: e.tensor_tensor(out=sl(4), in0=ps[0:4, 0:NSMP], in1=fi, op=ALU.add), r=[bps, b_rows], w=[b_srows])
        S.op("dve", lambda e: e.tensor_tensor(out=sl(0), in0=sl(4), in1=ii, op=ALU.max), r=[b_srows, b_rows], w=[b_srows])
        S.op("dve", lambda e: e.tensor_tensor(out=sl(1), in0=sl(4), in1=sl(0), op=ALU.subtract), r=[b_srows], w=[b_srows])
        S.op("act", lambda e: e.activation(out=sl(1), in_=sl(1), func=AF.Exp), r=[b_srows], w=[b_srows])
        S.op("dve", lambda e: e.tensor_tensor(out=sl(2), in0=ii, in1=sl(0), op=ALU.subtract), r=[b_srows, b_rows], w=[b_srows])
        S.op("act", lambda e: e.activation(out=sl(2), in_=sl(2), func=AF.Exp), r=[b_srows], w=[b_srows])
        S.op("act", lambda e: e.activation(out=sl(3), in_=sl(0), func=AF.Exp, scale=-1.0), r=[b_srows], w=[b_srows])
        rows_to_tok(4, 4)
        rep_rows(sl(1), 4, 0)
        S.op("dve", lambda e: e.tensor_copy(out=s_mtok[:, 4:8], in_=s_cols[:, 0:4]), r=[b_scols], w=[b_smtok])
        S.dma("sp", "o_sm", o_sm[l], s_mtok[:, 4:8], r=[b_smtok])

    def masked_q(src_f32, bsrc):
        S.op("dve", lambda e: e.tensor_tensor(out=Qm[:], in0=src_f32.unsqueeze(1).to_broadcast([128, NSMP, NSMP]), in1=eye3, op=ALU.mult), r=[bsrc] + CB, w=[b_Qm])

    def masked_k(src_tok, bsrc, scale_col=None, bscale=None):
        if scale_col is not None:
            S.op("dve", lambda e: e.tensor_scalar(out=s_t2[:, 0:128], in0=src_tok, scalar1=scale_col, scalar2=None, op0=ALU.mult), r=[bsrc, bscale], w=[b_st2])
            src_tok, bsrc = s_t2[:, 0:128], b_st2
        S.op("dve", lambda e: e.tensor_tensor(out=Km[:], in0=src_tok.unsqueeze(1).to_broadcast([NSMP, NSMP, 128]), in1=eyeP.unsqueeze(2).to_broadcast([NSMP, NSMP, 128]), op=ALU.mult),
             r=[bsrc] + CB, w=[b_Km])

    def dot_rows(a_f, ba, b_f, bb, out_col):
        S.op("dve", lambda e: e.tensor_tensor(out=s_fb[:], in0=a_f, in1=b_f, op=ALU.mult), r=[ba, bb], w=[b_sfb])
        ps, bps = psum()
        S.op("pe", lambda e: e.matmul(ps[0:NSMP, 0:1], lhsT=s_fb[:], rhs=onesb[:, 0:1], start=True, stop=True), r=[b_sfb] + CB, w=[bps])
        S.op("dve", lambda e: e.tensor_copy(out=out_col, in_=ps[0:NSMP, 0:1]), r=[bps], w=[b_scols])

    def mlstm_sample(l, h):
        if h == 0:
            mlstm_sample_prep(l)
        col = lambda q: s_cols[:, q * 4 + h:q * 4 + h + 1]
        masked_q(s_q[:], b_sq)
        masked_k(s_k[:], b_sk, scale_col=col(2), bscale=b_scols)
        S.op("act", lambda e: e.copy(out=s_vb[:], in_=s_v[:]), r=[b_sv], w=[b_svb])
        psA, bpsA = psum(pin=True)
        for r_ in range(NSMP):
            i = st2["rb"] % NRB
            st2["rb"] += 1
            S.dma("sp", "s_row%d" % i, rowf[i][:, 0:256], d_sC[l, r_, h], w=[b_rowf[i]])
            S.dma("sp", "s_row%d" % i, rowf[i][:, 256:257], d_sn[l, r_, h * 128:(h + 1) * 128].rearrange("(k o) -> k o", o=1), w=[b_rowf[i]])
            S.op("act", lambda e, i=i: e.copy(out=rowb[i][:], in_=rowf[i][:]), r=[b_rowf[i]], w=[b_rowb[i]])
            S.op("pe", lambda e, i=i, r_=r_: e.matmul(psA[0:NSMP, 0:257], lhsT=Qm[:, r_, :], rhs=rowb[i][:], start=(r_ == 0), stop=(r_ == NSMP - 1)), r=[b_Qm, b_rowb[i]], w=[bpsA])
            psC, bpsC = psum()
            S.op("pe", lambda e, r_=r_: e.matmul(psC[:, 0:257], lhsT=Km[:, r_, :], rhs=s_vb[:], start=True, stop=True), r=[b_Km, b_svb], w=[bpsC])
            S.op("dve", lambda e, i=i, r_=r_: e.scalar_tensor_tensor(out=rowf[i][:], in0=rowf[i][:], scalar=s_rep[:, h * NSMP + r_:h * NSMP + r_ + 1], in1=psC[:, 0:257],
                                                                     op0=ALU.mult, op1=ALU.add), r=[b_rowf[i], b_srep, bpsC], w=[b_rowf[i]])
            S.dma("sp", "o_sC%d" % i, o_sC[l, r_, h], rowf[i][:, 0:256], r=[b_rowf[i]])
            S.dma("sp", "o_sn%d" % i, o_sn[l, r_, h * 128:(h + 1) * 128].rearrange("(k o) -> k o", o=1), rowf[i][:, 256:257], r=[b_rowf[i]])
        dot_rows(s_q[:], b_sq, s_kf[:], b_skf, s_cols[:, 60:61])
        S.op("act", lambda e: e.activation(out=s_t1[:], in_=psA[0:NSMP, 0:257], func=AF.Copy, scale=col(1)), r=[bpsA, b_scols], w=[b_st1])
        unpin(bpsA)
        S.op("dve", lambda e: e.tensor_tensor(out=s_cols[:, 61:62], in0=s_cols[:, 60:61], in1=col(2), op=ALU.mult), r=[b_scols], w=[b_scols])
        S.op("dve", lambda e: e.scalar_tensor_tensor(out=s_t1[:], in0=s_v[:], scalar=s_cols[:, 61:62], in1=s_t1[:], op0=ALU.mult, op1=ALU.add), r=[b_sv, b_scols, b_st1], w=[b_st1])
        S.op("act", lambda e: e.activation(out=s_cols[:, 62:63], in_=s_t1[:, 256:257], func=AF.Abs), r=[b_st1], w=[b_scols])
        S.op("dve", lambda e: e.tensor_tensor(out=s_cols[:, 62:63], in0=s_cols[:, 62:63], in1=col(3), op=ALU.max), r=[b_scols], w=[b_scols])
        S.op("dve", lambda e: e.reciprocal(out=s_cols[:, 62:63], in_=s_cols[:, 62:63]), r=[b_scols], w=[b_scols])
        S.op("dve", lambda e: e.tensor_scalar(out=s_t1[:, 0:256], in0=s_t1[:, 0:256], scalar1=s_cols[:, 62:63], scalar2=None, op0=ALU.mult), r=[b_st1, b_scols], w=[b_st1])
        gated_norm_to_mix(s_t1[:, 0:256], 256, mnb[0:NSMP, h * 256:(h + 1) * 256], s_g[:, :], b_sg, [2 * h, 2 * h + 1], TILE, NSMP, [b_st1])

    s_cst = sb("s_cst", [NSMP, 3, 128]); b_scst = Buf()
    s_cf = sb("s_cf", [128, 3, NSMP]); b_scf = Buf()
    s_y = sb("s_y", [128, 3, NSMP]); b_sy = Buf()
    s_k2t = sb("s_k2t", [NSMP, 128]); b_sk2t = Buf()
    s_v2t = sb("s_v2t", [NSMP, 128]); b_sv2t = Buf()
    s_nt = sb("s_nt", [NSMP, 128]); b_snt = Buf()

    def gdn_sample_conv(l, h, qi):
        cc = qi * 8 + h
        if h == 0 and qi == 0:
            S.dma("sp", "o_scv", o_sconv[l, :, 0:2, :], d_sconv[l, :, 1:3, :])
        S.dma("sp", "s_cst", s_cst[:], d_sconv[l, :, :, cc * 128:(cc + 1) * 128], w=[b_scst])
        ps, bps = psum()
        for j in range(3):
            S.op("pe", lambda e, j=j: e.transpose(out=ps[:, j * NSMP:(j + 1) * NSMP], in_=s_cst[:, j, :], identity=eyeP), r=[b_scst] + CB, w=[bps])
        S.op("act", lambda e: e.copy(out=s_cf[:], in_=ps[:, 0:3 * NSMP].rearrange("p (a b) -> p a b", a=3)), r=[bps], w=[b_scf])
        wb = (l * 24 + cc) * 4
        S.op("dve", lambda e: e.tensor_scalar(out=s_y[:, qi, :], in0=s_pre[:, qi, :], scalar1=convw[:, wb + 3:wb + 4], scalar2=None, op0=ALU.mult), r=[b_spre, b_par], w=[b_sy])
        for j in range(3):
            S.op("dve", lambda e, j=j: e.scalar_tensor_tensor(out=s_y[:, qi, :], in0=s_cf[:, j, :], scalar=convw[:, wb + j:wb + j + 1], in1=s_y[:, qi, :], op0=ALU.mult, op1=ALU.add),
                 r=[b_scf, b_par, b_sy], w=[b_sy])
        S.op("act", lambda e: e.activation(out=s_y[:, qi, :], in_=s_y[:, qi, :], func=AF.Silu), r=[b_sy], w=[b_sy])
        ps2, bps2 = psum()
        S.op("pe", lambda e: e.transpose(out=ps2[0:NSMP, 0:128], in_=s_pre[:, qi, :], identity=identf), r=[b_spre] + CB, w=[bps2])
        S.op("dve", lambda e: e.tensor_copy(out=s_nt[:], in_=ps2[0:NSMP, 0:128]), r=[bps2], w=[b_snt])
        S.dma("sp", "o_scv2", o_sconv[l, :, 2, cc * 128:(cc + 1) * 128], s_nt[:], r=[b_snt])
        if qi < 2:
            S.op("act", lambda e: e.activation(out=s_fb[:], in_=s_y[:, qi, :], func=AF.Square), r=[b_sy], w=[b_sfb])
            ps3, bps3 = psum()
            S.op("pe", lambda e: e.matmul(ps3[:, 0:NSMP], lhsT=onesb[:], rhs=s_fb[:], start=True, stop=True), r=[b_sfb] + CB, w=[bps3])
            S.op("act", lambda e: e.activation(out=s_f1[:], in_=ps3[:, 0:NSMP], func=AF.Ln, bias=epsc[:], scale=1.0), r=[bps3, b_par], w=[b_sf1])
            S.op("act", lambda e: e.activation(out=s_f1[:], in_=s_f1[:], func=AF.Exp, scale=-0.5), r=[b_sf1], w=[b_sf1])
            sc = (128.0 ** -0.5) if qi == 0 else 1.0
            S.op("dve", lambda e: e.scalar_tensor_tensor(out=s_y[:, qi, :], in0=s_y[:, qi, :], scalar=sc, in1=s_f1[:], op0=ALU.mult, op1=ALU.mult), r=[b_sy, b_sf1], w=[b_sy])
        if qi >= 1:
            dst, bdst = (s_k2t, b_sk2t) if qi == 1 else (s_v2t, b_sv2t)
            ps4, bps4 = psum()
            S.op("pe", lambda e: e.transpose(out=ps4[0:NSMP, 0:128], in_=s_y[:, qi, :], identity=identf), r=[b_sy] + CB, w=[bps4])
            S.op("dve", lambda e: e.tensor_copy(out=dst[:], in_=ps4[0:NSMP, 0:128]), r=[bps4], w=[bdst])

    def gdn_sample_prep(l):
        sl = lambda q: s_rows[0:8, q * NSMP:(q + 1) * NSMP]
        S.op("act", lambda e: e.activation(out=sl(0), in_=rga[0:8, TILE:TILE + NSMP], func=AF.Exp), r=[b_rows], w=[b_srows])
        S.op("dve", lambda e: e.tensor_copy(out=sl(1), in_=rgb[0:8, TILE:TILE + NSMP]), r=[b_rows], w=[b_srows])
        rows_to_tok(8, 2)
        rep_rows(sl(0), 8, 0)

    def gdn_sample(l, h):
        if h == 0:
            gdn_sample_prep(l)
        col = lambda q: s_cols[:, q * 8 + h:q * 8 + h + 1]
        masked_q(s_y[:, 1, :], b_sy)
        psK, bpsK = psum(pin=True)
        rows_loaded = []
        for r_ in range(NSMP):
            i = st2["rb"] % NRB
            st2["rb"] += 1
            S.dma("sp", "s_row%d" % i, rowf[i][:, 0:128], d_sS[l, r_, h], w=[b_rowf[i]])
            S.op("act", lambda e, i=i: e.copy(out=rowb[i][:, 0:128], in_=rowf[i][:, 0:128]), r=[b_rowf[i]], w=[b_rowb[i]])
            S.op("pe", lambda e, i=i, r_=r_: e.matmul(psK[0:NSMP, 0:128], lhsT=Qm[:, r_, :], rhs=rowb[i][:, 0:128], start=(r_ == 0), stop=(r_ == NSMP - 1)), r=[b_Qm, b_rowb[i]], w=[bpsK])
        S.op("dve", lambda e: e.tensor_scalar(out=s_cols[:, 56:57], in0=col(0), scalar1=-1.0, scalar2=None, op0=ALU.mult), r=[b_scols], w=[b_scols])
        S.op("dve", lambda e: e.scalar_tensor_tensor(out=s_t1[:, 0:128], in0=psK[0:NSMP, 0:128], scalar=s_cols[:, 56:57], in1=s_v2t[:], op0=ALU.mult, op1=ALU.add),
             r=[bpsK, b_scols, b_sv2t], w=[b_st1])
        unpin(bpsK)
        S.op("dve", lambda e: e.tensor_scalar(out=s_t1[:, 0:128], in0=s_t1[:, 0:128], scalar1=col(1), scalar2=None, op0=ALU.mult), r=[b_st1, b_scols], w=[b_st1])
        S.op("act", lambda e: e.copy(out=s_vb[:, 0:128], in_=s_t1[:, 0:128]), r=[b_st1], w=[b_svb])
        masked_q(s_y[:, 0, :], b_sy)
        masked_k(s_k2t[:], b_sk2t)
        psQ, bpsQ = psum(pin=True)
        for r_ in range(NSMP):
            i = st2["rb"] % NRB
            st2["rb"] += 1
            S.dma("sp", "s_row%d" % i, rowf[i][:, 0:128], d_sS[l, r_, h], w=[b_rowf[i]])
            S.op("act", lambda e, i=i: e.copy(out=rowb[i][:, 0:128], in_=rowf[i][:, 0:128]), r=[b_rowf[i]], w=[b_rowb[i]])
            S.op("pe", lambda e, i=i, r_=r_: e.matmul(psQ[0:NSMP, 0:128], lhsT=Qm[:, r_, :], rhs=rowb[i][:, 0:128], start=(r_ == 0), stop=(r_ == NSMP - 1)), r=[b_Qm, b_rowb[i]], w=[bpsQ])
            psD, bpsD = psum()
            S.op("pe", lambda e, r_=r_: e.matmul(psD[:, 0:128], lhsT=Km[:, r_, :], rhs=s_vb[:, 0:128], start=True, stop=True), r=[b_Km, b_svb], w=[bpsD])
            S.op("dve", lambda e, i=i, r_=r_: e.scalar_tensor_tensor(out=rowf[i][:, 0:128], in0=rowf[i][:, 0:128], scalar=s_rep[:, h * NSMP + r_:h * NSMP + r_ + 1], in1=psD[:, 0:128],
                                                                     op0=ALU.mult, op1=ALU.add), r=[b_rowf[i], b_srep, bpsD], w=[b_rowf[i]])
            S.dma("sp", "o_sS%d" % i, o_sS[l, r_, h], rowf[i][:, 0:128], r=[b_rowf[i]])
        dot_rows(s_y[:, 0, :], b_sy, s_y[:, 1, :], b_sy, s_cols[:, 57:58])
        S.op("act", lambda e: e.activation(out=s_t2[:, 0:128], in_=psQ[0:NSMP, 0:128], func=AF.Copy, scale=col(0)), r=[bpsQ, b_scols], w=[b_st2])
        unpin(bpsQ)
        S.op("dve", lambda e: e.scalar_tensor_tensor(out=s_t2[:, 0:128], in0=s_t1[:, 0:128], scalar=s_cols[:, 57:58], in1=s_t2[:, 0:128], op0=ALU.mult, op1=ALU.add),
             r=[b_st1, b_scols, b_st2], w=[b_st2])
        gated_norm_to_mix(s_t2[:, 0:128], 128, gnb[0:NSMP, h * 128:(h + 1) * 128], s_z[:, :], b_sz, [8 + h], TILE, NSMP, [b_st2])

    kvf = sb("kvf", [128, 512]); b_kvf = Buf()
    kvb = sb("kvb", [128, 512], BF16); b_kvb = Buf()
    qtok = sb("qtok", [NSMP, 512], BF16); b_qtok = Buf()
    oh1 = sb("oh1", [NSMP, 128], BF16); b_ohR = Buf()
    sc_all = sb("sc_all", [128, 2, NSMP * 4]); b_sc = Buf()
    sm_p = sb("sm_p", [128, 256]); b_smp = Buf()
    pTs = sb("pTs", [128, 2, 64], BF16); b_pTs = Buf()

    def xattn_sample(l):
        ps, bps = psum()
        for hh in range(4):
            S.op("pe", lambda e, hh=hh: e.transpose(out=ps[0:NSMP, hh * 128:(hh + 1) * 128], in_=qxs[:, hh, :], identity=identf), r=[b_qxs] + CB, w=[bps])
        S.op("act", lambda e: e.copy(out=qtok[:], in_=ps[0:NSMP, 0:512]), r=[bps], w=[b_qtok])
        for r_ in range(NSMP):
            psb, bpsb = psum()
            S.op("dve", lambda e, r_=r_: e.tensor_copy(out=oh1[:], in_=eyeP[:, r_:r_ + 1].to_broadcast([NSMP, 128])), r=CB, w=[b_ohR])
            S.op("pe", lambda e, r_=r_: e.matmul(psb[:, 0:512], lhsT=oh1[:], rhs=qtok[:], start=True, stop=True), r=[b_ohR, b_qtok], w=[bpsb])
            for mt in range(2):
                S.dma("sp", "s_kv", kvf[:], d_ck[l, r_, mt * 128:(mt + 1) * 128, :], w=[b_kvf])
                S.op("dve", lambda e: e.tensor_tensor(out=kvf[:], in0=kvf[:], in1=psb[:, 0:512], op=ALU.mult), r=[b_kvf, bpsb], w=[b_kvf])
                S.op("dve", lambda e, r_=r_, mt=mt: e.tensor_reduce(out=sc_all[:, mt, r_ * 4:(r_ + 1) * 4], in_=kvf[:].rearrange("p (h d) -> p h d", h=4), axis=AX.X, op=ALU.add),
                     r=[b_kvf], w=[b_sc])
        ps2, bps2 = psum()
        for mt in range(2):
            S.op("pe", lambda e, mt=mt: e.transpose(out=ps2[0:64, mt * 128:(mt + 1) * 128], in_=sc_all[:, mt, :], identity=identf), r=[b_sc] + CB, w=[bps2])
        S.op("dve", lambda e: e.reduce_max(out=csm[0:64, 12:13], in_=ps2[0:64, 0:256], axis=AX.X), r=[bps2], w=[b_csm])
        S.op("dve", lambda e: e.tensor_scalar(out=csm[0:64, 12:13], in0=csm[0:64, 12:13], scalar1=-(128.0 ** -0.5), scalar2=None, op0=ALU.mult), r=[b_csm], w=[b_csm])
        S.op("act", lambda e: e.activation(out=sm_p[0:64, :], in_=ps2[0:64, 0:256], func=AF.Exp, bias=csm[0:64, 12:13], scale=128.0 ** -0.5, accum_out=csm[0:64, 13:14]),
             r=[bps2, b_csm], w=[b_smp, b_csm])
        S.op("dve", lambda e: e.reciprocal(out=csm[0:64, 14:15], in_=csm[0:64, 13:14]), r=[b_csm], w=[b_csm])
        S.op("dve", lambda e: e.tensor_scalar(out=sm_p[0:64, :], in0=sm_p[0:64, :], scalar1=csm[0:64, 14:15], scalar2=None, op0=ALU.mult), r=[b_smp, b_csm], w=[b_smp])
        ps3, bps3 = psum()
        for mt in range(2):
            S.op("pe", lambda e, mt=mt: e.transpose(out=ps3[:, mt * 64:(mt + 1) * 64], in_=sm_p[0:64, mt * 128:(mt + 1) * 128], identity=cst[0:64, C_ID:C_ID + 64]), r=[b_smp] + CB, w=[bps3])
        S.op("act", lambda e: e.copy(out=pTs[:], in_=ps3[:, 0:128].rearrange("p (a b) -> p a b", a=2)), r=[bps3], w=[b_pTs])
        pso, bpso = psum(pin=True)
        for r_ in range(NSMP):
            for mt in range(2):
                S.dma("sp", "s_kv", kvf[:], d_cv[l, r_, mt * 128:(mt + 1) * 128, :], w=[b_kvf])
                S.op("act", lambda e: e.copy(out=kvb[:], in_=kvf[:]), r=[b_kvf], w=[b_kvb])
                for hh in range(4):
                    S.op("pe", lambda e, hh=hh, mt=mt, r_=r_: e.matmul(pso[:, mt * 64 + hh * NSMP + r_:mt * 64 + hh * NSMP + r_ + 1], lhsT=kvb[:, hh * 128:(hh + 1) * 128],
                                                                      rhs=pTs[:, mt, r_ * 4 + hh:r_ * 4 + hh + 1], start=True, stop=True), r=[b_kvb, b_pTs], w=[bpso])
        S.op("act", lambda e: e.copy(out=sm_p[:, 0:64], in_=pso[:, 0:64]), r=[bpso], w=[b_smp])
        S.op("dve", lambda e: e.tensor_tensor(out=mixT[:, 0:4, TILE:TILE + NSMP], in0=sm_p[:, 0:64].rearrange("p (a b) -> p a b", a=4),
                                              in1=pso[:, 64:128].rearrange("p (a b) -> p a b", a=4), op=ALU.add), r=[bpso, b_smp], w=[b_mix])
        unpin(bpso)

    rga = rg; rgb = rf; rgc = rux; reg = ra; rngc = rnu
    rt1 = rm
    gcols = sb("gcols", [128, NCK * 3 * 8]); b_gcols = Buf()
    GBx = UBx
    b_GB = b_UB
    q2T = qTb
    b_q2 = b_qT
    k2T = kTb
    b_k2 = b_kT
    nega = sb("nega", [8, 2]); b_nega = Buf()
    ztok = sb("ztok", [128, NCK, 128], BF16); b_ztok = Buf()
    s_z = sb("s_z", [NSMP, 128]); b_sz = Buf()
    s_pre = sb("s_pre", [128, 3, NSMP]); b_spre = Buf()

    def gdn(l, ti):
        smp = (ti == 0 and with_sample)
        ncol = R if smp else TILE

        def ev_g(ps, bps, m, c0, n, gi, si):
            dst = rga if si == 0 else rgb
            if si == 0:
                S.op("dve", lambda e: e.tensor_scalar(out=dst[0:8, c0:c0 + n], in0=ps[0:8, 0:n], scalar1=gateb[0:8, l * 4 + 3:l * 4 + 4], scalar2=None, op0=ALU.add),
                     r=[bps, b_par], w=[b_rows])
            else:
                S.op("act", lambda e: e.activation(out=dst[0:8, c0:c0 + n], in_=ps[0:8, 0:n], func=AF.Sigmoid), r=[bps], w=[b_rows])
        proj(nT, b_n, ti, ev_g, subs=[(0, 8), (8, 16)])
        S.op("act", lambda e: e.activation(out=rt1[0:8, 0:ncol], in_=rga[0:8, 0:ncol], func=AF.Abs), r=[b_rows], w=[b_rows])
        S.op("act", lambda e: e.activation(out=rt1[0:8, 0:ncol], in_=rt1[0:8, 0:ncol], func=AF.Exp, scale=-1.0), r=[b_rows], w=[b_rows])
        S.op("act", lambda e: e.activation(out=rt1[0:8, 0:ncol], in_=rt1[0:8, 0:ncol], func=AF.Ln, bias=1.0), r=[b_rows], w=[b_rows])
        S.op("dve", lambda e: e.scalar_tensor_tensor(out=rt1[0:8, 0:ncol], in0=rga[0:8, 0:ncol], scalar=0.0, in1=rt1[0:8, 0:ncol], op0=ALU.max, op1=ALU.add), r=[b_rows], w=[b_rows])
        S.op("act", lambda e: e.activation(out=nega[0:8, 0:1], in_=gateb[0:8, l * 4 + 2:l * 4 + 3], func=AF.Exp), r=[b_par], w=[b_nega])
        S.op("dve", lambda e: e.tensor_scalar(out=nega[0:8, 0:1], in0=nega[0:8, 0:1], scalar1=-1.0, scalar2=None, op0=ALU.mult), r=[b_nega], w=[b_nega])
        S.op("dve", lambda e: e.tensor_scalar(out=rga[0:8, 0:ncol], in0=rt1[0:8, 0:ncol], scalar1=nega[0:8, 0:1], scalar2=None, op0=ALU.mult), r=[b_rows, b_nega], w=[b_rows])
        S.op("dve", lambda e: e.memset(rgc[0:8, 0:1], 0.0), w=[b_rows])
        S.op("dve", lambda e: e.tensor_tensor_scan(out=rgc[0:8, 1:(TILE + 1)], data0=rga[0:8, 0:TILE], data1=rz[0:8, :], initial=0.0, op0=ALU.add, op1=ALU.add), r=[b_rows], w=[b_rows])
        S.op("dve", lambda e: e.tensor_scalar(out=rngc[0:8, :], in0=rgc[0:8, :], scalar1=-1.0, scalar2=None, op0=ALU.mult), r=[b_rows], w=[b_rows])
        for c in range(NCK):
            S.op("act", lambda e, c=c: e.activation(out=reg[0:8, c * 128:(c + 1) * 128], in_=rgc[0:8, 1 + c * 128:1 + (c + 1) * 128], func=AF.Exp,
                                                    bias=rngc[0:8, c * 128:c * 128 + 1], scale=1.0), r=[b_rows], w=[b_rows])
        rows_to_cols([rgc[:, 1:(TILE + 1)], reg, rgb], [b_rows], 8, gcols, b_gcols)
        split3(rgc, b_rows, 8, (TILE + 1), pcs, b_pcs, ptmp, b_ptmp)
        S.dma("sp", "nb", gnb[:], d_gn[l].partition_broadcast(128), w=[b_nb])

        def gcol(c, qi, h):
            o = (c * 3 + qi) * 8 + h
            return gcols[:, o:o + 1]

        for h in range(8):
            bcast_rows(pcs, b_pcs, 8, h, (TILE + 1), GBx, b_GB)
            for qi in range(3):
                cc = qi * 8 + h
                tb = (l * 24 + cc) * 3
                S.op("pool", lambda e: e.tensor_copy(out=pre[:, 0:3], in_=ctail[:, tb:tb + 3]), r=[b_ctail], w=[b_pre])

                def ev_p(ps, bps, m, c0, n, gi, si):
                    if c0 == TILE:
                        S.op("dve", lambda e: e.tensor_copy(out=s_pre[:, qi, :], in_=ps[:, 0:n]), r=[bps], w=[b_spre])
                    else:
                        S.op("act", lambda e: e.copy(out=pre[:, 3 + c0:3 + c0 + n], in_=ps[:, 0:n]), r=[bps], w=[b_pre])
                proj(nT, b_n, ti, ev_p)
                S.op("pool", lambda e: e.tensor_copy(out=ctail[:, tb:tb + 3], in_=pre[:, TILE:(TILE + 3)]), r=[b_pre], w=[b_ctail])
                if ti == NT - 1:
                    S.dma("sp", "o_pcv", o_pconvT[l, cc * 128:(cc + 1) * 128, :], pre[:, TILE:(TILE + 3)], r=[b_pre])
                wb = (l * 24 + cc) * 4
                S.op("dve", lambda e: e.tensor_scalar(out=cvy[:, 0:TILE], in0=pre[:, 0:TILE], scalar1=convw[:, wb:wb + 1], scalar2=None, op0=ALU.mult), r=[b_pre, b_par], w=[b_cvy])
                for j in range(1, 4):
                    S.op("dve", lambda e, j=j: e.scalar_tensor_tensor(out=cvy[:, 0:TILE], in0=pre[:, j:j + TILE], scalar=convw[:, wb + j:wb + j + 1], in1=cvy[:, 0:TILE],
                                                                      op0=ALU.mult, op1=ALU.add), r=[b_pre, b_par, b_cvy], w=[b_cvy])
                S.op("act", lambda e: e.activation(out=cvy[:, 0:TILE], in_=cvy[:, 0:TILE], func=AF.Silu), r=[b_cvy], w=[b_cvy])
                if qi < 2:
                    dstb, bdst = (q2T, b_q2) if qi == 0 else (k2T, b_k2)
                    sc = (128.0 ** -0.5) if qi == 0 else 1.0
                    for c0 in range(0, TILE, 512):
                        S.op("act", lambda e: e.activation(out=dstb[:, c0:c0 + 512], in_=cvy[:, c0:c0 + 512], func=AF.Square), r=[b_cvy], w=[bdst])
                        ps, bps = psum()
                        S.op("pe", lambda e: e.matmul(ps[:, 0:512], lhsT=onesb[:], rhs=dstb[:, c0:c0 + 512], start=True, stop=True), r=[bdst] + CB, w=[bps])
                        S.op("act", lambda e: e.activation(out=sb_rstd[:, 0:512], in_=ps[:, 0:512], func=AF.Ln, bias=epsc[:], scale=1.0), r=[bps, b_par], w=[b_rstd])
                        S.op("act", lambda e: e.activation(out=sb_rstd[:, 0:512], in_=sb_rstd[:, 0:512], func=AF.Exp, scale=-0.5), r=[b_rstd], w=[b_rstd])
                        if qi == 0:
                            S.op("dve", lambda e: e.scalar_tensor_tensor(out=dstb[:, c0:c0 + 512], in0=cvy[:, c0:c0 + 512], scalar=sc, in1=sb_rstd[:, 0:512], op0=ALU.mult, op1=ALU.mult),
                                 r=[b_cvy, b_rstd], w=[bdst])
                        else:
                            S.op("dve", lambda e: e.tensor_tensor(out=k2f[:, c0:c0 + 512], in0=cvy[:, c0:c0 + 512], in1=sb_rstd[:, 0:512], op=ALU.mult), r=[b_cvy, b_rstd], w=[b_k2f])
                            S.op("act", lambda e: e.copy(out=dstb[:, c0:c0 + 512], in_=k2f[:, c0:c0 + 512]), r=[b_k2f], w=[bdst])
                    if qi == 1:
                        for b0 in range(0, NCK, 4):
                            ps, bps = psum()
                            for jj in range(4):
                                S.op("pe", lambda e, jj=jj: e.transpose(out=ps[:, jj * 128:(jj + 1) * 128], in_=k2f[:, (b0 + jj) * 128:(b0 + jj + 1) * 128], identity=identf),
                                     r=[b_k2f] + CB, w=[bps])
                            S.op("act", lambda e: e.copy(out=ktok[:, b0:b0 + 4, :], in_=ps[:, 0:512].rearrange("p (j c) -> p j c", j=4)), r=[bps], w=[b_ktok])
                else:
                    for b0 in range(0, NCK, 4):
                        ps, bps = psum()
                        for jj in range(4):
                            S.op("pe", lambda e, jj=jj: e.transpose(out=ps[:, jj * 128:(jj + 1) * 128], in_=cvy[:, (b0 + jj) * 128:(b0 + jj + 1) * 128], identity=identf),
                                 r=[b_cvy] + CB, w=[bps])
                        S.op("act", lambda e: e.copy(out=vtok[:, b0:b0 + 4, 0:128], in_=ps[:, 0:512].rearrange("p (j c) -> p j c", j=4)), r=[bps], w=[b_vtok])
                if smp:
                    gdn_sample_conv(l, h, qi)
            wv, bw, _ = getw()

            def ev_z(ps3, bps, j0, nj):
                S.op("act", lambda e: e.activation(out=ztok[:, j0:j0 + nj, :], in_=ps3, func=AF.Silu), r=[bps], w=[b_ztok])

            def ev_zs(ps, bps):
                S.op("act", lambda e: e.activation(out=s_z[:, :], in_=ps, func=AF.Silu), r=[bps], w=[b_sz])
            projT(wv, bw, nT, b_n, ti, ev_z, ev_zs)
            if ti == 0:
                S.op("dve", lambda e: e.memset(Sst[:], 0.0), w=[b_S])
            else:
                S.dma("sp", "scrS_r", Sst[:], d_scrS[l, h], r=[b_scr], w=[b_S])
            S.op("act", lambda e: e.copy(out=Sb[:], in_=Sst[:]), r=[b_S], w=[b_Sb])
            def phaseA(c):
                t0 = c * 128
                cX_, bX_, cE_, bE_, cAT, b_AT, cQK, b_QK = gX[c], b_gX[c], gE[c], b_gE[c], gAT[c], b_gAT[c], gQK[c], b_gQK[c]
                cBk, b_Bk, cP, b_P, cPT, b_PT, cWw, b_Ww = gBk[c], b_gBk[c], gP[c], b_gP[c], gPT[c], b_gPT[c], gW[c], b_gW[c]
                psa, bpsa = psum(pin=True)
                S.op("pe", lambda e: e.matmul(psa[:, 0:128], lhsT=k2T[:, t0:t0 + 128], rhs=k2T[:, t0:t0 + 128], start=True, stop=True), r=[b_k2], w=[bpsa])
                psq, bpsq = psum(pin=True)
                S.op("pe", lambda e: e.matmul(psq[:, 0:128], lhsT=k2T[:, t0:t0 + 128], rhs=q2T[:, t0:t0 + 128], start=True, stop=True), r=[b_k2, b_q2], w=[bpsq])
                S.op("dve", lambda e: e.scalar_tensor_tensor(out=cX_[:], in0=GBx[:, 1 + t0:1 + t0 + 128], scalar=gcol(c, 0, h), in1=negi, op0=ALU.subtract, op1=ALU.add),
                     r=[b_GB, b_gcols] + CB, w=[bX_])
                yield
                S.op("act", lambda e: e.activation(out=cE_[:], in_=cX_[:], func=AF.Exp), r=[bX_], w=[bE_])
                yield
                S.op("dve", lambda e: e.tensor_tensor(out=cQK[:], in0=cE_[:], in1=psq[:, 0:128], op=ALU.mult), r=[bE_, bpsq], w=[b_QK])
                S.op("dve", lambda e: e.tensor_tensor(out=cAT[:], in0=cE_[:], in1=psa[:, 0:128], op=ALU.mult), r=[bE_, bpsa], w=[b_AT])
                unpin(bpsa)
                unpin(bpsq)
                yield
                S.op("dve", lambda e: e.scalar_tensor_tensor(out=cAT[:], in0=cAT[:], scalar=gcol(c, 2, h), in1=mstrict, op0=ALU.mult, op1=ALU.mult), r=[b_AT, b_gcols] + CB, w=[b_AT])
                yield
                S.op("dve", lambda e: e.tensor_tensor(out=cX_[:], in0=cAT[:], in1=cst[:, C_MK:C_MK + 128], op=ALU.mult), r=[b_AT] + CB, w=[bX_])
                for lv in range(6):
                    S.op("dve", lambda e, lv=lv: e.tensor_tensor(out=cBk[lv][:], in0=cAT[:], in1=cst[:, C_MK + (lv + 1) * 128:C_MK + (lv + 2) * 128], op=ALU.mult),
                         r=[b_AT] + CB, w=[b_Bk[lv]])
                yield
                S.op("dve", lambda e: e.tensor_tensor(out=cX_[:], in0=identf, in1=cX_[:], op=ALU.subtract), r=[bX_] + CB, w=[bX_])
                yield
                S.op("act", lambda e: e.copy(out=cPT[0][:], in_=cX_[:]), r=[bX_], w=[b_PT[0]])
                pst, bpst = psum(pin=True)
                S.op("pe", lambda e: e.transpose(out=pst[:, 0:128], in_=cX_[:], identity=identf), r=[bX_] + CB, w=[bpst])
                yield
                S.op("act", lambda e: e.copy(out=cP[0][:], in_=pst[:, 0:128]), r=[bpst], w=[b_P[0]])
                unpin(bpst)
                yield
                pi = 0
                for lv in range(6):
                    po = 1 - pi
                    psw, bpsw = psum(pin=True)
                    S.op("pe", lambda e, lv=lv, pi=pi: e.matmul(psw[:, 0:128], lhsT=cBk[lv][:], rhs=cP[pi][:], start=True, stop=True), r=[b_Bk[lv], b_P[pi]], w=[bpsw])
                    yield
                    S.op("act", lambda e: e.copy(out=cWw[:], in_=psw[:, 0:128]), r=[bpsw], w=[b_Ww])
                    unpin(bpsw)
                    yield
                    ps1, bps1 = psum(pin=True)
                    S.op("pe", lambda e, pi=pi: e.matmul(ps1[:, 0:128], lhsT=cWw[:], rhs=cPT[pi][:], start=True, stop=True), r=[b_Ww, b_PT[pi]], w=[bps1])
                    if lv < 5:
                        ps2, bps2 = psum(pin=True)
                        S.op("pe", lambda e, pi=pi: e.matmul(ps2[:, 0:128], lhsT=cPT[pi][:], rhs=cWw[:], start=True, stop=True), r=[b_Ww, b_PT[pi]], w=[bps2])
                    yield
                    S.op("dve", lambda e, pi=pi, po=po: e.tensor_tensor(out=cPT[po][:], in0=cPT[pi][:], in1=ps1[:, 0:128], op=ALU.subtract), r=[b_PT[pi], bps1], w=[b_PT[po]])
                    unpin(bps1)
                    if lv < 5:
                        S.op("dve", lambda e, pi=pi, po=po: e.tensor_tensor(out=cP[po][:], in0=cP[pi][:], in1=ps2[:, 0:128], op=ALU.subtract), r=[b_P[pi], bps2], w=[b_P[po]])
                        unpin(bps2)
                    yield
                    pi = po
                TTres[c] = (cPT[pi], b_PT[pi])

            TTres = {}
            gens = [phaseA(c) for c in range(NCK)]
            while gens:
                for g_ in list(gens):
                    try:
                        next(g_)
                    except StopIteration:
                        gens.remove(g_)
            def crit(c):
                t0 = c * 128
                TTf, bTTf = TTres[c]
                cQK, b_QK = gQK[c], b_gQK[c]
                go_, bgo_ = go[c % 2], b_go[c % 2]
                psk, bpsk = psum(pin=True)
                S.op("pe", lambda e: e.matmul(psk[:, 0:128], lhsT=k2T[:, t0:t0 + 128], rhs=Sb[:], start=True, stop=True), r=[b_k2, b_Sb], w=[bpsk])
                psqs, bpsqs = psum(pin=True)
                S.op("pe", lambda e: e.matmul(psqs[:, 0:128], lhsT=q2T[:, t0:t0 + 128], rhs=Sb[:], start=True, stop=True), r=[b_q2, b_Sb], w=[bpsqs])
                S.op("dve", lambda e: e.tensor_scalar(out=csm[:, 6:7], in0=gcol(c, 1, h), scalar1=-1.0, scalar2=None, op0=ALU.mult), r=[b_gcols], w=[b_csm2])
                yield
                S.op("dve", lambda e: e.scalar_tensor_tensor(out=crhs[:], in0=psk[:, 0:128], scalar=csm[:, 6:7], in1=vtok[:, c, 0:128], op0=ALU.mult, op1=ALU.add),
                     r=[bpsk, b_csm2, b_vtok], w=[b_rhs])
                unpin(bpsk)
                S.op("act", lambda e: e.activation(out=co1[:], in_=psqs[:, 0:128], func=AF.Copy, scale=gcol(c, 1, h)), r=[bpsqs, b_gcols], w=[b_o1])
                unpin(bpsqs)
                yield
                psu, bpsu = psum(pin=True)
                S.op("pe", lambda e: e.matmul(psu[:, 0:128], lhsT=TTf[:], rhs=crhs[:], start=True, stop=True), r=[bTTf, b_rhs], w=[bpsu])
                S.op("act", lambda e: e.activation(out=csm[:, 7:8], in_=gcol(c, 0, h), func=AF.Exp, bias=GBx[:, t0 + 128:t0 + 129], scale=-1.0), r=[b_gcols, b_GB], w=[b_csm2])
                S.op("dve", lambda e: e.tensor_tensor(out=csm[:, 8:9], in0=GBx[:, t0 + 128:t0 + 129], in1=GBx[:, t0:t0 + 1], op=ALU.subtract), r=[b_GB], w=[b_csm2])
                yield
                S.op("act", lambda e: e.activation(out=cwu[:], in_=psu[:, 0:128], func=AF.Copy, scale=gcol(c, 2, h)), r=[bpsu, b_gcols], w=[b_wu])
                unpin(bpsu)
                S.op("dve", lambda e: e.tensor_scalar(out=ckw[:], in0=ktok[:, c, :], scalar1=csm[:, 7:8], scalar2=None, op0=ALU.mult), r=[b_ktok, b_csm2], w=[b_kw])
                S.op("act", lambda e: e.activation(out=csm[:, 8:9], in_=csm[:, 8:9], func=AF.Exp), r=[b_csm2], w=[b_csm2])
                yield
                psd, bpsd = psum(pin=True)
                S.op("pe", lambda e: e.matmul(psd[:, 0:128], lhsT=ckw[:], rhs=cwu[:], start=True, stop=True), r=[b_kw, b_wu], w=[bpsd])
                pso, bpso = psum(pin=True)
                S.op("pe", lambda e: e.matmul(pso[:, 0:128], lhsT=cQK[:], rhs=cwu[:], start=True, stop=True), r=[b_QK, b_wu], w=[bpso])
                yield
                S.op("dve", lambda e: e.scalar_tensor_tensor(out=Sst[:], in0=Sst[:], scalar=csm[:, 8:9], in1=psd[:, 0:128], op0=ALU.mult, op1=ALU.add), r=[b_S, b_csm2, bpsd], w=[b_S])
                unpin(bpsd)
                yield
                S.op("act", lambda e: e.copy(out=Sb[:], in_=Sst[:]), r=[b_S], w=[b_Sb])
                S.op("dve", lambda e: e.tensor_tensor(out=go_[:], in0=co1[:], in1=pso[:, 0:128], op=ALU.add), r=[b_o1, bpso], w=[bgo_])
                unpin(bpso)
                yield

            def post(c):
                t0 = c * 128
                go_, bgo_ = go[c % 2], b_go[c % 2]
                nrm_row = gnb[:, h * 128:(h + 1) * 128]
                S.op("act", lambda e: e.activation(out=cjunk[:, 0:128], in_=go_[:], func=AF.Square, accum_out=csm[:, 4:5]), r=[bgo_], w=[b_junk, b_csm])
                yield
                S.op("act", lambda e: e.activation(out=csm[:, 5:6], in_=csm[:, 4:5], func=AF.Ln, bias=epsc[:], scale=1.0 / 128), r=[b_csm, b_par], w=[b_csm])
                yield
                S.op("act", lambda e: e.activation(out=csm[:, 5:6], in_=csm[:, 5:6], func=AF.Exp, scale=-0.5), r=[b_csm], w=[b_csm])
                yield
                S.op("dve", lambda e: e.scalar_tensor_tensor(out=chm[:, 0:128], in0=go_[:], scalar=csm[:, 5:6], in1=nrm_row, op0=ALU.mult, op1=ALU.mult),
                     r=[bgo_, b_csm, b_nb], w=[b_chm])
                yield
                S.op("dve", lambda e: e.tensor_tensor(out=chm[:, 0:128], in0=chm[:, 0:128], in1=ztok[:, c, :], op=ALU.mult), r=[b_chm, b_ztok], w=[b_chm])
                yield
                ps, bps = psum(pin=True)
                S.op("pe", lambda e: e.transpose(out=ps[:, 0:128], in_=chm[:, 0:128], identity=identf), r=[b_chm] + CB, w=[bps])
                yield
                S.op("act", lambda e: e.copy(out=mixT[:, 8 + h, t0:t0 + 128], in_=ps[:, 0:128]), r=[bps], w=[b_mix])
                unpin(bps)
                yield

            def run_gens(gl):
                gl = list(gl)
                while gl:
                    for g_ in list(gl):
                        try:
                            next(g_)
                        except StopIteration:
                            gl.remove(g_)

            for c in range(NCK):
                run_gens([crit(c)] + ([post(c - 1)] if c > 0 else []))
            run_gens([post(NCK - 1)])
            if ti < NT - 1:
                S.dma("sp", "scrS_w", d_scrS[l, h], Sst[:], r=[b_S], w=[b_scr])
            else:
                S.dma("sp", "o_pS", o_pS[l, h], Sst[:], r=[b_S])
            if smp:
                gdn_sample(l, h)
    memh = arena[:, 0:2048].rearrange("p (c m) -> p c m", c=8)
    b_memh = b_rows
    d_memv = d_memT.rearrange("(c p) m -> p c m", p=128)

    def mem_norm(l):
        gcol0 = (l * 4 + 3) * 16
        ps, bps = psum()
        for hf in range(2):
            S.dma("sp", "mem", memh, d_memv[:, hf * 8:(hf + 1) * 8, :], w=[b_memh])
            S.op("act", lambda e, hf=hf: e.activation(out=mhT[:, hf * 8:(hf + 1) * 8, :], in_=memh, func=AF.Square), r=[b_memh], w=[b_mh])
            for k in range(8):
                kk = hf * 8 + k
                S.op("pe", lambda e, kk=kk: e.matmul(ps[:, 0:256], lhsT=onesb[:], rhs=mhT[:, kk, :], start=(kk == 0), stop=(kk == 15)), r=[b_mh] + CB, w=[bps])
        S.op("act", lambda e: e.activation(out=sb_rstd[:, 0:256], in_=ps[:, 0:256], func=AF.Ln, bias=epsc[:], scale=1.0 / D), r=[bps, b_par], w=[b_rstd])
        S.op("act", lambda e: e.activation(out=sb_rstd[:, 0:256], in_=sb_rstd[:, 0:256], func=AF.Exp, scale=-0.5), r=[b_rstd], w=[b_rstd])
        for hf in range(2):
            S.dma("sp", "mem", memh, d_memv[:, hf * 8:(hf + 1) * 8, :], w=[b_memh])
            for k in range(8):
                kk = hf * 8 + k
                S.op("dve", lambda e, k=k, kk=kk: e.scalar_tensor_tensor(out=mhT[:, kk, :], in0=memh[:, k, :], scalar=gains[:, gcol0 + kk:gcol0 + kk + 1], in1=sb_rstd[:, 0:256],
                                                                          op0=ALU.mult, op1=ALU.mult), r=[b_memh, b_rstd, b_par], w=[b_mh])
    mhT = mixT[:, :, :].rearrange("p c t -> p (c t)")[:, 0:4096].rearrange("p (c m) -> p c m", c=16)
    b_mh = b_mix
    KT = sb("KT", [128, 4, 256], BF16); b_KT = Buf()
    KTf = sb("KTf", [128, 256]); b_KTf = Buf()
    Vm = sb("Vm", [128, 2, 512], BF16); b_Vm = Buf()
    Vf = sb("Vf", [128, 128]); b_Vf = Buf()
    qx = sb("qx", [128, 4, R], BF16); b_qx = Buf()
    qxs = sb("qxs", [128, 4, NSMP]); b_qxs = Buf()
    pex = sb("pex", [128, 256]); b_pex = Buf()
    pTb = sb("pTb", [128, 2, 128], BF16); b_pT = Buf()

    def xattn(l, ti):
        smp = (ti == 0 and with_sample)
        mem_norm(l)
        if stage == 5.1:
            return
        for j in range(4):
            def ev_k(ps, bps, m, c0, n, gi, si, j=j):
                S.op("dve", lambda e: e.tensor_copy(out=KTf[:], in_=ps[:, 0:256]), r=[bps], w=[b_KTf])
                S.op("act", lambda e: e.copy(out=KT[:, j, :], in_=KTf[:]), r=[b_KTf], w=[b_KT])
                if ti == 0:
                    S.dma("sp", "o_pk", o_pkT[l, j * 128:(j + 1) * 128, :], KTf[:], r=[b_KTf])
            proj(mhT, b_mh, ti, ev_k, grp=[(0, 256)])
        if stage == 5.2:
            return
        for j in range(4):
            wv, bw, _ = getw()

            def ev_v(ps3, bps, j0, nj, j=j):
                for mt in range(2):
                    S.op("dve", lambda e, mt=mt: e.tensor_copy(out=Vf[:, 0:128], in_=ps3[:, mt, :]), r=[bps], w=[b_Vf])
                    S.op("act", lambda e, mt=mt: e.copy(out=Vm[:, mt, j * 128:(j + 1) * 128], in_=Vf[:, 0:128]), r=[b_Vf], w=[b_Vm])
                    if ti == 0:
                        S.dma("sp", "o_pv", o_pv[l, mt * 128:(mt + 1) * 128, j * 128:(j + 1) * 128], Vf[:, 0:128], r=[b_Vf])
            projT(wv, bw, mhT, b_mh, 1, ev_v, None, ntt=2)
        if stage == 5.3:
            return
        rmsnorm_F(ti, (l * 4 + 1) * 16, nT, b_n)
        for j in range(4):
            def ev_q(ps, bps, m, c0, n, gi, si, j=j):
                if c0 == TILE:
                    S.op("dve", lambda e: e.tensor_copy(out=qxs[:, j, :], in_=ps[:, 0:n]), r=[bps], w=[b_qxs])
                    S.op("act", lambda e: e.copy(out=qx[:, j, c0:c0 + n], in_=qxs[:, j, :]), r=[b_qxs], w=[b_qx])
                else:
                    S.op("act", lambda e: e.copy(out=qx[:, j, c0:c0 + n], in_=ps[:, 0:n]), r=[bps], w=[b_qx])
            proj(nT, b_n, ti, ev_q)
        if stage == 5.4:
            return
        for tt in range(NCK):
            t0 = tt * 128
            for hh in range(4):
                ps, bps = psum()
                S.op("pe", lambda e: e.matmul(ps[:, 0:256], lhsT=qx[:, hh, t0:t0 + 128], rhs=KT[:, hh, :], start=True, stop=True), r=[b_qx, b_KT], w=[bps])
                S.op("dve", lambda e: e.reduce_max(out=csm[:, 9:10], in_=ps[:, 0:256], axis=AX.X), r=[bps], w=[b_csm])
                S.op("dve", lambda e: e.tensor_scalar(out=csm[:, 9:10], in0=csm[:, 9:10], scalar1=-(128.0 ** -0.5), scalar2=None, op0=ALU.mult), r=[b_csm], w=[b_csm])
                S.op("act", lambda e: e.activation(out=pex[:], in_=ps[:, 0:256], func=AF.Exp, bias=csm[:, 9:10], scale=128.0 ** -0.5, accum_out=csm[:, 10:11]),
                     r=[bps, b_csm], w=[b_pex, b_csm])
                S.op("dve", lambda e: e.reciprocal(out=csm[:, 11:12], in_=csm[:, 10:11]), r=[b_csm], w=[b_csm])
                S.op("dve", lambda e: e.tensor_scalar(out=pex[:], in0=pex[:], scalar1=csm[:, 11:12], scalar2=None, op0=ALU.mult), r=[b_pex, b_csm], w=[b_pex])
                ps2, bps2 = psum()
                for mt in range(2):
                    S.op("pe", lambda e, mt=mt: e.transpose(out=ps2[:, mt * 128:(mt + 1) * 128], in_=pex[:, mt * 128:(mt + 1) * 128], identity=identf), r=[b_pex] + CB, w=[bps2])
                S.op("act", lambda e: e.copy(out=pTb[:], in_=ps2[:, 0:256].rearrange("p (a b) -> p a b", a=2)), r=[bps2], w=[b_pT])
                ps3, bps3 = psum()
                for mt in range(2):
                    S.op("pe", lambda e, mt=mt: e.matmul(ps3[:, 0:128], lhsT=Vm[:, mt, hh * 128:(hh + 1) * 128], rhs=pTb[:, mt, :], start=(mt == 0), stop=(mt == 1)),
                         r=[b_Vm, b_pT], w=[bps3])
                S.op("act", lambda e: e.copy(out=mixT[:, hh, t0:t0 + 128], in_=ps3[:, 0:128]), r=[bps3], w=[b_mix])
        if stage == 5.5:
            return
        if smp:
            xattn_sample(l)
        for j in range(16):
            proj(mixT, b_mix, ti, resid_evac(j), nk=4)

    hg = sb_rstd
    b_hg = b_rstd

    def ffn(l, ti):
        rmsnorm_F(ti, (l * 4 + 2) * 16, nT, b_n)
        for g in range(4):
            for c in range(11):
                wg, bwg, _ = getw()
                wu_, bwu, _ = getw()
                for (c0, n) in groups(ti):
                    psg, bpsg = psum()
                    psu, bpsu = psum()
                    for k in range(16):
                        S.op("pe", lambda e, k=k: e.matmul(psg[:, 0:n], lhsT=wg[:, k, :], rhs=nT[:, k, c0:c0 + n], start=(k == 0), stop=(k == 15)), r=[bwg, b_n], w=[bpsg])
                    for k in range(16):
                        S.op("pe", lambda e, k=k: e.matmul(psu[:, 0:n], lhsT=wu_[:, k, :], rhs=nT[:, k, c0:c0 + n], start=(k == 0), stop=(k == 15)), r=[bwu, b_n], w=[bpsu])
                    S.op("act", lambda e: e.activation(out=hg[:, 0:n], in_=psg[:, 0:n], func=AF.Silu), r=[bpsg], w=[b_hg])
                    S.op("dve", lambda e: e.tensor_tensor(out=mixT[:, c, c0:c0 + n], in0=hg[:, 0:n], in1=psu[:, 0:n], op=ALU.mult), r=[b_hg, bpsu], w=[b_mix])
            for j in range(16):
                proj(mixT, b_mix, ti, resid_evac(j), nk=11)

    def mixer(l, ti):
        rmsnorm_F(ti, (l * 4 + 0) * 16, nT, b_n)
        mlstm(l, ti)
        gdn(l, ti)
        for j in range(16):
            proj(mixT, b_mix, ti, resid_evac(j))

    for ti in range(NT):
        S.dma("sp", "x", xT[:, :, 0:TILE], d_xT[:, ti * TILE:(ti + 1) * TILE].rearrange("(c p) t -> p c t", p=128), w=[b_x])
        if ti == 0 and with_sample:
            S.dma("sp", "x", xT[:, :, TILE:TILE + NSMP], d_xsT.rearrange("(c p) t -> p c t", p=128), w=[b_x])
        for l in range(NL):
            st["layer"] = l
            st["wi"] = 0
            if stage >= 6:
                mixer(l, ti)
                xattn(l, ti)
                ffn(l, ti)
                assert st["wi"] == len(WBLOCKS), (st["wi"], len(WBLOCKS))
            else:
                rmsnorm_F(ti, (l * 4 + 0) * 16, nT, b_n)
                if stage >= 2:
                    mlstm(l, ti)
                if stage >= 3:
                    gdn(l, ti)
                if stage >= 4:
                    for j in range(16):
                        proj(mixT, b_mix, ti, resid_evac(j))
                if stage >= 5:
                    xattn(l, ti)
        for (c0, n) in groups(ti):
            S.op("act", lambda e: e.activation(out=nT[:, :, c0:c0 + n], in_=xT[:, :, c0:c0 + n], func=AF.Square), r=[b_x], w=[b_n])
            ps, bps = psum()
            for k in range(NCH):
                S.op("pe", lambda e, k=k: e.matmul(ps[:, 0:n], lhsT=onesb[:], rhs=nT[:, k, c0:c0 + n], start=(k == 0), stop=(k == NCH - 1)), r=[b_n] + CB, w=[bps])
            S.op("act", lambda e: e.activation(out=sb_rstd[:, 0:n], in_=ps[:, 0:n], func=AF.Ln, bias=epsc[:], scale=1.0 / D), r=[bps, b_par], w=[b_rstd])
            S.op("act", lambda e: e.activation(out=sb_rstd[:, 0:n], in_=sb_rstd[:, 0:n], func=AF.Exp, scale=-0.5), r=[b_rstd], w=[b_rstd])
            gc = NL * 4 * 16
            for k in range(NCH):
                S.op("dve", lambda e, k=k: e.scalar_tensor_tensor(out=xT[:, k, c0:c0 + n], in0=xT[:, k, c0:c0 + n], scalar=gains[:, gc + k:gc + k + 1],
                                                                  in1=sb_rstd[:, 0:n], op0=ALU.mult, op1=ALU.mult), r=[b_x, b_rstd, b_par], w=[b_x])
        S.dma("sp", "oy", o_yT[:, ti * TILE:(ti + 1) * TILE].rearrange("(c p) t -> p c t", p=128), xT[:, :, 0:TILE], r=[b_x])
        if ti == 0 and with_sample:
            S.dma("sp", "oy", o_ysT.rearrange("(c p) t -> p c t", p=128), xT[:, :, TILE:TILE + NSMP], r=[b_x])
    S.finish()
    print('sbuf bytes remaining', nc.sbuf_bytes_remaining, 'instr counts', dict(S.cnt))
    return nc, es


def _consts():
    c = np.zeros((128, CW), np.float32)
    c[:, C_ID:C_ID + 128] = np.eye(128, dtype=np.float32)
    s = np.arange(128)[:, None]
    t = np.arange(128)[None, :]
    c[:, C_NI:C_NI + 128] = np.where(s <= t, 0.0, NEG)
    c[:, C_MS:C_MS + 128] = (s < t).astype(np.float32)
    c[:, C_ONE:C_ONE + 128] = 1.0
    c[:, C_MK:C_MK + 128] = (s // 2 == t // 2)
    bs = 2
    for lv in range(6):
        c[:, C_MK + (lv + 1) * 128:C_MK + (lv + 2) * 128] = (s // (2 * bs) == t // (2 * bs)) & (s // bs != t // bs)
        bs *= 2
    c[:, C_E16:C_E16 + 256] = np.eye(16, dtype=np.float32).reshape(1, 256)
    return c


def _pack_weights(inp, NL):
    w = np.empty((NL, 128, WTOT), np.float32)
    for i, (mat, k0, nk, c0, ncw) in enumerate(WBLOCKS):
        src = inp[mat]
        for l in range(NL):
            blk = src[l, k0 * 128:(k0 + nk) * 128, c0:c0 + ncw].reshape(nk, 128, ncw)
            w[l, :, WOFF[i]:WOFF[i + 1]] = blk.transpose(1, 0, 2).reshape(128, nk * ncw)
    return w


def _fm(v):
    return np.ascontiguousarray(v.reshape(16, 128).T)


def run(inp, NL=DEPTH, NT=SEQ // TILE, with_sample=True, trace=False, stage=99, ncores=8):
    inp = {k: np.asarray(v) for k, v in inp.items()}
    nc, es = build(NL=NL, NT=NT, with_sample=with_sample, stage=stage)
    wts = _pack_weights(inp, NL)
    cst = _consts()
    gains = np.zeros((128, (NL * 4 + 1) * 16), np.float32)
    for l in range(NL):
        for wi, nm in enumerate(("norm_mix", "norm_xattn", "norm_ffn", "norm_mem")):
            gains[:, (l * 4 + wi) * 16:(l * 4 + wi + 1) * 16] = _fm(inp[nm][l])
    gains[:, NL * 64:NL * 64 + 16] = _fm(inp["norm_final"])
    gateb = np.zeros((8, NL * 4), np.float32)
    for l in range(NL):
        gateb[0:4, l * 4 + 0] = inp["mlstm_b_i"][l]
        gateb[0:4, l * 4 + 1] = inp["mlstm_b_f"][l]
        gateb[0:8, l * 4 + 2] = inp["gdn_A_log"][l]
        gateb[0:8, l * 4 + 3] = inp["gdn_dt_bias"][l]
    mnorm = np.ascontiguousarray(inp["mlstm_norm"][:NL].reshape(NL, 1024))
    gnorm = np.ascontiguousarray(inp["gdn_norm"][:NL].reshape(NL, 1024))
    convw = np.zeros((128, NL * 24 * 4), np.float32)
    for l in range(NL):
        cw = inp["gdn_conv_w"][l]
        convw[:, l * 96:(l + 1) * 96] = cw.reshape(4, 24, 128).transpose(2, 1, 0).reshape(128, 96)
    in_maps = []
    sel = np.zeros((8, 1024), np.float32)
    for h in range(8):
        sel[h, h * 128:(h + 1) * 128] = 1.0
    for c in range(8):
        b = c % 4
        r0 = c * NSMP
        m = {
            "xT": np.ascontiguousarray(inp["x_prompt"][b].T),
            "xsT": np.ascontiguousarray(inp["x_sample"][r0:r0 + NSMP, 0, :].T),
            "memT": np.ascontiguousarray(inp["mem_prompt"][b].T),
            "wts": wts, "cst": cst, "sel": sel, "gains": gains, "gateb": gateb, "mnorm": mnorm, "gnorm": gnorm, "convw": convw,
            "sC": np.ascontiguousarray(inp["state_mlstm_C"][:NL, r0:r0 + NSMP]),
            "sn": np.ascontiguousarray(inp["state_mlstm_n"][:NL, r0:r0 + NSMP].reshape(NL, NSMP, 512)),
            "sm": np.ascontiguousarray(inp["state_mlstm_m"][:NL, r0:r0 + NSMP]),
            "sS": np.ascontiguousarray(inp["state_gdn_S"][:NL, r0:r0 + NSMP]),
            "sconv": np.ascontiguousarray(inp["state_gdn_conv"][:NL, r0:r0 + NSMP]),
            "ck": np.ascontiguousarray(inp["cache_mem_k"][:NL, r0:r0 + NSMP].reshape(NL, NSMP, 256, 512)),
            "cv": np.ascontiguousarray(inp["cache_mem_v"][:NL, r0:r0 + NSMP].reshape(NL, NSMP, 256, 512)),
        }
        in_maps.append(m)
    res = run_bass_kernel_spmd(nc, in_maps[:ncores], core_ids=list(range(ncores)), trace=trace)
    R_ = list(res.results) + [res.results[0]] * (8 - ncores)
    B = 4
    y_prompt = np.stack([R_[b]["o_yT"].T for b in range(B)])
    y_sample = np.concatenate([R_[c]["o_ysT"].T for c in range(8)], 0)[:, None, :]
    pC = np.stack([R_[b]["o_pC"] for b in range(B)], 1)
    pn = np.stack([R_[b]["o_pn"] for b in range(B)], 1)
    pm = np.stack([R_[b]["o_pm"] for b in range(B)], 1)
    pS = np.stack([R_[b]["o_pS"] for b in range(B)], 1)
    pconv = np.stack([R_[b]["o_pconvT"].transpose(0, 2, 1) for b in range(B)], 1)
    pk = np.stack([R_[b]["o_pkT"].transpose(0, 2, 1).reshape(NL, 256, 4, 128) for b in range(B)], 1)
    pv = np.stack([R_[b]["o_pv"].reshape(NL, 256, 4, 128) for b in range(B)], 1)
    sC = np.concatenate([R_[c]["o_sC"] for c in range(8)], 1)
    sn = np.concatenate([R_[c]["o_sn"].reshape(NL, NSMP, 4, 128) for c in range(8)], 1)
    sm = np.concatenate([R_[c]["o_sm"] for c in range(8)], 1)
    sS = np.concatenate([R_[c]["o_sS"] for c in range(8)], 1)
    sconv = np.concatenate([R_[c]["o_sconv"] for c in range(8)], 1)
    outs = (y_prompt, y_sample, pC, pn, pm, pS, pconv, pk, pv, sC, sn, sm, sS, sconv)
    outs = tuple(np.ascontiguousarray(o, dtype=np.float32) for o in outs)
    if trace:
        return outs, res
    return outs


def kernel(**inputs):
    return run(inputs)
```
